# Optimizing a Trainium2 kernel written in Bass

```python
import math
import numpy as np
import jax
import jax.numpy as jnp
from jax import lax

D_MODEL = 2048
BATCH = 4
SEQ = 4096
DEPTH = 2

CHUNK = 64
Q_BLOCK = 128
ROPE_THETA = 500000.0
HEAD_DIM = 128
BRANCH_HEADS = 4
N_BRANCHES = 4
BRANCH_WIDTH = BRANCH_HEADS * HEAD_DIM
DIFF_QK = 64
DIFF_ROT = DIFF_QK // 4
MLA_Q_LORA = 512
MLA_KV_LORA = 256
MLA_NOPE = 128
MLA_ROPE = 64
N_GROUPS = 4
EXPERTS_PER_GROUP = 8
N_EXPERTS = N_GROUPS * EXPERTS_PER_GROUP
EXPERT_HIDDEN = 512
TOP_K_FINE = 2
MOE_BLOCK = 128
PLE_DIM = 256
LN_EPS = 1e-5
RMS_EPS = 1e-6
DEEPNORM_ALPHA = (2 * DEPTH) ** 0.25
DEEPNORM_BETA = (8 * DEPTH) ** -0.25

IN_WIDTHS = (
    ('diff_q', BRANCH_HEADS * 2 * DIFF_QK),
    ('diff_k', BRANCH_HEADS * 2 * DIFF_QK),
    ('diff_v', BRANCH_WIDTH),
    ('fox_q', BRANCH_WIDTH),
    ('fox_k', BRANCH_WIDTH),
    ('fox_v', BRANCH_WIDTH),
    ('fox_f', BRANCH_HEADS),
    ('mla_cq', MLA_Q_LORA),
    ('mla_ckv', MLA_KV_LORA),
    ('mla_kr', MLA_ROPE),
    ('sb_q', BRANCH_WIDTH),
    ('sb_k', BRANCH_WIDTH),
    ('sb_v', BRANCH_WIDTH),
    ('gates', N_BRANCHES * D_MODEL),
)
IN_DIM = sum(w for _, w in IN_WIDTHS)

kernel_name = 'hybrid_chunk_causal_moe_encoder'


def layer_norm(x, g, b):
    xf = x.astype(jnp.float32)
    mu = jnp.mean(xf, axis=-1, keepdims=True)
    var = jnp.mean(jnp.square(xf - mu), axis=-1, keepdims=True)
    y = (xf - mu) * lax.rsqrt(var + LN_EPS) * g.astype(jnp.float32) + b.astype(jnp.float32)
    return y.astype(x.dtype)


def rms_norm(x, g):
    xf = x.astype(jnp.float32)
    y = xf * lax.rsqrt(jnp.mean(jnp.square(xf), axis=-1, keepdims=True) + RMS_EPS) * g.astype(jnp.float32)
    return y.astype(x.dtype)


def rope(x, pos, rot_dim):
    half = rot_dim // 2
    inv_freq = ROPE_THETA ** (-jnp.arange(half, dtype=jnp.float32) / half)
    ang = pos.astype(jnp.float32)[:, :, None] * inv_freq
    ang = ang.reshape(ang.shape[:2] + (1,) * (x.ndim - 3) + (half,))
    cos, sin = jnp.cos(ang), jnp.sin(ang)
    xr = x[..., :rot_dim].astype(jnp.float32)
    x1, x2 = xr[..., :half], xr[..., half:]
    rot = jnp.concatenate([x1 * cos - x2 * sin, x2 * cos + x1 * sin], axis=-1).astype(x.dtype)
    return jnp.concatenate([rot, x[..., rot_dim:]], axis=-1)


def masked_softmax(scores, mask):
    return jax.nn.softmax(jnp.where(mask, scores, -jnp.inf), axis=-1)


def query_positions(bi):
    return bi * Q_BLOCK + jnp.arange(Q_BLOCK)


def chunk_causal_mask(bi, s):
    return (jnp.arange(s) // CHUNK)[None, :] <= (query_positions(bi) // CHUNK)[:, None]


def sweep_query_blocks(fn, *q_arrays):
    b, s = q_arrays[0].shape[:2]
    nb = s // Q_BLOCK
    blocks = tuple(jnp.moveaxis(a.reshape((b, nb, Q_BLOCK) + a.shape[2:]), 1, 0) for a in q_arrays)
    out = lax.map(lambda args: fn(*args), (jnp.arange(nb),) + blocks)
    out = jnp.moveaxis(out, 0, 1)
    return out.reshape((b, s) + out.shape[3:])


def split_columns(u):
    offsets = np.cumsum([w for _, w in IN_WIDTHS])[:-1].tolist()
    parts = jnp.split(u, offsets, axis=-1)
    return {name: part for (name, _), part in zip(IN_WIDTHS, parts)}


def differential_attention(q, k, v, lam_params, subln_g, pos, layer_idx):
    b, s = q.shape[:2]
    q = rope(q, pos, DIFF_ROT)
    k = rope(k, pos, DIFF_ROT)
    lam_init = 0.8 - 0.6 * math.exp(-0.3 * layer_idx)
    lp = lam_params.astype(jnp.float32)
    lam = jnp.exp(jnp.sum(lp[0] * lp[1])) - jnp.exp(jnp.sum(lp[2] * lp[3])) + lam_init
    scale = DIFF_QK ** -0.5

    def block(bi, qb):
        sc = jnp.einsum('bqhcd,bkhcd->bchqk', qb, k).astype(jnp.float32) * scale
        a = masked_softmax(sc, chunk_causal_mask(bi, s))
        a = a[:, 0] - lam * a[:, 1]
        return jnp.einsum('bhqk,bkhd->bqhd', a.astype(v.dtype), v)

    o = sweep_query_blocks(block, q)
    o = rms_norm(o, subln_g) * (1.0 - lam_init)
    return o.reshape(b, s, BRANCH_WIDTH)


def forgetting_attention(q, k, v, f_logit, f_bias):
    b, s = q.shape[:2]
    log_f = jax.nn.log_sigmoid(f_logit.astype(jnp.float32) + f_bias.astype(jnp.float32))
    cum = jnp.cumsum(log_f, axis=1)
    cum_k = jnp.transpose(cum, (0, 2, 1))
    scale = HEAD_DIM ** -0.5
    key_pos = jnp.arange(s)

    def block(bi, qb, cum_q):
        sc = jnp.einsum('bqhd,bkhd->bhqk', qb, k).astype(jnp.float32) * scale
        sc = sc + jnp.transpose(cum_q, (0, 2, 1))[..., None] - cum_k[:, :, None, :]
        mask = key_pos[None, :] <= query_positions(bi)[:, None]
        a = masked_softmax(sc, mask)
        return jnp.einsum('bhqk,bkhd->bqhd', a.astype(v.dtype), v)

    o = sweep_query_blocks(block, q, cum)
    return o.reshape(b, s, BRANCH_WIDTH)


def latent_attention(cq, ckv, kr, q_norm_g, kv_norm_g, w_uq, w_ukv, pos):
    b, s = cq.shape[:2]
    q = (rms_norm(cq, q_norm_g) @ w_uq).reshape(b, s, BRANCH_HEADS, MLA_NOPE + MLA_ROPE)
    q_nope = q[..., :MLA_NOPE]
    q_rope = rope(q[..., MLA_NOPE:], pos, MLA_ROPE)
    kv = (rms_norm(ckv, kv_norm_g) @ w_ukv).reshape(b, s, BRANCH_HEADS, MLA_NOPE + HEAD_DIM)
    k_nope, v = kv[..., :MLA_NOPE], kv[..., MLA_NOPE:]
    k_rope = rope(kr, pos, MLA_ROPE)
    scale = (MLA_NOPE + MLA_ROPE) ** -0.5

    def block(bi, qn, qr):
        sc = (jnp.einsum('bqhd,bkhd->bhqk', qn, k_nope)
              + jnp.einsum('bqhr,bkr->bhqk', qr, k_rope)).astype(jnp.float32) * scale
        a = masked_softmax(sc, chunk_causal_mask(bi, s))
        return jnp.einsum('bhqk,bkhd->bqhd', a.astype(v.dtype), v)

    o = sweep_query_blocks(block, q_nope, q_rope)
    return o.reshape(b, s, BRANCH_WIDTH)


def stick_breaking_attention(q, k, v):
    b, s = q.shape[:2]
    scale = HEAD_DIM ** -0.5
    key_pos = jnp.arange(s)

    def block(bi, qb):
        z = jnp.einsum('bqhd,bkhd->bhqk', qb, k).astype(jnp.float32) * scale
        mask = key_pos[None, :] < query_positions(bi)[:, None]
        log_beta = jax.nn.log_sigmoid(z)
        log_keep = jnp.where(mask, jax.nn.log_sigmoid(-z), 0.0)
        later = lax.cumsum(log_keep, axis=3, reverse=True) - log_keep
        w = jnp.where(mask, jnp.exp(log_beta + later), 0.0)
        return jnp.einsum('bhqk,bkhd->bqhd', w.astype(v.dtype), v)

    o = sweep_query_blocks(block, q)
    return o.reshape(b, s, BRANCH_WIDTH)


def hybrid_mixer(x, positions, w_in, fox_f_bias, diff_lambda, diff_subln, mla_q_norm,
                 mla_kv_norm, mla_w_uq, mla_w_ukv, w_branch, w_o, layer_idx):
    b, s, _ = x.shape
    u = split_columns(x @ w_in)

    def heads(t):
        return t.reshape(b, s, BRANCH_HEADS, HEAD_DIM)

    o_diff = differential_attention(
        u['diff_q'].reshape(b, s, BRANCH_HEADS, 2, DIFF_QK),
        u['diff_k'].reshape(b, s, BRANCH_HEADS, 2, DIFF_QK),
        heads(u['diff_v']), diff_lambda, diff_subln, positions, layer_idx)
    o_fox = forgetting_attention(heads(u['fox_q']), heads(u['fox_k']), heads(u['fox_v']),
                                 u['fox_f'], fox_f_bias)
    o_mla = latent_attention(u['mla_cq'], u['mla_ckv'], u['mla_kr'], mla_q_norm, mla_kv_norm,
                             mla_w_uq, mla_w_ukv, positions)
    o_sb = stick_breaking_attention(heads(u['sb_q']), heads(u['sb_k']), heads(u['sb_v']))

    gates = jax.nn.sigmoid(u['gates'].reshape(b, s, N_BRANCHES, D_MODEL))
    merged = jnp.zeros_like(x)
    for n, o in enumerate((o_diff, o_fox, o_mla, o_sb)):
        merged = merged + gates[:, :, n] * (o @ w_branch[n])
    return merged @ w_o


def routed_experts(xt, expert_idx, gate_w, w_gate, w_up, w_down):
    t, d = xt.shape
    a = t * TOP_K_FINE
    flat_e = expert_idx.reshape(a)
    flat_tok = jnp.repeat(jnp.arange(t, dtype=jnp.int32), TOP_K_FINE)
    flat_w = gate_w.reshape(a)
    order = jnp.argsort(flat_e)
    e_sorted, tok_sorted, w_sorted = flat_e[order], flat_tok[order], flat_w[order]
    sizes = jnp.bincount(flat_e, length=N_EXPERTS)
    padded = (sizes + MOE_BLOCK - 1) // MOE_BLOCK * MOE_BLOCK
    pad_end = jnp.cumsum(padded)
    pad_start = pad_end - padded
    start = jnp.cumsum(sizes) - sizes
    dest = pad_start[e_sorted] + jnp.arange(a) - start[e_sorted]
    n_rows = a + N_EXPERTS * MOE_BLOCK
    n_blk = n_rows // MOE_BLOCK
    rows = jnp.zeros((n_rows, d), xt.dtype).at[dest].set(xt[tok_sorted])
    blk_e = jnp.minimum(jnp.searchsorted(pad_end, jnp.arange(n_blk) * MOE_BLOCK, side='right'),
                        N_EXPERTS - 1)

    def expert_block(args):
        xb, e = args
        hid = jax.nn.silu(xb @ w_gate[e]) * (xb @ w_up[e])
        return hid @ w_down[e]

    y_rows = lax.map(expert_block, (rows.reshape(n_blk, MOE_BLOCK, d), blk_e)).reshape(n_rows, d)
    y_assign = y_rows[dest] * w_sorted[:, None].astype(xt.dtype)
    return jax.ops.segment_sum(y_assign, tok_sorted, num_segments=t)


def hierarchical_moe(h, w_rg, b_rg, w_re, b_re, w_eg, w_eu, w_ed):
    b, s, d = h.shape
    t = b * s
    xt = h.reshape(t, d)
    g_logits = (xt @ w_rg).astype(jnp.float32) + b_rg.astype(jnp.float32)
    grp = jnp.argmax(g_logits, axis=-1)
    p_grp = jnp.take_along_axis(jax.nn.softmax(g_logits, axis=-1), grp[:, None], axis=-1)
    e_logits = ((xt @ w_re).astype(jnp.float32) + b_re.astype(jnp.float32)).reshape(
        t, N_GROUPS, EXPERTS_PER_GROUP)
    e_in = e_logits[jnp.arange(t), grp]
    top_p, top_i = lax.top_k(jax.nn.softmax(e_in, axis=-1), TOP_K_FINE)
    weights = p_grp * top_p / jnp.sum(top_p, axis=-1, keepdims=True)
    expert_idx = grp[:, None].astype(jnp.int32) * EXPERTS_PER_GROUP + top_i.astype(jnp.int32)
    y = routed_experts(xt, expert_idx, weights, w_eg, w_eu, w_ed)
    return y.reshape(b, s, d)


def setup_inputs(seed: int = 0) -> dict:
    key = jax.random.key(seed)
    ks = jax.random.split(key, 32)
    d = D_MODEL

    def normal(i, shape, scale):
        return jax.random.normal(ks[i], shape, jnp.float32) * scale

    x = normal(0, (BATCH, SEQ, d), 1.0)
    p = normal(1, (DEPTH, BATCH, SEQ, PLE_DIM), 1.0)
    stream_start = jax.random.randint(ks[2], (BATCH, 1), 0, 64, dtype=jnp.int32) * CHUNK
    positions = (stream_start + jnp.arange(SEQ, dtype=jnp.int32)[None, :]).astype(jnp.int32)
    return {
        'x': x,
        'p': p,
        'positions': positions,
        'w_in': normal(3, (DEPTH, d, IN_DIM), d ** -0.5),
        'fox_f_bias': normal(4, (DEPTH, BRANCH_HEADS), 0.1),
        'diff_lambda': normal(5, (DEPTH, 4, DIFF_QK), 0.1),
        'diff_subln': 1.0 + normal(6, (DEPTH, HEAD_DIM), 0.02),
        'mla_q_norm': 1.0 + normal(7, (DEPTH, MLA_Q_LORA), 0.02),
        'mla_kv_norm': 1.0 + normal(8, (DEPTH, MLA_KV_LORA), 0.02),
        'mla_w_uq': normal(9, (DEPTH, MLA_Q_LORA, BRANCH_HEADS * (MLA_NOPE + MLA_ROPE)), MLA_Q_LORA ** -0.5),
        'mla_w_ukv': normal(10, (DEPTH, MLA_KV_LORA, BRANCH_HEADS * (MLA_NOPE + HEAD_DIM)), MLA_KV_LORA ** -0.5),
        'w_branch': normal(11, (DEPTH, N_BRANCHES, BRANCH_WIDTH, d), BRANCH_WIDTH ** -0.5),
        'w_o': normal(12, (DEPTH, d, d), DEEPNORM_BETA * d ** -0.5),
        'ln1_g': 1.0 + normal(13, (DEPTH, d), 0.02),
        'ln1_b': normal(14, (DEPTH, d), 0.02),
        'w_router_group': normal(15, (DEPTH, d, N_GROUPS), d ** -0.5),
        'b_router_group': normal(16, (DEPTH, N_GROUPS), 0.01),
        'w_router_expert': normal(17, (DEPTH, d, N_EXPERTS), d ** -0.5),
        'b_router_expert': normal(18, (DEPTH, N_EXPERTS), 0.01),
        'w_expert_gate': normal(19, (DEPTH, N_EXPERTS, d, EXPERT_HIDDEN), d ** -0.5),
        'w_expert_up': normal(20, (DEPTH, N_EXPERTS, d, EXPERT_HIDDEN), d ** -0.5),
        'w_expert_down': normal(21, (DEPTH, N_EXPERTS, EXPERT_HIDDEN, d), DEEPNORM_BETA * EXPERT_HIDDEN ** -0.5),
        'w_ple_gate': normal(22, (DEPTH, d, d), d ** -0.5),
        'w_ple_proj': normal(23, (DEPTH, PLE_DIM, d), DEEPNORM_BETA * PLE_DIM ** -0.5),
        'ln2_g': 1.0 + normal(24, (DEPTH, d), 0.02),
        'ln2_b': normal(25, (DEPTH, d), 0.02),
    }


def reference(x, p, positions, w_in, fox_f_bias, diff_lambda, diff_subln, mla_q_norm,
              mla_kv_norm, mla_w_uq, mla_w_ukv, w_branch, w_o, ln1_g, ln1_b,
              w_router_group, b_router_group, w_router_expert, b_router_expert,
              w_expert_gate, w_expert_up, w_expert_down, w_ple_gate, w_ple_proj,
              ln2_g, ln2_b):
    for i in range(DEPTH):
        mix = hybrid_mixer(x, positions, w_in[i], fox_f_bias[i], diff_lambda[i], diff_subln[i],
                           mla_q_norm[i], mla_kv_norm[i], mla_w_uq[i], mla_w_ukv[i],
                           w_branch[i], w_o[i], i)
        h = layer_norm(DEEPNORM_ALPHA * x + mix, ln1_g[i], ln1_b[i])
        ffn = hierarchical_moe(h, w_router_group[i], b_router_group[i], w_router_expert[i],
                               b_router_expert[i], w_expert_gate[i], w_expert_up[i],
                               w_expert_down[i])
        ple = jax.nn.sigmoid(h @ w_ple_gate[i]) * (p[i] @ w_ple_proj[i])
        x = layer_norm(DEEPNORM_ALPHA * h + ffn + ple, ln2_g[i], ln2_b[i])
    return x
```

```python
import math
import numpy as np
from contextlib import ExitStack
import concourse.bass as bass
import concourse.mybir as mybir
from concourse.bass_utils import run_bass_kernel_spmd

F32 = mybir.dt.float32
BF16 = mybir.dt.bfloat16
I32 = mybir.dt.int32
AF = mybir.ActivationFunctionType
ALU = mybir.AluOpType

D = 2048
DEPTH = 2
NEXP = 32
EH = 512
IN_DIM = 13636
ALPHA = 4.0 ** 0.25
LN_EPS = 1e-5
RMS_EPS = 1e-6
ROPE_THETA = 500000.0
OFF = dict(diff_q=0, diff_k=512, diff_v=1024, fox_q=1536, fox_k=2048, fox_v=2560, fox_f=3072,
           mla_cq=3076, mla_ckv=3588, mla_kr=3844, sb_q=3908, sb_k=4420, sb_v=4932, gates=5444)
NEG = -30000.0
TWO_PI = 6.283185307179586

C_IDENT, C_ONES, C_TRIS, C_TRIL, C_RD, C_RM = 0, 128, 256, 384, 512, 640
C_MADD_CH, C_MADD_CA, C_MMUL = 768, 768 + 2048, 768 + 4096
C_INVF_D, C_INVF_M, C_ECAP = 768 + 6144, 768 + 6145, 768 + 6146
NCONST = 768 + 6146 + 32


class Sched:
    NDMA = 16

    def __init__(self, nc, es):
        self.nc = nc
        self.es = es
        self.eng = {'pe': nc.tensor, 'dve': nc.vector, 'act': nc.scalar, 'pool': nc.gpsimd, 'sp': nc.sync}
        self.sems = {}
        self.cnt = {}
        for e in self.eng:
            self.sems[e] = es.enter_context(nc.semaphore('s_' + e))
            self.cnt[e] = 0
        self.dma_rr = {}
        for q in ('sp', 'act', 'pool'):
            self.dma_rr[q] = 0
            for i in range(self.NDMA):
                k = ('d', q, i)
                self.sems[k] = es.enter_context(nc.semaphore('d_%s_%d' % (q, i)))
                self.cnt[k] = 0
        self.seen = {e: {} for e in self.eng}
        self.res = {}
        self.nins = 0

    def _need(self, reads, writes):
        need = {}
        for r in reads:
            st = self.res.get(r)
            if st and st[0] is not None:
                k, v = st[0]
                if need.get(k, 0) < v:
                    need[k] = v
        for w in writes:
            st = self.res.get(w)
            if st:
                if st[0] is not None:
                    k, v = st[0]
                    if need.get(k, 0) < v:
                        need[k] = v
                for k, v in st[1].items():
                    if need.get(k, 0) < v:
                        need[k] = v
        return need

    def _commit(self, ev, reads, writes):
        k, v = ev
        for r in reads:
            st = self.res.get(r)
            if st is None:
                st = self.res[r] = [None, {}]
            if st[1].get(k, 0) < v:
                st[1][k] = v
        for w in writes:
            self.res[w] = [ev, {}]

    def _waits(self, e, need):
        eng = self.eng[e]
        seen = self.seen[e]
        for k, v in need.items():
            if e == 'pe' and k == 'pe':
                continue
            if seen.get(k, 0) < v:
                eng.wait_ge(self.sems[k], v)
                seen[k] = v

    def op(self, e, fn, reads=(), writes=()):
        self._waits(e, self._need(reads, writes))
        ins = fn(self.eng[e])
        self.cnt[e] += 1
        ins.then_inc(self.sems[e], 1)
        self._commit((e, self.cnt[e]), reads, writes)
        self.nins += 1

    def dma(self, q, out, in_, reads=(), writes=(), fn=None, **kw):
        if q == 'sp' and fn is None and str(out.space) == 'DRAM' and str(in_.space) != 'DRAM':
            q = 'act'
        i = self.dma_rr[q]
        self.dma_rr[q] = (i + 1) % self.NDMA
        k = ('d', q, i)
        need = self._need(reads, writes)
        if self.cnt[k]:
            need[k] = max(need.get(k, 0), self.cnt[k])
        self._waits(q, need)
        if fn is not None:
            ins = fn(self.eng[q])
        else:
            ins = self.eng[q].dma_start(out=out, in_=in_, **kw)
        self.cnt[k] += 16
        ins.then_inc(self.sems[k], 16)
        self._commit((k, self.cnt[k]), reads, writes)
        self.nins += 1

    def finish(self, keys):
        self._waits('sp', self._need(keys, ()))

    def barrier(self):
        need = {k: v for k, v in self.cnt.items() if v > 0}
        for e in self.eng:
            eng = self.eng[e]
            seen = self.seen[e]
            for k, v in need.items():
                if seen.get(k, 0) < v:
                    eng.wait_ge(self.sems[k], v)
                    seen[k] = v


class Ring:
    def __init__(self, tiles):
        self.tiles = tiles
        self.i = 0

    def get(self):
        t = self.tiles[self.i]
        self.i = (self.i + 1) % len(self.tiles)
        return t


class T:
    def __init__(self, ap, key):
        self.t = ap
        self.k = key


class Prog:
    def __init__(self, S_len, nlayers=DEPTH, dbg=False, phases=None):
        self.SL = S_len
        self.TG = min(2048, S_len)
        self.NTG = S_len // self.TG
        self.NCH = S_len // 128
        self.NQT = S_len // 512
        avg = 2 * S_len // NEXP
        sd = math.sqrt(2 * S_len / 32.0)
        self.CAPB = int(math.ceil((avg + 8.0 * sd) / 128.0))
        self.CAP = self.CAPB * 128
        self.nlayers = nlayers
        self.WD = DEPTH if (nlayers == DEPTH) else nlayers
        self.NE_DECL = NEXP if (phases is None or 'exp' in phases) else 1
        self.dbg = dbg
        self.phases = phases
        self.nc = bass.Bass("TRN2", target_bir_lowering=False)
        self.scopes = []
        self.uid = 0
        self.cast_rr = 0

    def din(self, name, shape, dt=F32):
        return self.nc.dram_tensor(name, list(shape), dt, kind="ExternalInput").ap()

    def dscr(self, name, shape, dt):
        kind = "ExternalOutput" if (self.dbg and name in self.dbg) else "Internal"
        return self.nc.dram_tensor(name, list(shape), dt, kind=kind).ap()

    def sb(self, name, shape, dt):
        self.uid += 1
        t = self.cur.enter_context(self.nc.sbuf_tensor("%s_%d" % (name, self.uid), list(shape), dt))
        return T(t, name)

    def ps(self, name, shape, dt=F32):
        self.uid += 1
        t = self.cur.enter_context(self.nc.psum_tensor("%s_%d" % (name, self.uid), list(shape), dt))
        return T(t, name)

    def ring(self, name, n, shape, dt, psum=False):
        return Ring([(self.ps if psum else self.sb)("%s%d" % (name, i), shape, dt) for i in range(n)])

    def wload(self, src_ap, a, b, q='sp'):
        st = self.wst.get()
        wb = self.wbf.get()
        S = self.S
        stv = st.t[:, 0:a * b].rearrange("p (a b) -> p a b", a=a)
        step = max(1, 4 * 128 // 128) if b <= 512 else 1
        step = 4
        keys = []
        for j, a0 in enumerate(range(0, a, step)):
            a1 = min(a, a0 + step)
            S.dma(q, stv[:, a0:a1, :], src_ap[:, a0:a1, :], writes=[(st.k, j)])
            keys.append((st.k, j))
        self.cast(wb.t[:, 0:a * b], st.t[:, 0:a * b], keys, [wb.k])
        return wb

    def cast(self, out_ap, in_ap, reads, writes):
        i = self.cast_rr
        self.cast_rr = (i + 1) % 3
        if i == 0:
            self.S.op('pool', lambda e: e.tensor_copy(out=out_ap, in_=in_ap), reads=reads, writes=writes)
        elif i == 1:
            self.S.op('act', lambda e: e.activation(out=out_ap, in_=in_ap, func=AF.Copy), reads=reads, writes=writes)
        else:
            self.S.op('dve', lambda e: e.tensor_copy(out=out_ap, in_=in_ap), reads=reads, writes=writes)

    def build(self):
        nc = self.nc
        SL, TG, NCH = self.SL, self.TG, self.NCH
        L = self.nlayers
        self.x_tm = self.din("x_tm", [SL, D])
        self.x_fm = self.din("x_fm", [D, SL])
        self.pT = self.din("pT", [DEPTH, 256, SL])
        self.pos = self.din("pos", [1, SL], I32)
        self.consts = self.din("consts", [128, NCONST])
        self.w_in = self.din("w_in", [DEPTH, D, IN_DIM])
        self.fox_f_bias = self.din("fox_f_bias", [DEPTH, 4, 1])
        self.diff_lambda = self.din("diff_lambda", [DEPTH, 1, 256])
        self.diff_subln = self.din("diff_subln", [DEPTH, 128, 1])
        self.mla_q_norm = self.din("mla_q_norm", [DEPTH, 128, 4])
        self.mla_kv_norm = self.din("mla_kv_norm", [DEPTH, 128, 2])
        self.mla_w_uq = self.din("mla_w_uq", [DEPTH, 512, 768])
        self.mla_w_ukv = self.din("mla_w_ukv", [DEPTH, 256, 1024])
        self.w_branch = self.din("w_branch", [DEPTH, 4, 512, D])
        self.w_o = self.din("w_o", [DEPTH, D, D])
        self.ln1_g = self.din("ln1_g", [DEPTH, 1, D])
        self.ln1_b = self.din("ln1_b", [DEPTH, 1, D])
        self.w_rg = self.din("w_router_group", [DEPTH, D, 4])
        self.b_rg = self.din("b_router_group", [DEPTH, 1, 4])
        self.w_re = self.din("w_router_expert", [DEPTH, D, 32])
        self.b_re = self.din("b_router_expert", [DEPTH, 1, 32])
        self.w_eg = self.din("w_expert_gate", [self.WD, self.NE_DECL, D, EH])
        self.w_eu = self.din("w_expert_up", [self.WD, self.NE_DECL, D, EH])
        self.w_ed = self.din("w_expert_down", [self.WD, self.NE_DECL, EH, D])
        self.w_pg = self.din("w_ple_gate", [DEPTH, D, D])
        self.w_pp = self.din("w_ple_proj", [DEPTH, 256, D])
        self.ln2_g = self.din("ln2_g", [DEPTH, 1, D])
        self.ln2_b = self.din("ln2_b", [DEPTH, 1, D])
        self.out = nc.dram_tensor("out", [SL, D], F32, kind="ExternalOutput").ap()
        self.xT = self.dscr("xT", [D, SL], BF16)
        self.xres = self.dscr("xres", [SL, D], F32)
        self.ropeD = self.dscr("ropeD", [2, 128, SL], BF16)
        self.ropeM = self.dscr("ropeM", [2, 64, SL], BF16)
        self.qT = self.dscr("qT", [16, 128, SL], BF16)
        self.kT = self.dscr("kT", [16, 128, SL], BF16)
        self.vv = self.dscr("vv", [16, SL, 128], BF16)
        self.qrT = self.dscr("qrT", [4, 64, SL], BF16)
        self.krT = self.dscr("krT", [64, SL], BF16)
        self.fT = self.dscr("fT", [4, SL], F32)
        self.cumT = self.dscr("cumT", [4, SL], F32)
        self.qaug = self.dscr("qaug", [4, 3, SL], BF16)
        self.gatesT = self.dscr("gatesT", [4 * D, SL], BF16)
        self.oT = self.dscr("oT", [16, 128, SL], BF16)
        self.mergedT = self.dscr("mergedT", [D, SL], BF16)
        self.hres = self.dscr("hres", [SL, D], F32)
        self.hT = self.dscr("hT", [D, SL], BF16)
        self.hrows = self.dscr("hrows", [NEXP * self.CAP, D], BF16)
        self.yrows = self.dscr("yrows", [NEXP * self.CAP, D], F32)

        with ExitStack() as es:
            self.S = Sched(nc, es)
            self.glob = es
            self.cur = es
            self.cb = self.sb("cb", [128, C_INVF_D], BF16)
            self.cf = self.sb("cf", [128, 34], F32)
            self.S.dma('sp', self.cf.t[:], self.consts[:, C_INVF_D:C_INVF_D + 34], writes=['cf'])
            with self.scope() as es0:
                for c0 in range(0, C_INVF_D, 2048):
                    c1 = min(C_INVF_D, c0 + 2048)
                    cft = self.sb("cft%d" % c0, [128, 2048], F32)
                    self.S.dma('sp', cft.t[:, 0:c1 - c0], self.consts[:, c0:c1], writes=[cft.k])
                    self.S.op('dve', lambda e: e.tensor_copy(out=self.cb.t[:, c0:c1], in_=cft.t[:, 0:c1 - c0]), reads=[cft.k], writes=['cb'])
            self.dest_all = self.sb("dest_all", [128, NCH, 2], I32)
            self.w_all = self.sb("w_all", [128, NCH, 2], F32)
            self.wst = self.ring("wst", 2, [128, 2048], F32)
            self.wbf = self.ring("wbf", 3, [128, 2048], BF16)
            self.epsln = self.sb("epsln", [128, 1], F32)
            self.S.op('dve', lambda e: e.memset(self.epsln.t[:], LN_EPS), writes=['epsln'])
            self.epsrms = self.sb("epsrms", [128, 1], F32)
            self.S.op('dve', lambda e: e.memset(self.epsrms.t[:], RMS_EPS), writes=['epsrms'])

            ph = self.phases
            if ph is None or 'pre' in ph:
                self.phase_pre()
            for l in range(L):
                if ph is None or 'proj' in ph:
                    self.phase_proj(l)
                if ph is None or 'fox' in ph:
                    self.phase_foxprep(l)
                if ph is None or 'attn' in ph:
                    self.phase_attn(l)
                if ph is None or 'merge' in ph:
                    self.phase_merge(l)
                if ph is None or 'wo' in ph:
                    self.phase_wo(l)
                if ph is None or 'exp' in ph:
                    self.phase_experts(l)
                if ph is None or 'ple' in ph:
                    self.phase_ple(l, last=(l == L - 1))
            keys = [k for k in self.S.res.keys() if isinstance(k, tuple) and k and k[0] == 'D']
            self.S.finish(keys)
        return nc

    def cbs(self, c0, n, p=128):
        return self.cb.t[0:p, c0:c0 + n]

    def scope(self):
        return _Scope(self)

    def phase_pre(self):
        S, SL = self.S, self.SL
        with self.scope() as es:
            CB = min(2048, SL)
            posi = self.sb("posi", [128, CB], I32)
            posf = self.sb("posf", [128, CB], F32)
            ang = self.sb("ang", [128, CB], F32)
            ki = self.sb("ki", [128, CB], I32)
            kf = self.sb("kf", [128, CB], F32)
            tb = self.ring("tb", 2, [128, CB], BF16)
            for cb0 in range(0, SL, CB):
                S.dma('sp', posi.t[:], self.pos[0:1, cb0:cb0 + CB].partition_broadcast(128), writes=['posi'])
                S.op('dve', lambda e: e.tensor_copy(out=posf.t[:], in_=posi.t[:]), reads=['posi'], writes=['posf'])
                for kind, (ccol, npart, dst) in enumerate([(0, 128, self.ropeD), (1, 64, self.ropeM)]):
                    for cs, shift in enumerate([math.pi / 2, 0.0]):
                        P = npart
                        S.op('dve', lambda e: e.tensor_scalar(out=ang.t[0:P], in0=posf.t[0:P], scalar1=self.cf.t[0:P, ccol:ccol + 1],
                                                              scalar2=shift, op0=ALU.mult, op1=ALU.add),
                             reads=['posf', 'cf'], writes=['ang'])
                        S.op('dve', lambda e: e.tensor_scalar(out=ki.t[0:P], in0=ang.t[0:P], scalar1=1.0 / TWO_PI, scalar2=None, op0=ALU.mult),
                             reads=['ang'], writes=['ki'])
                        S.op('dve', lambda e: e.tensor_copy(out=kf.t[0:P], in_=ki.t[0:P]), reads=['ki'], writes=['kf'])
                        S.op('dve', lambda e: e.scalar_tensor_tensor(out=ang.t[0:P], in0=kf.t[0:P], scalar=-TWO_PI, in1=ang.t[0:P],
                                                                     op0=ALU.mult, op1=ALU.add), reads=['kf', 'ang'], writes=['ang'])
                        S.op('dve', lambda e: e.tensor_scalar(out=ang.t[0:P], in0=ang.t[0:P], scalar1=-3.1415925, scalar2=3.1415925,
                                                              op0=ALU.max, op1=ALU.min), reads=['ang'], writes=['ang'])
                        o = tb.get()
                        S.op('act', lambda e: e.activation(out=o.t[0:P], in_=ang.t[0:P], func=AF.Sin), reads=['ang'], writes=[o.k])
                        S.dma('sp', dst[cs, :, cb0:cb0 + CB], o.t[0:P], reads=[o.k], writes=[('D', 'rope', kind, cs, cb0)])
            xs = self.ring("xs", 2, [128, CB], F32)
            xb = self.ring("xbp", 2, [128, CB], BF16)
            for k in range(16):
                for cb0 in range(0, SL, CB):
                    a = xs.get()
                    b = xb.get()
                    S.dma('sp', a.t[:], self.x_fm[k * 128:(k + 1) * 128, cb0:cb0 + CB], writes=[a.k])
                    S.op('pool', lambda e: e.tensor_copy(out=b.t[:], in_=a.t[:]), reads=[a.k], writes=[b.k])
                    S.dma('sp', self.xT[k * 128:(k + 1) * 128, cb0:cb0 + CB], b.t[:], reads=[b.k], writes=[('D', 'xT0', k, cb0 // self.TG)])

    def phase_proj(self, l):
        S, SL, TG = self.S, self.SL, self.TG
        NT = TG // 512
        w_in = self.w_in[l].rearrange("(k p) c -> p k c", p=128)
        for tg in range(self.NTG):
            t0 = tg * TG
            with self.scope() as es:
                xt = self.sb("xt", [128, 16, TG], BF16)
                for k in range(16):
                    S.dma('sp', xt.t[:, k, :], self.xT[k * 128:(k + 1) * 128, t0:t0 + TG], reads=([('D', 'xT0', k, tg)] if l == 0 else [('D', 'xT1', c_) for c_ in range(t0 // 128, (t0 + TG) // 128)]), writes=['xt'])
                rD = self.sb("rD", [128, 2, TG], BF16)
                rM = self.sb("rM", [64, 2, TG], BF16)
                for cs in range(2):
                    S.dma('sp', rD.t[:, cs, :], self.ropeD[cs, :, t0:t0 + TG], reads=[('D', 'rope', 0, cs, t0)], writes=['rD'])
                    S.dma('sp', rM.t[:, cs, :], self.ropeM[cs, :, t0:t0 + TG], reads=[('D', 'rope', 1, cs, t0)], writes=['rM'])
                pp = self.ring("pp", 4, [128, 512], F32, psum=True)
                pr = self.ring("prr", 2, [128, 512], F32, psum=True)
                ptr = self.ring("ptr", 2, [128, 1024], BF16, psum=True)
                ob = self.ring("ob", 3, [128, TG], BF16)
                vtb = self.ring("vtb", 2, [128, TG // 128, 128], BF16)
                tmp = self.ring("ptmp", 3, [128, 512], F32)
                cqg = self.sb("cqg", [128, 4, TG], BF16)
                sqq = self.sb("sqq", [128, 4, TG], BF16)
                rq = self.sb("rq", [128, TG], F32)
                gq = self.sb("gq", [128, 4], F32)
                gkv = self.sb("gkv", [128, 2], F32)
                fb = self.sb("fb", [4, 1], F32)
                S.dma('sp', gq.t[:], self.mla_q_norm[l], writes=['gq'])
                S.dma('sp', gkv.t[:], self.mla_kv_norm[l], writes=['gkv'])
                S.dma('sp', fb.t[:], self.fox_f_bias[l], writes=['fb'])
                eng_alt = [0]

                def evac_copy(dst_ap, src_ap, r, w, func=AF.Copy):
                    if func == AF.Copy and eng_alt[0] % 2 == 0:
                        S.op('dve', lambda e: e.tensor_copy(out=dst_ap, in_=src_ap), reads=r, writes=w)
                    else:
                        S.op('act', lambda e: e.activation(out=dst_ap, in_=src_ap, func=func), reads=r, writes=w)
                    eng_alt[0] += 1

                def project(wb, nk, M, rhs_t, rhs_key):
                    outs = []
                    for n in range(NT):
                        p = pp.get()
                        for k in range(nk):
                            S.op('pe', lambda e: e.matmul(p.t[0:M, :], lhsT=wb.t[:, k * M:(k + 1) * M],
                                                          rhs=rhs_t[:, k, n * 512:(n + 1) * 512], start=(k == 0), stop=(k == nk - 1)),
                                 reads=[wb.k, rhs_key], writes=[p.k])
                        outs.append(p)
                    return outs

                def rope_store(o, M, rtab, rmat_c, dst_ap, dkey):
                    o2 = ob.get()
                    for n in range(NT):
                        sl = slice(n * 512, (n + 1) * 512)
                        p = pr.get()
                        S.op('pe', lambda e: e.matmul(p.t[0:M, :], lhsT=self.cbs(rmat_c, M, M), rhs=o.t[0:M, sl], start=True, stop=True),
                             reads=[o.k, 'cb'], writes=[p.k])
                        t1 = tmp.get()
                        S.op('dve', lambda e: e.tensor_tensor(out=t1.t[0:M, :], in0=o.t[0:M, sl], in1=rtab.t[0:M, 0, sl], op=ALU.mult),
                             reads=[o.k, rtab.k], writes=[t1.k])
                        t2 = tmp.get()
                        S.op('dve', lambda e: e.tensor_tensor(out=t2.t[0:M, :], in0=p.t[0:M, :], in1=rtab.t[0:M, 1, sl], op=ALU.mult),
                             reads=[p.k, rtab.k], writes=[t2.k])
                        S.op('dve', lambda e: e.tensor_tensor(out=o2.t[0:M, sl], in0=t1.t[0:M, :], in1=t2.t[0:M, :], op=ALU.add),
                             reads=[t1.k, t2.k], writes=[o2.k])
                    S.dma('sp', dst_ap, o2.t[0:M, :], reads=[o2.k], writes=[dkey])

                def v_store(o, idx):
                    vt = vtb.get()
                    for c8 in range(0, TG // 128, 8):
                        p = ptr.get()
                        nn = min(8, TG // 128 - c8)
                        for j in range(nn):
                            c = c8 + j
                            S.op('pe', lambda e: e.transpose(p.t[:, j * 128:(j + 1) * 128], o.t[:, c * 128:(c + 1) * 128], self.cbs(C_IDENT, 128)),
                                 reads=[o.k, 'cb'], writes=[p.k])
                        evac_copy(vt.t[:, c8:c8 + nn, :], p.t[:, 0:nn * 128].rearrange("p (c d) -> p c d", c=nn), [p.k], [vt.k])
                    S.dma('sp', self.vv[idx, t0:t0 + TG, :].rearrange("(c p) d -> p c d", p=128), vt.t[:], reads=[vt.k],
                          writes=[('D', 'vv', idx, tg)])

                import os
                fams = os.environ.get("PROJ_FAMS")

                def do_tile(fam, c0, M, info):
                    if fams and fam not in fams.split(','):
                        return
                    wb = self.wload(w_in[:, :, c0:c0 + M], 16, M)
                    outs = project(wb, 16, M, xt.t, 'xt')
                    if fam in ('plain', 'gate', 'v', 'rope_d', 'kr'):
                        o = ob.get()
                        for n, p in enumerate(outs):
                            evac_copy(o.t[0:M, n * 512:(n + 1) * 512], p.t[0:M, :], [p.k], [o.k],
                                      func=(AF.Sigmoid if fam == 'gate' else AF.Copy))
                        if fam == 'plain':
                            dst = self.qT if info[0] == 'qT' else self.kT
                            S.dma('sp', dst[info[1], :, t0:t0 + TG], o.t[:], reads=[o.k], writes=[('D', info[0], info[1], tg)])
                        elif fam == 'gate':
                            S.dma('sp', self.gatesT[info * 128:(info + 1) * 128, t0:t0 + TG], o.t[:], reads=[o.k],
                                  writes=[('D', 'gatesT', info, tg)])
                        elif fam == 'v':
                            v_store(o, info)
                        elif fam == 'rope_d':
                            dst = self.qT if info[0] == 'qT' else self.kT
                            rope_store(o, 128, rD, C_RD, dst[info[1], :, t0:t0 + TG], ('D', info[0], info[1], tg))
                        elif fam == 'kr':
                            rope_store(o, 64, rM, C_RM, self.krT[:, t0:t0 + TG], ('D', 'krT', tg))
                    elif fam in ('cq', 'ckv'):
                        gg = gq if fam == 'cq' else gkv
                        for n, p in enumerate(outs):
                            sl = slice(n * 512, (n + 1) * 512)
                            tq = tmp.get()
                            S.op('act', lambda e: e.activation(out=tq.t[:], in_=p.t[:], func=AF.Copy), reads=[p.k], writes=[tq.k])
                            S.op('dve', lambda e: e.tensor_tensor(out=sqq.t[:, info, sl], in0=tq.t[:], in1=tq.t[:], op=ALU.mult), reads=[tq.k], writes=['sqq'])
                            S.op('dve', lambda e: e.tensor_scalar(out=cqg.t[:, info, sl], in0=tq.t[:], scalar1=gg.t[:, info:info + 1], scalar2=None,
                                                                  op0=ALU.mult), reads=[tq.k, gg.k], writes=['cqg'])
                    elif fam == 'f':
                        for n, p in enumerate(outs):
                            ft = tmp.get()
                            S.op('dve', lambda e: e.tensor_scalar(out=ft.t[0:4, :], in0=p.t[0:4, :], scalar1=fb.t[0:4, 0:1], scalar2=None, op0=ALU.add),
                                 reads=[p.k, 'fb'], writes=[ft.k])
                            S.dma('sp', self.fT[:, t0 + n * 512:t0 + (n + 1) * 512], ft.t[0:4, :], reads=[ft.k], writes=[('D', 'fT', tg, n)])

                def rms_scale(nk, dim):
                    for n in range(NT):
                        sl = slice(n * 512, (n + 1) * 512)
                        p = pp.get()
                        for k in range(nk):
                            S.op('pe', lambda e: e.matmul(p.t[:], lhsT=self.cbs(C_ONES, 128), rhs=sqq.t[:, k, sl], start=(k == 0), stop=(k == nk - 1)),
                                 reads=['sqq', 'cb'], writes=[p.k])
                        S.op('act', lambda e: e.activation(out=rq.t[:, sl], in_=p.t[:], func=AF.Sqrt, bias=self.epsrms.t[:, 0:1], scale=1.0 / dim),
                             reads=[p.k, 'epsrms'], writes=['rq'])
                        S.op('dve', lambda e: e.reciprocal(out=rq.t[:, sl], in_=rq.t[:, sl]), reads=['rq'], writes=['rq'])

                def scaled(outs, M):
                    o = ob.get()
                    for n, p in enumerate(outs):
                        sl = slice(n * 512, (n + 1) * 512)
                        S.op('dve', lambda e: e.tensor_tensor(out=o.t[0:M, sl], in0=p.t[0:M, :], in1=rq.t[0:M, sl], op=ALU.mult),
                             reads=[p.k, 'rq'], writes=[o.k])
                    return o
                wuq = self.mla_w_uq[l].rearrange("(k p) c -> p k c", p=128)
                wukv = self.mla_w_ukv[l].rearrange("(k p) c -> p k c", p=128)
                mla_on = os.environ.get("PROJ_MLA", "1") == "1"
                for h in range(4 if mla_on else 0):
                    do_tile('cq', OFF['mla_cq'] + h * 128, 128, h)
                if mla_on:
                    rms_scale(4, 512)
                for h in range(4 if mla_on else 0):
                    wb = self.wload(wuq[:, :, h * 192:h * 192 + 128], 4, 128)
                    o = scaled(project(wb, 4, 128, cqg.t, 'cqg'), 128)
                    S.dma('sp', self.qT[8 + h, :, t0:t0 + TG], o.t[:], reads=[o.k], writes=[('D', 'qT', 8 + h, tg)])
                    wb = self.wload(wuq[:, :, h * 192 + 128:h * 192 + 192], 4, 64)
                    o = scaled(project(wb, 4, 64, cqg.t, 'cqg'), 64)
                    rope_store(o, 64, rM, C_RM, self.qrT[h, :, t0:t0 + TG], ('D', 'qrT', h, tg))
                for j in range(2 if mla_on else 0):
                    do_tile('ckv', OFF['mla_ckv'] + j * 128, 128, j)
                if mla_on:
                    rms_scale(2, 256)
                for h in range(4 if mla_on else 0):
                    wb = self.wload(wukv[:, :, h * 256:h * 256 + 128], 2, 128)
                    o = scaled(project(wb, 2, 128, cqg.t, 'cqg'), 128)
                    S.dma('sp', self.kT[8 + h, :, t0:t0 + TG], o.t[:], reads=[o.k], writes=[('D', 'kT', 8 + h, tg)])
                    wb = self.wload(wukv[:, :, h * 256 + 128:h * 256 + 256], 2, 128)
                    o = scaled(project(wb, 2, 128, cqg.t, 'cqg'), 128)
                    v_store(o, 8 + h)
                do_tile('kr', OFF['mla_kr'], 64, 0)
                do_tile('f', OFF['fox_f'], 4, 0)
                for h in range(4):
                    do_tile('rope_d', OFF['diff_q'] + h * 128, 128, ('qT', 0 + h))
                    do_tile('rope_d', OFF['diff_k'] + h * 128, 128, ('kT', 0 + h))
                    do_tile('v', OFF['diff_v'] + h * 128, 128, 0 + h)
                    do_tile('plain', OFF['fox_q'] + h * 128, 128, ('qT', 4 + h))
                    do_tile('plain', OFF['fox_k'] + h * 128, 128, ('kT', 4 + h))
                    do_tile('v', OFF['fox_v'] + h * 128, 128, 4 + h)
                    do_tile('plain', OFF['sb_q'] + h * 128, 128, ('qT', 12 + h))
                    do_tile('plain', OFF['sb_k'] + h * 128, 128, ('kT', 12 + h))
                    do_tile('v', OFF['sb_v'] + h * 128, 128, 12 + h)
                for g in range(64):
                    do_tile('gate', OFF['gates'] + g * 128, 128, g)

    def phase_foxprep(self, l):
        S, SL = self.S, self.SL
        with self.scope() as es:
            z = self.sb("fz", [4, SL], F32)
            a = self.sb("fa", [4, SL], F32)
            b = self.sb("fb2", [4, SL], F32)
            hb = [self.sb("fh%d" % i, [4, SL], BF16) for i in range(3)]
            rk = [('D', 'fT', tg, n) for tg in range(self.NTG) for n in range(self.TG // 512)]
            S.dma('sp', z.t[:], self.fT[:, :], reads=rk, writes=['fz'])
            S.op('act', lambda e: e.activation(out=a.t[:], in_=z.t[:], func=AF.Exp, scale=-1.0), reads=['fz'], writes=['fa'])
            S.op('act', lambda e: e.activation(out=a.t[:], in_=a.t[:], func=AF.Ln, bias=1.0), reads=['fa'], writes=['fa'])
            S.op('dve', lambda e: e.tensor_scalar(out=a.t[:], in0=a.t[:], scalar1=-1.0, scalar2=None, op0=ALU.mult), reads=['fa'], writes=['fa'])
            S.op('dve', lambda e: e.memset(b.t[:], 1.0), writes=['fb2'])
            S.op('dve', lambda e: e.tensor_tensor_scan(out=z.t[:], data0=b.t[:], data1=a.t[:], initial=0.0, op0=ALU.mult, op1=ALU.add),
                 reads=['fa', 'fb2'], writes=['fz'])
            S.dma('sp', self.cumT[:, :], z.t[:], reads=['fz'], writes=[('D', 'cumT')])
            S.op('dve', lambda e: e.tensor_scalar(out=a.t[:], in0=z.t[:], scalar1=math.sqrt(128.0), scalar2=None, op0=ALU.mult), reads=['fz'], writes=['fa'])
            for i in range(3):
                S.op('dve', lambda e: e.tensor_copy(out=hb[i].t[:], in_=a.t[:]), reads=['fa'], writes=[hb[i].k])
                if i < 2:
                    S.op('dve', lambda e: e.tensor_tensor(out=a.t[:], in0=a.t[:], in1=hb[i].t[:], op=ALU.subtract), reads=['fa', hb[i].k], writes=['fa'])
                for h in range(4):
                    S.dma('sp', self.qaug[h, i:i + 1, :], hb[i].t[h:h + 1, :], reads=[hb[i].k], writes=[('D', 'qaug', h, i)])

    def phase_attn(self, l):
        S, SL, NCH, NQT = self.S, self.SL, self.NCH, self.NQT
        NTG = self.NTG
        with self.scope() as es:
            KT = self.ring("KT", 2, [128, SL], BF16)
            QT = self.ring("QT", 2, [128, SL], BF16)
            VV = self.ring("VV", 2, [128, NCH, 128], BF16)
            OT = self.ring("OT", 2, [128, SL], BF16)
            KR = self.sb("KR", [64, SL], BF16)
            QR = self.ring("QR", 2, [64, SL], BF16)
            QA = self.ring("QA", 2, [3, SL], BF16)
            NCUM = self.ring("NCUM", 2, [128, NCH], F32)
            sps = self.ring("sps", 4, [128, 512], F32, psum=True)
            ops_ = self.ring("ops", 2, [128, 512], F32, psum=True)
            dps = self.ring("dps", 2, [128, 512], F32, psum=True)
            lps = sps
            PT = self.ring("PT", 6, [128, 512], BF16)
            rec = self.ring("rec", 2, [128, 512], F32)
            o0 = self.ring("o0", 2, [128, 512], F32)
            o1 = self.ring("o1", 2, [128, 512], F32)
            f32t = self.ring("f32t", 4, [128, 512], F32)
            sqb = self.ring("sqb", 2, [128, 512], BF16)
            lp = self.sb("lp", [128, 256], F32)
            lpp = self.sb("lpp", [128, 128], F32)
            lam2 = self.sb("lam2", [128, 2], F32)
            neglam = self.sb("neglam", [128, 1], F32)
            gsub = self.sb("gsub", [128, 1], F32)
            lam_init = 0.8 - 0.6 * math.exp(-0.3 * l)
            S.dma('sp', lp.t[:], self.diff_lambda[l, 0:1, :].partition_broadcast(128), writes=['lp'])
            S.dma('sp', gsub.t[:], self.diff_subln[l], writes=['gsub'])
            S.op('dve', lambda e: e.tensor_scalar(out=gsub.t[:], in0=gsub.t[:], scalar1=(1.0 - lam_init), scalar2=None, op0=ALU.mult), reads=['gsub'], writes=['gsub'])
            lp4 = lp.t[:].rearrange("p (a d) -> p a d", a=4)
            lpp2 = lpp.t[:].rearrange("p (a d) -> p a d", a=2)
            S.op('dve', lambda e: e.tensor_tensor(out=lpp2[:, 0, :], in0=lp4[:, 0, :], in1=lp4[:, 1, :], op=ALU.mult), reads=['lp'], writes=['lpp'])
            S.op('dve', lambda e: e.tensor_tensor(out=lpp2[:, 1, :], in0=lp4[:, 2, :], in1=lp4[:, 3, :], op=ALU.mult), reads=['lp'], writes=['lpp'])
            S.op('dve', lambda e: e.reduce_sum(out=lam2.t[:], in_=lpp2, axis=mybir.AxisListType.X), reads=['lpp'], writes=['lam2'])
            S.op('act', lambda e: e.activation(out=lam2.t[:], in_=lam2.t[:], func=AF.Exp), reads=['lam2'], writes=['lam2'])
            S.op('dve', lambda e: e.tensor_tensor(out=neglam.t[:], in0=lam2.t[:, 1:2], in1=lam2.t[:, 0:1], op=ALU.subtract), reads=['lam2'], writes=['neglam'])
            S.op('dve', lambda e: e.tensor_scalar(out=neglam.t[:], in0=neglam.t[:], scalar1=-lam_init, scalar2=None, op0=ALU.add), reads=['neglam'], writes=['neglam'])

            def dkeys(name, idx):
                return [('D', name, idx, tg) for tg in range(NTG)]

            def load_head(idx, need_v=True):
                kt = KT.get()
                qt = QT.get()
                S.dma('sp', kt.t[:], self.kT[idx], reads=dkeys('kT', idx), writes=[kt.k])
                S.dma('sp', qt.t[:], self.qT[idx], reads=dkeys('qT', idx), writes=[qt.k])
                vt = VV.get()
                vsrc = self.vv[idx].rearrange("(c p) d -> p c d", p=128)
                for c0 in range(0, NCH, 8):
                    S.dma('sp', vt.t[:, c0:min(NCH, c0 + 8), :], vsrc[:, c0:min(NCH, c0 + 8), :], reads=dkeys('vv', idx), writes=[(vt.k, c0 // 8)])
                return kt, qt, vt

            def drive(streams):
                live = list(streams)
                while live:
                    nxt = []
                    for g_ in live:
                        try:
                            next(g_)
                            nxt.append(g_)
                        except StopIteration:
                            pass
                    live = nxt

            def softmax_tile(i, parts, scale, madd_c, vt, O, Dn, bias_t=None, aug=None):
                last = 4 * i + 3

                def emit_qk(g):
                    sp_ = sps.get()
                    j0 = g - 4 * i
                    nmm = len(parts) + (1 if aug is not None else 0) + (1 if j0 >= 0 else 0)
                    c = 0
                    for (kt, qt, p0, p1) in parts:
                        S.op('pe', lambda e: e.matmul(sp_.t[:], lhsT=kt.t[p0:p1, g * 128:(g + 1) * 128], rhs=qt.t[p0:p1, i * 512:(i + 1) * 512],
                                                      start=(c == 0), stop=(c == nmm - 1)), reads=[kt.k, qt.k], writes=[sp_.k])
                        c += 1
                    if aug is not None:
                        S.op('pe', lambda e: e.matmul(sp_.t[:], lhsT=self.cb.t[0:3, C_ONES:C_ONES + 128], rhs=aug.t[0:3, i * 512:(i + 1) * 512],
                                                      start=False, stop=(c == nmm - 1)), reads=[aug.k, 'cb'], writes=[sp_.k])
                        c += 1
                    if j0 >= 0:
                        S.op('pe', lambda e: e.matmul(sp_.t[:], lhsT=self.cbs(C_IDENT, 128), rhs=self.cbs(madd_c + j0 * 512, 512),
                                                      start=False, stop=True), reads=['cb'], writes=[sp_.k])
                    return sp_
                nxt = emit_qk(0)
                for g in range(last + 1):
                    sp_ = nxt
                    if g < last:
                        nxt = emit_qk(g + 1)
                    pt = PT.get()
                    if bias_t is not None:
                        S.op('act', lambda e: e.activation(out=pt.t[:], in_=sp_.t[:], func=AF.Exp, scale=scale, bias=bias_t.t[:, g:g + 1]),
                             reads=[sp_.k, bias_t.k], writes=[pt.k])
                    else:
                        S.op('act', lambda e: e.activation(out=pt.t[:], in_=sp_.t[:], func=AF.Exp, scale=scale), reads=[sp_.k], writes=[pt.k])
                    S.op('pe', lambda e: e.matmul(O.t[:], lhsT=vt.t[:, g, :], rhs=pt.t[:], start=(g == 0), stop=(g == last)),
                         reads=[(vt.k, g // 8), pt.k], writes=[O.k])
                    S.op('pe', lambda e: e.matmul(Dn.t[:], lhsT=self.cbs(C_ONES, 128), rhs=pt.t[:], start=(g == 0), stop=(g == last)),
                         reads=['cb', pt.k], writes=[Dn.k])
                    yield

            def normalize(O, Dn, dst_ap, wkey):
                r = rec.get()
                S.op('dve', lambda e: e.reciprocal(out=r.t[:], in_=Dn.t[:]), reads=[Dn.k], writes=[r.k])
                S.op('dve', lambda e: e.tensor_tensor(out=dst_ap, in0=O.t[:], in1=r.t[:], op=ALU.mult), reads=[O.k, r.k], writes=[wkey])

            for h in range(4):
                idx = h
                kt, qt, vt = load_head(idx)
                ot = OT.get()
                for i in range(NQT):
                    accs = [(ops_.get(), dps.get()) for c in range(2)]
                    drive([softmax_tile(i, [(kt, qt, c * 64, (c + 1) * 64)], 64 ** -0.5, C_MADD_CH, vt, accs[c][0], accs[c][1]) for c in range(2)])
                    oc = []
                    for c in range(2):
                        dst = (o0 if c == 0 else o1).get()
                        normalize(accs[c][0], accs[c][1], dst.t[:], dst.k)
                        oc.append(dst)
                    d = f32t.get()
                    S.op('dve', lambda e: e.scalar_tensor_tensor(out=d.t[:], in0=oc[1].t[:], scalar=neglam.t[:, 0:1], in1=oc[0].t[:],
                                                                 op0=ALU.mult, op1=ALU.add), reads=[oc[0].k, oc[1].k, 'neglam'], writes=[d.k])
                    sq = sqb.get()
                    S.op('pool', lambda e: e.tensor_tensor(out=sq.t[:], in0=d.t[:], in1=d.t[:], op=ALU.mult), reads=[d.k], writes=[sq.k])
                    nps = lps.get()
                    S.op('pe', lambda e: e.matmul(nps.t[:], lhsT=self.cbs(C_ONES, 128), rhs=sq.t[:], start=True, stop=True), reads=[sq.k, 'cb'], writes=[nps.k])
                    rt = f32t.get()
                    S.op('act', lambda e: e.activation(out=rt.t[:], in_=nps.t[:], func=AF.Sqrt, bias=self.epsrms.t[:, 0:1], scale=1.0 / 128),
                         reads=[nps.k, 'epsrms'], writes=[rt.k])
                    S.op('dve', lambda e: e.reciprocal(out=rt.t[:], in_=rt.t[:]), reads=[rt.k], writes=[rt.k])
                    S.op('dve', lambda e: e.scalar_tensor_tensor(out=ot.t[:, i * 512:(i + 1) * 512], in0=d.t[:], scalar=gsub.t[:, 0:1], in1=rt.t[:],
                                                                 op0=ALU.mult, op1=ALU.mult), reads=[d.k, rt.k, 'gsub'], writes=[ot.k])
                S.dma('sp', self.oT[idx], ot.t[:], reads=[ot.k], writes=[('D', 'oT', idx)])

            def head_stream(idx, parts_fn, scale, madd_c, bias_t=None, aug=None):
                kt, qt, vt = load_head(idx)
                parts = parts_fn(kt, qt)
                ot = OT.get()
                for i in range(NQT):
                    O = ops_.get()
                    Dn = dps.get()
                    for _ in softmax_tile(i, parts, scale, madd_c, vt, O, Dn, bias_t=bias_t, aug=aug):
                        yield
                    normalize(O, Dn, ot.t[:, i * 512:(i + 1) * 512], ot.k)
                S.dma('sp', self.oT[idx], ot.t[:], reads=[ot.k], writes=[('D', 'oT', idx)])

            def fox_stream(h):
                qa = QA.get()
                S.dma('sp', qa.t[:], self.qaug[h], reads=[('D', 'qaug', h, i) for i in range(3)], writes=[qa.k])
                ncum = NCUM.get()
                with self.nc.allow_non_contiguous_dma(reason="tiny strided column load"):
                    csrc = self.cumT[h].rearrange("(c p) -> p c", p=128)
                    for c0 in range(0, NCH, 8):
                        S.dma('sp', ncum.t[:, c0:min(NCH, c0 + 8)], csrc[:, c0:min(NCH, c0 + 8)], reads=[('D', 'cumT')], writes=[(ncum.k, 'ld', c0 // 8), ncum.k])
                S.op('dve', lambda e: e.tensor_scalar(out=ncum.t[:], in0=ncum.t[:], scalar1=-1.0, scalar2=None, op0=ALU.mult),
                     reads=[(ncum.k, 'ld', j) for j in range((NCH + 7) // 8)], writes=[ncum.k])
                return head_stream(4 + h, lambda kt, qt: [(kt, qt, 0, 128)], 128 ** -0.5, C_MADD_CA, bias_t=ncum, aug=qa)
            for h0 in (0, 2):
                drive([fox_stream(h0), fox_stream(h0 + 1)])

            S.dma('sp', KR.t[:], self.krT[:, :], reads=[('D', 'krT', tg) for tg in range(NTG)], writes=['KR'])

            def mla_stream(h):
                qr = QR.get()
                S.dma('sp', qr.t[:], self.qrT[h], reads=dkeys('qrT', h), writes=[qr.k])
                return head_stream(8 + h, lambda kt, qt: [(kt, qt, 0, 128), (KR, qr, 0, 64)], 192 ** -0.5, C_MADD_CH)
            for h0 in (0, 2):
                drive([mla_stream(h0), mla_stream(h0 + 1)])

        with self.scope() as es:
            KT = self.ring("KT", 2, [128, SL], BF16)
            QT = self.ring("QT", 2, [128, SL], BF16)
            VV = self.ring("VV", 2, [128, NCH, 128], BF16)
            OT = self.ring("OT", 2, [128, SL], BF16)
            sps = self.ring("sps", 4, [128, 512], F32, psum=True)
            ops_ = self.ring("ops", 2, [128, 512], F32, psum=True)
            lps = self.ring("lps", 2, [128, 512], F32, psum=True)
            PT = self.ring("PT", 8, [128, 512], BF16)
            f32t = self.ring("f32t", 10, [128, 512], F32)
            sp32 = self.ring("sp32", 6, [128, 512], F32)
            spb = self.ring("spb", 6, [128, 512], BF16)
            sc = 128 ** -0.5

            def sb_stream(h, cast_eng):
                idx = 12 + h
                kt = KT.get()
                qt = QT.get()
                S.dma('sp', kt.t[:], self.kT[idx], reads=dkeys('kT', idx), writes=[kt.k])
                S.dma('sp', qt.t[:], self.qT[idx], reads=dkeys('qT', idx), writes=[qt.k])
                vt = VV.get()
                vsrc = self.vv[idx].rearrange("(c p) d -> p c d", p=128)
                for c0 in range(0, NCH, 8):
                    S.dma('sp', vt.t[:, c0:min(NCH, c0 + 8), :], vsrc[:, c0:min(NCH, c0 + 8), :], reads=dkeys('vv', idx), writes=[(vt.k, c0 // 8)])
                ot = OT.get()
                spsum = self.sb("spsum%d" % h, [128, 512], F32)
                spsumb = self.ring("spsumb%d" % h, 4, [128, 512], BF16)
                for i in range(NQT):
                    O = ops_.get()
                    last = 4 * i + 3
                    order = list(range(last, -1, -1))
                    n = len(order)
                    st1 = {}
                    st2 = {}
                    ssb_prev = [None]

                    def stage1(g):
                        j0 = g - 4 * i
                        z = sps.get()
                        S.op('pe', lambda e: e.matmul(z.t[:], lhsT=kt.t[:, g * 128:(g + 1) * 128], rhs=qt.t[:, i * 512:(i + 1) * 512], start=True, stop=True),
                             reads=[kt.k, qt.k], writes=[z.k])
                        ee = f32t.get()
                        S.op('act', lambda e: e.activation(out=ee.t[:], in_=z.t[:], func=AF.Exp, scale=sc), reads=[z.k], writes=[ee.k])
                        s32 = sp32.get()
                        S.op('act', lambda e: e.activation(out=s32.t[:], in_=ee.t[:], func=AF.Ln, bias=1.0), reads=[ee.k], writes=[s32.k])
                        sb_ = spb.get()
                        if j0 >= 0:
                            S.op('pool', lambda e: e.tensor_tensor(out=sb_.t[:], in0=s32.t[:], in1=self.cbs(C_MMUL + j0 * 512, 512), op=ALU.mult),
                                 reads=[s32.k, 'cb'], writes=[sb_.k])
                        else:
                            S.op('pool', lambda e: e.tensor_copy(out=sb_.t[:], in_=s32.t[:]), reads=[s32.k], writes=[sb_.k])
                        ssb_in = ssb_prev[0]
                        if g > 0:
                            if g == last:
                                S.op('dve', lambda e: e.tensor_copy(out=spsum.t[:], in_=sb_.t[:]), reads=[sb_.k], writes=[spsum.k])
                            else:
                                S.op('dve', lambda e: e.tensor_tensor(out=spsum.t[:], in0=spsum.t[:], in1=sb_.t[:], op=ALU.add), reads=[spsum.k, sb_.k], writes=[spsum.k])
                            ssb = spsumb.get()
                            S.op('act', lambda e: e.activation(out=ssb.t[:], in_=spsum.t[:], func=AF.Copy), reads=[spsum.k], writes=[ssb.k])
                            ssb_prev[0] = ssb
                        st1[g] = (z, s32, sb_, ssb_in)

                    def stage2(g):
                        j0 = g - 4 * i
                        z, s32, sb_, ssb_in = st1.pop(g)
                        first = (g == last)
                        Lp = lps.get()
                        S.op('pe', lambda e: e.matmul(Lp.t[:], lhsT=self.cbs(C_TRIS, 128), rhs=sb_.t[:], start=True, stop=first), reads=[sb_.k, 'cb'], writes=[Lp.k])
                        if not first:
                            ssb = ssb_in
                            S.op('pe', lambda e: e.matmul(Lp.t[:], lhsT=self.cbs(C_ONES, 128), rhs=ssb.t[:], start=False, stop=True), reads=[ssb.k, 'cb'], writes=[Lp.k])
                        t1 = f32t.get()
                        S.op('dve', lambda e: e.scalar_tensor_tensor(out=t1.t[:], in0=z.t[:], scalar=sc, in1=s32.t[:], op0=ALU.mult, op1=ALU.subtract),
                             reads=[z.k, s32.k], writes=[t1.k])
                        S.op('dve', lambda e: e.tensor_tensor(out=t1.t[:], in0=t1.t[:], in1=Lp.t[:], op=ALU.subtract), reads=[t1.k, Lp.k], writes=[t1.k])
                        wt = PT.get()
                        S.op('act', lambda e: e.activation(out=wt.t[:], in_=t1.t[:], func=AF.Exp), reads=[t1.k], writes=[wt.k])
                        if j0 >= 0:
                            S.op('pool', lambda e: e.tensor_tensor(out=wt.t[:], in0=wt.t[:], in1=self.cbs(C_MMUL + j0 * 512, 512), op=ALU.mult),
                                 reads=[wt.k, 'cb'], writes=[wt.k])
                        st2[g] = wt

                    def stage3(g):
                        wt = st2.pop(g)
                        S.op('pe', lambda e: e.matmul(O.t[:], lhsT=vt.t[:, g, :], rhs=wt.t[:], start=(g == last), stop=(g == 0)), reads=[(vt.k, g // 8), wt.k], writes=[O.k])
                    for step in range(n + 2):
                        if step < n:
                            stage1(order[step])
                        if 0 <= step - 1 < n:
                            stage2(order[step - 1])
                        if 0 <= step - 2 < n:
                            stage3(order[step - 2])
                        yield
                    S.op('act', lambda e: e.activation(out=ot.t[:, i * 512:(i + 1) * 512], in_=O.t[:], func=AF.Copy), reads=[O.k], writes=[ot.k])
                S.dma('sp', self.oT[idx], ot.t[:], reads=[ot.k], writes=[('D', 'oT', idx)])
            for h0 in (0, 2):
                drive([sb_stream(h0, 'dve'), sb_stream(h0 + 1, 'dve')])

    def phase_merge(self, l):
        S, SL, TG = self.S, self.SL, self.TG
        NT = TG // 512
        for tg in range(self.NTG):
            t0 = tg * TG
            with self.scope() as es:
                ot = self.sb("mot", [128, 16, TG], BF16)
                for i in range(16):
                    S.dma('sp', ot.t[:, i, :], self.oT[i, :, t0:t0 + TG], reads=[('D', 'oT', i)], writes=['mot'])
                pp = self.ring("mpp", 4, [128, 512], F32, psum=True)
                gt = self.ring("mgt", 2, [128, TG], BF16)
                acc = self.sb("macc", [128, TG], F32)
                mo = self.ring("mmo", 2, [128, TG], BF16)
                tmp = self.ring("mtmp", 2, [128, 512], F32)
                for c in range(16):
                    for n_ in range(4):
                        wb = self.wload(self.w_branch[l, n_].rearrange("(k p) c -> p k c", p=128)[:, :, c * 128:(c + 1) * 128], 4, 128)
                        g = gt.get()
                        grow = n_ * 16 + c
                        S.dma('sp', g.t[:], self.gatesT[grow * 128:(grow + 1) * 128, t0:t0 + TG], reads=[('D', 'gatesT', grow, tg)], writes=[g.k])
                        wv = wb.t[:, 0:512].rearrange("p (k m) -> p k m", k=4)
                        for n in range(NT):
                            sl = slice(n * 512, (n + 1) * 512)
                            p = pp.get()
                            for k in range(4):
                                S.op('pe', lambda e: e.matmul(p.t[:], lhsT=wv[:, k, :], rhs=ot.t[:, n_ * 4 + k, sl], start=(k == 0), stop=(k == 3)),
                                     reads=[wb.k, 'mot'], writes=[p.k])
                            if n_ == 0:
                                S.op('dve', lambda e: e.tensor_tensor(out=acc.t[:, sl], in0=p.t[:], in1=g.t[:, sl], op=ALU.mult), reads=[p.k, g.k], writes=['macc'])
                            else:
                                t = tmp.get()
                                S.op('dve', lambda e: e.tensor_tensor(out=t.t[:], in0=p.t[:], in1=g.t[:, sl], op=ALU.mult), reads=[p.k, g.k], writes=[t.k])
                                S.op('pool', lambda e: e.tensor_tensor(out=acc.t[:, sl], in0=acc.t[:, sl], in1=t.t[:], op=ALU.add), reads=['macc', t.k], writes=['macc'])
                    m = mo.get()
                    S.op('act', lambda e: e.activation(out=m.t[:], in_=acc.t[:], func=AF.Copy), reads=['macc'], writes=[m.k])
                    S.dma('sp', self.mergedT[c * 128:(c + 1) * 128, t0:t0 + TG], m.t[:], reads=[m.k], writes=[('D', 'mergedT', c, tg)])

    def load_resident(self, dst, w_ap, nk):
        S = self.S
        wv = w_ap.rearrange("(k p) c -> p k c", p=128)
        for k in range(nk):
            st = self.wst.get()
            S.dma('sp', st.t[:], wv[:, k, :], writes=[st.k])
            self.cast(dst.t[:, k, :], st.t[:], [st.k], [dst.k])

    def layer_norm(self, v, stt, mvt, g_t, b_t, out):
        S = self.S
        for j in range(4):
            S.op('dve', lambda e: e.bn_stats(out=stt.t[:, j * 6:(j + 1) * 6], in_=v.t[:, j * 512:(j + 1) * 512]), reads=[v.k], writes=[stt.k])
        S.op('dve', lambda e: e.bn_aggr(out=mvt.t[:, 0:2], in_=stt.t[:]), reads=[stt.k], writes=[mvt.k])
        S.op('act', lambda e: e.activation(out=mvt.t[:, 2:3], in_=mvt.t[:, 1:2], func=AF.Sqrt, bias=self.epsln.t[:, 0:1], scale=1.0),
             reads=[mvt.k, 'epsln'], writes=[mvt.k])
        S.op('dve', lambda e: e.reciprocal(out=mvt.t[:, 3:4], in_=mvt.t[:, 2:3]), reads=[mvt.k], writes=[mvt.k])
        S.op('dve', lambda e: e.tensor_scalar(out=out.t[:], in0=v.t[:], scalar1=mvt.t[:, 0:1], scalar2=mvt.t[:, 3:4], op0=ALU.subtract, op1=ALU.mult),
             reads=[v.k, mvt.k], writes=[out.k])
        S.op('pool', lambda e: e.tensor_tensor(out=out.t[:], in0=out.t[:], in1=g_t.t[:], op=ALU.mult), reads=[out.k, g_t.k], writes=[out.k])
        S.op('dve', lambda e: e.tensor_tensor(out=out.t[:], in0=out.t[:], in1=b_t.t[:], op=ALU.add), reads=[out.k, b_t.k], writes=[out.k])

    def transpose_store(self, hb, ptr, dstT_tile, evac_eng='act'):
        S = self.S
        for k8 in range(0, 16, 8):
            p = ptr.get()
            for j in range(8):
                k = k8 + j
                S.op('pe', lambda e: e.transpose(p.t[:, j * 128:(j + 1) * 128], hb.t[:, k * 128:(k + 1) * 128], self.cbs(C_IDENT, 128)),
                     reads=[hb.k, 'cb'], writes=[p.k])
            S.op('act', lambda e: e.activation(out=dstT_tile.t[:, k8:k8 + 8, :], in_=p.t[:].rearrange("p (c d) -> p c d", c=8), func=AF.Copy),
                 reads=[p.k], writes=[dstT_tile.k])

    def phase_wo(self, l):
        S, SL, NCH = self.S, self.SL, self.NCH
        CAP = self.CAP
        with self.scope() as es:
            wo = self.sb("wo", [128, 16, D], BF16)
            self.load_resident(wo, self.w_o[l], 16)
            wr = self.sb("wr", [128, 16, 36], BF16)
            wrs = self.sb("wrs", [128, 16, 36], F32)
            S.dma('sp', wrs.t[:, :, 0:4], self.w_rg[l].rearrange("(k p) c -> p k c", p=128), writes=['wrs'])
            S.dma('sp', wrs.t[:, :, 4:36], self.w_re[l].rearrange("(k p) c -> p k c", p=128), writes=['wrs'])
            S.op('dve', lambda e: e.tensor_copy(out=wr.t[:], in_=wrs.t[:]), reads=['wrs'], writes=['wr'])
            brt = self.sb("brt", [128, 36], F32)
            S.dma('sp', brt.t[:, 0:4], self.b_rg[l, 0:1, :].partition_broadcast(128), writes=['brt'])
            S.dma('sp', brt.t[:, 4:36], self.b_re[l, 0:1, :].partition_broadcast(128), writes=['brt'])
            g_t = self.sb("l1g", [128, D], F32)
            b_t = self.sb("l1b", [128, D], F32)
            S.dma('sp', g_t.t[:], self.ln1_g[l, 0:1, :].partition_broadcast(128), writes=['l1g'])
            S.dma('sp', b_t.t[:], self.ln1_b[l, 0:1, :].partition_broadcast(128), writes=['l1b'])
            base = self.sb("rbase", [128, 32], F32)
            S.op('dve', lambda e: e.memset(base.t[:], 0.0), writes=['rbase'])
            mT = self.ring("wmT", 2, [128, 16, 128], BF16)
            xr = self.ring("wxr", 2, [128, D], F32)
            ht = self.ring("wh", 2, [128, D], F32)
            hb = self.ring("whb", 2, [128, D], BF16)
            hTt = self.ring("whT", 2, [128, 16, 128], BF16)
            stt = self.ring("wstt", 2, [128, 24], F32)
            mvt = self.ring("wmv", 2, [128, 4], F32)
            pp = self.ring("wpp", 4, [128, 512], F32, psum=True)
            ptr = self.ring("wptr", 2, [128, 1024], BF16, psum=True)
            prt = self.ring("wprt", 2, [128, 64], F32, psum=True)
            sm = self.ring("wsm", 2, [128, 256], F32)
            mb = self.ring("wmb", 2, [128, 32], BF16)
            x_src = self.x_tm if l == 0 else self.xres
            mres = [('D', 'mergedT', c, tg) for c in range(16) for tg in range(self.NTG)]
            def stageA(c):
                r0 = c * 128
                m = mT.get()
                S.dma('sp', m.t[:], self.mergedT[:, r0:r0 + 128].rearrange("(k p) t -> p k t", p=128), reads=mres, writes=[m.k])
                x = xr.get()
                S.dma('sp', x.t[:], x_src[r0:r0 + 128, :], reads=([('D', 'xres', c)] if l > 0 else []), writes=[x.k])
                v = x
                for ct in range(4):
                    p = pp.get()
                    for k in range(16):
                        S.op('pe', lambda e: e.matmul(p.t[:], lhsT=m.t[:, k, :], rhs=wo.t[:, k, ct * 512:(ct + 1) * 512], start=(k == 0), stop=(k == 15)),
                             reads=[m.k, 'wo'], writes=[p.k])
                    S.op('dve', lambda e: e.scalar_tensor_tensor(out=v.t[:, ct * 512:(ct + 1) * 512], in0=x.t[:, ct * 512:(ct + 1) * 512], scalar=ALPHA,
                                                                 in1=p.t[:], op0=ALU.mult, op1=ALU.add), reads=[x.k, p.k], writes=[v.k])
                h = ht.get()
                self.layer_norm(v, stt.get(), mvt.get(), g_t, b_t, h)
                hbb = hb.get()
                S.op('act', lambda e: e.activation(out=hbb.t[:], in_=h.t[:], func=AF.Copy), reads=[h.k], writes=[hbb.k])
                return h, hbb

            def stageB(c, h, hbb):
                r0 = c * 128
                S.dma('sp', self.hres[r0:r0 + 128, :], h.t[:], reads=[h.k], writes=[('D', 'hres', c)])
                hT = hTt.get()
                self.transpose_store(hbb, ptr, hT)
                S.dma('sp', self.hT[:, r0:r0 + 128].rearrange("(k p) t -> p k t", p=128), hT.t[:], reads=[hT.k], writes=[('D', 'hT', c)])
                pr = prt.get()
                for k in range(16):
                    S.op('pe', lambda e: e.matmul(pr.t[:, 0:36], lhsT=hT.t[:, k, :], rhs=wr.t[:, k, :], start=(k == 0), stop=(k == 15)),
                         reads=[hT.k, 'wr'], writes=[pr.k])
                s = sm.get()
                sk = s.k
                A = s.t

                def dv(fn, extra_r=(), w=None):
                    S.op('dve', fn, reads=[sk] + list(extra_r), writes=[w or sk])
                dv(lambda e: e.tensor_tensor(out=A[:, 0:36], in0=pr.t[:, 0:36], in1=brt.t[:], op=ALU.add), extra_r=[pr.k, 'brt'])
                dv(lambda e: e.reduce_max(out=A[:, 36:37], in_=A[:, 0:4], axis=mybir.AxisListType.X))
                dv(lambda e: e.tensor_scalar(out=A[:, 224:228], in0=A[:, 0:4], scalar1=A[:, 36:37], scalar2=None, op0=ALU.subtract))
                S.op('act', lambda e: e.activation(out=A[:, 224:228], in_=A[:, 224:228], func=AF.Exp), reads=[sk], writes=[sk])
                dv(lambda e: e.reduce_sum(out=A[:, 37:38], in_=A[:, 224:228], axis=mybir.AxisListType.X))
                dv(lambda e: e.reciprocal(out=A[:, 38:39], in_=A[:, 37:38]))
                dv(lambda e: e.tensor_scalar(out=A[:, 40:44], in0=A[:, 0:4], scalar1=A[:, 36:37], scalar2=None, op0=ALU.is_equal))
                dv(lambda e: e.tensor_scalar(out=A[:, 44:52], in0=A[:, 4:12], scalar1=A[:, 40:41], scalar2=None, op0=ALU.mult))
                for j in range(1, 4):
                    dv(lambda e: e.scalar_tensor_tensor(out=A[:, 44:52], in0=A[:, 4 + 8 * j:12 + 8 * j], scalar=A[:, 40 + j:41 + j], in1=A[:, 44:52],
                                                        op0=ALU.mult, op1=ALU.add))
                dv(lambda e: e.reduce_max(out=A[:, 52:53], in_=A[:, 44:52], axis=mybir.AxisListType.X))
                dv(lambda e: e.tensor_scalar(out=A[:, 56:64], in0=A[:, 44:52], scalar1=A[:, 52:53], scalar2=None, op0=ALU.is_equal))
                dv(lambda e: e.scalar_tensor_tensor(out=A[:, 72:80], in0=A[:, 56:64], scalar=-1e30, in1=A[:, 44:52], op0=ALU.mult, op1=ALU.add))
                dv(lambda e: e.reduce_max(out=A[:, 53:54], in_=A[:, 72:80], axis=mybir.AxisListType.X))
                dv(lambda e: e.tensor_scalar(out=A[:, 64:72], in0=A[:, 72:80], scalar1=A[:, 53:54], scalar2=None, op0=ALU.is_equal))
                dv(lambda e: e.tensor_tensor(out=A[:, 80:81], in0=A[:, 53:54], in1=A[:, 52:53], op=ALU.subtract))
                S.op('act', lambda e: e.activation(out=A[:, 80:81], in_=A[:, 80:81], func=AF.Exp), reads=[sk], writes=[sk])
                dv(lambda e: e.tensor_scalar(out=A[:, 81:82], in0=A[:, 80:81], scalar1=1.0, scalar2=None, op0=ALU.add))
                dv(lambda e: e.reciprocal(out=A[:, 81:82], in_=A[:, 81:82]))
                dv(lambda e: e.tensor_tensor(out=A[:, 81:82], in0=A[:, 81:82], in1=A[:, 38:39], op=ALU.mult))
                dv(lambda e: e.tensor_tensor(out=A[:, 82:83], in0=A[:, 38:39], in1=A[:, 81:82], op=ALU.subtract))
                S.op('dve', lambda e: e.tensor_copy(out=self.w_all.t[:, c, :], in_=A[:, 81:83]), reads=[sk], writes=['w_all'])
                for j in range(4):
                    dv(lambda e: e.tensor_scalar(out=A[:, 96 + 8 * j:104 + 8 * j], in0=A[:, 56:64], scalar1=A[:, 40 + j:41 + j], scalar2=None, op0=ALU.mult))
                    dv(lambda e: e.tensor_scalar(out=A[:, 128 + 8 * j:136 + 8 * j], in0=A[:, 64:72], scalar1=A[:, 40 + j:41 + j], scalar2=None, op0=ALU.mult))
                dv(lambda e: e.tensor_tensor(out=A[:, 160:192], in0=A[:, 96:128], in1=A[:, 128:160], op=ALU.add))
                mbb = mb.get()
                S.op('dve', lambda e: e.tensor_copy(out=mbb.t[:], in_=A[:, 160:192]), reads=[sk], writes=[mbb.k])
                pc = prt.get()
                S.op('pe', lambda e: e.matmul(pc.t[:, 0:32], lhsT=self.cbs(C_TRIL, 128), rhs=mbb.t[:], start=True, stop=True), reads=[mbb.k, 'cb'], writes=[pc.k])
                S.op('pe', lambda e: e.matmul(pc.t[:, 32:64], lhsT=self.cbs(C_ONES, 128), rhs=mbb.t[:], start=True, stop=True), reads=[mbb.k, 'cb'], writes=[pc.k])
                dv(lambda e: e.tensor_tensor(out=A[:, 192:224], in0=pc.t[:, 0:32], in1=base.t[:], op=ALU.add), extra_r=[pc.k, 'rbase'])
                dv(lambda e: e.tensor_tensor(out=A[:, 192:224], in0=A[:, 192:224], in1=self.cf.t[:, 2:34], op=ALU.add), extra_r=['cf'])
                S.op('dve', lambda e: e.tensor_tensor(out=base.t[:], in0=base.t[:], in1=pc.t[:, 32:64], op=ALU.add), reads=['rbase', pc.k, sk], writes=['rbase'])
                dv(lambda e: e.tensor_tensor(out=A[:, 96:128], in0=A[:, 96:128], in1=A[:, 192:224], op=ALU.mult))
                dv(lambda e: e.tensor_tensor(out=A[:, 128:160], in0=A[:, 128:160], in1=A[:, 192:224], op=ALU.mult))
                dv(lambda e: e.reduce_sum(out=A[:, 228:229], in_=A[:, 96:128], axis=mybir.AxisListType.X))
                dv(lambda e: e.reduce_sum(out=A[:, 229:230], in_=A[:, 128:160], axis=mybir.AxisListType.X))
                S.op('dve', lambda e: e.tensor_copy(out=self.dest_all.t[:, c, :], in_=A[:, 228:230]), reads=[sk], writes=['dest_all'])
                for kk in range(2):
                    S.dma('pool', None, None, reads=[hbb.k, 'dest_all'], writes=[('D', 'hrows', c, kk)],
                          fn=lambda e: e.indirect_dma_start(out=self.hrows[:, :], out_offset=bass.IndirectOffsetOnAxis(ap=self.dest_all.t[:, c, kk:kk + 1], axis=0),
                                                            in_=hbb.t[:], in_offset=None))

            cur = stageA(0)
            for c in range(NCH):
                nxt = stageA(c + 1) if c + 1 < NCH else None
                stageB(c, *cur)
                cur = nxt

    def phase_experts(self, l):
        S, NCH, CAP, CAPB = self.S, self.NCH, self.CAP, self.CAPB
        with self.scope() as es:
            rows = self.ring("erow", 2, [128, D], BF16)
            rT = self.ring("erT", 2, [128, 16, CAP], BF16)
            hidT = self.ring("ehid", 2, [128, 4, CAP], BF16)
            sil = self.ring("esil", 2, [128, CAP], F32)
            ysb = self.ring("eysb", CAPB, [128, D], F32)
            ptr = self.ring("eptr", 2, [128, 1024], BF16, psum=True)
            pg = self.ring("epg", 2, [128, 512], F32, psum=True)
            pu = self.ring("epu", 2, [128, 512], F32, psum=True)
            py = self.ring("epy", 2, [128, 512], F32, psum=True)
            hr = [('D', 'hrows', c, kk) for c in range(NCH) for kk in range(2)]
            EST = self.ring("est", 4, [128, 2048], F32)
            WG = self.ring("ewg", 2, [128, 16, EH], BF16)
            WU = self.ring("ewu", 2, [128, 16, EH], BF16)
            def load_gu(ex_):
                weg_ = self.w_eg[l, ex_].rearrange("(k p) c -> p k c", p=128)
                weu_ = self.w_eu[l, ex_].rearrange("(k p) c -> p k c", p=128)
                wg_ = WG.get()
                wu_ = WU.get()
                for (src, dstb) in ((weg_, wg_), (weu_, wu_)):
                    for kq in range(4):
                        st = EST.get()
                        sk4 = [(st.k, jj) for jj in range(4)]
                        S.dma('sp', st.t[:].rearrange("p (a b) -> p a b", a=4), src[:, kq * 4:(kq + 1) * 4, :], writes=sk4)
                        self.cast(dstb.t[:, kq * 4:(kq + 1) * 4, :], st.t[:].rearrange("p (a b) -> p a b", a=4), sk4, [(dstb.k, kq)])
                return wg_, wu_
            nxt_w = None
            for ex in range(NEXP):
                rt = rT.get()
                for b in range(CAPB):
                    r = rows.get()
                    S.dma('sp', r.t[:], self.hrows[ex * CAP + b * 128:ex * CAP + (b + 1) * 128, :], reads=hr, writes=[r.k])
                    for k8 in range(0, 16, 8):
                        p = ptr.get()
                        for j in range(8):
                            k = k8 + j
                            S.op('pe', lambda e: e.transpose(p.t[:, j * 128:(j + 1) * 128], r.t[:, k * 128:(k + 1) * 128], self.cbs(C_IDENT, 128)),
                                 reads=[r.k, 'cb'], writes=[p.k])
                        S.op('act' if k8 == 0 else 'dve',
                             (lambda e: e.activation(out=rt.t[:, k8:k8 + 8, b * 128:(b + 1) * 128], in_=p.t[:].rearrange("p (c d) -> p c d", c=8), func=AF.Copy)) if k8 == 0 else
                             (lambda e: e.tensor_copy(out=rt.t[:, k8:k8 + 8, b * 128:(b + 1) * 128], in_=p.t[:].rearrange("p (c d) -> p c d", c=8))),
                             reads=[p.k], writes=[rt.k])
                hd = hidT.get()
                weg = self.w_eg[l, ex].rearrange("(k p) c -> p k c", p=128)
                weu = self.w_eu[l, ex].rearrange("(k p) c -> p k c", p=128)
                if ex == 0:
                    nxt_w = load_gu(0)
                wg, wu = nxt_w
                if ex + 1 < NEXP:
                    nxt_w = load_gu(ex + 1)
                for j in range(4):
                    g_ = pg.get()
                    u_ = pu.get()
                    for k in range(16):
                        S.op('pe', lambda e: e.matmul(g_.t[:, 0:CAP], lhsT=wg.t[:, k, j * 128:(j + 1) * 128], rhs=rt.t[:, k, :], start=(k == 0), stop=(k == 15)),
                             reads=[(wg.k, k // 4), rt.k], writes=[g_.k])
                    for k in range(16):
                        S.op('pe', lambda e: e.matmul(u_.t[:, 0:CAP], lhsT=wu.t[:, k, j * 128:(j + 1) * 128], rhs=rt.t[:, k, :], start=(k == 0), stop=(k == 15)),
                             reads=[(wu.k, k // 4), rt.k], writes=[u_.k])
                    s_ = sil.get()
                    S.op('act', lambda e: e.activation(out=s_.t[:], in_=g_.t[:, 0:CAP], func=AF.Silu), reads=[g_.k], writes=[s_.k])
                    S.op('dve', lambda e: e.tensor_tensor(out=hd.t[:, j, :], in0=s_.t[:], in1=u_.t[:, 0:CAP], op=ALU.mult), reads=[s_.k, u_.k], writes=[hd.k])
                wed = self.w_ed[l, ex].rearrange("(k p) c -> p k c", p=128)
                ys = [ysb.get() for _ in range(CAPB)]
                for ct in range(4):
                    wd = self.wload(wed[:, :, ct * 512:(ct + 1) * 512], 4, 512)
                    wdv = wd.t[:].rearrange("p (k m) -> p k m", k=4)
                    for b in range(CAPB):
                        y_ = py.get()
                        for j in range(4):
                            S.op('pe', lambda e: e.matmul(y_.t[:], lhsT=hd.t[:, j, b * 128:(b + 1) * 128], rhs=wdv[:, j, :], start=(j == 0), stop=(j == 3)),
                                 reads=[hd.k, wd.k], writes=[y_.k])
                        if (ct + b) % 2 == 0:
                            S.op('act', lambda e: e.activation(out=ys[b].t[:, ct * 512:(ct + 1) * 512], in_=y_.t[:], func=AF.Copy), reads=[y_.k], writes=[ys[b].k])
                        else:
                            S.op('dve', lambda e: e.tensor_copy(out=ys[b].t[:, ct * 512:(ct + 1) * 512], in_=y_.t[:]), reads=[y_.k], writes=[ys[b].k])
                for b in range(CAPB):
                    S.dma('sp', self.yrows[ex * CAP + b * 128:ex * CAP + (b + 1) * 128, :], ys[b].t[:], reads=[ys[b].k], writes=[('D', 'yrows', ex, b)])

    def phase_ple(self, l, last):
        S, SL, NCH, CAPB = self.S, self.SL, self.NCH, self.CAPB
        with self.scope() as es:
            wpg = self.sb("wpg", [128, 16, D], BF16)
            self.load_resident(wpg, self.w_pg[l], 16)
            wpp = self.sb("wpp", [128, 2, D], BF16)
            self.load_resident(wpp, self.w_pp[l], 2)
            g_t = self.sb("l2g", [128, D], F32)
            b_t = self.sb("l2b", [128, D], F32)
            S.dma('sp', g_t.t[:], self.ln2_g[l, 0:1, :].partition_broadcast(128), writes=['l2g'])
            S.dma('sp', b_t.t[:], self.ln2_b[l, 0:1, :].partition_broadcast(128), writes=['l2b'])
            hTt = self.ring("phT", 2, [128, 16, 128], BF16)
            pTs = self.ring("ppTs", 2, [128, 2, 128], F32)
            pTb = self.ring("ppTb", 2, [128, 2, 128], BF16)
            hr = self.ring("phr", 2, [128, D], F32)
            y1 = self.ring("py1", 1, [128, D], F32)
            y2 = self.ring("py2", 1, [128, D], F32)
            sg = self.ring("psg", 1, [128, D], F32)
            xb = self.ring("pxb", 1, [128, D], BF16)
            xTt = self.ring("pxT", 2, [128, 16, 128], BF16)
            stt = self.ring("pstt", 2, [128, 24], F32)
            mvt = self.ring("pmv", 2, [128, 4], F32)
            ppg = self.ring("ppg", 3, [128, 512], F32, psum=True)
            ppp = self.ring("ppp", 3, [128, 512], F32, psum=True)
            ptr = self.ring("pptr", 2, [128, 1024], BF16, psum=True)
            yr = [('D', 'yrows', ex, b) for ex in range(NEXP) for b in range(CAPB)]
            for c in range(NCH):
                r0 = c * 128
                hT = hTt.get()
                S.dma('sp', hT.t[:], self.hT[:, r0:r0 + 128].rearrange("(k p) t -> p k t", p=128), reads=[('D', 'hT', c)], writes=[hT.k])
                ps_ = pTs.get()
                S.dma('sp', ps_.t[:], self.pT[l, :, r0:r0 + 128].rearrange("(k p) t -> p k t", p=128), writes=[ps_.k])
                pb = pTb.get()
                S.op('pool', lambda e: e.tensor_copy(out=pb.t[:], in_=ps_.t[:]), reads=[ps_.k], writes=[pb.k])
                h = hr.get()
                S.dma('sp', h.t[:], self.hres[r0:r0 + 128, :], reads=[('D', 'hres', c)], writes=[h.k])
                ya = y1.get()
                yb = y2.get()
                for kk, yy in ((0, ya), (1, yb)):
                    S.dma('pool', None, None, reads=yr + ['dest_all'], writes=[yy.k],
                          fn=lambda e: e.indirect_dma_start(out=yy.t[:], out_offset=None, in_=self.yrows[:, :],
                                                            in_offset=bass.IndirectOffsetOnAxis(ap=self.dest_all.t[:, c, kk:kk + 1], axis=0)))
                s_ = sg.get()
                v = s_
                for ct in range(4):
                    sl = slice(ct * 512, (ct + 1) * 512)
                    pg_ = ppg.get()
                    for k in range(16):
                        S.op('pe', lambda e: e.matmul(pg_.t[:], lhsT=hT.t[:, k, :], rhs=wpg.t[:, k, sl], start=(k == 0), stop=(k == 15)), reads=[hT.k, 'wpg'], writes=[pg_.k])
                    pp_ = ppp.get()
                    for k in range(2):
                        S.op('pe', lambda e: e.matmul(pp_.t[:], lhsT=pb.t[:, k, :], rhs=wpp.t[:, k, sl], start=(k == 0), stop=(k == 1)), reads=[pb.k, 'wpp'], writes=[pp_.k])
                    S.op('act', lambda e: e.activation(out=s_.t[:, sl], in_=pg_.t[:], func=AF.Sigmoid), reads=[pg_.k], writes=[s_.k])
                    S.op('dve', lambda e: e.tensor_tensor(out=s_.t[:, sl], in0=s_.t[:, sl], in1=pp_.t[:], op=ALU.mult), reads=[s_.k, pp_.k], writes=[s_.k])
                S.op('dve', lambda e: e.scalar_tensor_tensor(out=v.t[:], in0=h.t[:], scalar=ALPHA, in1=s_.t[:], op0=ALU.mult, op1=ALU.add), reads=[h.k, s_.k], writes=[v.k])
                S.op('dve', lambda e: e.scalar_tensor_tensor(out=v.t[:], in0=ya.t[:], scalar=self.w_all.t[:, c, 0:1], in1=v.t[:], op0=ALU.mult, op1=ALU.add),
                     reads=[ya.k, 'w_all', v.k], writes=[v.k])
                S.op('dve', lambda e: e.scalar_tensor_tensor(out=v.t[:], in0=yb.t[:], scalar=self.w_all.t[:, c, 1:2], in1=v.t[:], op0=ALU.mult, op1=ALU.add),
                     reads=[yb.k, 'w_all', v.k], writes=[v.k])
                x = h
                self.layer_norm(v, stt.get(), mvt.get(), g_t, b_t, x)
                if last:
                    S.dma('sp', self.out[r0:r0 + 128, :], x.t[:], reads=[x.k], writes=[('D', 'out', c)])
                else:
                    S.dma('sp', self.xres[r0:r0 + 128, :], x.t[:], reads=[x.k], writes=[('D', 'xres', c)])
                    xbb = xb.get()
                    S.op('act', lambda e: e.activation(out=xbb.t[:], in_=x.t[:], func=AF.Copy), reads=[x.k], writes=[xbb.k])
                    xT = xTt.get()
                    self.transpose_store(xbb, ptr, xT)
                    S.dma('sp', self.xT[:, r0:r0 + 128].rearrange("(k p) t -> p k t", p=128), xT.t[:], reads=[xT.k], writes=[('D', 'xT1', c)])


class _Scope:
    def __init__(self, prog):
        self.p = prog
        self.es = ExitStack()

    def __enter__(self):
        self.prev = self.p.cur
        self.p.cur = self.es
        self.es.__enter__()
        return self.es

    def __exit__(self, *a):
        self.p.S.barrier()
        r = self.es.__exit__(*a)
        self.p.cur = self.prev
        return r


class nc_allow:
    def __init__(self, nc):
        self.cm = nc.allow_non_contiguous_dma(reason="tiny strided column load")

    def __enter__(self):
        return self.cm.__enter__()

    def __exit__(self, *a):
        return self.cm.__exit__(*a)


def make_consts(CAP):
    c = np.zeros((128, NCONST), np.float32)
    i = np.arange(128)
    c[:, C_IDENT:C_IDENT + 128] = np.eye(128)
    c[:, C_ONES:C_ONES + 128] = 1.0
    c[:, C_TRIS:C_TRIS + 128] = (i[:, None] > i[None, :])
    c[:, C_TRIL:C_TRIL + 128] = (i[:, None] < i[None, :])
    Rd = np.zeros((128, 128), np.float32)
    for base in (0, 64):
        for j in range(8):
            Rd[base + j, base + j + 8] = -1.0
            Rd[base + j + 8, base + j] = 1.0
    c[:, C_RD:C_RD + 128] = Rd.T
    Rm = np.zeros((128, 128), np.float32)
    for j in range(32):
        Rm[j, j + 32] = -1.0
        Rm[j + 32, j] = 1.0
    c[:, C_RM:C_RM + 128] = Rm.T
    kk = i[:, None]
    qq = i[None, :]
    inblk = {
        'ch': (kk // 64 <= qq // 64),
        'ca': (kk <= qq),
        'st': (kk < qq),
    }
    for j0 in range(4):
        for b in range(4):
            for name, col, kind in (('ch', C_MADD_CH, 'add'), ('ca', C_MADD_CA, 'add'), ('st', C_MMUL, 'mul')):
                if b < j0:
                    valid = np.zeros((128, 128), bool)
                elif b == j0:
                    valid = inblk[name]
                else:
                    valid = np.ones((128, 128), bool)
                blk = np.where(valid, 0.0, NEG) if kind == 'add' else valid.astype(np.float32)
                c[:, col + j0 * 512 + b * 128:col + j0 * 512 + (b + 1) * 128] = blk
    invf_d = np.zeros(128, np.float32)
    fd = (np.float32(ROPE_THETA) ** (-np.arange(8, dtype=np.float32) / np.float32(8))).astype(np.float32)
    for base in (0, 64):
        invf_d[base:base + 8] = fd
        invf_d[base + 8:base + 16] = fd
    c[:, C_INVF_D] = invf_d
    fm = (np.float32(ROPE_THETA) ** (-np.arange(32, dtype=np.float32) / np.float32(32))).astype(np.float32)
    invf_m = np.zeros(128, np.float32)
    invf_m[0:32] = fm
    invf_m[32:64] = fm
    c[:, C_INVF_M] = invf_m
    c[:, C_ECAP:C_ECAP + 32] = (np.arange(32) * CAP)[None, :]
    return c


_CACHE = {}


def get_prog(S_len, **kw):
    key = (S_len, tuple(sorted((k, str(v)) for k, v in kw.items())))
    if key not in _CACHE:
        p = Prog(S_len, **kw)
        p.build()
        _CACHE[key] = p
    return _CACHE[key]


def make_in_map(p, b, inputs):
    f = lambda a: np.ascontiguousarray(np.asarray(a, dtype=np.float32))
    x = np.asarray(inputs['x'])[b]
    m = {
        'x_tm': f(x), 'x_fm': f(x.T),
        'pT': f(np.transpose(np.asarray(inputs['p'])[:, b], (0, 2, 1))),
        'pos': np.ascontiguousarray(np.asarray(inputs['positions'])[b:b + 1].astype(np.int32)),
        'consts': make_consts(p.CAP),
        'w_in': f(inputs['w_in']),
        'fox_f_bias': f(inputs['fox_f_bias']).reshape(DEPTH, 4, 1),
        'diff_lambda': f(inputs['diff_lambda']).reshape(DEPTH, 1, 256),
        'diff_subln': f(inputs['diff_subln']).reshape(DEPTH, 128, 1),
        'mla_q_norm': f(f(inputs['mla_q_norm']).reshape(DEPTH, 4, 128).transpose(0, 2, 1)),
        'mla_kv_norm': f(f(inputs['mla_kv_norm']).reshape(DEPTH, 2, 128).transpose(0, 2, 1)),
        'mla_w_uq': f(inputs['mla_w_uq']), 'mla_w_ukv': f(inputs['mla_w_ukv']),
        'w_branch': f(inputs['w_branch']), 'w_o': f(inputs['w_o']),
        'ln1_g': f(inputs['ln1_g']).reshape(DEPTH, 1, D), 'ln1_b': f(inputs['ln1_b']).reshape(DEPTH, 1, D),
        'w_router_group': f(inputs['w_router_group']), 'b_router_group': f(inputs['b_router_group']).reshape(DEPTH, 1, 4),
        'w_router_expert': f(inputs['w_router_expert']), 'b_router_expert': f(inputs['b_router_expert']).reshape(DEPTH, 1, 32),
        'w_expert_gate': f(inputs['w_expert_gate']), 'w_expert_up': f(inputs['w_expert_up']),
        'w_expert_down': f(inputs['w_expert_down']),
        'w_ple_gate': f(inputs['w_ple_gate']), 'w_ple_proj': f(inputs['w_ple_proj']),
        'ln2_g': f(inputs['ln2_g']).reshape(DEPTH, 1, D), 'ln2_b': f(inputs['ln2_b']).reshape(DEPTH, 1, D),
    }
    return m


def kernel(**inputs):
    x = np.asarray(inputs['x'])
    B, SL, _ = x.shape
    p = get_prog(SL)
    in_maps = [make_in_map(p, b, inputs) for b in range(B)]
    res = run_bass_kernel_spmd(p.nc, in_maps, core_ids=list(range(B)))
    out = np.stack([np.asarray(r['out']) for r in res.results], axis=0)
    return out.astype(np.float32)
```

```python
import math
import numpy as np
from contextlib import ExitStack
import concourse.bass as bass
import concourse.mybir as mybir
from concourse.bass_utils import run_bass_kernel_spmd

F32 = mybir.dt.float32
BF16 = mybir.dt.bfloat16
I32 = mybir.dt.int32
AF = mybir.ActivationFunctionType
ALU = mybir.AluOpType

D = 2048
DEPTH = 2
NEXP = 32
EH = 512
IN_DIM = 13636
ALPHA = 4.0 ** 0.25
LN_EPS = 1e-5
RMS_EPS = 1e-6
ROPE_THETA = 500000.0
OFF = dict(diff_q=0, diff_k=512, diff_v=1024, fox_q=1536, fox_k=2048, fox_v=2560, fox_f=3072,
           mla_cq=3076, mla_ckv=3588, mla_kr=3844, sb_q=3908, sb_k=4420, sb_v=4932, gates=5444)
NEG = -30000.0
TWO_PI = 6.283185307179586

C_IDENT, C_ONES, C_TRIS, C_TRIL, C_RD, C_RM = 0, 128, 256, 384, 512, 640
C_MADD_CH, C_MADD_CA, C_MMUL = 768, 768 + 2048, 768 + 4096
C_INVF_D, C_INVF_M, C_ECAP = 768 + 6144, 768 + 6145, 768 + 6146
NCONST = 768 + 6146 + 32


class Sched:
    NDMA = 16

    def __init__(self, nc, es):
        self.nc = nc
        self.es = es
        self.eng = {'pe': nc.tensor, 'dve': nc.vector, 'act': nc.scalar, 'pool': nc.gpsimd, 'sp': nc.sync}
        self.sems = {}
        self.cnt = {}
        for e in self.eng:
            self.sems[e] = es.enter_context(nc.semaphore('s_' + e))
            self.cnt[e] = 0
        self.dma_rr = {}
        for q in ('sp', 'act', 'pool'):
            self.dma_rr[q] = 0
            for i in range(self.NDMA):
                k = ('d', q, i)
                self.sems[k] = es.enter_context(nc.semaphore('d_%s_%d' % (q, i)))
                self.cnt[k] = 0
        self.seen = {e: {} for e in self.eng}
        self.res = {}
        self.nins = 0

    def _need(self, reads, writes):
        need = {}
        for r in reads:
            st = self.res.get(r)
            if st and st[0] is not None:
                k, v = st[0]
                if need.get(k, 0) < v:
                    need[k] = v
        for w in writes:
            st = self.res.get(w)
            if st:
                if st[0] is not None:
                    k, v = st[0]
                    if need.get(k, 0) < v:
                        need[k] = v
                for k, v in st[1].items():
                    if need.get(k, 0) < v:
                        need[k] = v
        return need

    def _commit(self, ev, reads, writes):
        k, v = ev
        for r in reads:
            st = self.res.get(r)
            if st is None:
                st = self.res[r] = [None, {}]
            if st[1].get(k, 0) < v:
                st[1][k] = v
        for w in writes:
            self.res[w] = [ev, {}]

    def _waits(self, e, need):
        eng = self.eng[e]
        seen = self.seen[e]
        for k, v in need.items():
            if e == 'pe' and k == 'pe':
                continue
            if seen.get(k, 0) < v:
                eng.wait_ge(self.sems[k], v)
                seen[k] = v

    def op(self, e, fn, reads=(), writes=()):
        self._waits(e, self._need(reads, writes))
        ins = fn(self.eng[e])
        self.cnt[e] += 1
        ins.then_inc(self.sems[e], 1)
        self._commit((e, self.cnt[e]), reads, writes)
        self.nins += 1

    def dma(self, q, out, in_, reads=(), writes=(), fn=None, **kw):
        if q == 'sp' and fn is None and str(out.space) == 'DRAM' and str(in_.space) != 'DRAM':
            q = 'act'
        i = self.dma_rr[q]
        self.dma_rr[q] = (i + 1) % self.NDMA
        k = ('d', q, i)
        need = self._need(reads, writes)
        if self.cnt[k]:
            need[k] = max(need.get(k, 0), self.cnt[k])
        self._waits(q, need)
        if fn is not None:
            ins = fn(self.eng[q])
        else:
            ins = self.eng[q].dma_start(out=out, in_=in_, **kw)
        self.cnt[k] += 16
        ins.then_inc(self.sems[k], 16)
        self._commit((k, self.cnt[k]), reads, writes)
        self.nins += 1

    def finish(self, keys):
        self._waits('sp', self._need(keys, ()))

    def barrier(self):
        need = {k: v for k, v in self.cnt.items() if v > 0}
        for e in self.eng:
            eng = self.eng[e]
            seen = self.seen[e]
            for k, v in need.items():
                if seen.get(k, 0) < v:
                    eng.wait_ge(self.sems[k], v)
                    seen[k] = v


class Ring:
    def __init__(self, tiles):
        self.tiles = tiles
        self.i = 0

    def get(self):
        t = self.tiles[self.i]
        self.i = (self.i + 1) % len(self.tiles)
        return t


class T:
    def __init__(self, ap, key):
        self.t = ap
        self.k = key


class Prog:
    def __init__(self, S_len, nlayers=DEPTH, dbg=False, phases=None):
        self.SL = S_len
        self.TG = min(2048, S_len)
        self.NTG = S_len // self.TG
        self.NCH = S_len // 128
        self.NQT = S_len // 512
        avg = 2 * S_len // NEXP
        sd = math.sqrt(2 * S_len / 32.0)
        self.CAPB = int(math.ceil((avg + 8.0 * sd) / 128.0))
        self.CAP = self.CAPB * 128
        self.nlayers = nlayers
        self.WD = DEPTH if (nlayers == DEPTH) else nlayers
        self.NE_DECL = NEXP if (phases is None or 'exp' in phases) else 1
        self.dbg = dbg
        self.phases = phases
        self.nc = bass.Bass("TRN2", target_bir_lowering=False)
        self.scopes = []
        self.uid = 0
        self.cast_rr = 0

    def din(self, name, shape, dt=F32):
        return self.nc.dram_tensor(name, list(shape), dt, kind="ExternalInput").ap()

    def dscr(self, name, shape, dt):
        kind = "ExternalOutput" if (self.dbg and name in self.dbg) else "Internal"
        return self.nc.dram_tensor(name, list(shape), dt, kind=kind).ap()

    def sb(self, name, shape, dt):
        self.uid += 1
        t = self.cur.enter_context(self.nc.sbuf_tensor("%s_%d" % (name, self.uid), list(shape), dt))
        return T(t, name)

    def ps(self, name, shape, dt=F32):
        self.uid += 1
        t = self.cur.enter_context(self.nc.psum_tensor("%s_%d" % (name, self.uid), list(shape), dt))
        return T(t, name)

    def ring(self, name, n, shape, dt, psum=False):
        return Ring([(self.ps if psum else self.sb)("%s%d" % (name, i), shape, dt) for i in range(n)])

    def wload(self, src_ap, a, b, q='sp'):
        st = self.wst.get()
        wb = self.wbf.get()
        S = self.S
        stv = st.t[:, 0:a * b].rearrange("p (a b) -> p a b", a=a)
        step = max(1, 4 * 128 // 128) if b <= 512 else 1
        step = 4
        keys = []
        for j, a0 in enumerate(range(0, a, step)):
            a1 = min(a, a0 + step)
            S.dma(q, stv[:, a0:a1, :], src_ap[:, a0:a1, :], writes=[(st.k, j)])
            keys.append((st.k, j))
        self.cast(wb.t[:, 0:a * b], st.t[:, 0:a * b], keys, [wb.k])
        return wb

    def cast(self, out_ap, in_ap, reads, writes):
        i = self.cast_rr
        self.cast_rr = (i + 1) % 3
        if i != 2:
            self.S.op('act', lambda e: e.activation(out=out_ap, in_=in_ap, func=AF.Copy), reads=reads, writes=writes)
        else:
            self.S.op('dve', lambda e: e.tensor_copy(out=out_ap, in_=in_ap), reads=reads, writes=writes)

    def build(self):
        nc = self.nc
        SL, TG, NCH = self.SL, self.TG, self.NCH
        L = self.nlayers
        self.x_tm = self.din("x_tm", [SL, D])
        self.x_fm = self.din("x_fm", [D, SL])
        self.pT = self.din("pT", [DEPTH, 256, SL])
        self.pos = self.din("pos", [1, SL], I32)
        self.consts = self.din("consts", [128, NCONST])
        self.w_in = self.din("w_in", [DEPTH, D, IN_DIM])
        self.fox_f_bias = self.din("fox_f_bias", [DEPTH, 4, 1])
        self.diff_lambda = self.din("diff_lambda", [DEPTH, 1, 256])
        self.diff_subln = self.din("diff_subln", [DEPTH, 128, 1])
        self.mla_q_norm = self.din("mla_q_norm", [DEPTH, 128, 4])
        self.mla_kv_norm = self.din("mla_kv_norm", [DEPTH, 128, 2])
        self.mla_w_uq = self.din("mla_w_uq", [DEPTH, 512, 768])
        self.mla_w_ukv = self.din("mla_w_ukv", [DEPTH, 256, 1024])
        self.w_branch = self.din("w_branch", [DEPTH, 4, 512, D])
        self.w_o = self.din("w_o", [DEPTH, D, D])
        self.ln1_g = self.din("ln1_g", [DEPTH, 1, D])
        self.ln1_b = self.din("ln1_b", [DEPTH, 1, D])
        self.w_rg = self.din("w_router_group", [DEPTH, D, 4])
        self.b_rg = self.din("b_router_group", [DEPTH, 1, 4])
        self.w_re = self.din("w_router_expert", [DEPTH, D, 32])
        self.b_re = self.din("b_router_expert", [DEPTH, 1, 32])
        self.w_eg = self.din("w_expert_gate", [self.WD, self.NE_DECL, D, EH])
        self.w_eu = self.din("w_expert_up", [self.WD, self.NE_DECL, D, EH])
        self.w_ed = self.din("w_expert_down", [self.WD, self.NE_DECL, EH, D])
        self.w_pg = self.din("w_ple_gate", [DEPTH, D, D])
        self.w_pp = self.din("w_ple_proj", [DEPTH, 256, D])
        self.ln2_g = self.din("ln2_g", [DEPTH, 1, D])
        self.ln2_b = self.din("ln2_b", [DEPTH, 1, D])
        self.out = nc.dram_tensor("out", [SL, D], F32, kind="ExternalOutput").ap()
        self.xT = self.dscr("xT", [D, SL], BF16)
        self.xres = self.dscr("xres", [SL, D], F32)
        self.ropeD = self.dscr("ropeD", [2, 128, SL], BF16)
        self.ropeM = self.dscr("ropeM", [2, 64, SL], BF16)
        self.qT = self.dscr("qT", [16, 128, SL], BF16)
        self.kT = self.dscr("kT", [16, 128, SL], BF16)
        self.vv = self.dscr("vv", [16, SL, 128], BF16)
        self.qrT = self.dscr("qrT", [4, 64, SL], BF16)
        self.krT = self.dscr("krT", [64, SL], BF16)
        self.fT = self.dscr("fT", [4, SL], F32)
        self.cumT = self.dscr("cumT", [4, SL], F32)
        self.qaug = self.dscr("qaug", [4, 3, SL], BF16)
        self.gatesT = self.dscr("gatesT", [4 * D, SL], BF16)
        self.oT = self.dscr("oT", [16, 128, SL], BF16)
        self.mergedT = self.dscr("mergedT", [D, SL], BF16)
        self.hres = self.dscr("hres", [SL, D], F32)
        self.hT = self.dscr("hT", [D, SL], BF16)
        self.hrows = self.dscr("hrows", [NEXP * self.CAP, D], BF16)
        self.yrows = self.dscr("yrows", [NEXP * self.CAP, D], F32)

        with ExitStack() as es:
            self.S = Sched(nc, es)
            self.glob = es
            self.cur = es
            self.cb = self.sb("cb", [128, C_INVF_D], BF16)
            self.cf = self.sb("cf", [128, 34], F32)
            self.S.dma('sp', self.cf.t[:], self.consts[:, C_INVF_D:C_INVF_D + 34], writes=['cf'])
            with self.scope() as es0:
                for c0 in range(0, C_INVF_D, 2048):
                    c1 = min(C_INVF_D, c0 + 2048)
                    cft = self.sb("cft%d" % c0, [128, 2048], F32)
                    self.S.dma('sp', cft.t[:, 0:c1 - c0], self.consts[:, c0:c1], writes=[cft.k])
                    self.S.op('dve', lambda e: e.tensor_copy(out=self.cb.t[:, c0:c1], in_=cft.t[:, 0:c1 - c0]), reads=[cft.k], writes=['cb'])
            self.dest_all = self.sb("dest_all", [128, NCH, 2], I32)
            self.w_all = self.sb("w_all", [128, NCH, 2], F32)
            self.wst = self.ring("wst", 2, [128, 2048], F32)
            self.wbf = self.ring("wbf", 3, [128, 2048], BF16)
            self.epsln = self.sb("epsln", [128, 1], F32)
            self.S.op('dve', lambda e: e.memset(self.epsln.t[:], LN_EPS), writes=['epsln'])
            self.epsrms = self.sb("epsrms", [128, 1], F32)
            self.S.op('dve', lambda e: e.memset(self.epsrms.t[:], RMS_EPS), writes=['epsrms'])

            ph = self.phases
            if ph is None or 'pre' in ph:
                self.phase_pre()
            for l in range(L):
                if ph is None or 'proj' in ph:
                    self.phase_proj(l)
                if ph is None or 'fox' in ph:
                    self.phase_foxprep(l)
                if ph is None or 'attn' in ph:
                    self.phase_attn(l)
                if ph is None or 'merge' in ph:
                    self.phase_merge(l)
                if ph is None or 'wo' in ph:
                    self.phase_wo(l)
                if ph is None or 'exp' in ph:
                    self.phase_experts(l)
                if ph is None or 'ple' in ph:
                    self.phase_ple(l, last=(l == L - 1))
            keys = [k for k in self.S.res.keys() if isinstance(k, tuple) and k and k[0] == 'D']
            self.S.finish(keys)
        return nc

    def cbs(self, c0, n, p=128):
        return self.cb.t[0:p, c0:c0 + n]

    def scope(self):
        return _Scope(self)

    def phase_pre(self):
        S, SL = self.S, self.SL
        with self.scope() as es:
            CB = min(2048, SL)
            posi = self.sb("posi", [128, CB], I32)
            posf = self.sb("posf", [128, CB], F32)
            ang = self.sb("ang", [128, CB], F32)
            ki = self.sb("ki", [128, CB], I32)
            kf = self.sb("kf", [128, CB], F32)
            tb = self.ring("tb", 2, [128, CB], BF16)
            for cb0 in range(0, SL, CB):
                S.dma('sp', posi.t[:], self.pos[0:1, cb0:cb0 + CB].partition_broadcast(128), writes=['posi'])
                S.op('dve', lambda e: e.tensor_copy(out=posf.t[:], in_=posi.t[:]), reads=['posi'], writes=['posf'])
                for kind, (ccol, npart, dst) in enumerate([(0, 128, self.ropeD), (1, 64, self.ropeM)]):
                    for cs, shift in enumerate([math.pi / 2, 0.0]):
                        P = npart
                        S.op('dve', lambda e: e.tensor_scalar(out=ang.t[0:P], in0=posf.t[0:P], scalar1=self.cf.t[0:P, ccol:ccol + 1],
                                                              scalar2=shift, op0=ALU.mult, op1=ALU.add),
                             reads=['posf', 'cf'], writes=['ang'])
                        S.op('dve', lambda e: e.tensor_scalar(out=ki.t[0:P], in0=ang.t[0:P], scalar1=1.0 / TWO_PI, scalar2=None, op0=ALU.mult),
                             reads=['ang'], writes=['ki'])
                        S.op('dve', lambda e: e.tensor_copy(out=kf.t[0:P], in_=ki.t[0:P]), reads=['ki'], writes=['kf'])
                        S.op('dve', lambda e: e.scalar_tensor_tensor(out=ang.t[0:P], in0=kf.t[0:P], scalar=-TWO_PI, in1=ang.t[0:P],
                                                                     op0=ALU.mult, op1=ALU.add), reads=['kf', 'ang'], writes=['ang'])
                        S.op('dve', lambda e: e.tensor_scalar(out=ang.t[0:P], in0=ang.t[0:P], scalar1=-3.1415925, scalar2=3.1415925,
                                                              op0=ALU.max, op1=ALU.min), reads=['ang'], writes=['ang'])
                        o = tb.get()
                        S.op('act', lambda e: e.activation(out=o.t[0:P], in_=ang.t[0:P], func=AF.Sin), reads=['ang'], writes=[o.k])
                        S.dma('sp', dst[cs, :, cb0:cb0 + CB], o.t[0:P], reads=[o.k], writes=[('D', 'rope', kind, cs, cb0)])
            xs = self.ring("xs", 2, [128, CB], F32)
            xb = self.ring("xbp", 2, [128, CB], BF16)
            for k in range(16):
                for cb0 in range(0, SL, CB):
                    a = xs.get()
                    b = xb.get()
                    S.dma('sp', a.t[:], self.x_fm[k * 128:(k + 1) * 128, cb0:cb0 + CB], writes=[a.k])
                    S.op('pool', lambda e: e.tensor_copy(out=b.t[:], in_=a.t[:]), reads=[a.k], writes=[b.k])
                    S.dma('sp', self.xT[k * 128:(k + 1) * 128, cb0:cb0 + CB], b.t[:], reads=[b.k], writes=[('D', 'xT0', k, cb0 // self.TG)])

    def phase_proj(self, l):
        S, SL, TG = self.S, self.SL, self.TG
        NT = TG // 512
        w_in = self.w_in[l].rearrange("(k p) c -> p k c", p=128)
        for tg in range(self.NTG):
            t0 = tg * TG
            with self.scope() as es:
                xt = self.sb("xt", [128, 16, TG], BF16)
                for k in range(16):
                    S.dma('sp', xt.t[:, k, :], self.xT[k * 128:(k + 1) * 128, t0:t0 + TG], reads=([('D', 'xT0', k, tg)] if l == 0 else [('D', 'xT1', c_) for c_ in range(t0 // 128, (t0 + TG) // 128)]), writes=['xt'])
                rD = self.sb("rD", [128, 2, TG], BF16)
                rM = self.sb("rM", [64, 2, TG], BF16)
                for cs in range(2):
                    S.dma('sp', rD.t[:, cs, :], self.ropeD[cs, :, t0:t0 + TG], reads=[('D', 'rope', 0, cs, t0)], writes=['rD'])
                    S.dma('sp', rM.t[:, cs, :], self.ropeM[cs, :, t0:t0 + TG], reads=[('D', 'rope', 1, cs, t0)], writes=['rM'])
                pp = self.ring("pp", 4, [128, 512], F32, psum=True)
                pr = self.ring("prr", 2, [128, 512], F32, psum=True)
                ptr = self.ring("ptr", 2, [128, 1024], BF16, psum=True)
                ob = self.ring("ob", 3, [128, TG], BF16)
                vtb = self.ring("vtb", 2, [128, TG // 128, 128], BF16)
                tmp = self.ring("ptmp", 3, [128, 512], F32)
                cqg = self.sb("cqg", [128, 4, TG], BF16)
                sqq = self.sb("sqq", [128, 4, TG], BF16)
                rq = self.sb("rq", [128, TG], F32)
                gq = self.sb("gq", [128, 4], F32)
                gkv = self.sb("gkv", [128, 2], F32)
                fb = self.sb("fb", [4, 1], F32)
                S.dma('sp', gq.t[:], self.mla_q_norm[l], writes=['gq'])
                S.dma('sp', gkv.t[:], self.mla_kv_norm[l], writes=['gkv'])
                S.dma('sp', fb.t[:], self.fox_f_bias[l], writes=['fb'])
                eng_alt = [0]

                def evac_copy(dst_ap, src_ap, r, w, func=AF.Copy):
                    if func == AF.Copy and eng_alt[0] % 2 == 0:
                        S.op('dve', lambda e: e.tensor_copy(out=dst_ap, in_=src_ap), reads=r, writes=w)
                    else:
                        S.op('act', lambda e: e.activation(out=dst_ap, in_=src_ap, func=func), reads=r, writes=w)
                    eng_alt[0] += 1

                def project(wb, nk, M, rhs_t, rhs_key):
                    outs = []
                    for n in range(NT):
                        p = pp.get()
                        for k in range(nk):
                            S.op('pe', lambda e: e.matmul(p.t[0:M, :], lhsT=wb.t[:, k * M:(k + 1) * M],
                                                          rhs=rhs_t[:, k, n * 512:(n + 1) * 512], start=(k == 0), stop=(k == nk - 1)),
                                 reads=[wb.k, rhs_key], writes=[p.k])
                        outs.append(p)
                    return outs

                def rope_store(o, M, rtab, rmat_c, dst_ap, dkey):
                    o2 = ob.get()
                    for n in range(NT):
                        sl = slice(n * 512, (n + 1) * 512)
                        p = pr.get()
                        S.op('pe', lambda e: e.matmul(p.t[0:M, :], lhsT=self.cbs(rmat_c, M, M), rhs=o.t[0:M, sl], start=True, stop=True),
                             reads=[o.k, 'cb'], writes=[p.k])
                        t1 = tmp.get()
                        S.op('dve', lambda e: e.tensor_tensor(out=t1.t[0:M, :], in0=o.t[0:M, sl], in1=rtab.t[0:M, 0, sl], op=ALU.mult),
                             reads=[o.k, rtab.k], writes=[t1.k])
                        t2 = tmp.get()
                        S.op('dve', lambda e: e.tensor_tensor(out=t2.t[0:M, :], in0=p.t[0:M, :], in1=rtab.t[0:M, 1, sl], op=ALU.mult),
                             reads=[p.k, rtab.k], writes=[t2.k])
                        S.op('dve', lambda e: e.tensor_tensor(out=o2.t[0:M, sl], in0=t1.t[0:M, :], in1=t2.t[0:M, :], op=ALU.add),
                             reads=[t1.k, t2.k], writes=[o2.k])
                    S.dma('sp', dst_ap, o2.t[0:M, :], reads=[o2.k], writes=[dkey])

                def v_store(o, idx):
                    vt = vtb.get()
                    for c8 in range(0, TG // 128, 8):
                        p = ptr.get()
                        nn = min(8, TG // 128 - c8)
                        for j in range(nn):
                            c = c8 + j
                            S.op('pe', lambda e: e.transpose(p.t[:, j * 128:(j + 1) * 128], o.t[:, c * 128:(c + 1) * 128], self.cbs(C_IDENT, 128)),
                                 reads=[o.k, 'cb'], writes=[p.k])
                        evac_copy(vt.t[:, c8:c8 + nn, :], p.t[:, 0:nn * 128].rearrange("p (c d) -> p c d", c=nn), [p.k], [vt.k])
                    S.dma('sp', self.vv[idx, t0:t0 + TG, :].rearrange("(c p) d -> p c d", p=128), vt.t[:], reads=[vt.k],
                          writes=[('D', 'vv', idx, tg)])

                import os
                fams = os.environ.get("PROJ_FAMS")

                def do_tile(fam, c0, M, info):
                    if fams and fam not in fams.split(','):
                        return
                    wb = self.wload(w_in[:, :, c0:c0 + M], 16, M)
                    outs = project(wb, 16, M, xt.t, 'xt')
                    if fam in ('plain', 'gate', 'v', 'rope_d', 'kr'):
                        o = ob.get()
                        for n, p in enumerate(outs):
                            evac_copy(o.t[0:M, n * 512:(n + 1) * 512], p.t[0:M, :], [p.k], [o.k],
                                      func=(AF.Sigmoid if fam == 'gate' else AF.Copy))
                        if fam == 'plain':
                            dst = self.qT if info[0] == 'qT' else self.kT
                            S.dma('sp', dst[info[1], :, t0:t0 + TG], o.t[:], reads=[o.k], writes=[('D', info[0], info[1], tg)])
                        elif fam == 'gate':
                            S.dma('sp', self.gatesT[info * 128:(info + 1) * 128, t0:t0 + TG], o.t[:], reads=[o.k],
                                  writes=[('D', 'gatesT', info, tg)])
                        elif fam == 'v':
                            v_store(o, info)
                        elif fam == 'rope_d':
                            dst = self.qT if info[0] == 'qT' else self.kT
                            rope_store(o, 128, rD, C_RD, dst[info[1], :, t0:t0 + TG], ('D', info[0], info[1], tg))
                        elif fam == 'kr':
                            rope_store(o, 64, rM, C_RM, self.krT[:, t0:t0 + TG], ('D', 'krT', tg))
                    elif fam in ('cq', 'ckv'):
                        gg = gq if fam == 'cq' else gkv
                        for n, p in enumerate(outs):
                            sl = slice(n * 512, (n + 1) * 512)
                            tq = tmp.get()
                            S.op('act', lambda e: e.activation(out=tq.t[:], in_=p.t[:], func=AF.Copy), reads=[p.k], writes=[tq.k])
                            S.op('dve', lambda e: e.tensor_tensor(out=sqq.t[:, info, sl], in0=tq.t[:], in1=tq.t[:], op=ALU.mult), reads=[tq.k], writes=['sqq'])
                            S.op('dve', lambda e: e.tensor_scalar(out=cqg.t[:, info, sl], in0=tq.t[:], scalar1=gg.t[:, info:info + 1], scalar2=None,
                                                                  op0=ALU.mult), reads=[tq.k, gg.k], writes=['cqg'])
                    elif fam == 'f':
                        for n, p in enumerate(outs):
                            ft = tmp.get()
                            S.op('dve', lambda e: e.tensor_scalar(out=ft.t[0:4, :], in0=p.t[0:4, :], scalar1=fb.t[0:4, 0:1], scalar2=None, op0=ALU.add),
                                 reads=[p.k, 'fb'], writes=[ft.k])
                            S.dma('sp', self.fT[:, t0 + n * 512:t0 + (n + 1) * 512], ft.t[0:4, :], reads=[ft.k], writes=[('D', 'fT', tg, n)])

                def rms_scale(nk, dim):
                    for n in range(NT):
                        sl = slice(n * 512, (n + 1) * 512)
                        p = pp.get()
                        for k in range(nk):
                            S.op('pe', lambda e: e.matmul(p.t[:], lhsT=self.cbs(C_ONES, 128), rhs=sqq.t[:, k, sl], start=(k == 0), stop=(k == nk - 1)),
                                 reads=['sqq', 'cb'], writes=[p.k])
                        S.op('act', lambda e: e.activation(out=rq.t[:, sl], in_=p.t[:], func=AF.Sqrt, bias=self.epsrms.t[:, 0:1], scale=1.0 / dim),
                             reads=[p.k, 'epsrms'], writes=['rq'])
                        S.op('dve', lambda e: e.reciprocal(out=rq.t[:, sl], in_=rq.t[:, sl]), reads=['rq'], writes=['rq'])

                def scaled(outs, M):
                    o = ob.get()
                    for n, p in enumerate(outs):
                        sl = slice(n * 512, (n + 1) * 512)
                        S.op('dve', lambda e: e.tensor_tensor(out=o.t[0:M, sl], in0=p.t[0:M, :], in1=rq.t[0:M, sl], op=ALU.mult),
                             reads=[p.k, 'rq'], writes=[o.k])
                    return o
                wuq = self.mla_w_uq[l].rearrange("(k p) c -> p k c", p=128)
                wukv = self.mla_w_ukv[l].rearrange("(k p) c -> p k c", p=128)
                mla_on = os.environ.get("PROJ_MLA", "1") == "1"
                for h in range(4 if mla_on else 0):
                    do_tile('cq', OFF['mla_cq'] + h * 128, 128, h)
                if mla_on:
                    rms_scale(4, 512)
                for h in range(4 if mla_on else 0):
                    wb = self.wload(wuq[:, :, h * 192:h * 192 + 128], 4, 128)
                    o = scaled(project(wb, 4, 128, cqg.t, 'cqg'), 128)
                    S.dma('sp', self.qT[8 + h, :, t0:t0 + TG], o.t[:], reads=[o.k], writes=[('D', 'qT', 8 + h, tg)])
                    wb = self.wload(wuq[:, :, h * 192 + 128:h * 192 + 192], 4, 64)
                    o = scaled(project(wb, 4, 64, cqg.t, 'cqg'), 64)
                    rope_store(o, 64, rM, C_RM, self.qrT[h, :, t0:t0 + TG], ('D', 'qrT', h, tg))
                for j in range(2 if mla_on else 0):
                    do_tile('ckv', OFF['mla_ckv'] + j * 128, 128, j)
                if mla_on:
                    rms_scale(2, 256)
                for h in range(4 if mla_on else 0):
                    wb = self.wload(wukv[:, :, h * 256:h * 256 + 128], 2, 128)
                    o = scaled(project(wb, 2, 128, cqg.t, 'cqg'), 128)
                    S.dma('sp', self.kT[8 + h, :, t0:t0 + TG], o.t[:], reads=[o.k], writes=[('D', 'kT', 8 + h, tg)])
                    wb = self.wload(wukv[:, :, h * 256 + 128:h * 256 + 256], 2, 128)
                    o = scaled(project(wb, 2, 128, cqg.t, 'cqg'), 128)
                    v_store(o, 8 + h)
                do_tile('kr', OFF['mla_kr'], 64, 0)
                do_tile('f', OFF['fox_f'], 4, 0)
                for h in range(4):
                    do_tile('rope_d', OFF['diff_q'] + h * 128, 128, ('qT', 0 + h))
                    do_tile('rope_d', OFF['diff_k'] + h * 128, 128, ('kT', 0 + h))
                    do_tile('v', OFF['diff_v'] + h * 128, 128, 0 + h)
                    do_tile('plain', OFF['fox_q'] + h * 128, 128, ('qT', 4 + h))
                    do_tile('plain', OFF['fox_k'] + h * 128, 128, ('kT', 4 + h))
                    do_tile('v', OFF['fox_v'] + h * 128, 128, 4 + h)
                    do_tile('plain', OFF['sb_q'] + h * 128, 128, ('qT', 12 + h))
                    do_tile('plain', OFF['sb_k'] + h * 128, 128, ('kT', 12 + h))
                    do_tile('v', OFF['sb_v'] + h * 128, 128, 12 + h)
                for g in range(64):
                    do_tile('gate', OFF['gates'] + g * 128, 128, g)

    def phase_foxprep(self, l):
        S, SL = self.S, self.SL
        with self.scope() as es:
            z = self.sb("fz", [4, SL], F32)
            a = self.sb("fa", [4, SL], F32)
            b = self.sb("fb2", [4, SL], F32)
            hb = [self.sb("fh%d" % i, [4, SL], BF16) for i in range(3)]
            rk = [('D', 'fT', tg, n) for tg in range(self.NTG) for n in range(self.TG // 512)]
            S.dma('sp', z.t[:], self.fT[:, :], reads=rk, writes=['fz'])
            S.op('act', lambda e: e.activation(out=a.t[:], in_=z.t[:], func=AF.Exp, scale=-1.0), reads=['fz'], writes=['fa'])
            S.op('act', lambda e: e.activation(out=a.t[:], in_=a.t[:], func=AF.Ln, bias=1.0), reads=['fa'], writes=['fa'])
            S.op('dve', lambda e: e.tensor_scalar(out=a.t[:], in0=a.t[:], scalar1=-1.0, scalar2=None, op0=ALU.mult), reads=['fa'], writes=['fa'])
            S.op('dve', lambda e: e.memset(b.t[:], 1.0), writes=['fb2'])
            S.op('dve', lambda e: e.tensor_tensor_scan(out=z.t[:], data0=b.t[:], data1=a.t[:], initial=0.0, op0=ALU.mult, op1=ALU.add),
                 reads=['fa', 'fb2'], writes=['fz'])
            S.dma('sp', self.cumT[:, :], z.t[:], reads=['fz'], writes=[('D', 'cumT')])
            S.op('dve', lambda e: e.tensor_scalar(out=a.t[:], in0=z.t[:], scalar1=math.sqrt(128.0), scalar2=None, op0=ALU.mult), reads=['fz'], writes=['fa'])
            for i in range(3):
                S.op('dve', lambda e: e.tensor_copy(out=hb[i].t[:], in_=a.t[:]), reads=['fa'], writes=[hb[i].k])
                if i < 2:
                    S.op('dve', lambda e: e.tensor_tensor(out=a.t[:], in0=a.t[:], in1=hb[i].t[:], op=ALU.subtract), reads=['fa', hb[i].k], writes=['fa'])
                for h in range(4):
                    S.dma('sp', self.qaug[h, i:i + 1, :], hb[i].t[h:h + 1, :], reads=[hb[i].k], writes=[('D', 'qaug', h, i)])

    def phase_attn(self, l):
        S, SL, NCH, NQT = self.S, self.SL, self.NCH, self.NQT
        NTG = self.NTG
        with self.scope() as es:
            KT = self.ring("KT", 2, [128, SL], BF16)
            QT = self.ring("QT", 2, [128, SL], BF16)
            VV = self.ring("VV", 2, [128, NCH, 128], BF16)
            OT = self.ring("OT", 2, [128, SL], BF16)
            KR = self.sb("KR", [64, SL], BF16)
            QR = self.ring("QR", 2, [64, SL], BF16)
            QA = self.ring("QA", 2, [3, SL], BF16)
            NCUM = self.ring("NCUM", 2, [128, NCH], F32)
            sps = self.ring("sps", 4, [128, 512], F32, psum=True)
            ops_ = self.ring("ops", 2, [128, 512], F32, psum=True)
            dps = self.ring("dps", 2, [128, 512], F32, psum=True)
            lps = sps
            PT = self.ring("PT", 6, [128, 512], BF16)
            rec = self.ring("rec", 2, [128, 512], F32)
            o0 = self.ring("o0", 2, [128, 512], F32)
            o1 = self.ring("o1", 2, [128, 512], F32)
            f32t = self.ring("f32t", 4, [128, 512], F32)
            sqb = self.ring("sqb", 2, [128, 512], BF16)
            lp = self.sb("lp", [128, 256], F32)
            lpp = self.sb("lpp", [128, 128], F32)
            lam2 = self.sb("lam2", [128, 2], F32)
            neglam = self.sb("neglam", [128, 1], F32)
            gsub = self.sb("gsub", [128, 1], F32)
            lam_init = 0.8 - 0.6 * math.exp(-0.3 * l)
            S.dma('sp', lp.t[:], self.diff_lambda[l, 0:1, :].partition_broadcast(128), writes=['lp'])
            S.dma('sp', gsub.t[:], self.diff_subln[l], writes=['gsub'])
            S.op('dve', lambda e: e.tensor_scalar(out=gsub.t[:], in0=gsub.t[:], scalar1=(1.0 - lam_init), scalar2=None, op0=ALU.mult), reads=['gsub'], writes=['gsub'])
            lp4 = lp.t[:].rearrange("p (a d) -> p a d", a=4)
            lpp2 = lpp.t[:].rearrange("p (a d) -> p a d", a=2)
            S.op('dve', lambda e: e.tensor_tensor(out=lpp2[:, 0, :], in0=lp4[:, 0, :], in1=lp4[:, 1, :], op=ALU.mult), reads=['lp'], writes=['lpp'])
            S.op('dve', lambda e: e.tensor_tensor(out=lpp2[:, 1, :], in0=lp4[:, 2, :], in1=lp4[:, 3, :], op=ALU.mult), reads=['lp'], writes=['lpp'])
            S.op('dve', lambda e: e.reduce_sum(out=lam2.t[:], in_=lpp2, axis=mybir.AxisListType.X), reads=['lpp'], writes=['lam2'])
            S.op('act', lambda e: e.activation(out=lam2.t[:], in_=lam2.t[:], func=AF.Exp), reads=['lam2'], writes=['lam2'])
            S.op('dve', lambda e: e.tensor_tensor(out=neglam.t[:], in0=lam2.t[:, 1:2], in1=lam2.t[:, 0:1], op=ALU.subtract), reads=['lam2'], writes=['neglam'])
            S.op('dve', lambda e: e.tensor_scalar(out=neglam.t[:], in0=neglam.t[:], scalar1=-lam_init, scalar2=None, op0=ALU.add), reads=['neglam'], writes=['neglam'])

            def dkeys(name, idx):
                return [('D', name, idx, tg) for tg in range(NTG)]

            def load_head(idx, need_v=True):
                kt = KT.get()
                qt = QT.get()
                S.dma('sp', kt.t[:], self.kT[idx], reads=dkeys('kT', idx), writes=[kt.k])
                S.dma('sp', qt.t[:], self.qT[idx], reads=dkeys('qT', idx), writes=[qt.k])
                vt = VV.get()
                vsrc = self.vv[idx].rearrange("(c p) d -> p c d", p=128)
                for c0 in range(0, NCH, 8):
                    S.dma('sp', vt.t[:, c0:min(NCH, c0 + 8), :], vsrc[:, c0:min(NCH, c0 + 8), :], reads=dkeys('vv', idx), writes=[(vt.k, c0 // 8)])
                return kt, qt, vt

            def drive(streams):
                live = list(streams)
                while live:
                    nxt = []
                    for g_ in live:
                        try:
                            next(g_)
                            nxt.append(g_)
                        except StopIteration:
                            pass
                    live = nxt

            def softmax_tile(i, parts, scale, madd_c, vt, O, Dn, bias_t=None, aug=None):
                last = 4 * i + 3

                def emit_qk(g):
                    sp_ = sps.get()
                    j0 = g - 4 * i
                    nmm = len(parts) + (1 if aug is not None else 0) + (1 if j0 >= 0 else 0)
                    c = 0
                    for (kt, qt, p0, p1) in parts:
                        S.op('pe', lambda e: e.matmul(sp_.t[:], lhsT=kt.t[p0:p1, g * 128:(g + 1) * 128], rhs=qt.t[p0:p1, i * 512:(i + 1) * 512],
                                                      start=(c == 0), stop=(c == nmm - 1)), reads=[kt.k, qt.k], writes=[sp_.k])
                        c += 1
                    if aug is not None:
                        S.op('pe', lambda e: e.matmul(sp_.t[:], lhsT=self.cb.t[0:3, C_ONES:C_ONES + 128], rhs=aug.t[0:3, i * 512:(i + 1) * 512],
                                                      start=False, stop=(c == nmm - 1)), reads=[aug.k, 'cb'], writes=[sp_.k])
                        c += 1
                    if j0 >= 0:
                        S.op('pe', lambda e: e.matmul(sp_.t[:], lhsT=self.cbs(C_IDENT, 128), rhs=self.cbs(madd_c + j0 * 512, 512),
                                                      start=False, stop=True), reads=['cb'], writes=[sp_.k])
                    return sp_
                nxt = emit_qk(0)
                for g in range(last + 1):
                    sp_ = nxt
                    if g < last:
                        nxt = emit_qk(g + 1)
                    pt = PT.get()
                    if bias_t is not None:
                        S.op('act', lambda e: e.activation(out=pt.t[:], in_=sp_.t[:], func=AF.Exp, scale=scale, bias=bias_t.t[:, g:g + 1]),
                             reads=[sp_.k, bias_t.k], writes=[pt.k])
                    else:
                        S.op('act', lambda e: e.activation(out=pt.t[:], in_=sp_.t[:], func=AF.Exp, scale=scale), reads=[sp_.k], writes=[pt.k])
                    S.op('pe', lambda e: e.matmul(O.t[:], lhsT=vt.t[:, g, :], rhs=pt.t[:], start=(g == 0), stop=(g == last)),
                         reads=[(vt.k, g // 8), pt.k], writes=[O.k])
                    S.op('pe', lambda e: e.matmul(Dn.t[:], lhsT=self.cbs(C_ONES, 128), rhs=pt.t[:], start=(g == 0), stop=(g == last)),
                         reads=['cb', pt.k], writes=[Dn.k])
                    yield

            def normalize(O, Dn, dst_ap, wkey):
                r = rec.get()
                S.op('dve', lambda e: e.reciprocal(out=r.t[:], in_=Dn.t[:]), reads=[Dn.k], writes=[r.k])
                S.op('dve', lambda e: e.tensor_tensor(out=dst_ap, in0=O.t[:], in1=r.t[:], op=ALU.mult), reads=[O.k, r.k], writes=[wkey])

            for h in range(4):
                idx = h
                kt, qt, vt = load_head(idx)
                ot = OT.get()
                for i in range(NQT):
                    accs = [(ops_.get(), dps.get()) for c in range(2)]
                    drive([softmax_tile(i, [(kt, qt, c * 64, (c + 1) * 64)], 64 ** -0.5, C_MADD_CH, vt, accs[c][0], accs[c][1]) for c in range(2)])
                    oc = []
                    for c in range(2):
                        dst = (o0 if c == 0 else o1).get()
                        normalize(accs[c][0], accs[c][1], dst.t[:], dst.k)
                        oc.append(dst)
                    d = f32t.get()
                    S.op('dve', lambda e: e.scalar_tensor_tensor(out=d.t[:], in0=oc[1].t[:], scalar=neglam.t[:, 0:1], in1=oc[0].t[:],
                                                                 op0=ALU.mult, op1=ALU.add), reads=[oc[0].k, oc[1].k, 'neglam'], writes=[d.k])
                    sq = sqb.get()
                    S.op('pool', lambda e: e.tensor_tensor(out=sq.t[:], in0=d.t[:], in1=d.t[:], op=ALU.mult), reads=[d.k], writes=[sq.k])
                    nps = lps.get()
                    S.op('pe', lambda e: e.matmul(nps.t[:], lhsT=self.cbs(C_ONES, 128), rhs=sq.t[:], start=True, stop=True), reads=[sq.k, 'cb'], writes=[nps.k])
                    rt = f32t.get()
                    S.op('act', lambda e: e.activation(out=rt.t[:], in_=nps.t[:], func=AF.Sqrt, bias=self.epsrms.t[:, 0:1], scale=1.0 / 128),
                         reads=[nps.k, 'epsrms'], writes=[rt.k])
                    S.op('dve', lambda e: e.reciprocal(out=rt.t[:], in_=rt.t[:]), reads=[rt.k], writes=[rt.k])
                    S.op('dve', lambda e: e.scalar_tensor_tensor(out=ot.t[:, i * 512:(i + 1) * 512], in0=d.t[:], scalar=gsub.t[:, 0:1], in1=rt.t[:],
                                                                 op0=ALU.mult, op1=ALU.mult), reads=[d.k, rt.k, 'gsub'], writes=[ot.k])
                S.dma('sp', self.oT[idx], ot.t[:], reads=[ot.k], writes=[('D', 'oT', idx)])

            def head_stream(idx, parts_fn, scale, madd_c, bias_t=None, aug=None):
                kt, qt, vt = load_head(idx)
                parts = parts_fn(kt, qt)
                ot = OT.get()
                for i in range(NQT):
                    O = ops_.get()
                    Dn = dps.get()
                    for _ in softmax_tile(i, parts, scale, madd_c, vt, O, Dn, bias_t=bias_t, aug=aug):
                        yield
                    normalize(O, Dn, ot.t[:, i * 512:(i + 1) * 512], ot.k)
                S.dma('sp', self.oT[idx], ot.t[:], reads=[ot.k], writes=[('D', 'oT', idx)])

            def fox_stream(h):
                qa = QA.get()
                S.dma('sp', qa.t[:], self.qaug[h], reads=[('D', 'qaug', h, i) for i in range(3)], writes=[qa.k])
                ncum = NCUM.get()
                with self.nc.allow_non_contiguous_dma(reason="tiny strided column load"):
                    csrc = self.cumT[h].rearrange("(c p) -> p c", p=128)
                    for c0 in range(0, NCH, 8):
                        S.dma('sp', ncum.t[:, c0:min(NCH, c0 + 8)], csrc[:, c0:min(NCH, c0 + 8)], reads=[('D', 'cumT')], writes=[(ncum.k, 'ld', c0 // 8), ncum.k])
                S.op('dve', lambda e: e.tensor_scalar(out=ncum.t[:], in0=ncum.t[:], scalar1=-1.0, scalar2=None, op0=ALU.mult),
                     reads=[(ncum.k, 'ld', j) for j in range((NCH + 7) // 8)], writes=[ncum.k])
                return head_stream(4 + h, lambda kt, qt: [(kt, qt, 0, 128)], 128 ** -0.5, C_MADD_CA, bias_t=ncum, aug=qa)
            for h0 in (0, 2):
                drive([fox_stream(h0), fox_stream(h0 + 1)])

            S.dma('sp', KR.t[:], self.krT[:, :], reads=[('D', 'krT', tg) for tg in range(NTG)], writes=['KR'])

            def mla_stream(h):
                qr = QR.get()
                S.dma('sp', qr.t[:], self.qrT[h], reads=dkeys('qrT', h), writes=[qr.k])
                return head_stream(8 + h, lambda kt, qt: [(kt, qt, 0, 128), (KR, qr, 0, 64)], 192 ** -0.5, C_MADD_CH)
            for h0 in (0, 2):
                drive([mla_stream(h0), mla_stream(h0 + 1)])

        with self.scope() as es:
            KT = self.ring("KT", 2, [128, SL], BF16)
            QT = self.ring("QT", 2, [128, SL], BF16)
            VV = self.ring("VV", 2, [128, NCH, 128], BF16)
            OT = self.ring("OT", 2, [128, SL], BF16)
            sps = self.ring("sps", 4, [128, 512], F32, psum=True)
            ops_ = self.ring("ops", 2, [128, 512], F32, psum=True)
            lps = self.ring("lps", 2, [128, 512], F32, psum=True)
            PT = self.ring("PT", 8, [128, 512], BF16)
            f32t = self.ring("f32t", 10, [128, 512], F32)
            sp32 = self.ring("sp32", 6, [128, 512], F32)
            spb = self.ring("spb", 6, [128, 512], BF16)
            sc = 128 ** -0.5

            def sb_stream(h, cast_eng):
                idx = 12 + h
                kt = KT.get()
                qt = QT.get()
                S.dma('sp', kt.t[:], self.kT[idx], reads=dkeys('kT', idx), writes=[kt.k])
                S.dma('sp', qt.t[:], self.qT[idx], reads=dkeys('qT', idx), writes=[qt.k])
                vt = VV.get()
                vsrc = self.vv[idx].rearrange("(c p) d -> p c d", p=128)
                for c0 in range(0, NCH, 8):
                    S.dma('sp', vt.t[:, c0:min(NCH, c0 + 8), :], vsrc[:, c0:min(NCH, c0 + 8), :], reads=dkeys('vv', idx), writes=[(vt.k, c0 // 8)])
                ot = OT.get()
                spsum = self.sb("spsum%d" % h, [128, 512], F32)
                spsumb = self.ring("spsumb%d" % h, 3, [128, 512], BF16)
                for i in range(NQT):
                    O = ops_.get()
                    last = 4 * i + 3
                    order = list(range(last, -1, -1))
                    n = len(order)
                    st1 = {}
                    st2 = {}
                    ssb_prev = [None]

                    def stage1(g):
                        j0 = g - 4 * i
                        z = sps.get()
                        S.op('pe', lambda e: e.matmul(z.t[:], lhsT=kt.t[:, g * 128:(g + 1) * 128], rhs=qt.t[:, i * 512:(i + 1) * 512], start=True, stop=True),
                             reads=[kt.k, qt.k], writes=[z.k])
                        ee = f32t.get()
                        S.op('act', lambda e: e.activation(out=ee.t[:], in_=z.t[:], func=AF.Exp, scale=sc), reads=[z.k], writes=[ee.k])
                        s32 = sp32.get()
                        S.op('act', lambda e: e.activation(out=s32.t[:], in_=ee.t[:], func=AF.Ln, bias=1.0), reads=[ee.k], writes=[s32.k])
                        sb_ = spb.get()
                        if j0 >= 0:
                            S.op('pool', lambda e: e.tensor_tensor(out=sb_.t[:], in0=s32.t[:], in1=self.cbs(C_MMUL + j0 * 512, 512), op=ALU.mult),
                                 reads=[s32.k, 'cb'], writes=[sb_.k])
                        else:
                            S.op('dve', lambda e: e.tensor_copy(out=sb_.t[:], in_=s32.t[:]), reads=[s32.k], writes=[sb_.k])
                        st1[g] = (z, s32, sb_)

                    def stage2(g):
                        j0 = g - 4 * i
                        z, s32, sb_ = st1.pop(g)
                        first = (g == last)
                        Lp = lps.get()
                        S.op('pe', lambda e: e.matmul(Lp.t[:], lhsT=self.cbs(C_TRIS, 128), rhs=sb_.t[:], start=True, stop=first), reads=[sb_.k, 'cb'], writes=[Lp.k])
                        if not first:
                            ssb = ssb_prev[0]
                            S.op('pe', lambda e: e.matmul(Lp.t[:], lhsT=self.cbs(C_ONES, 128), rhs=ssb.t[:], start=False, stop=True), reads=[ssb.k, 'cb'], writes=[Lp.k])
                        t1 = f32t.get()
                        S.op('dve', lambda e: e.scalar_tensor_tensor(out=t1.t[:], in0=z.t[:], scalar=sc, in1=s32.t[:], op0=ALU.mult, op1=ALU.subtract),
                             reads=[z.k, s32.k], writes=[t1.k])
                        S.op('dve', lambda e: e.tensor_tensor(out=t1.t[:], in0=t1.t[:], in1=Lp.t[:], op=ALU.subtract), reads=[t1.k, Lp.k], writes=[t1.k])
                        wt = PT.get()
                        S.op('act', lambda e: e.activation(out=wt.t[:], in_=t1.t[:], func=AF.Exp), reads=[t1.k], writes=[wt.k])
                        if j0 >= 0:
                            S.op('pool', lambda e: e.tensor_tensor(out=wt.t[:], in0=wt.t[:], in1=self.cbs(C_MMUL + j0 * 512, 512), op=ALU.mult),
                                 reads=[wt.k, 'cb'], writes=[wt.k])
                        if g > 0:
                            if first:
                                S.op('pool', lambda e: e.tensor_copy(out=spsum.t[:], in_=sb_.t[:]), reads=[sb_.k], writes=[spsum.k])
                            else:
                                S.op('dve', lambda e: e.tensor_tensor(out=spsum.t[:], in0=spsum.t[:], in1=sb_.t[:], op=ALU.add), reads=[spsum.k, sb_.k], writes=[spsum.k])
                            ssb = spsumb.get()
                            S.op('act', lambda e: e.activation(out=ssb.t[:], in_=spsum.t[:], func=AF.Copy), reads=[spsum.k], writes=[ssb.k])
                            ssb_prev[0] = ssb
                        st2[g] = wt

                    def stage3(g):
                        wt = st2.pop(g)
                        S.op('pe', lambda e: e.matmul(O.t[:], lhsT=vt.t[:, g, :], rhs=wt.t[:], start=(g == last), stop=(g == 0)), reads=[(vt.k, g // 8), wt.k], writes=[O.k])
                    for step in range(n + 2):
                        if step < n:
                            stage1(order[step])
                        if 0 <= step - 1 < n:
                            stage2(order[step - 1])
                        if 0 <= step - 2 < n:
                            stage3(order[step - 2])
                        yield
                    S.op('act', lambda e: e.activation(out=ot.t[:, i * 512:(i + 1) * 512], in_=O.t[:], func=AF.Copy), reads=[O.k], writes=[ot.k])
                S.dma('sp', self.oT[idx], ot.t[:], reads=[ot.k], writes=[('D', 'oT', idx)])
            for h0 in (0, 2):
                drive([sb_stream(h0, 'dve'), sb_stream(h0 + 1, 'dve')])

    def phase_merge(self, l):
        S, SL, TG = self.S, self.SL, self.TG
        NT = TG // 512
        for tg in range(self.NTG):
            t0 = tg * TG
            with self.scope() as es:
                ot = self.sb("mot", [128, 16, TG], BF16)
                for i in range(16):
                    S.dma('sp', ot.t[:, i, :], self.oT[i, :, t0:t0 + TG], reads=[('D', 'oT', i)], writes=['mot'])
                pp = self.ring("mpp", 4, [128, 512], F32, psum=True)
                gt = self.ring("mgt", 2, [128, TG], BF16)
                acc = self.sb("macc", [128, TG], F32)
                mo = self.ring("mmo", 2, [128, TG], BF16)
                tmp = self.ring("mtmp", 2, [128, 512], F32)
                for c in range(16):
                    for n_ in range(4):
                        wb = self.wload(self.w_branch[l, n_].rearrange("(k p) c -> p k c", p=128)[:, :, c * 128:(c + 1) * 128], 4, 128)
                        g = gt.get()
                        grow = n_ * 16 + c
                        S.dma('sp', g.t[:], self.gatesT[grow * 128:(grow + 1) * 128, t0:t0 + TG], reads=[('D', 'gatesT', grow, tg)], writes=[g.k])
                        wv = wb.t[:, 0:512].rearrange("p (k m) -> p k m", k=4)
                        for n in range(NT):
                            sl = slice(n * 512, (n + 1) * 512)
                            p = pp.get()
                            for k in range(4):
                                S.op('pe', lambda e: e.matmul(p.t[:], lhsT=wv[:, k, :], rhs=ot.t[:, n_ * 4 + k, sl], start=(k == 0), stop=(k == 3)),
                                     reads=[wb.k, 'mot'], writes=[p.k])
                            if n_ == 0:
                                S.op('dve', lambda e: e.tensor_tensor(out=acc.t[:, sl], in0=p.t[:], in1=g.t[:, sl], op=ALU.mult), reads=[p.k, g.k], writes=['macc'])
                            else:
                                t = tmp.get()
                                S.op('dve', lambda e: e.tensor_tensor(out=t.t[:], in0=p.t[:], in1=g.t[:, sl], op=ALU.mult), reads=[p.k, g.k], writes=[t.k])
                                S.op('pool', lambda e: e.tensor_tensor(out=acc.t[:, sl], in0=acc.t[:, sl], in1=t.t[:], op=ALU.add), reads=['macc', t.k], writes=['macc'])
                    m = mo.get()
                    S.op('act', lambda e: e.activation(out=m.t[:], in_=acc.t[:], func=AF.Copy), reads=['macc'], writes=[m.k])
                    S.dma('sp', self.mergedT[c * 128:(c + 1) * 128, t0:t0 + TG], m.t[:], reads=[m.k], writes=[('D', 'mergedT', c, tg)])

    def load_resident(self, dst, w_ap, nk):
        S = self.S
        wv = w_ap.rearrange("(k p) c -> p k c", p=128)
        for k in range(nk):
            st = self.wst.get()
            S.dma('sp', st.t[:], wv[:, k, :], writes=[st.k])
            self.cast(dst.t[:, k, :], st.t[:], [st.k], [dst.k])

    def layer_norm(self, v, stt, mvt, g_t, b_t, out):
        S = self.S
        for j in range(4):
            S.op('dve', lambda e: e.bn_stats(out=stt.t[:, j * 6:(j + 1) * 6], in_=v.t[:, j * 512:(j + 1) * 512]), reads=[v.k], writes=[stt.k])
        S.op('dve', lambda e: e.bn_aggr(out=mvt.t[:, 0:2], in_=stt.t[:]), reads=[stt.k], writes=[mvt.k])
        S.op('act', lambda e: e.activation(out=mvt.t[:, 2:3], in_=mvt.t[:, 1:2], func=AF.Sqrt, bias=self.epsln.t[:, 0:1], scale=1.0),
             reads=[mvt.k, 'epsln'], writes=[mvt.k])
        S.op('dve', lambda e: e.reciprocal(out=mvt.t[:, 3:4], in_=mvt.t[:, 2:3]), reads=[mvt.k], writes=[mvt.k])
        S.op('dve', lambda e: e.tensor_scalar(out=out.t[:], in0=v.t[:], scalar1=mvt.t[:, 0:1], scalar2=mvt.t[:, 3:4], op0=ALU.subtract, op1=ALU.mult),
             reads=[v.k, mvt.k], writes=[out.k])
        S.op('pool', lambda e: e.tensor_tensor(out=out.t[:], in0=out.t[:], in1=g_t.t[:], op=ALU.mult), reads=[out.k, g_t.k], writes=[out.k])
        S.op('dve', lambda e: e.tensor_tensor(out=out.t[:], in0=out.t[:], in1=b_t.t[:], op=ALU.add), reads=[out.k, b_t.k], writes=[out.k])

    def transpose_store(self, hb, ptr, dstT_tile, evac_eng='act'):
        S = self.S
        for k8 in range(0, 16, 8):
            p = ptr.get()
            for j in range(8):
                k = k8 + j
                S.op('pe', lambda e: e.transpose(p.t[:, j * 128:(j + 1) * 128], hb.t[:, k * 128:(k + 1) * 128], self.cbs(C_IDENT, 128)),
                     reads=[hb.k, 'cb'], writes=[p.k])
            S.op('act', lambda e: e.activation(out=dstT_tile.t[:, k8:k8 + 8, :], in_=p.t[:].rearrange("p (c d) -> p c d", c=8), func=AF.Copy),
                 reads=[p.k], writes=[dstT_tile.k])

    def phase_wo(self, l):
        S, SL, NCH = self.S, self.SL, self.NCH
        CAP = self.CAP
        with self.scope() as es:
            wo = self.sb("wo", [128, 16, D], BF16)
            self.load_resident(wo, self.w_o[l], 16)
            wr = self.sb("wr", [128, 16, 36], BF16)
            wrs = self.sb("wrs", [128, 16, 36], F32)
            S.dma('sp', wrs.t[:, :, 0:4], self.w_rg[l].rearrange("(k p) c -> p k c", p=128), writes=['wrs'])
            S.dma('sp', wrs.t[:, :, 4:36], self.w_re[l].rearrange("(k p) c -> p k c", p=128), writes=['wrs'])
            S.op('dve', lambda e: e.tensor_copy(out=wr.t[:], in_=wrs.t[:]), reads=['wrs'], writes=['wr'])
            brt = self.sb("brt", [128, 36], F32)
            S.dma('sp', brt.t[:, 0:4], self.b_rg[l, 0:1, :].partition_broadcast(128), writes=['brt'])
            S.dma('sp', brt.t[:, 4:36], self.b_re[l, 0:1, :].partition_broadcast(128), writes=['brt'])
            g_t = self.sb("l1g", [128, D], F32)
            b_t = self.sb("l1b", [128, D], F32)
            S.dma('sp', g_t.t[:], self.ln1_g[l, 0:1, :].partition_broadcast(128), writes=['l1g'])
            S.dma('sp', b_t.t[:], self.ln1_b[l, 0:1, :].partition_broadcast(128), writes=['l1b'])
            base = self.sb("rbase", [128, 32], F32)
            S.op('dve', lambda e: e.memset(base.t[:], 0.0), writes=['rbase'])
            mT = self.ring("wmT", 2, [128, 16, 128], BF16)
            xr = self.ring("wxr", 2, [128, D], F32)
            ht = self.ring("wh", 2, [128, D], F32)
            hb = self.ring("whb", 2, [128, D], BF16)
            hTt = self.ring("whT", 2, [128, 16, 128], BF16)
            stt = self.ring("wstt", 2, [128, 24], F32)
            mvt = self.ring("wmv", 2, [128, 4], F32)
            pp = self.ring("wpp", 4, [128, 512], F32, psum=True)
            ptr = self.ring("wptr", 2, [128, 1024], BF16, psum=True)
            prt = self.ring("wprt", 2, [128, 64], F32, psum=True)
            sm = self.ring("wsm", 2, [128, 256], F32)
            mb = self.ring("wmb", 2, [128, 32], BF16)
            x_src = self.x_tm if l == 0 else self.xres
            mres = [('D', 'mergedT', c, tg) for c in range(16) for tg in range(self.NTG)]
            def stageA(c):
                r0 = c * 128
                m = mT.get()
                S.dma('sp', m.t[:], self.mergedT[:, r0:r0 + 128].rearrange("(k p) t -> p k t", p=128), reads=mres, writes=[m.k])
                x = xr.get()
                S.dma('sp', x.t[:], x_src[r0:r0 + 128, :], reads=([('D', 'xres', c)] if l > 0 else []), writes=[x.k])
                v = x
                for ct in range(4):
                    p = pp.get()
                    for k in range(16):
                        S.op('pe', lambda e: e.matmul(p.t[:], lhsT=m.t[:, k, :], rhs=wo.t[:, k, ct * 512:(ct + 1) * 512], start=(k == 0), stop=(k == 15)),
                             reads=[m.k, 'wo'], writes=[p.k])
                    S.op('dve', lambda e: e.scalar_tensor_tensor(out=v.t[:, ct * 512:(ct + 1) * 512], in0=x.t[:, ct * 512:(ct + 1) * 512], scalar=ALPHA,
                                                                 in1=p.t[:], op0=ALU.mult, op1=ALU.add), reads=[x.k, p.k], writes=[v.k])
                h = ht.get()
                self.layer_norm(v, stt.get(), mvt.get(), g_t, b_t, h)
                hbb = hb.get()
                S.op('act', lambda e: e.activation(out=hbb.t[:], in_=h.t[:], func=AF.Copy), reads=[h.k], writes=[hbb.k])
                return h, hbb

            def stageB(c, h, hbb):
                r0 = c * 128
                S.dma('sp', self.hres[r0:r0 + 128, :], h.t[:], reads=[h.k], writes=[('D', 'hres', c)])
                hT = hTt.get()
                self.transpose_store(hbb, ptr, hT)
                S.dma('sp', self.hT[:, r0:r0 + 128].rearrange("(k p) t -> p k t", p=128), hT.t[:], reads=[hT.k], writes=[('D', 'hT', c)])
                pr = prt.get()
                for k in range(16):
                    S.op('pe', lambda e: e.matmul(pr.t[:, 0:36], lhsT=hT.t[:, k, :], rhs=wr.t[:, k, :], start=(k == 0), stop=(k == 15)),
                         reads=[hT.k, 'wr'], writes=[pr.k])
                s = sm.get()
                sk = s.k
                A = s.t

                def dv(fn, extra_r=(), w=None):
                    S.op('dve', fn, reads=[sk] + list(extra_r), writes=[w or sk])
                dv(lambda e: e.tensor_tensor(out=A[:, 0:36], in0=pr.t[:, 0:36], in1=brt.t[:], op=ALU.add), extra_r=[pr.k, 'brt'])
                dv(lambda e: e.reduce_max(out=A[:, 36:37], in_=A[:, 0:4], axis=mybir.AxisListType.X))
                dv(lambda e: e.tensor_scalar(out=A[:, 224:228], in0=A[:, 0:4], scalar1=A[:, 36:37], scalar2=None, op0=ALU.subtract))
                S.op('act', lambda e: e.activation(out=A[:, 224:228], in_=A[:, 224:228], func=AF.Exp), reads=[sk], writes=[sk])
                dv(lambda e: e.reduce_sum(out=A[:, 37:38], in_=A[:, 224:228], axis=mybir.AxisListType.X))
                dv(lambda e: e.reciprocal(out=A[:, 38:39], in_=A[:, 37:38]))
                dv(lambda e: e.tensor_scalar(out=A[:, 40:44], in0=A[:, 0:4], scalar1=A[:, 36:37], scalar2=None, op0=ALU.is_equal))
                dv(lambda e: e.tensor_scalar(out=A[:, 44:52], in0=A[:, 4:12], scalar1=A[:, 40:41], scalar2=None, op0=ALU.mult))
                for j in range(1, 4):
                    dv(lambda e: e.scalar_tensor_tensor(out=A[:, 44:52], in0=A[:, 4 + 8 * j:12 + 8 * j], scalar=A[:, 40 + j:41 + j], in1=A[:, 44:52],
                                                        op0=ALU.mult, op1=ALU.add))
                dv(lambda e: e.reduce_max(out=A[:, 52:53], in_=A[:, 44:52], axis=mybir.AxisListType.X))
                dv(lambda e: e.tensor_scalar(out=A[:, 56:64], in0=A[:, 44:52], scalar1=A[:, 52:53], scalar2=None, op0=ALU.is_equal))
                dv(lambda e: e.scalar_tensor_tensor(out=A[:, 72:80], in0=A[:, 56:64], scalar=-1e30, in1=A[:, 44:52], op0=ALU.mult, op1=ALU.add))
                dv(lambda e: e.reduce_max(out=A[:, 53:54], in_=A[:, 72:80], axis=mybir.AxisListType.X))
                dv(lambda e: e.tensor_scalar(out=A[:, 64:72], in0=A[:, 72:80], scalar1=A[:, 53:54], scalar2=None, op0=ALU.is_equal))
                dv(lambda e: e.tensor_tensor(out=A[:, 80:81], in0=A[:, 53:54], in1=A[:, 52:53], op=ALU.subtract))
                S.op('act', lambda e: e.activation(out=A[:, 80:81], in_=A[:, 80:81], func=AF.Exp), reads=[sk], writes=[sk])
                dv(lambda e: e.tensor_scalar(out=A[:, 81:82], in0=A[:, 80:81], scalar1=1.0, scalar2=None, op0=ALU.add))
                dv(lambda e: e.reciprocal(out=A[:, 81:82], in_=A[:, 81:82]))
                dv(lambda e: e.tensor_tensor(out=A[:, 81:82], in0=A[:, 81:82], in1=A[:, 38:39], op=ALU.mult))
                dv(lambda e: e.tensor_tensor(out=A[:, 82:83], in0=A[:, 38:39], in1=A[:, 81:82], op=ALU.subtract))
                S.op('dve', lambda e: e.tensor_copy(out=self.w_all.t[:, c, :], in_=A[:, 81:83]), reads=[sk], writes=['w_all'])
                for j in range(4):
                    dv(lambda e: e.tensor_scalar(out=A[:, 96 + 8 * j:104 + 8 * j], in0=A[:, 56:64], scalar1=A[:, 40 + j:41 + j], scalar2=None, op0=ALU.mult))
                    dv(lambda e: e.tensor_scalar(out=A[:, 128 + 8 * j:136 + 8 * j], in0=A[:, 64:72], scalar1=A[:, 40 + j:41 + j], scalar2=None, op0=ALU.mult))
                dv(lambda e: e.tensor_tensor(out=A[:, 160:192], in0=A[:, 96:128], in1=A[:, 128:160], op=ALU.add))
                mbb = mb.get()
                S.op('dve', lambda e: e.tensor_copy(out=mbb.t[:], in_=A[:, 160:192]), reads=[sk], writes=[mbb.k])
                pc = prt.get()
                S.op('pe', lambda e: e.matmul(pc.t[:, 0:32], lhsT=self.cbs(C_TRIL, 128), rhs=mbb.t[:], start=True, stop=True), reads=[mbb.k, 'cb'], writes=[pc.k])
                S.op('pe', lambda e: e.matmul(pc.t[:, 32:64], lhsT=self.cbs(C_ONES, 128), rhs=mbb.t[:], start=True, stop=True), reads=[mbb.k, 'cb'], writes=[pc.k])
                dv(lambda e: e.tensor_tensor(out=A[:, 192:224], in0=pc.t[:, 0:32], in1=base.t[:], op=ALU.add), extra_r=[pc.k, 'rbase'])
                dv(lambda e: e.tensor_tensor(out=A[:, 192:224], in0=A[:, 192:224], in1=self.cf.t[:, 2:34], op=ALU.add), extra_r=['cf'])
                S.op('dve', lambda e: e.tensor_tensor(out=base.t[:], in0=base.t[:], in1=pc.t[:, 32:64], op=ALU.add), reads=['rbase', pc.k, sk], writes=['rbase'])
                dv(lambda e: e.tensor_tensor(out=A[:, 96:128], in0=A[:, 96:128], in1=A[:, 192:224], op=ALU.mult))
                dv(lambda e: e.tensor_tensor(out=A[:, 128:160], in0=A[:, 128:160], in1=A[:, 192:224], op=ALU.mult))
                dv(lambda e: e.reduce_sum(out=A[:, 228:229], in_=A[:, 96:128], axis=mybir.AxisListType.X))
                dv(lambda e: e.reduce_sum(out=A[:, 229:230], in_=A[:, 128:160], axis=mybir.AxisListType.X))
                S.op('dve', lambda e: e.tensor_copy(out=self.dest_all.t[:, c, :], in_=A[:, 228:230]), reads=[sk], writes=['dest_all'])
                for kk in range(2):
                    S.dma('pool', None, None, reads=[hbb.k, 'dest_all'], writes=[('D', 'hrows', c, kk)],
                          fn=lambda e: e.indirect_dma_start(out=self.hrows[:, :], out_offset=bass.IndirectOffsetOnAxis(ap=self.dest_all.t[:, c, kk:kk + 1], axis=0),
                                                            in_=hbb.t[:], in_offset=None))

            cur = stageA(0)
            for c in range(NCH):
                nxt = stageA(c + 1) if c + 1 < NCH else None
                stageB(c, *cur)
                cur = nxt

    def phase_experts(self, l):
        S, NCH, CAP, CAPB = self.S, self.NCH, self.CAP, self.CAPB
        with self.scope() as es:
            rows = self.ring("erow", 2, [128, D], BF16)
            rT = self.ring("erT", 2, [128, 16, CAP], BF16)
            hidT = self.ring("ehid", 2, [128, 4, CAP], BF16)
            sil = self.ring("esil", 2, [128, CAP], F32)
            ysb = self.ring("eysb", CAPB, [128, D], F32)
            ptr = self.ring("eptr", 2, [128, 1024], BF16, psum=True)
            pg = self.ring("epg", 2, [128, 512], F32, psum=True)
            pu = self.ring("epu", 2, [128, 512], F32, psum=True)
            py = self.ring("epy", 2, [128, 512], F32, psum=True)
            hr = [('D', 'hrows', c, kk) for c in range(NCH) for kk in range(2)]
            EST = self.ring("est", 4, [128, 2048], F32)
            WG = self.ring("ewg", 2, [128, 16, EH], BF16)
            WU = self.ring("ewu", 2, [128, 16, EH], BF16)
            def load_gu(ex_):
                weg_ = self.w_eg[l, ex_].rearrange("(k p) c -> p k c", p=128)
                weu_ = self.w_eu[l, ex_].rearrange("(k p) c -> p k c", p=128)
                wg_ = WG.get()
                wu_ = WU.get()
                for (src, dstb) in ((weg_, wg_), (weu_, wu_)):
                    for kq in range(4):
                        st = EST.get()
                        sk4 = [(st.k, jj) for jj in range(4)]
                        S.dma('sp', st.t[:].rearrange("p (a b) -> p a b", a=4), src[:, kq * 4:(kq + 1) * 4, :], writes=sk4)
                        self.cast(dstb.t[:, kq * 4:(kq + 1) * 4, :], st.t[:].rearrange("p (a b) -> p a b", a=4), sk4, [(dstb.k, kq)])
                return wg_, wu_
            nxt_w = None
            for ex in range(NEXP):
                rt = rT.get()
                for b in range(CAPB):
                    r = rows.get()
                    S.dma('sp', r.t[:], self.hrows[ex * CAP + b * 128:ex * CAP + (b + 1) * 128, :], reads=hr, writes=[r.k])
                    for k8 in range(0, 16, 8):
                        p = ptr.get()
                        for j in range(8):
                            k = k8 + j
                            S.op('pe', lambda e: e.transpose(p.t[:, j * 128:(j + 1) * 128], r.t[:, k * 128:(k + 1) * 128], self.cbs(C_IDENT, 128)),
                                 reads=[r.k, 'cb'], writes=[p.k])
                        S.op('act' if k8 == 0 else 'dve',
                             (lambda e: e.activation(out=rt.t[:, k8:k8 + 8, b * 128:(b + 1) * 128], in_=p.t[:].rearrange("p (c d) -> p c d", c=8), func=AF.Copy)) if k8 == 0 else
                             (lambda e: e.tensor_copy(out=rt.t[:, k8:k8 + 8, b * 128:(b + 1) * 128], in_=p.t[:].rearrange("p (c d) -> p c d", c=8))),
                             reads=[p.k], writes=[rt.k])
                hd = hidT.get()
                weg = self.w_eg[l, ex].rearrange("(k p) c -> p k c", p=128)
                weu = self.w_eu[l, ex].rearrange("(k p) c -> p k c", p=128)
                if ex == 0:
                    nxt_w = load_gu(0)
                wg, wu = nxt_w
                if ex + 1 < NEXP:
                    nxt_w = load_gu(ex + 1)
                for j in range(4):
                    g_ = pg.get()
                    u_ = pu.get()
                    for k in range(16):
                        S.op('pe', lambda e: e.matmul(g_.t[:, 0:CAP], lhsT=wg.t[:, k, j * 128:(j + 1) * 128], rhs=rt.t[:, k, :], start=(k == 0), stop=(k == 15)),
                             reads=[(wg.k, k // 4), rt.k], writes=[g_.k])
                    for k in range(16):
                        S.op('pe', lambda e: e.matmul(u_.t[:, 0:CAP], lhsT=wu.t[:, k, j * 128:(j + 1) * 128], rhs=rt.t[:, k, :], start=(k == 0), stop=(k == 15)),
                             reads=[(wu.k, k // 4), rt.k], writes=[u_.k])
                    s_ = sil.get()
                    S.op('act', lambda e: e.activation(out=s_.t[:], in_=g_.t[:, 0:CAP], func=AF.Silu), reads=[g_.k], writes=[s_.k])
                    S.op('dve', lambda e: e.tensor_tensor(out=hd.t[:, j, :], in0=s_.t[:], in1=u_.t[:, 0:CAP], op=ALU.mult), reads=[s_.k, u_.k], writes=[hd.k])
                wed = self.w_ed[l, ex].rearrange("(k p) c -> p k c", p=128)
                ys = [ysb.get() for _ in range(CAPB)]
                for ct in range(4):
                    wd = self.wload(wed[:, :, ct * 512:(ct + 1) * 512], 4, 512)
                    wdv = wd.t[:].rearrange("p (k m) -> p k m", k=4)
                    for b in range(CAPB):
                        y_ = py.get()
                        for j in range(4):
                            S.op('pe', lambda e: e.matmul(y_.t[:], lhsT=hd.t[:, j, b * 128:(b + 1) * 128], rhs=wdv[:, j, :], start=(j == 0), stop=(j == 3)),
                                 reads=[hd.k, wd.k], writes=[y_.k])
                        if (ct + b) % 2 == 0:
                            S.op('act', lambda e: e.activation(out=ys[b].t[:, ct * 512:(ct + 1) * 512], in_=y_.t[:], func=AF.Copy), reads=[y_.k], writes=[ys[b].k])
                        else:
                            S.op('dve', lambda e: e.tensor_copy(out=ys[b].t[:, ct * 512:(ct + 1) * 512], in_=y_.t[:]), reads=[y_.k], writes=[ys[b].k])
                for b in range(CAPB):
                    S.dma('sp', self.yrows[ex * CAP + b * 128:ex * CAP + (b + 1) * 128, :], ys[b].t[:], reads=[ys[b].k], writes=[('D', 'yrows', ex, b)])

    def phase_ple(self, l, last):
        S, SL, NCH, CAPB = self.S, self.SL, self.NCH, self.CAPB
        with self.scope() as es:
            wpg = self.sb("wpg", [128, 16, D], BF16)
            self.load_resident(wpg, self.w_pg[l], 16)
            wpp = self.sb("wpp", [128, 2, D], BF16)
            self.load_resident(wpp, self.w_pp[l], 2)
            g_t = self.sb("l2g", [128, D], F32)
            b_t = self.sb("l2b", [128, D], F32)
            S.dma('sp', g_t.t[:], self.ln2_g[l, 0:1, :].partition_broadcast(128), writes=['l2g'])
            S.dma('sp', b_t.t[:], self.ln2_b[l, 0:1, :].partition_broadcast(128), writes=['l2b'])
            hTt = self.ring("phT", 2, [128, 16, 128], BF16)
            pTs = self.ring("ppTs", 2, [128, 2, 128], F32)
            pTb = self.ring("ppTb", 2, [128, 2, 128], BF16)
            hr = self.ring("phr", 2, [128, D], F32)
            y1 = self.ring("py1", 1, [128, D], F32)
            y2 = self.ring("py2", 1, [128, D], F32)
            sg = self.ring("psg", 1, [128, D], F32)
            xb = self.ring("pxb", 1, [128, D], BF16)
            xTt = self.ring("pxT", 2, [128, 16, 128], BF16)
            stt = self.ring("pstt", 2, [128, 24], F32)
            mvt = self.ring("pmv", 2, [128, 4], F32)
            ppg = self.ring("ppg", 3, [128, 512], F32, psum=True)
            ppp = self.ring("ppp", 3, [128, 512], F32, psum=True)
            ptr = self.ring("pptr", 2, [128, 1024], BF16, psum=True)
            yr = [('D', 'yrows', ex, b) for ex in range(NEXP) for b in range(CAPB)]
            for c in range(NCH):
                r0 = c * 128
                hT = hTt.get()
                S.dma('sp', hT.t[:], self.hT[:, r0:r0 + 128].rearrange("(k p) t -> p k t", p=128), reads=[('D', 'hT', c)], writes=[hT.k])
                ps_ = pTs.get()
                S.dma('sp', ps_.t[:], self.pT[l, :, r0:r0 + 128].rearrange("(k p) t -> p k t", p=128), writes=[ps_.k])
                pb = pTb.get()
                S.op('pool', lambda e: e.tensor_copy(out=pb.t[:], in_=ps_.t[:]), reads=[ps_.k], writes=[pb.k])
                h = hr.get()
                S.dma('sp', h.t[:], self.hres[r0:r0 + 128, :], reads=[('D', 'hres', c)], writes=[h.k])
                ya = y1.get()
                yb = y2.get()
                for kk, yy in ((0, ya), (1, yb)):
                    S.dma('pool', None, None, reads=yr + ['dest_all'], writes=[yy.k],
                          fn=lambda e: e.indirect_dma_start(out=yy.t[:], out_offset=None, in_=self.yrows[:, :],
                                                            in_offset=bass.IndirectOffsetOnAxis(ap=self.dest_all.t[:, c, kk:kk + 1], axis=0)))
                s_ = sg.get()
                v = s_
                for ct in range(4):
                    sl = slice(ct * 512, (ct + 1) * 512)
                    pg_ = ppg.get()
                    for k in range(16):
                        S.op('pe', lambda e: e.matmul(pg_.t[:], lhsT=hT.t[:, k, :], rhs=wpg.t[:, k, sl], start=(k == 0), stop=(k == 15)), reads=[hT.k, 'wpg'], writes=[pg_.k])
                    pp_ = ppp.get()
                    for k in range(2):
                        S.op('pe', lambda e: e.matmul(pp_.t[:], lhsT=pb.t[:, k, :], rhs=wpp.t[:, k, sl], start=(k == 0), stop=(k == 1)), reads=[pb.k, 'wpp'], writes=[pp_.k])
                    S.op('act', lambda e: e.activation(out=s_.t[:, sl], in_=pg_.t[:], func=AF.Sigmoid), reads=[pg_.k], writes=[s_.k])
                    S.op('dve', lambda e: e.tensor_tensor(out=s_.t[:, sl], in0=s_.t[:, sl], in1=pp_.t[:], op=ALU.mult), reads=[s_.k, pp_.k], writes=[s_.k])
                S.op('dve', lambda e: e.scalar_tensor_tensor(out=v.t[:], in0=h.t[:], scalar=ALPHA, in1=s_.t[:], op0=ALU.mult, op1=ALU.add), reads=[h.k, s_.k], writes=[v.k])
                S.op('dve', lambda e: e.scalar_tensor_tensor(out=v.t[:], in0=ya.t[:], scalar=self.w_all.t[:, c, 0:1], in1=v.t[:], op0=ALU.mult, op1=ALU.add),
                     reads=[ya.k, 'w_all', v.k], writes=[v.k])
                S.op('dve', lambda e: e.scalar_tensor_tensor(out=v.t[:], in0=yb.t[:], scalar=self.w_all.t[:, c, 1:2], in1=v.t[:], op0=ALU.mult, op1=ALU.add),
                     reads=[yb.k, 'w_all', v.k], writes=[v.k])
                x = h
                self.layer_norm(v, stt.get(), mvt.get(), g_t, b_t, x)
                if last:
                    S.dma('sp', self.out[r0:r0 + 128, :], x.t[:], reads=[x.k], writes=[('D', 'out', c)])
                else:
                    S.dma('sp', self.xres[r0:r0 + 128, :], x.t[:], reads=[x.k], writes=[('D', 'xres', c)])
                    xbb = xb.get()
                    S.op('act', lambda e: e.activation(out=xbb.t[:], in_=x.t[:], func=AF.Copy), reads=[x.k], writes=[xbb.k])
                    xT = xTt.get()
                    self.transpose_store(xbb, ptr, xT)
                    S.dma('sp', self.xT[:, r0:r0 + 128].rearrange("(k p) t -> p k t", p=128), xT.t[:], reads=[xT.k], writes=[('D', 'xT1', c)])


class _Scope:
    def __init__(self, prog):
        self.p = prog
        self.es = ExitStack()

    def __enter__(self):
        self.prev = self.p.cur
        self.p.cur = self.es
        self.es.__enter__()
        return self.es

    def __exit__(self, *a):
        self.p.S.barrier()
        r = self.es.__exit__(*a)
        self.p.cur = self.prev
        return r


class nc_allow:
    def __init__(self, nc):
        self.cm = nc.allow_non_contiguous_dma(reason="tiny strided column load")

    def __enter__(self):
        return self.cm.__enter__()

    def __exit__(self, *a):
        return self.cm.__exit__(*a)


def make_consts(CAP):
    c = np.zeros((128, NCONST), np.float32)
    i = np.arange(128)
    c[:, C_IDENT:C_IDENT + 128] = np.eye(128)
    c[:, C_ONES:C_ONES + 128] = 1.0
    c[:, C_TRIS:C_TRIS + 128] = (i[:, None] > i[None, :])
    c[:, C_TRIL:C_TRIL + 128] = (i[:, None] < i[None, :])
    Rd = np.zeros((128, 128), np.float32)
    for base in (0, 64):
        for j in range(8):
            Rd[base + j, base + j + 8] = -1.0
            Rd[base + j + 8, base + j] = 1.0
    c[:, C_RD:C_RD + 128] = Rd.T
    Rm = np.zeros((128, 128), np.float32)
    for j in range(32):
        Rm[j, j + 32] = -1.0
        Rm[j + 32, j] = 1.0
    c[:, C_RM:C_RM + 128] = Rm.T
    kk = i[:, None]
    qq = i[None, :]
    inblk = {
        'ch': (kk // 64 <= qq // 64),
        'ca': (kk <= qq),
        'st': (kk < qq),
    }
    for j0 in range(4):
        for b in range(4):
            for name, col, kind in (('ch', C_MADD_CH, 'add'), ('ca', C_MADD_CA, 'add'), ('st', C_MMUL, 'mul')):
                if b < j0:
                    valid = np.zeros((128, 128), bool)
                elif b == j0:
                    valid = inblk[name]
                else:
                    valid = np.ones((128, 128), bool)
                blk = np.where(valid, 0.0, NEG) if kind == 'add' else valid.astype(np.float32)
                c[:, col + j0 * 512 + b * 128:col + j0 * 512 + (b + 1) * 128] = blk
    invf_d = np.zeros(128, np.float32)
    fd = (np.float32(ROPE_THETA) ** (-np.arange(8, dtype=np.float32) / np.float32(8))).astype(np.float32)
    for base in (0, 64):
        invf_d[base:base + 8] = fd
        invf_d[base + 8:base + 16] = fd
    c[:, C_INVF_D] = invf_d
    fm = (np.float32(ROPE_THETA) ** (-np.arange(32, dtype=np.float32) / np.float32(32))).astype(np.float32)
    invf_m = np.zeros(128, np.float32)
    invf_m[0:32] = fm
    invf_m[32:64] = fm
    c[:, C_INVF_M] = invf_m
    c[:, C_ECAP:C_ECAP + 32] = (np.arange(32) * CAP)[None, :]
    return c


_CACHE = {}


def get_prog(S_len, **kw):
    key = (S_len, tuple(sorted((k, str(v)) for k, v in kw.items())))
    if key not in _CACHE:
        p = Prog(S_len, **kw)
        p.build()
        _CACHE[key] = p
    return _CACHE[key]


def make_in_map(p, b, inputs):
    f = lambda a: np.ascontiguousarray(np.asarray(a, dtype=np.float32))
    x = np.asarray(inputs['x'])[b]
    m = {
        'x_tm': f(x), 'x_fm': f(x.T),
        'pT': f(np.transpose(np.asarray(inputs['p'])[:, b], (0, 2, 1))),
        'pos': np.ascontiguousarray(np.asarray(inputs['positions'])[b:b + 1].astype(np.int32)),
        'consts': make_consts(p.CAP),
        'w_in': f(inputs['w_in']),
        'fox_f_bias': f(inputs['fox_f_bias']).reshape(DEPTH, 4, 1),
        'diff_lambda': f(inputs['diff_lambda']).reshape(DEPTH, 1, 256),
        'diff_subln': f(inputs['diff_subln']).reshape(DEPTH, 128, 1),
        'mla_q_norm': f(f(inputs['mla_q_norm']).reshape(DEPTH, 4, 128).transpose(0, 2, 1)),
        'mla_kv_norm': f(f(inputs['mla_kv_norm']).reshape(DEPTH, 2, 128).transpose(0, 2, 1)),
        'mla_w_uq': f(inputs['mla_w_uq']), 'mla_w_ukv': f(inputs['mla_w_ukv']),
        'w_branch': f(inputs['w_branch']), 'w_o': f(inputs['w_o']),
        'ln1_g': f(inputs['ln1_g']).reshape(DEPTH, 1, D), 'ln1_b': f(inputs['ln1_b']).reshape(DEPTH, 1, D),
        'w_router_group': f(inputs['w_router_group']), 'b_router_group': f(inputs['b_router_group']).reshape(DEPTH, 1, 4),
        'w_router_expert': f(inputs['w_router_expert']), 'b_router_expert': f(inputs['b_router_expert']).reshape(DEPTH, 1, 32),
        'w_expert_gate': f(inputs['w_expert_gate']), 'w_expert_up': f(inputs['w_expert_up']),
        'w_expert_down': f(inputs['w_expert_down']),
        'w_ple_gate': f(inputs['w_ple_gate']), 'w_ple_proj': f(inputs['w_ple_proj']),
        'ln2_g': f(inputs['ln2_g']).reshape(DEPTH, 1, D), 'ln2_b': f(inputs['ln2_b']).reshape(DEPTH, 1, D),
    }
    return m


def kernel(**inputs):
    x = np.asarray(inputs['x'])
    B, SL, _ = x.shape
    p = get_prog(SL)
    in_maps = [make_in_map(p, b, inputs) for b in range(B)]
    res = run_bass_kernel_spmd(p.nc, in_maps, core_ids=list(range(B)))
    out = np.stack([np.asarray(r['out']) for r in res.results], axis=0)
    return out.astype(np.float32)
```

```python
import math
import numpy as np
from contextlib import ExitStack
import concourse.bass as bass
import concourse.mybir as mybir
from concourse.bass_utils import run_bass_kernel_spmd

F32 = mybir.dt.float32
BF16 = mybir.dt.bfloat16
I32 = mybir.dt.int32
AF = mybir.ActivationFunctionType
ALU = mybir.AluOpType

D = 2048
DEPTH = 2
NEXP = 32
EH = 512
IN_DIM = 13636
ALPHA = 4.0 ** 0.25
LN_EPS = 1e-5
RMS_EPS = 1e-6
ROPE_THETA = 500000.0
OFF = dict(diff_q=0, diff_k=512, diff_v=1024, fox_q=1536, fox_k=2048, fox_v=2560, fox_f=3072,
           mla_cq=3076, mla_ckv=3588, mla_kr=3844, sb_q=3908, sb_k=4420, sb_v=4932, gates=5444)
NEG = -30000.0
TWO_PI = 6.283185307179586

C_IDENT, C_ONES, C_TRIS, C_TRIL, C_RD, C_RM = 0, 128, 256, 384, 512, 640
C_MADD_CH, C_MADD_CA, C_MMUL = 768, 768 + 2048, 768 + 4096
C_INVF_D, C_INVF_M, C_ECAP = 768 + 6144, 768 + 6145, 768 + 6146
NCONST = 768 + 6146 + 32


class Sched:
    NDMA = 16

    def __init__(self, nc, es):
        self.nc = nc
        self.es = es
        self.eng = {'pe': nc.tensor, 'dve': nc.vector, 'act': nc.scalar, 'pool': nc.gpsimd, 'sp': nc.sync}
        self.sems = {}
        self.cnt = {}
        for e in self.eng:
            self.sems[e] = es.enter_context(nc.semaphore('s_' + e))
            self.cnt[e] = 0
        self.dma_rr = {}
        for q in ('sp', 'act', 'pool'):
            self.dma_rr[q] = 0
            for i in range(self.NDMA):
                k = ('d', q, i)
                self.sems[k] = es.enter_context(nc.semaphore('d_%s_%d' % (q, i)))
                self.cnt[k] = 0
        self.seen = {e: {} for e in self.eng}
        self.res = {}
        self.nins = 0

    def _need(self, reads, writes):
        need = {}
        for r in reads:
            st = self.res.get(r)
            if st and st[0] is not None:
                k, v = st[0]
                if need.get(k, 0) < v:
                    need[k] = v
        for w in writes:
            st = self.res.get(w)
            if st:
                if st[0] is not None:
                    k, v = st[0]
                    if need.get(k, 0) < v:
                        need[k] = v
                for k, v in st[1].items():
                    if need.get(k, 0) < v:
                        need[k] = v
        return need

    def _commit(self, ev, reads, writes):
        k, v = ev
        for r in reads:
            st = self.res.get(r)
            if st is None:
                st = self.res[r] = [None, {}]
            if st[1].get(k, 0) < v:
                st[1][k] = v
        for w in writes:
            self.res[w] = [ev, {}]

    def _waits(self, e, need):
        eng = self.eng[e]
        seen = self.seen[e]
        for k, v in need.items():
            if e == 'pe' and k == 'pe':
                continue
            if seen.get(k, 0) < v:
                eng.wait_ge(self.sems[k], v)
                seen[k] = v

    def op(self, e, fn, reads=(), writes=()):
        self._waits(e, self._need(reads, writes))
        ins = fn(self.eng[e])
        self.cnt[e] += 1
        ins.then_inc(self.sems[e], 1)
        self._commit((e, self.cnt[e]), reads, writes)
        self.nins += 1

    def dma(self, q, out, in_, reads=(), writes=(), fn=None, **kw):
        if q == 'sp' and fn is None and str(out.space) == 'DRAM' and str(in_.space) != 'DRAM':
            q = 'act'
        i = self.dma_rr[q]
        self.dma_rr[q] = (i + 1) % self.NDMA
        k = ('d', q, i)
        need = self._need(reads, writes)
        if self.cnt[k]:
            need[k] = max(need.get(k, 0), self.cnt[k])
        self._waits(q, need)
        if fn is not None:
            ins = fn(self.eng[q])
        else:
            ins = self.eng[q].dma_start(out=out, in_=in_, **kw)
        self.cnt[k] += 16
        ins.then_inc(self.sems[k], 16)
        self._commit((k, self.cnt[k]), reads, writes)
        self.nins += 1

    def finish(self, keys):
        self._waits('sp', self._need(keys, ()))

    def barrier(self):
        need = {k: v for k, v in self.cnt.items() if v > 0}
        for e in self.eng:
            eng = self.eng[e]
            seen = self.seen[e]
            for k, v in need.items():
                if seen.get(k, 0) < v:
                    eng.wait_ge(self.sems[k], v)
                    seen[k] = v


class Ring:
    def __init__(self, tiles):
        self.tiles = tiles
        self.i = 0

    def get(self):
        t = self.tiles[self.i]
        self.i = (self.i + 1) % len(self.tiles)
        return t


class T:
    def __init__(self, ap, key):
        self.t = ap
        self.k = key


class Prog:
    def __init__(self, S_len, nlayers=DEPTH, dbg=False, phases=None):
        self.SL = S_len
        self.TG = min(2048, S_len)
        self.NTG = S_len // self.TG
        self.NCH = S_len // 128
        self.NQT = S_len // 512
        avg = 2 * S_len // NEXP
        sd = math.sqrt(2 * S_len / 32.0)
        self.CAPB = int(math.ceil((avg + 8.0 * sd) / 128.0))
        self.CAP = self.CAPB * 128
        self.nlayers = nlayers
        self.WD = DEPTH if (nlayers == DEPTH) else nlayers
        self.NE_DECL = NEXP if (phases is None or 'exp' in phases) else 1
        self.dbg = dbg
        self.phases = phases
        self.nc = bass.Bass("TRN2", target_bir_lowering=False)
        self.scopes = []
        self.uid = 0
        self.cast_rr = 0

    def din(self, name, shape, dt=F32):
        return self.nc.dram_tensor(name, list(shape), dt, kind="ExternalInput").ap()

    def dscr(self, name, shape, dt):
        kind = "ExternalOutput" if (self.dbg and name in self.dbg) else "Internal"
        return self.nc.dram_tensor(name, list(shape), dt, kind=kind).ap()

    def sb(self, name, shape, dt):
        self.uid += 1
        t = self.cur.enter_context(self.nc.sbuf_tensor("%s_%d" % (name, self.uid), list(shape), dt))
        return T(t, name)

    def ps(self, name, shape, dt=F32):
        self.uid += 1
        t = self.cur.enter_context(self.nc.psum_tensor("%s_%d" % (name, self.uid), list(shape), dt))
        return T(t, name)

    def ring(self, name, n, shape, dt, psum=False):
        return Ring([(self.ps if psum else self.sb)("%s%d" % (name, i), shape, dt) for i in range(n)])

    def wload(self, src_ap, a, b, q='sp'):
        st = self.wst.get()
        wb = self.wbf.get()
        S = self.S
        stv = st.t[:, 0:a * b].rearrange("p (a b) -> p a b", a=a)
        step = max(1, 4 * 128 // 128) if b <= 512 else 1
        step = 4
        keys = []
        for j, a0 in enumerate(range(0, a, step)):
            a1 = min(a, a0 + step)
            S.dma(q, stv[:, a0:a1, :], src_ap[:, a0:a1, :], writes=[(st.k, j)])
            keys.append((st.k, j))
        self.cast(wb.t[:, 0:a * b], st.t[:, 0:a * b], keys, [wb.k])
        return wb

    def cast(self, out_ap, in_ap, reads, writes):
        i = self.cast_rr
        self.cast_rr = (i + 1) % 3
        if i != 2:
            self.S.op('act', lambda e: e.activation(out=out_ap, in_=in_ap, func=AF.Copy), reads=reads, writes=writes)
        else:
            self.S.op('dve', lambda e: e.tensor_copy(out=out_ap, in_=in_ap), reads=reads, writes=writes)

    def build(self):
        nc = self.nc
        SL, TG, NCH = self.SL, self.TG, self.NCH
        L = self.nlayers
        self.x_tm = self.din("x_tm", [SL, D])
        self.x_fm = self.din("x_fm", [D, SL])
        self.pT = self.din("pT", [DEPTH, 256, SL])
        self.pos = self.din("pos", [1, SL], I32)
        self.consts = self.din("consts", [128, NCONST])
        self.w_in = self.din("w_in", [DEPTH, D, IN_DIM])
        self.fox_f_bias = self.din("fox_f_bias", [DEPTH, 4, 1])
        self.diff_lambda = self.din("diff_lambda", [DEPTH, 1, 256])
        self.diff_subln = self.din("diff_subln", [DEPTH, 128, 1])
        self.mla_q_norm = self.din("mla_q_norm", [DEPTH, 128, 4])
        self.mla_kv_norm = self.din("mla_kv_norm", [DEPTH, 128, 2])
        self.mla_w_uq = self.din("mla_w_uq", [DEPTH, 512, 768])
        self.mla_w_ukv = self.din("mla_w_ukv", [DEPTH, 256, 1024])
        self.w_branch = self.din("w_branch", [DEPTH, 4, 512, D])
        self.w_o = self.din("w_o", [DEPTH, D, D])
        self.ln1_g = self.din("ln1_g", [DEPTH, 1, D])
        self.ln1_b = self.din("ln1_b", [DEPTH, 1, D])
        self.w_rg = self.din("w_router_group", [DEPTH, D, 4])
        self.b_rg = self.din("b_router_group", [DEPTH, 1, 4])
        self.w_re = self.din("w_router_expert", [DEPTH, D, 32])
        self.b_re = self.din("b_router_expert", [DEPTH, 1, 32])
        self.w_eg = self.din("w_expert_gate", [self.WD, self.NE_DECL, D, EH])
        self.w_eu = self.din("w_expert_up", [self.WD, self.NE_DECL, D, EH])
        self.w_ed = self.din("w_expert_down", [self.WD, self.NE_DECL, EH, D])
        self.w_pg = self.din("w_ple_gate", [DEPTH, D, D])
        self.w_pp = self.din("w_ple_proj", [DEPTH, 256, D])
        self.ln2_g = self.din("ln2_g", [DEPTH, 1, D])
        self.ln2_b = self.din("ln2_b", [DEPTH, 1, D])
        self.out = nc.dram_tensor("out", [SL, D], F32, kind="ExternalOutput").ap()
        self.xT = self.dscr("xT", [D, SL], BF16)
        self.xres = self.dscr("xres", [SL, D], F32)
        self.ropeD = self.dscr("ropeD", [2, 128, SL], BF16)
        self.ropeM = self.dscr("ropeM", [2, 64, SL], BF16)
        self.qT = self.dscr("qT", [16, 128, SL], BF16)
        self.kT = self.dscr("kT", [16, 128, SL], BF16)
        self.vv = self.dscr("vv", [16, SL, 128], BF16)
        self.qrT = self.dscr("qrT", [4, 64, SL], BF16)
        self.krT = self.dscr("krT", [64, SL], BF16)
        self.fT = self.dscr("fT", [4, SL], F32)
        self.cumT = self.dscr("cumT", [4, SL], F32)
        self.qaug = self.dscr("qaug", [4, 3, SL], BF16)
        self.gatesT = self.dscr("gatesT", [4 * D, SL], BF16)
        self.oT = self.dscr("oT", [16, 128, SL], BF16)
        self.mergedT = self.dscr("mergedT", [D, SL], BF16)
        self.hres = self.dscr("hres", [SL, D], F32)
        self.hT = self.dscr("hT", [D, SL], BF16)
        self.hrows = self.dscr("hrows", [NEXP * self.CAP, D], BF16)
        self.yrows = self.dscr("yrows", [NEXP * self.CAP, D], F32)

        with ExitStack() as es:
            self.S = Sched(nc, es)
            self.glob = es
            self.cur = es
            self.cb = self.sb("cb", [128, C_INVF_D], BF16)
            self.cf = self.sb("cf", [128, 34], F32)
            self.S.dma('sp', self.cf.t[:], self.consts[:, C_INVF_D:C_INVF_D + 34], writes=['cf'])
            with self.scope() as es0:
                for c0 in range(0, C_INVF_D, 2048):
                    c1 = min(C_INVF_D, c0 + 2048)
                    cft = self.sb("cft%d" % c0, [128, 2048], F32)
                    self.S.dma('sp', cft.t[:, 0:c1 - c0], self.consts[:, c0:c1], writes=[cft.k])
                    self.S.op('dve', lambda e: e.tensor_copy(out=self.cb.t[:, c0:c1], in_=cft.t[:, 0:c1 - c0]), reads=[cft.k], writes=['cb'])
            self.dest_all = self.sb("dest_all", [128, NCH, 2], I32)
            self.w_all = self.sb("w_all", [128, NCH, 2], F32)
            self.wst = self.ring("wst", 2, [128, 2048], F32)
            self.wbf = self.ring("wbf", 3, [128, 2048], BF16)
            self.epsln = self.sb("epsln", [128, 1], F32)
            self.S.op('dve', lambda e: e.memset(self.epsln.t[:], LN_EPS), writes=['epsln'])
            self.epsrms = self.sb("epsrms", [128, 1], F32)
            self.S.op('dve', lambda e: e.memset(self.epsrms.t[:], RMS_EPS), writes=['epsrms'])

            ph = self.phases
            if ph is None or 'pre' in ph:
                self.phase_pre()
            for l in range(L):
                if ph is None or 'proj' in ph:
                    self.phase_proj(l)
                if ph is None or 'fox' in ph:
                    self.phase_foxprep(l)
                if ph is None or 'attn' in ph:
                    self.phase_attn(l)
                if ph is None or 'merge' in ph:
                    self.phase_merge(l)
                if ph is None or 'wo' in ph:
                    self.phase_wo(l)
                if ph is None or 'exp' in ph:
                    self.phase_experts(l)
                if ph is None or 'ple' in ph:
                    self.phase_ple(l, last=(l == L - 1))
            keys = [k for k in self.S.res.keys() if isinstance(k, tuple) and k and k[0] == 'D']
            self.S.finish(keys)
        return nc

    def cbs(self, c0, n, p=128):
        return self.cb.t[0:p, c0:c0 + n]

    def scope(self):
        return _Scope(self)

    def phase_pre(self):
        S, SL = self.S, self.SL
        with self.scope() as es:
            CB = min(2048, SL)
            posi = self.sb("posi", [128, CB], I32)
            posf = self.sb("posf", [128, CB], F32)
            ang = self.sb("ang", [128, CB], F32)
            ki = self.sb("ki", [128, CB], I32)
            kf = self.sb("kf", [128, CB], F32)
            tb = self.ring("tb", 2, [128, CB], BF16)
            for cb0 in range(0, SL, CB):
                S.dma('sp', posi.t[:], self.pos[0:1, cb0:cb0 + CB].partition_broadcast(128), writes=['posi'])
                S.op('dve', lambda e: e.tensor_copy(out=posf.t[:], in_=posi.t[:]), reads=['posi'], writes=['posf'])
                for kind, (ccol, npart, dst) in enumerate([(0, 128, self.ropeD), (1, 64, self.ropeM)]):
                    for cs, shift in enumerate([math.pi / 2, 0.0]):
                        P = npart
                        S.op('dve', lambda e: e.tensor_scalar(out=ang.t[0:P], in0=posf.t[0:P], scalar1=self.cf.t[0:P, ccol:ccol + 1],
                                                              scalar2=shift, op0=ALU.mult, op1=ALU.add),
                             reads=['posf', 'cf'], writes=['ang'])
                        S.op('dve', lambda e: e.tensor_scalar(out=ki.t[0:P], in0=ang.t[0:P], scalar1=1.0 / TWO_PI, scalar2=None, op0=ALU.mult),
                             reads=['ang'], writes=['ki'])
                        S.op('dve', lambda e: e.tensor_copy(out=kf.t[0:P], in_=ki.t[0:P]), reads=['ki'], writes=['kf'])
                        S.op('dve', lambda e: e.scalar_tensor_tensor(out=ang.t[0:P], in0=kf.t[0:P], scalar=-TWO_PI, in1=ang.t[0:P],
                                                                     op0=ALU.mult, op1=ALU.add), reads=['kf', 'ang'], writes=['ang'])
                        S.op('dve', lambda e: e.tensor_scalar(out=ang.t[0:P], in0=ang.t[0:P], scalar1=-3.1415925, scalar2=3.1415925,
                                                              op0=ALU.max, op1=ALU.min), reads=['ang'], writes=['ang'])
                        o = tb.get()
                        S.op('act', lambda e: e.activation(out=o.t[0:P], in_=ang.t[0:P], func=AF.Sin), reads=['ang'], writes=[o.k])
                        S.dma('sp', dst[cs, :, cb0:cb0 + CB], o.t[0:P], reads=[o.k], writes=[('D', 'rope', kind, cs, cb0)])
            xs = self.ring("xs", 2, [128, CB], F32)
            xb = self.ring("xbp", 2, [128, CB], BF16)
            for k in range(16):
                for cb0 in range(0, SL, CB):
                    a = xs.get()
                    b = xb.get()
                    S.dma('sp', a.t[:], self.x_fm[k * 128:(k + 1) * 128, cb0:cb0 + CB], writes=[a.k])
                    S.op('pool', lambda e: e.tensor_copy(out=b.t[:], in_=a.t[:]), reads=[a.k], writes=[b.k])
                    S.dma('sp', self.xT[k * 128:(k + 1) * 128, cb0:cb0 + CB], b.t[:], reads=[b.k], writes=[('D', 'xT0', k, cb0 // self.TG)])

    def phase_proj(self, l):
        S, SL, TG = self.S, self.SL, self.TG
        NT = TG // 512
        w_in = self.w_in[l].rearrange("(k p) c -> p k c", p=128)
        for tg in range(self.NTG):
            t0 = tg * TG
            with self.scope() as es:
                xt = self.sb("xt", [128, 16, TG], BF16)
                for k in range(16):
                    S.dma('sp', xt.t[:, k, :], self.xT[k * 128:(k + 1) * 128, t0:t0 + TG], reads=([('D', 'xT0', k, tg)] if l == 0 else [('D', 'xT1', c_) for c_ in range(t0 // 128, (t0 + TG) // 128)]), writes=['xt'])
                rD = self.sb("rD", [128, 2, TG], BF16)
                rM = self.sb("rM", [64, 2, TG], BF16)
                for cs in range(2):
                    S.dma('sp', rD.t[:, cs, :], self.ropeD[cs, :, t0:t0 + TG], reads=[('D', 'rope', 0, cs, t0)], writes=['rD'])
                    S.dma('sp', rM.t[:, cs, :], self.ropeM[cs, :, t0:t0 + TG], reads=[('D', 'rope', 1, cs, t0)], writes=['rM'])
                pp = self.ring("pp", 4, [128, 512], F32, psum=True)
                pr = self.ring("prr", 2, [128, 512], F32, psum=True)
                ptr = self.ring("ptr", 2, [128, 1024], BF16, psum=True)
                ob = self.ring("ob", 3, [128, TG], BF16)
                vtb = self.ring("vtb", 2, [128, TG // 128, 128], BF16)
                tmp = self.ring("ptmp", 3, [128, 512], F32)
                cqg = self.sb("cqg", [128, 4, TG], BF16)
                sqq = self.sb("sqq", [128, 4, TG], BF16)
                rq = self.sb("rq", [128, TG], F32)
                gq = self.sb("gq", [128, 4], F32)
                gkv = self.sb("gkv", [128, 2], F32)
                fb = self.sb("fb", [4, 1], F32)
                S.dma('sp', gq.t[:], self.mla_q_norm[l], writes=['gq'])
                S.dma('sp', gkv.t[:], self.mla_kv_norm[l], writes=['gkv'])
                S.dma('sp', fb.t[:], self.fox_f_bias[l], writes=['fb'])
                eng_alt = [0]

                def evac_copy(dst_ap, src_ap, r, w, func=AF.Copy):
                    if func == AF.Copy and eng_alt[0] % 2 == 0:
                        S.op('dve', lambda e: e.tensor_copy(out=dst_ap, in_=src_ap), reads=r, writes=w)
                    else:
                        S.op('act', lambda e: e.activation(out=dst_ap, in_=src_ap, func=func), reads=r, writes=w)
                    eng_alt[0] += 1

                def project(wb, nk, M, rhs_t, rhs_key):
                    outs = []
                    for n in range(NT):
                        p = pp.get()
                        for k in range(nk):
                            S.op('pe', lambda e: e.matmul(p.t[0:M, :], lhsT=wb.t[:, k * M:(k + 1) * M],
                                                          rhs=rhs_t[:, k, n * 512:(n + 1) * 512], start=(k == 0), stop=(k == nk - 1)),
                                 reads=[wb.k, rhs_key], writes=[p.k])
                        outs.append(p)
                    return outs

                def rope_store(o, M, rtab, rmat_c, dst_ap, dkey):
                    o2 = ob.get()
                    for n in range(NT):
                        sl = slice(n * 512, (n + 1) * 512)
                        p = pr.get()
                        S.op('pe', lambda e: e.matmul(p.t[0:M, :], lhsT=self.cbs(rmat_c, M, M), rhs=o.t[0:M, sl], start=True, stop=True),
                             reads=[o.k, 'cb'], writes=[p.k])
                        t1 = tmp.get()
                        S.op('dve', lambda e: e.tensor_tensor(out=t1.t[0:M, :], in0=o.t[0:M, sl], in1=rtab.t[0:M, 0, sl], op=ALU.mult),
                             reads=[o.k, rtab.k], writes=[t1.k])
                        t2 = tmp.get()
                        S.op('dve', lambda e: e.tensor_tensor(out=t2.t[0:M, :], in0=p.t[0:M, :], in1=rtab.t[0:M, 1, sl], op=ALU.mult),
                             reads=[p.k, rtab.k], writes=[t2.k])
                        S.op('dve', lambda e: e.tensor_tensor(out=o2.t[0:M, sl], in0=t1.t[0:M, :], in1=t2.t[0:M, :], op=ALU.add),
                             reads=[t1.k, t2.k], writes=[o2.k])
                    S.dma('sp', dst_ap, o2.t[0:M, :], reads=[o2.k], writes=[dkey])

                def v_store(o, idx):
                    vt = vtb.get()
                    for c8 in range(0, TG // 128, 8):
                        p = ptr.get()
                        nn = min(8, TG // 128 - c8)
                        for j in range(nn):
                            c = c8 + j
                            S.op('pe', lambda e: e.transpose(p.t[:, j * 128:(j + 1) * 128], o.t[:, c * 128:(c + 1) * 128], self.cbs(C_IDENT, 128)),
                                 reads=[o.k, 'cb'], writes=[p.k])
                        evac_copy(vt.t[:, c8:c8 + nn, :], p.t[:, 0:nn * 128].rearrange("p (c d) -> p c d", c=nn), [p.k], [vt.k])
                    S.dma('sp', self.vv[idx, t0:t0 + TG, :].rearrange("(c p) d -> p c d", p=128), vt.t[:], reads=[vt.k],
                          writes=[('D', 'vv', idx, tg)])

                import os
                fams = os.environ.get("PROJ_FAMS")

                def do_tile(fam, c0, M, info):
                    if fams and fam not in fams.split(','):
                        return
                    wb = self.wload(w_in[:, :, c0:c0 + M], 16, M)
                    outs = project(wb, 16, M, xt.t, 'xt')
                    if fam in ('plain', 'gate', 'v', 'rope_d', 'kr'):
                        o = ob.get()
                        for n, p in enumerate(outs):
                            evac_copy(o.t[0:M, n * 512:(n + 1) * 512], p.t[0:M, :], [p.k], [o.k],
                                      func=(AF.Sigmoid if fam == 'gate' else AF.Copy))
                        if fam == 'plain':
                            dst = self.qT if info[0] == 'qT' else self.kT
                            S.dma('sp', dst[info[1], :, t0:t0 + TG], o.t[:], reads=[o.k], writes=[('D', info[0], info[1], tg)])
                        elif fam == 'gate':
                            S.dma('sp', self.gatesT[info * 128:(info + 1) * 128, t0:t0 + TG], o.t[:], reads=[o.k],
                                  writes=[('D', 'gatesT', info, tg)])
                        elif fam == 'v':
                            v_store(o, info)
                        elif fam == 'rope_d':
                            dst = self.qT if info[0] == 'qT' else self.kT
                            rope_store(o, 128, rD, C_RD, dst[info[1], :, t0:t0 + TG], ('D', info[0], info[1], tg))
                        elif fam == 'kr':
                            rope_store(o, 64, rM, C_RM, self.krT[:, t0:t0 + TG], ('D', 'krT', tg))
                    elif fam in ('cq', 'ckv'):
                        gg = gq if fam == 'cq' else gkv
                        for n, p in enumerate(outs):
                            sl = slice(n * 512, (n + 1) * 512)
                            tq = tmp.get()
                            S.op('act', lambda e: e.activation(out=tq.t[:], in_=p.t[:], func=AF.Copy), reads=[p.k], writes=[tq.k])
                            S.op('dve', lambda e: e.tensor_tensor(out=sqq.t[:, info, sl], in0=tq.t[:], in1=tq.t[:], op=ALU.mult), reads=[tq.k], writes=['sqq'])
                            S.op('dve', lambda e: e.tensor_scalar(out=cqg.t[:, info, sl], in0=tq.t[:], scalar1=gg.t[:, info:info + 1], scalar2=None,
                                                                  op0=ALU.mult), reads=[tq.k, gg.k], writes=['cqg'])
                    elif fam == 'f':
                        for n, p in enumerate(outs):
                            ft = tmp.get()
                            S.op('dve', lambda e: e.tensor_scalar(out=ft.t[0:4, :], in0=p.t[0:4, :], scalar1=fb.t[0:4, 0:1], scalar2=None, op0=ALU.add),
                                 reads=[p.k, 'fb'], writes=[ft.k])
                            S.dma('sp', self.fT[:, t0 + n * 512:t0 + (n + 1) * 512], ft.t[0:4, :], reads=[ft.k], writes=[('D', 'fT', tg, n)])

                def rms_scale(nk, dim):
                    for n in range(NT):
                        sl = slice(n * 512, (n + 1) * 512)
                        p = pp.get()
                        for k in range(nk):
                            S.op('pe', lambda e: e.matmul(p.t[:], lhsT=self.cbs(C_ONES, 128), rhs=sqq.t[:, k, sl], start=(k == 0), stop=(k == nk - 1)),
                                 reads=['sqq', 'cb'], writes=[p.k])
                        S.op('act', lambda e: e.activation(out=rq.t[:, sl], in_=p.t[:], func=AF.Sqrt, bias=self.epsrms.t[:, 0:1], scale=1.0 / dim),
                             reads=[p.k, 'epsrms'], writes=['rq'])
                        S.op('dve', lambda e: e.reciprocal(out=rq.t[:, sl], in_=rq.t[:, sl]), reads=['rq'], writes=['rq'])

                def scaled(outs, M):
                    o = ob.get()
                    for n, p in enumerate(outs):
                        sl = slice(n * 512, (n + 1) * 512)
                        S.op('dve', lambda e: e.tensor_tensor(out=o.t[0:M, sl], in0=p.t[0:M, :], in1=rq.t[0:M, sl], op=ALU.mult),
                             reads=[p.k, 'rq'], writes=[o.k])
                    return o
                wuq = self.mla_w_uq[l].rearrange("(k p) c -> p k c", p=128)
                wukv = self.mla_w_ukv[l].rearrange("(k p) c -> p k c", p=128)
                mla_on = os.environ.get("PROJ_MLA", "1") == "1"
                for h in range(4 if mla_on else 0):
                    do_tile('cq', OFF['mla_cq'] + h * 128, 128, h)
                if mla_on:
                    rms_scale(4, 512)
                for h in range(4 if mla_on else 0):
                    wb = self.wload(wuq[:, :, h * 192:h * 192 + 128], 4, 128)
                    o = scaled(project(wb, 4, 128, cqg.t, 'cqg'), 128)
                    S.dma('sp', self.qT[8 + h, :, t0:t0 + TG], o.t[:], reads=[o.k], writes=[('D', 'qT', 8 + h, tg)])
                    wb = self.wload(wuq[:, :, h * 192 + 128:h * 192 + 192], 4, 64)
                    o = scaled(project(wb, 4, 64, cqg.t, 'cqg'), 64)
                    rope_store(o, 64, rM, C_RM, self.qrT[h, :, t0:t0 + TG], ('D', 'qrT', h, tg))
                for j in range(2 if mla_on else 0):
                    do_tile('ckv', OFF['mla_ckv'] + j * 128, 128, j)
                if mla_on:
                    rms_scale(2, 256)
                for h in range(4 if mla_on else 0):
                    wb = self.wload(wukv[:, :, h * 256:h * 256 + 128], 2, 128)
                    o = scaled(project(wb, 2, 128, cqg.t, 'cqg'), 128)
                    S.dma('sp', self.kT[8 + h, :, t0:t0 + TG], o.t[:], reads=[o.k], writes=[('D', 'kT', 8 + h, tg)])
                    wb = self.wload(wukv[:, :, h * 256 + 128:h * 256 + 256], 2, 128)
                    o = scaled(project(wb, 2, 128, cqg.t, 'cqg'), 128)
                    v_store(o, 8 + h)
                do_tile('kr', OFF['mla_kr'], 64, 0)
                do_tile('f', OFF['fox_f'], 4, 0)
                for h in range(4):
                    do_tile('rope_d', OFF['diff_q'] + h * 128, 128, ('qT', 0 + h))
                    do_tile('rope_d', OFF['diff_k'] + h * 128, 128, ('kT', 0 + h))
                    do_tile('v', OFF['diff_v'] + h * 128, 128, 0 + h)
                    do_tile('plain', OFF['fox_q'] + h * 128, 128, ('qT', 4 + h))
                    do_tile('plain', OFF['fox_k'] + h * 128, 128, ('kT', 4 + h))
                    do_tile('v', OFF['fox_v'] + h * 128, 128, 4 + h)
                    do_tile('plain', OFF['sb_q'] + h * 128, 128, ('qT', 12 + h))
                    do_tile('plain', OFF['sb_k'] + h * 128, 128, ('kT', 12 + h))
                    do_tile('v', OFF['sb_v'] + h * 128, 128, 12 + h)
                for g in range(64):
                    do_tile('gate', OFF['gates'] + g * 128, 128, g)

    def phase_foxprep(self, l):
        S, SL = self.S, self.SL
        with self.scope() as es:
            z = self.sb("fz", [4, SL], F32)
            a = self.sb("fa", [4, SL], F32)
            b = self.sb("fb2", [4, SL], F32)
            hb = [self.sb("fh%d" % i, [4, SL], BF16) for i in range(3)]
            rk = [('D', 'fT', tg, n) for tg in range(self.NTG) for n in range(self.TG // 512)]
            S.dma('sp', z.t[:], self.fT[:, :], reads=rk, writes=['fz'])
            S.op('act', lambda e: e.activation(out=a.t[:], in_=z.t[:], func=AF.Exp, scale=-1.0), reads=['fz'], writes=['fa'])
            S.op('act', lambda e: e.activation(out=a.t[:], in_=a.t[:], func=AF.Ln, bias=1.0), reads=['fa'], writes=['fa'])
            S.op('dve', lambda e: e.tensor_scalar(out=a.t[:], in0=a.t[:], scalar1=-1.0, scalar2=None, op0=ALU.mult), reads=['fa'], writes=['fa'])
            S.op('dve', lambda e: e.memset(b.t[:], 1.0), writes=['fb2'])
            S.op('dve', lambda e: e.tensor_tensor_scan(out=z.t[:], data0=b.t[:], data1=a.t[:], initial=0.0, op0=ALU.mult, op1=ALU.add),
                 reads=['fa', 'fb2'], writes=['fz'])
            S.dma('sp', self.cumT[:, :], z.t[:], reads=['fz'], writes=[('D', 'cumT')])
            S.op('dve', lambda e: e.tensor_scalar(out=a.t[:], in0=z.t[:], scalar1=math.sqrt(128.0), scalar2=None, op0=ALU.mult), reads=['fz'], writes=['fa'])
            for i in range(3):
                S.op('dve', lambda e: e.tensor_copy(out=hb[i].t[:], in_=a.t[:]), reads=['fa'], writes=[hb[i].k])
                if i < 2:
                    S.op('dve', lambda e: e.tensor_tensor(out=a.t[:], in0=a.t[:], in1=hb[i].t[:], op=ALU.subtract), reads=['fa', hb[i].k], writes=['fa'])
                for h in range(4):
                    S.dma('sp', self.qaug[h, i:i + 1, :], hb[i].t[h:h + 1, :], reads=[hb[i].k], writes=[('D', 'qaug', h, i)])

    def phase_attn(self, l):
        S, SL, NCH, NQT = self.S, self.SL, self.NCH, self.NQT
        NTG = self.NTG
        with self.scope() as es:
            KT = self.ring("KT", 2, [128, SL], BF16)
            QT = self.ring("QT", 2, [128, SL], BF16)
            VV = self.ring("VV", 2, [128, NCH, 128], BF16)
            OT = self.ring("OT", 2, [128, SL], BF16)
            KR = self.sb("KR", [64, SL], BF16)
            QR = self.ring("QR", 2, [64, SL], BF16)
            QA = self.ring("QA", 2, [3, SL], BF16)
            NCUM = self.ring("NCUM", 2, [128, NCH], F32)
            sps = self.ring("sps", 4, [128, 512], F32, psum=True)
            ops_ = self.ring("ops", 2, [128, 512], F32, psum=True)
            dps = self.ring("dps", 2, [128, 512], F32, psum=True)
            lps = sps
            PT = self.ring("PT", 6, [128, 512], BF16)
            rec = self.ring("rec", 2, [128, 512], F32)
            o0 = self.ring("o0", 2, [128, 512], F32)
            o1 = self.ring("o1", 2, [128, 512], F32)
            f32t = self.ring("f32t", 4, [128, 512], F32)
            sqb = self.ring("sqb", 2, [128, 512], BF16)
            lp = self.sb("lp", [128, 256], F32)
            lpp = self.sb("lpp", [128, 128], F32)
            lam2 = self.sb("lam2", [128, 2], F32)
            neglam = self.sb("neglam", [128, 1], F32)
            gsub = self.sb("gsub", [128, 1], F32)
            lam_init = 0.8 - 0.6 * math.exp(-0.3 * l)
            S.dma('sp', lp.t[:], self.diff_lambda[l, 0:1, :].partition_broadcast(128), writes=['lp'])
            S.dma('sp', gsub.t[:], self.diff_subln[l], writes=['gsub'])
            S.op('dve', lambda e: e.tensor_scalar(out=gsub.t[:], in0=gsub.t[:], scalar1=(1.0 - lam_init), scalar2=None, op0=ALU.mult), reads=['gsub'], writes=['gsub'])
            lp4 = lp.t[:].rearrange("p (a d) -> p a d", a=4)
            lpp2 = lpp.t[:].rearrange("p (a d) -> p a d", a=2)
            S.op('dve', lambda e: e.tensor_tensor(out=lpp2[:, 0, :], in0=lp4[:, 0, :], in1=lp4[:, 1, :], op=ALU.mult), reads=['lp'], writes=['lpp'])
            S.op('dve', lambda e: e.tensor_tensor(out=lpp2[:, 1, :], in0=lp4[:, 2, :], in1=lp4[:, 3, :], op=ALU.mult), reads=['lp'], writes=['lpp'])
            S.op('dve', lambda e: e.reduce_sum(out=lam2.t[:], in_=lpp2, axis=mybir.AxisListType.X), reads=['lpp'], writes=['lam2'])
            S.op('act', lambda e: e.activation(out=lam2.t[:], in_=lam2.t[:], func=AF.Exp), reads=['lam2'], writes=['lam2'])
            S.op('dve', lambda e: e.tensor_tensor(out=neglam.t[:], in0=lam2.t[:, 1:2], in1=lam2.t[:, 0:1], op=ALU.subtract), reads=['lam2'], writes=['neglam'])
            S.op('dve', lambda e: e.tensor_scalar(out=neglam.t[:], in0=neglam.t[:], scalar1=-lam_init, scalar2=None, op0=ALU.add), reads=['neglam'], writes=['neglam'])

            def dkeys(name, idx):
                return [('D', name, idx, tg) for tg in range(NTG)]

            def load_head(idx, need_v=True):
                kt = KT.get()
                qt = QT.get()
                S.dma('sp', kt.t[:], self.kT[idx], reads=dkeys('kT', idx), writes=[kt.k])
                S.dma('sp', qt.t[:], self.qT[idx], reads=dkeys('qT', idx), writes=[qt.k])
                vt = VV.get()
                vsrc = self.vv[idx].rearrange("(c p) d -> p c d", p=128)
                for c0 in range(0, NCH, 8):
                    S.dma('sp', vt.t[:, c0:min(NCH, c0 + 8), :], vsrc[:, c0:min(NCH, c0 + 8), :], reads=dkeys('vv', idx), writes=[(vt.k, c0 // 8)])
                return kt, qt, vt

            def drive(streams):
                live = list(streams)
                while live:
                    nxt = []
                    for g_ in live:
                        try:
                            next(g_)
                            nxt.append(g_)
                        except StopIteration:
                            pass
                    live = nxt

            def softmax_tile(i, parts, scale, madd_c, vt, O, Dn, bias_t=None, aug=None):
                last = 4 * i + 3

                def emit_qk(g):
                    sp_ = sps.get()
                    j0 = g - 4 * i
                    nmm = len(parts) + (1 if aug is not None else 0) + (1 if j0 >= 0 else 0)
                    c = 0
                    for (kt, qt, p0, p1) in parts:
                        S.op('pe', lambda e: e.matmul(sp_.t[:], lhsT=kt.t[p0:p1, g * 128:(g + 1) * 128], rhs=qt.t[p0:p1, i * 512:(i + 1) * 512],
                                                      start=(c == 0), stop=(c == nmm - 1)), reads=[kt.k, qt.k], writes=[sp_.k])
                        c += 1
                    if aug is not None:
                        S.op('pe', lambda e: e.matmul(sp_.t[:], lhsT=self.cb.t[0:3, C_ONES:C_ONES + 128], rhs=aug.t[0:3, i * 512:(i + 1) * 512],
                                                      start=False, stop=(c == nmm - 1)), reads=[aug.k, 'cb'], writes=[sp_.k])
                        c += 1
                    if j0 >= 0:
                        S.op('pe', lambda e: e.matmul(sp_.t[:], lhsT=self.cbs(C_IDENT, 128), rhs=self.cbs(madd_c + j0 * 512, 512),
                                                      start=False, stop=True), reads=['cb'], writes=[sp_.k])
                    return sp_
                nxt = emit_qk(0)
                for g in range(last + 1):
                    sp_ = nxt
                    if g < last:
                        nxt = emit_qk(g + 1)
                    pt = PT.get()
                    if bias_t is not None:
                        S.op('act', lambda e: e.activation(out=pt.t[:], in_=sp_.t[:], func=AF.Exp, scale=scale, bias=bias_t.t[:, g:g + 1]),
                             reads=[sp_.k, bias_t.k], writes=[pt.k])
                    else:
                        S.op('act', lambda e: e.activation(out=pt.t[:], in_=sp_.t[:], func=AF.Exp, scale=scale), reads=[sp_.k], writes=[pt.k])
                    S.op('pe', lambda e: e.matmul(O.t[:], lhsT=vt.t[:, g, :], rhs=pt.t[:], start=(g == 0), stop=(g == last)),
                         reads=[(vt.k, g // 8), pt.k], writes=[O.k])
                    S.op('pe', lambda e: e.matmul(Dn.t[:], lhsT=self.cbs(C_ONES, 128), rhs=pt.t[:], start=(g == 0), stop=(g == last)),
                         reads=['cb', pt.k], writes=[Dn.k])
                    yield

            def normalize(O, Dn, dst_ap, wkey):
                r = rec.get()
                S.op('dve', lambda e: e.reciprocal(out=r.t[:], in_=Dn.t[:]), reads=[Dn.k], writes=[r.k])
                S.op('dve', lambda e: e.tensor_tensor(out=dst_ap, in0=O.t[:], in1=r.t[:], op=ALU.mult), reads=[O.k, r.k], writes=[wkey])

            for h in range(4):
                idx = h
                kt, qt, vt = load_head(idx)
                ot = OT.get()
                for i in range(NQT):
                    accs = [(ops_.get(), dps.get()) for c in range(2)]
                    drive([softmax_tile(i, [(kt, qt, c * 64, (c + 1) * 64)], 64 ** -0.5, C_MADD_CH, vt, accs[c][0], accs[c][1]) for c in range(2)])
                    oc = []
                    for c in range(2):
                        dst = (o0 if c == 0 else o1).get()
                        normalize(accs[c][0], accs[c][1], dst.t[:], dst.k)
                        oc.append(dst)
                    d = f32t.get()
                    S.op('dve', lambda e: e.scalar_tensor_tensor(out=d.t[:], in0=oc[1].t[:], scalar=neglam.t[:, 0:1], in1=oc[0].t[:],
                                                                 op0=ALU.mult, op1=ALU.add), reads=[oc[0].k, oc[1].k, 'neglam'], writes=[d.k])
                    sq = sqb.get()
                    S.op('pool', lambda e: e.tensor_tensor(out=sq.t[:], in0=d.t[:], in1=d.t[:], op=ALU.mult), reads=[d.k], writes=[sq.k])
                    nps = lps.get()
                    S.op('pe', lambda e: e.matmul(nps.t[:], lhsT=self.cbs(C_ONES, 128), rhs=sq.t[:], start=True, stop=True), reads=[sq.k, 'cb'], writes=[nps.k])
                    rt = f32t.get()
                    S.op('act', lambda e: e.activation(out=rt.t[:], in_=nps.t[:], func=AF.Sqrt, bias=self.epsrms.t[:, 0:1], scale=1.0 / 128),
                         reads=[nps.k, 'epsrms'], writes=[rt.k])
                    S.op('dve', lambda e: e.reciprocal(out=rt.t[:], in_=rt.t[:]), reads=[rt.k], writes=[rt.k])
                    S.op('dve', lambda e: e.scalar_tensor_tensor(out=ot.t[:, i * 512:(i + 1) * 512], in0=d.t[:], scalar=gsub.t[:, 0:1], in1=rt.t[:],
                                                                 op0=ALU.mult, op1=ALU.mult), reads=[d.k, rt.k, 'gsub'], writes=[ot.k])
                S.dma('sp', self.oT[idx], ot.t[:], reads=[ot.k], writes=[('D', 'oT', idx)])

            def head_stream(idx, parts_fn, scale, madd_c, bias_t=None, aug=None):
                kt, qt, vt = load_head(idx)
                parts = parts_fn(kt, qt)
                ot = OT.get()
                for i in range(NQT):
                    O = ops_.get()
                    Dn = dps.get()
                    for _ in softmax_tile(i, parts, scale, madd_c, vt, O, Dn, bias_t=bias_t, aug=aug):
                        yield
                    normalize(O, Dn, ot.t[:, i * 512:(i + 1) * 512], ot.k)
                S.dma('sp', self.oT[idx], ot.t[:], reads=[ot.k], writes=[('D', 'oT', idx)])

            def fox_stream(h):
                qa = QA.get()
                S.dma('sp', qa.t[:], self.qaug[h], reads=[('D', 'qaug', h, i) for i in range(3)], writes=[qa.k])
                ncum = NCUM.get()
                with self.nc.allow_non_contiguous_dma(reason="tiny strided column load"):
                    csrc = self.cumT[h].rearrange("(c p) -> p c", p=128)
                    for c0 in range(0, NCH, 8):
                        S.dma('sp', ncum.t[:, c0:min(NCH, c0 + 8)], csrc[:, c0:min(NCH, c0 + 8)], reads=[('D', 'cumT')], writes=[(ncum.k, 'ld', c0 // 8), ncum.k])
                S.op('dve', lambda e: e.tensor_scalar(out=ncum.t[:], in0=ncum.t[:], scalar1=-1.0, scalar2=None, op0=ALU.mult),
                     reads=[(ncum.k, 'ld', j) for j in range((NCH + 7) // 8)], writes=[ncum.k])
                return head_stream(4 + h, lambda kt, qt: [(kt, qt, 0, 128)], 128 ** -0.5, C_MADD_CA, bias_t=ncum, aug=qa)
            for h0 in (0, 2):
                drive([fox_stream(h0), fox_stream(h0 + 1)])

            S.dma('sp', KR.t[:], self.krT[:, :], reads=[('D', 'krT', tg) for tg in range(NTG)], writes=['KR'])

            def mla_stream(h):
                qr = QR.get()
                S.dma('sp', qr.t[:], self.qrT[h], reads=dkeys('qrT', h), writes=[qr.k])
                return head_stream(8 + h, lambda kt, qt: [(kt, qt, 0, 128), (KR, qr, 0, 64)], 192 ** -0.5, C_MADD_CH)
            for h0 in (0, 2):
                drive([mla_stream(h0), mla_stream(h0 + 1)])

        with self.scope() as es:
            KT = self.ring("KT", 2, [128, SL], BF16)
            QT = self.ring("QT", 2, [128, SL], BF16)
            VV = self.ring("VV", 2, [128, NCH, 128], BF16)
            OT = self.ring("OT", 2, [128, SL], BF16)
            sps = self.ring("sps", 4, [128, 512], F32, psum=True)
            ops_ = self.ring("ops", 2, [128, 512], F32, psum=True)
            lps = self.ring("lps", 2, [128, 512], F32, psum=True)
            PT = self.ring("PT", 8, [128, 512], BF16)
            f32t = self.ring("f32t", 10, [128, 512], F32)
            sp32 = self.ring("sp32", 6, [128, 512], F32)
            spb = self.ring("spb", 6, [128, 512], BF16)
            sc = 128 ** -0.5

            def sb_stream(h, cast_eng):
                idx = 12 + h
                kt = KT.get()
                qt = QT.get()
                S.dma('sp', kt.t[:], self.kT[idx], reads=dkeys('kT', idx), writes=[kt.k])
                S.dma('sp', qt.t[:], self.qT[idx], reads=dkeys('qT', idx), writes=[qt.k])
                vt = VV.get()
                vsrc = self.vv[idx].rearrange("(c p) d -> p c d", p=128)
                for c0 in range(0, NCH, 8):
                    S.dma('sp', vt.t[:, c0:min(NCH, c0 + 8), :], vsrc[:, c0:min(NCH, c0 + 8), :], reads=dkeys('vv', idx), writes=[(vt.k, c0 // 8)])
                ot = OT.get()
                spsum = self.sb("spsum%d" % h, [128, 512], F32)
                spsumb = self.ring("spsumb%d" % h, 3, [128, 512], BF16)
                for i in range(NQT):
                    O = ops_.get()
                    last = 4 * i + 3
                    order = list(range(last, -1, -1))
                    n = len(order)
                    st1 = {}
                    st2 = {}
                    ssb_prev = [None]

                    def stage1(g):
                        j0 = g - 4 * i
                        z = sps.get()
                        S.op('pe', lambda e: e.matmul(z.t[:], lhsT=kt.t[:, g * 128:(g + 1) * 128], rhs=qt.t[:, i * 512:(i + 1) * 512], start=True, stop=True),
                             reads=[kt.k, qt.k], writes=[z.k])
                        ee = f32t.get()
                        S.op('act', lambda e: e.activation(out=ee.t[:], in_=z.t[:], func=AF.Exp, scale=sc), reads=[z.k], writes=[ee.k])
                        s32 = sp32.get()
                        S.op('act', lambda e: e.activation(out=s32.t[:], in_=ee.t[:], func=AF.Ln, bias=1.0), reads=[ee.k], writes=[s32.k])
                        sb_ = spb.get()
                        if j0 >= 0:
                            S.op('pool', lambda e: e.tensor_tensor(out=sb_.t[:], in0=s32.t[:], in1=self.cbs(C_MMUL + j0 * 512, 512), op=ALU.mult),
                                 reads=[s32.k, 'cb'], writes=[sb_.k])
                        else:
                            S.op('dve', lambda e: e.tensor_copy(out=sb_.t[:], in_=s32.t[:]), reads=[s32.k], writes=[sb_.k])
                        st1[g] = (z, s32, sb_)

                    def stage2(g):
                        j0 = g - 4 * i
                        z, s32, sb_ = st1.pop(g)
                        first = (g == last)
                        Lp = lps.get()
                        S.op('pe', lambda e: e.matmul(Lp.t[:], lhsT=self.cbs(C_TRIS, 128), rhs=sb_.t[:], start=True, stop=first), reads=[sb_.k, 'cb'], writes=[Lp.k])
                        if not first:
                            ssb = ssb_prev[0]
                            S.op('pe', lambda e: e.matmul(Lp.t[:], lhsT=self.cbs(C_ONES, 128), rhs=ssb.t[:], start=False, stop=True), reads=[ssb.k, 'cb'], writes=[Lp.k])
                        t1 = f32t.get()
                        S.op('dve', lambda e: e.scalar_tensor_tensor(out=t1.t[:], in0=z.t[:], scalar=sc, in1=s32.t[:], op0=ALU.mult, op1=ALU.subtract),
                             reads=[z.k, s32.k], writes=[t1.k])
                        S.op('dve', lambda e: e.tensor_tensor(out=t1.t[:], in0=t1.t[:], in1=Lp.t[:], op=ALU.subtract), reads=[t1.k, Lp.k], writes=[t1.k])
                        wt = PT.get()
                        S.op('act', lambda e: e.activation(out=wt.t[:], in_=t1.t[:], func=AF.Exp), reads=[t1.k], writes=[wt.k])
                        if j0 >= 0:
                            S.op('pool', lambda e: e.tensor_tensor(out=wt.t[:], in0=wt.t[:], in1=self.cbs(C_MMUL + j0 * 512, 512), op=ALU.mult),
                                 reads=[wt.k, 'cb'], writes=[wt.k])
                        if g > 0:
                            if first:
                                S.op('pool', lambda e: e.tensor_copy(out=spsum.t[:], in_=sb_.t[:]), reads=[sb_.k], writes=[spsum.k])
                            else:
                                S.op('dve', lambda e: e.tensor_tensor(out=spsum.t[:], in0=spsum.t[:], in1=sb_.t[:], op=ALU.add), reads=[spsum.k, sb_.k], writes=[spsum.k])
                            ssb = spsumb.get()
                            S.op('act', lambda e: e.activation(out=ssb.t[:], in_=spsum.t[:], func=AF.Copy), reads=[spsum.k], writes=[ssb.k])
                            ssb_prev[0] = ssb
                        st2[g] = wt

                    def stage3(g):
                        wt = st2.pop(g)
                        S.op('pe', lambda e: e.matmul(O.t[:], lhsT=vt.t[:, g, :], rhs=wt.t[:], start=(g == last), stop=(g == 0)), reads=[(vt.k, g // 8), wt.k], writes=[O.k])
                    for step in range(n + 2):
                        if step < n:
                            stage1(order[step])
                        if 0 <= step - 1 < n:
                            stage2(order[step - 1])
                        if 0 <= step - 2 < n:
                            stage3(order[step - 2])
                        yield
                    S.op('act', lambda e: e.activation(out=ot.t[:, i * 512:(i + 1) * 512], in_=O.t[:], func=AF.Copy), reads=[O.k], writes=[ot.k])
                S.dma('sp', self.oT[idx], ot.t[:], reads=[ot.k], writes=[('D', 'oT', idx)])
            for h0 in (0, 2):
                drive([sb_stream(h0, 'dve'), sb_stream(h0 + 1, 'dve')])

    def phase_merge(self, l):
        S, SL, TG = self.S, self.SL, self.TG
        NT = TG // 512
        for tg in range(self.NTG):
            t0 = tg * TG
            with self.scope() as es:
                ot = self.sb("mot", [128, 16, TG], BF16)
                for i in range(16):
                    S.dma('sp', ot.t[:, i, :], self.oT[i, :, t0:t0 + TG], reads=[('D', 'oT', i)], writes=['mot'])
                pp = self.ring("mpp", 4, [128, 512], F32, psum=True)
                gt = self.ring("mgt", 3, [128, TG], BF16)
                acc = self.sb("macc", [128, TG], F32)
                mo = self.ring("mmo", 2, [128, TG], BF16)
                tmp = self.ring("mtmp", 2, [128, 512], F32)
                def fetch(c, n_):
                    wb_ = self.wload(self.w_branch[l, n_].rearrange("(k p) c -> p k c", p=128)[:, :, c * 128:(c + 1) * 128], 4, 128)
                    g_ = gt.get()
                    grow = n_ * 16 + c
                    S.dma('sp', g_.t[:], self.gatesT[grow * 128:(grow + 1) * 128, t0:t0 + TG], reads=[('D', 'gatesT', grow, tg)], writes=[g_.k])
                    return wb_, g_
                nxt_f = fetch(0, 0)
                for c in range(16):
                    for n_ in range(4):
                        wb, g = nxt_f
                        if not (c == 15 and n_ == 3):
                            nxt_f = fetch(c + (n_ + 1) // 4, (n_ + 1) % 4)
                        wv = wb.t[:, 0:512].rearrange("p (k m) -> p k m", k=4)
                        for n in range(NT):
                            sl = slice(n * 512, (n + 1) * 512)
                            p = pp.get()
                            for k in range(4):
                                S.op('pe', lambda e: e.matmul(p.t[:], lhsT=wv[:, k, :], rhs=ot.t[:, n_ * 4 + k, sl], start=(k == 0), stop=(k == 3)),
                                     reads=[wb.k, 'mot'], writes=[p.k])
                            if n_ == 0:
                                S.op('dve', lambda e: e.tensor_tensor(out=acc.t[:, sl], in0=p.t[:], in1=g.t[:, sl], op=ALU.mult), reads=[p.k, g.k], writes=['macc'])
                            else:
                                t = tmp.get()
                                S.op('dve', lambda e: e.tensor_tensor(out=t.t[:], in0=p.t[:], in1=g.t[:, sl], op=ALU.mult), reads=[p.k, g.k], writes=[t.k])
                                S.op('pool', lambda e: e.tensor_tensor(out=acc.t[:, sl], in0=acc.t[:, sl], in1=t.t[:], op=ALU.add), reads=['macc', t.k], writes=['macc'])
                    m = mo.get()
                    S.op('act', lambda e: e.activation(out=m.t[:], in_=acc.t[:], func=AF.Copy), reads=['macc'], writes=[m.k])
                    S.dma('sp', self.mergedT[c * 128:(c + 1) * 128, t0:t0 + TG], m.t[:], reads=[m.k], writes=[('D', 'mergedT', c, tg)])

    def load_resident(self, dst, w_ap, nk):
        S = self.S
        wv = w_ap.rearrange("(k p) c -> p k c", p=128)
        for k in range(nk):
            st = self.wst.get()
            S.dma('sp', st.t[:], wv[:, k, :], writes=[st.k])
            self.cast(dst.t[:, k, :], st.t[:], [st.k], [dst.k])

    def layer_norm(self, v, stt, mvt, g_t, b_t, out):
        S = self.S
        for j in range(4):
            S.op('dve', lambda e: e.bn_stats(out=stt.t[:, j * 6:(j + 1) * 6], in_=v.t[:, j * 512:(j + 1) * 512]), reads=[v.k], writes=[stt.k])
        S.op('dve', lambda e: e.bn_aggr(out=mvt.t[:, 0:2], in_=stt.t[:]), reads=[stt.k], writes=[mvt.k])
        S.op('act', lambda e: e.activation(out=mvt.t[:, 2:3], in_=mvt.t[:, 1:2], func=AF.Sqrt, bias=self.epsln.t[:, 0:1], scale=1.0),
             reads=[mvt.k, 'epsln'], writes=[mvt.k])
        S.op('dve', lambda e: e.reciprocal(out=mvt.t[:, 3:4], in_=mvt.t[:, 2:3]), reads=[mvt.k], writes=[mvt.k])
        S.op('dve', lambda e: e.tensor_scalar(out=out.t[:], in0=v.t[:], scalar1=mvt.t[:, 0:1], scalar2=mvt.t[:, 3:4], op0=ALU.subtract, op1=ALU.mult),
             reads=[v.k, mvt.k], writes=[out.k])
        S.op('pool', lambda e: e.tensor_tensor(out=out.t[:], in0=out.t[:], in1=g_t.t[:], op=ALU.mult), reads=[out.k, g_t.k], writes=[out.k])
        S.op('dve', lambda e: e.tensor_tensor(out=out.t[:], in0=out.t[:], in1=b_t.t[:], op=ALU.add), reads=[out.k, b_t.k], writes=[out.k])

    def transpose_store(self, hb, ptr, dstT_tile, evac_eng='act'):
        S = self.S
        for k8 in range(0, 16, 8):
            p = ptr.get()
            for j in range(8):
                k = k8 + j
                S.op('pe', lambda e: e.transpose(p.t[:, j * 128:(j + 1) * 128], hb.t[:, k * 128:(k + 1) * 128], self.cbs(C_IDENT, 128)),
                     reads=[hb.k, 'cb'], writes=[p.k])
            S.op('act', lambda e: e.activation(out=dstT_tile.t[:, k8:k8 + 8, :], in_=p.t[:].rearrange("p (c d) -> p c d", c=8), func=AF.Copy),
                 reads=[p.k], writes=[dstT_tile.k])

    def phase_wo(self, l):
        S, SL, NCH = self.S, self.SL, self.NCH
        CAP = self.CAP
        with self.scope() as es:
            wo = self.sb("wo", [128, 16, D], BF16)
            self.load_resident(wo, self.w_o[l], 16)
            wr = self.sb("wr", [128, 16, 36], BF16)
            wrs = self.sb("wrs", [128, 16, 36], F32)
            S.dma('sp', wrs.t[:, :, 0:4], self.w_rg[l].rearrange("(k p) c -> p k c", p=128), writes=['wrs'])
            S.dma('sp', wrs.t[:, :, 4:36], self.w_re[l].rearrange("(k p) c -> p k c", p=128), writes=['wrs'])
            S.op('dve', lambda e: e.tensor_copy(out=wr.t[:], in_=wrs.t[:]), reads=['wrs'], writes=['wr'])
            brt = self.sb("brt", [128, 36], F32)
            S.dma('sp', brt.t[:, 0:4], self.b_rg[l, 0:1, :].partition_broadcast(128), writes=['brt'])
            S.dma('sp', brt.t[:, 4:36], self.b_re[l, 0:1, :].partition_broadcast(128), writes=['brt'])
            g_t = self.sb("l1g", [128, D], F32)
            b_t = self.sb("l1b", [128, D], F32)
            S.dma('sp', g_t.t[:], self.ln1_g[l, 0:1, :].partition_broadcast(128), writes=['l1g'])
            S.dma('sp', b_t.t[:], self.ln1_b[l, 0:1, :].partition_broadcast(128), writes=['l1b'])
            base = self.sb("rbase", [128, 32], F32)
            S.op('dve', lambda e: e.memset(base.t[:], 0.0), writes=['rbase'])
            mT = self.ring("wmT", 2, [128, 16, 128], BF16)
            xr = self.ring("wxr", 2, [128, D], F32)
            ht = self.ring("wh", 2, [128, D], F32)
            hb = self.ring("whb", 2, [128, D], BF16)
            hTt = self.ring("whT", 2, [128, 16, 128], BF16)
            stt = self.ring("wstt", 2, [128, 24], F32)
            mvt = self.ring("wmv", 2, [128, 4], F32)
            pp = self.ring("wpp", 4, [128, 512], F32, psum=True)
            ptr = self.ring("wptr", 2, [128, 1024], BF16, psum=True)
            prt = self.ring("wprt", 2, [128, 64], F32, psum=True)
            sm = self.ring("wsm", 2, [128, 256], F32)
            mb = self.ring("wmb", 2, [128, 32], BF16)
            x_src = self.x_tm if l == 0 else self.xres
            mres = [('D', 'mergedT', c, tg) for c in range(16) for tg in range(self.NTG)]
            def stageA(c):
                r0 = c * 128
                m = mT.get()
                S.dma('sp', m.t[:], self.mergedT[:, r0:r0 + 128].rearrange("(k p) t -> p k t", p=128), reads=mres, writes=[m.k])
                x = xr.get()
                S.dma('sp', x.t[:], x_src[r0:r0 + 128, :], reads=([('D', 'xres', c)] if l > 0 else []), writes=[x.k])
                v = x
                for ct in range(4):
                    p = pp.get()
                    for k in range(16):
                        S.op('pe', lambda e: e.matmul(p.t[:], lhsT=m.t[:, k, :], rhs=wo.t[:, k, ct * 512:(ct + 1) * 512], start=(k == 0), stop=(k == 15)),
                             reads=[m.k, 'wo'], writes=[p.k])
                    S.op('dve', lambda e: e.scalar_tensor_tensor(out=v.t[:, ct * 512:(ct + 1) * 512], in0=x.t[:, ct * 512:(ct + 1) * 512], scalar=ALPHA,
                                                                 in1=p.t[:], op0=ALU.mult, op1=ALU.add), reads=[x.k, p.k], writes=[v.k])
                h = ht.get()
                self.layer_norm(v, stt.get(), mvt.get(), g_t, b_t, h)
                hbb = hb.get()
                S.op('act', lambda e: e.activation(out=hbb.t[:], in_=h.t[:], func=AF.Copy), reads=[h.k], writes=[hbb.k])
                return h, hbb

            def stageB(c, h, hbb):
                r0 = c * 128
                S.dma('sp', self.hres[r0:r0 + 128, :], h.t[:], reads=[h.k], writes=[('D', 'hres', c)])
                hT = hTt.get()
                self.transpose_store(hbb, ptr, hT)
                S.dma('sp', self.hT[:, r0:r0 + 128].rearrange("(k p) t -> p k t", p=128), hT.t[:], reads=[hT.k], writes=[('D', 'hT', c)])
                pr = prt.get()
                for k in range(16):
                    S.op('pe', lambda e: e.matmul(pr.t[:, 0:36], lhsT=hT.t[:, k, :], rhs=wr.t[:, k, :], start=(k == 0), stop=(k == 15)),
                         reads=[hT.k, 'wr'], writes=[pr.k])
                s = sm.get()
                sk = s.k
                A = s.t

                def dv(fn, extra_r=(), w=None):
                    S.op('dve', fn, reads=[sk] + list(extra_r), writes=[w or sk])
                dv(lambda e: e.tensor_tensor(out=A[:, 0:36], in0=pr.t[:, 0:36], in1=brt.t[:], op=ALU.add), extra_r=[pr.k, 'brt'])
                dv(lambda e: e.reduce_max(out=A[:, 36:37], in_=A[:, 0:4], axis=mybir.AxisListType.X))
                dv(lambda e: e.tensor_scalar(out=A[:, 224:228], in0=A[:, 0:4], scalar1=A[:, 36:37], scalar2=None, op0=ALU.subtract))
                S.op('act', lambda e: e.activation(out=A[:, 224:228], in_=A[:, 224:228], func=AF.Exp), reads=[sk], writes=[sk])
                dv(lambda e: e.reduce_sum(out=A[:, 37:38], in_=A[:, 224:228], axis=mybir.AxisListType.X))
                dv(lambda e: e.reciprocal(out=A[:, 38:39], in_=A[:, 37:38]))
                dv(lambda e: e.tensor_scalar(out=A[:, 40:44], in0=A[:, 0:4], scalar1=A[:, 36:37], scalar2=None, op0=ALU.is_equal))
                dv(lambda e: e.tensor_scalar(out=A[:, 44:52], in0=A[:, 4:12], scalar1=A[:, 40:41], scalar2=None, op0=ALU.mult))
                for j in range(1, 4):
                    dv(lambda e: e.scalar_tensor_tensor(out=A[:, 44:52], in0=A[:, 4 + 8 * j:12 + 8 * j], scalar=A[:, 40 + j:41 + j], in1=A[:, 44:52],
                                                        op0=ALU.mult, op1=ALU.add))
                dv(lambda e: e.reduce_max(out=A[:, 52:53], in_=A[:, 44:52], axis=mybir.AxisListType.X))
                dv(lambda e: e.tensor_scalar(out=A[:, 56:64], in0=A[:, 44:52], scalar1=A[:, 52:53], scalar2=None, op0=ALU.is_equal))
                dv(lambda e: e.scalar_tensor_tensor(out=A[:, 72:80], in0=A[:, 56:64], scalar=-1e30, in1=A[:, 44:52], op0=ALU.mult, op1=ALU.add))
                dv(lambda e: e.reduce_max(out=A[:, 53:54], in_=A[:, 72:80], axis=mybir.AxisListType.X))
                dv(lambda e: e.tensor_scalar(out=A[:, 64:72], in0=A[:, 72:80], scalar1=A[:, 53:54], scalar2=None, op0=ALU.is_equal))
                dv(lambda e: e.tensor_tensor(out=A[:, 80:81], in0=A[:, 53:54], in1=A[:, 52:53], op=ALU.subtract))
                S.op('act', lambda e: e.activation(out=A[:, 80:81], in_=A[:, 80:81], func=AF.Exp), reads=[sk], writes=[sk])
                dv(lambda e: e.tensor_scalar(out=A[:, 81:82], in0=A[:, 80:81], scalar1=1.0, scalar2=None, op0=ALU.add))
                dv(lambda e: e.reciprocal(out=A[:, 81:82], in_=A[:, 81:82]))
                dv(lambda e: e.tensor_tensor(out=A[:, 81:82], in0=A[:, 81:82], in1=A[:, 38:39], op=ALU.mult))
                dv(lambda e: e.tensor_tensor(out=A[:, 82:83], in0=A[:, 38:39], in1=A[:, 81:82], op=ALU.subtract))
                S.op('dve', lambda e: e.tensor_copy(out=self.w_all.t[:, c, :], in_=A[:, 81:83]), reads=[sk], writes=['w_all'])
                for j in range(4):
                    dv(lambda e: e.tensor_scalar(out=A[:, 96 + 8 * j:104 + 8 * j], in0=A[:, 56:64], scalar1=A[:, 40 + j:41 + j], scalar2=None, op0=ALU.mult))
                    dv(lambda e: e.tensor_scalar(out=A[:, 128 + 8 * j:136 + 8 * j], in0=A[:, 64:72], scalar1=A[:, 40 + j:41 + j], scalar2=None, op0=ALU.mult))
                dv(lambda e: e.tensor_tensor(out=A[:, 160:192], in0=A[:, 96:128], in1=A[:, 128:160], op=ALU.add))
                mbb = mb.get()
                S.op('dve', lambda e: e.tensor_copy(out=mbb.t[:], in_=A[:, 160:192]), reads=[sk], writes=[mbb.k])
                pc = prt.get()
                S.op('pe', lambda e: e.matmul(pc.t[:, 0:32], lhsT=self.cbs(C_TRIL, 128), rhs=mbb.t[:], start=True, stop=True), reads=[mbb.k, 'cb'], writes=[pc.k])
                S.op('pe', lambda e: e.matmul(pc.t[:, 32:64], lhsT=self.cbs(C_ONES, 128), rhs=mbb.t[:], start=True, stop=True), reads=[mbb.k, 'cb'], writes=[pc.k])
                dv(lambda e: e.tensor_tensor(out=A[:, 192:224], in0=pc.t[:, 0:32], in1=base.t[:], op=ALU.add), extra_r=[pc.k, 'rbase'])
                dv(lambda e: e.tensor_tensor(out=A[:, 192:224], in0=A[:, 192:224], in1=self.cf.t[:, 2:34], op=ALU.add), extra_r=['cf'])
                S.op('dve', lambda e: e.tensor_tensor(out=base.t[:], in0=base.t[:], in1=pc.t[:, 32:64], op=ALU.add), reads=['rbase', pc.k, sk], writes=['rbase'])
                dv(lambda e: e.tensor_tensor(out=A[:, 96:128], in0=A[:, 96:128], in1=A[:, 192:224], op=ALU.mult))
                dv(lambda e: e.tensor_tensor(out=A[:, 128:160], in0=A[:, 128:160], in1=A[:, 192:224], op=ALU.mult))
                dv(lambda e: e.reduce_sum(out=A[:, 228:229], in_=A[:, 96:128], axis=mybir.AxisListType.X))
                dv(lambda e: e.reduce_sum(out=A[:, 229:230], in_=A[:, 128:160], axis=mybir.AxisListType.X))
                S.op('dve', lambda e: e.tensor_copy(out=self.dest_all.t[:, c, :], in_=A[:, 228:230]), reads=[sk], writes=['dest_all'])
                for kk in range(2):
                    S.dma('pool', None, None, reads=[hbb.k, 'dest_all'], writes=[('D', 'hrows', c, kk)],
                          fn=lambda e: e.indirect_dma_start(out=self.hrows[:, :], out_offset=bass.IndirectOffsetOnAxis(ap=self.dest_all.t[:, c, kk:kk + 1], axis=0),
                                                            in_=hbb.t[:], in_offset=None))

            cur = stageA(0)
            for c in range(NCH):
                nxt = stageA(c + 1) if c + 1 < NCH else None
                stageB(c, *cur)
                cur = nxt

    def phase_experts(self, l):
        S, NCH, CAP, CAPB = self.S, self.NCH, self.CAP, self.CAPB
        with self.scope() as es:
            rows = self.ring("erow", 2, [128, D], BF16)
            rT = self.ring("erT", 2, [128, 16, CAP], BF16)
            hidT = self.ring("ehid", 2, [128, 4, CAP], BF16)
            sil = self.ring("esil", 2, [128, CAP], F32)
            ysb = self.ring("eysb", CAPB, [128, D], F32)
            ptr = self.ring("eptr", 2, [128, 1024], BF16, psum=True)
            pg = self.ring("epg", 2, [128, 512], F32, psum=True)
            pu = self.ring("epu", 2, [128, 512], F32, psum=True)
            py = self.ring("epy", 2, [128, 512], F32, psum=True)
            hr = [('D', 'hrows', c, kk) for c in range(NCH) for kk in range(2)]
            EST = self.ring("est", 4, [128, 2048], F32)
            WG = self.ring("ewg", 2, [128, 16, EH], BF16)
            WU = self.ring("ewu", 2, [128, 16, EH], BF16)
            def load_gu(ex_):
                weg_ = self.w_eg[l, ex_].rearrange("(k p) c -> p k c", p=128)
                weu_ = self.w_eu[l, ex_].rearrange("(k p) c -> p k c", p=128)
                wg_ = WG.get()
                wu_ = WU.get()
                for (src, dstb) in ((weg_, wg_), (weu_, wu_)):
                    for kq in range(4):
                        st = EST.get()
                        sk4 = [(st.k, jj) for jj in range(4)]
                        S.dma('sp', st.t[:].rearrange("p (a b) -> p a b", a=4), src[:, kq * 4:(kq + 1) * 4, :], writes=sk4)
                        self.cast(dstb.t[:, kq * 4:(kq + 1) * 4, :], st.t[:].rearrange("p (a b) -> p a b", a=4), sk4, [(dstb.k, kq)])
                        yield (wg_, wu_)
            nxt_w = None
            for ex in range(NEXP):
                rt = rT.get()
                for b in range(CAPB):
                    r = rows.get()
                    S.dma('act', r.t[:], self.hrows[ex * CAP + b * 128:ex * CAP + (b + 1) * 128, :], reads=hr, writes=[r.k])
                    for k8 in range(0, 16, 8):
                        p = ptr.get()
                        for j in range(8):
                            k = k8 + j
                            S.op('pe', lambda e: e.transpose(p.t[:, j * 128:(j + 1) * 128], r.t[:, k * 128:(k + 1) * 128], self.cbs(C_IDENT, 128)),
                                 reads=[r.k, 'cb'], writes=[p.k])
                        S.op('act' if k8 == 0 else 'dve',
                             (lambda e: e.activation(out=rt.t[:, k8:k8 + 8, b * 128:(b + 1) * 128], in_=p.t[:].rearrange("p (c d) -> p c d", c=8), func=AF.Copy)) if k8 == 0 else
                             (lambda e: e.tensor_copy(out=rt.t[:, k8:k8 + 8, b * 128:(b + 1) * 128], in_=p.t[:].rearrange("p (c d) -> p c d", c=8))),
                             reads=[p.k], writes=[rt.k])
                hd = hidT.get()
                weg = self.w_eg[l, ex].rearrange("(k p) c -> p k c", p=128)
                weu = self.w_eu[l, ex].rearrange("(k p) c -> p k c", p=128)
                if ex == 0:
                    for nxt_w in load_gu(0):
                        pass
                wg, wu = nxt_w
                gen = load_gu(ex + 1) if ex + 1 < NEXP else iter(())
                for j in range(4):
                    for _ in range(2):
                        nxt_w = next(gen, nxt_w)
                    g_ = pg.get()
                    u_ = pu.get()
                    for k in range(16):
                        S.op('pe', lambda e: e.matmul(g_.t[:, 0:CAP], lhsT=wg.t[:, k, j * 128:(j + 1) * 128], rhs=rt.t[:, k, :], start=(k == 0), stop=(k == 15)),
                             reads=[(wg.k, k // 4), rt.k], writes=[g_.k])
                    for k in range(16):
                        S.op('pe', lambda e: e.matmul(u_.t[:, 0:CAP], lhsT=wu.t[:, k, j * 128:(j + 1) * 128], rhs=rt.t[:, k, :], start=(k == 0), stop=(k == 15)),
                             reads=[(wu.k, k // 4), rt.k], writes=[u_.k])
                    s_ = sil.get()
                    S.op('act', lambda e: e.activation(out=s_.t[:], in_=g_.t[:, 0:CAP], func=AF.Silu), reads=[g_.k], writes=[s_.k])
                    S.op('dve', lambda e: e.tensor_tensor(out=hd.t[:, j, :], in0=s_.t[:], in1=u_.t[:, 0:CAP], op=ALU.mult), reads=[s_.k, u_.k], writes=[hd.k])
                wed = self.w_ed[l, ex].rearrange("(k p) c -> p k c", p=128)
                ys = [ysb.get() for _ in range(CAPB)]
                wd_n = self.wload(wed[:, :, 0:512], 4, 512)
                for ct in range(4):
                    wd = wd_n
                    if ct < 3:
                        wd_n = self.wload(wed[:, :, (ct + 1) * 512:(ct + 2) * 512], 4, 512)
                    wdv = wd.t[:].rearrange("p (k m) -> p k m", k=4)
                    for b in range(CAPB):
                        y_ = py.get()
                        for j in range(4):
                            S.op('pe', lambda e: e.matmul(y_.t[:], lhsT=hd.t[:, j, b * 128:(b + 1) * 128], rhs=wdv[:, j, :], start=(j == 0), stop=(j == 3)),
                                 reads=[hd.k, wd.k], writes=[y_.k])
                        if (ct + b) % 2 == 0:
                            S.op('act', lambda e: e.activation(out=ys[b].t[:, ct * 512:(ct + 1) * 512], in_=y_.t[:], func=AF.Copy), reads=[y_.k], writes=[ys[b].k])
                        else:
                            S.op('dve', lambda e: e.tensor_copy(out=ys[b].t[:, ct * 512:(ct + 1) * 512], in_=y_.t[:]), reads=[y_.k], writes=[ys[b].k])
                for b in range(CAPB):
                    S.dma('sp', self.yrows[ex * CAP + b * 128:ex * CAP + (b + 1) * 128, :], ys[b].t[:], reads=[ys[b].k], writes=[('D', 'yrows', ex, b)])

    def phase_ple(self, l, last):
        S, SL, NCH, CAPB = self.S, self.SL, self.NCH, self.CAPB
        with self.scope() as es:
            wpg = self.sb("wpg", [128, 16, D], BF16)
            self.load_resident(wpg, self.w_pg[l], 16)
            wpp = self.sb("wpp", [128, 2, D], BF16)
            self.load_resident(wpp, self.w_pp[l], 2)
            g_t = self.sb("l2g", [128, D], F32)
            b_t = self.sb("l2b", [128, D], F32)
            S.dma('sp', g_t.t[:], self.ln2_g[l, 0:1, :].partition_broadcast(128), writes=['l2g'])
            S.dma('sp', b_t.t[:], self.ln2_b[l, 0:1, :].partition_broadcast(128), writes=['l2b'])
            hTt = self.ring("phT", 2, [128, 16, 128], BF16)
            pTs = self.ring("ppTs", 2, [128, 2, 128], F32)
            pTb = self.ring("ppTb", 2, [128, 2, 128], BF16)
            hr = self.ring("phr", 2, [128, D], F32)
            y1 = self.ring("py1", 1, [128, D], F32)
            y2 = self.ring("py2", 1, [128, D], F32)
            sg = self.ring("psg", 1, [128, D], F32)
            xb = self.ring("pxb", 1, [128, D], BF16)
            xTt = self.ring("pxT", 2, [128, 16, 128], BF16)
            stt = self.ring("pstt", 2, [128, 24], F32)
            mvt = self.ring("pmv", 2, [128, 4], F32)
            ppg = self.ring("ppg", 3, [128, 512], F32, psum=True)
            ppp = self.ring("ppp", 3, [128, 512], F32, psum=True)
            ptr = self.ring("pptr", 2, [128, 1024], BF16, psum=True)
            yr = [('D', 'yrows', ex, b) for ex in range(NEXP) for b in range(CAPB)]
            for c in range(NCH):
                r0 = c * 128
                hT = hTt.get()
                S.dma('sp', hT.t[:], self.hT[:, r0:r0 + 128].rearrange("(k p) t -> p k t", p=128), reads=[('D', 'hT', c)], writes=[hT.k])
                ps_ = pTs.get()
                S.dma('sp', ps_.t[:], self.pT[l, :, r0:r0 + 128].rearrange("(k p) t -> p k t", p=128), writes=[ps_.k])
                pb = pTb.get()
                S.op('pool', lambda e: e.tensor_copy(out=pb.t[:], in_=ps_.t[:]), reads=[ps_.k], writes=[pb.k])
                h = hr.get()
                S.dma('sp', h.t[:], self.hres[r0:r0 + 128, :], reads=[('D', 'hres', c)], writes=[h.k])
                ya = y1.get()
                yb = y2.get()
                for kk, yy in ((0, ya), (1, yb)):
                    S.dma('pool', None, None, reads=yr + ['dest_all'], writes=[yy.k],
                          fn=lambda e: e.indirect_dma_start(out=yy.t[:], out_offset=None, in_=self.yrows[:, :],
                                                            in_offset=bass.IndirectOffsetOnAxis(ap=self.dest_all.t[:, c, kk:kk + 1], axis=0)))
                s_ = sg.get()
                v = s_
                for ct in range(4):
                    sl = slice(ct * 512, (ct + 1) * 512)
                    pg_ = ppg.get()
                    for k in range(16):
                        S.op('pe', lambda e: e.matmul(pg_.t[:], lhsT=hT.t[:, k, :], rhs=wpg.t[:, k, sl], start=(k == 0), stop=(k == 15)), reads=[hT.k, 'wpg'], writes=[pg_.k])
                    pp_ = ppp.get()
                    for k in range(2):
                        S.op('pe', lambda e: e.matmul(pp_.t[:], lhsT=pb.t[:, k, :], rhs=wpp.t[:, k, sl], start=(k == 0), stop=(k == 1)), reads=[pb.k, 'wpp'], writes=[pp_.k])
                    S.op('act', lambda e: e.activation(out=s_.t[:, sl], in_=pg_.t[:], func=AF.Sigmoid), reads=[pg_.k], writes=[s_.k])
                    S.op('dve', lambda e: e.tensor_tensor(out=s_.t[:, sl], in0=s_.t[:, sl], in1=pp_.t[:], op=ALU.mult), reads=[s_.k, pp_.k], writes=[s_.k])
                S.op('dve', lambda e: e.scalar_tensor_tensor(out=v.t[:], in0=h.t[:], scalar=ALPHA, in1=s_.t[:], op0=ALU.mult, op1=ALU.add), reads=[h.k, s_.k], writes=[v.k])
                S.op('dve', lambda e: e.scalar_tensor_tensor(out=v.t[:], in0=ya.t[:], scalar=self.w_all.t[:, c, 0:1], in1=v.t[:], op0=ALU.mult, op1=ALU.add),
                     reads=[ya.k, 'w_all', v.k], writes=[v.k])
                S.op('dve', lambda e: e.scalar_tensor_tensor(out=v.t[:], in0=yb.t[:], scalar=self.w_all.t[:, c, 1:2], in1=v.t[:], op0=ALU.mult, op1=ALU.add),
                     reads=[yb.k, 'w_all', v.k], writes=[v.k])
                x = h
                self.layer_norm(v, stt.get(), mvt.get(), g_t, b_t, x)
                if last:
                    S.dma('sp', self.out[r0:r0 + 128, :], x.t[:], reads=[x.k], writes=[('D', 'out', c)])
                else:
                    S.dma('sp', self.xres[r0:r0 + 128, :], x.t[:], reads=[x.k], writes=[('D', 'xres', c)])
                    xbb = xb.get()
                    S.op('act', lambda e: e.activation(out=xbb.t[:], in_=x.t[:], func=AF.Copy), reads=[x.k], writes=[xbb.k])
                    xT = xTt.get()
                    self.transpose_store(xbb, ptr, xT)
                    S.dma('sp', self.xT[:, r0:r0 + 128].rearrange("(k p) t -> p k t", p=128), xT.t[:], reads=[xT.k], writes=[('D', 'xT1', c)])


class _Scope:
    def __init__(self, prog):
        self.p = prog
        self.es = ExitStack()

    def __enter__(self):
        self.prev = self.p.cur
        self.p.cur = self.es
        self.es.__enter__()
        return self.es

    def __exit__(self, *a):
        self.p.S.barrier()
        r = self.es.__exit__(*a)
        self.p.cur = self.prev
        return r


class nc_allow:
    def __init__(self, nc):
        self.cm = nc.allow_non_contiguous_dma(reason="tiny strided column load")

    def __enter__(self):
        return self.cm.__enter__()

    def __exit__(self, *a):
        return self.cm.__exit__(*a)


def make_consts(CAP):
    c = np.zeros((128, NCONST), np.float32)
    i = np.arange(128)
    c[:, C_IDENT:C_IDENT + 128] = np.eye(128)
    c[:, C_ONES:C_ONES + 128] = 1.0
    c[:, C_TRIS:C_TRIS + 128] = (i[:, None] > i[None, :])
    c[:, C_TRIL:C_TRIL + 128] = (i[:, None] < i[None, :])
    Rd = np.zeros((128, 128), np.float32)
    for base in (0, 64):
        for j in range(8):
            Rd[base + j, base + j + 8] = -1.0
            Rd[base + j + 8, base + j] = 1.0
    c[:, C_RD:C_RD + 128] = Rd.T
    Rm = np.zeros((128, 128), np.float32)
    for j in range(32):
        Rm[j, j + 32] = -1.0
        Rm[j + 32, j] = 1.0
    c[:, C_RM:C_RM + 128] = Rm.T
    kk = i[:, None]
    qq = i[None, :]
    inblk = {
        'ch': (kk // 64 <= qq // 64),
        'ca': (kk <= qq),
        'st': (kk < qq),
    }
    for j0 in range(4):
        for b in range(4):
            for name, col, kind in (('ch', C_MADD_CH, 'add'), ('ca', C_MADD_CA, 'add'), ('st', C_MMUL, 'mul')):
                if b < j0:
                    valid = np.zeros((128, 128), bool)
                elif b == j0:
                    valid = inblk[name]
                else:
                    valid = np.ones((128, 128), bool)
                blk = np.where(valid, 0.0, NEG) if kind == 'add' else valid.astype(np.float32)
                c[:, col + j0 * 512 + b * 128:col + j0 * 512 + (b + 1) * 128] = blk
    invf_d = np.zeros(128, np.float32)
    fd = (np.float32(ROPE_THETA) ** (-np.arange(8, dtype=np.float32) / np.float32(8))).astype(np.float32)
    for base in (0, 64):
        invf_d[base:base + 8] = fd
        invf_d[base + 8:base + 16] = fd
    c[:, C_INVF_D] = invf_d
    fm = (np.float32(ROPE_THETA) ** (-np.arange(32, dtype=np.float32) / np.float32(32))).astype(np.float32)
    invf_m = np.zeros(128, np.float32)
    invf_m[0:32] = fm
    invf_m[32:64] = fm
    c[:, C_INVF_M] = invf_m
    c[:, C_ECAP:C_ECAP + 32] = (np.arange(32) * CAP)[None, :]
    return c


_CACHE = {}


def get_prog(S_len, **kw):
    key = (S_len, tuple(sorted((k, str(v)) for k, v in kw.items())))
    if key not in _CACHE:
        p = Prog(S_len, **kw)
        p.build()
        _CACHE[key] = p
    return _CACHE[key]


def make_in_map(p, b, inputs):
    f = lambda a: np.ascontiguousarray(np.asarray(a, dtype=np.float32))
    x = np.asarray(inputs['x'])[b]
    m = {
        'x_tm': f(x), 'x_fm': f(x.T),
        'pT': f(np.transpose(np.asarray(inputs['p'])[:, b], (0, 2, 1))),
        'pos': np.ascontiguousarray(np.asarray(inputs['positions'])[b:b + 1].astype(np.int32)),
        'consts': make_consts(p.CAP),
        'w_in': f(inputs['w_in']),
        'fox_f_bias': f(inputs['fox_f_bias']).reshape(DEPTH, 4, 1),
        'diff_lambda': f(inputs['diff_lambda']).reshape(DEPTH, 1, 256),
        'diff_subln': f(inputs['diff_subln']).reshape(DEPTH, 128, 1),
        'mla_q_norm': f(f(inputs['mla_q_norm']).reshape(DEPTH, 4, 128).transpose(0, 2, 1)),
        'mla_kv_norm': f(f(inputs['mla_kv_norm']).reshape(DEPTH, 2, 128).transpose(0, 2, 1)),
        'mla_w_uq': f(inputs['mla_w_uq']), 'mla_w_ukv': f(inputs['mla_w_ukv']),
        'w_branch': f(inputs['w_branch']), 'w_o': f(inputs['w_o']),
        'ln1_g': f(inputs['ln1_g']).reshape(DEPTH, 1, D), 'ln1_b': f(inputs['ln1_b']).reshape(DEPTH, 1, D),
        'w_router_group': f(inputs['w_router_group']), 'b_router_group': f(inputs['b_router_group']).reshape(DEPTH, 1, 4),
        'w_router_expert': f(inputs['w_router_expert']), 'b_router_expert': f(inputs['b_router_expert']).reshape(DEPTH, 1, 32),
        'w_expert_gate': f(inputs['w_expert_gate']), 'w_expert_up': f(inputs['w_expert_up']),
        'w_expert_down': f(inputs['w_expert_down']),
        'w_ple_gate': f(inputs['w_ple_gate']), 'w_ple_proj': f(inputs['w_ple_proj']),
        'ln2_g': f(inputs['ln2_g']).reshape(DEPTH, 1, D), 'ln2_b': f(inputs['ln2_b']).reshape(DEPTH, 1, D),
    }
    return m


def kernel(**inputs):
    x = np.asarray(inputs['x'])
    B, SL, _ = x.shape
    p = get_prog(SL)
    in_maps = [make_in_map(p, b, inputs) for b in range(B)]
    res = run_bass_kernel_spmd(p.nc, in_maps, core_ids=list(range(B)))
    out = np.stack([np.asarray(r['out']) for r in res.results], axis=0)
    return out.astype(np.float32)
```

```python
import math
import numpy as np
from contextlib import ExitStack
import concourse.bass as bass
import concourse.mybir as mybir
from concourse.bass_utils import run_bass_kernel_spmd

F32 = mybir.dt.float32
BF16 = mybir.dt.bfloat16
I32 = mybir.dt.int32
AF = mybir.ActivationFunctionType
ALU = mybir.AluOpType

D = 2048
DEPTH = 2
NEXP = 32
EH = 512
IN_DIM = 13636
ALPHA = 4.0 ** 0.25
LN_EPS = 1e-5
RMS_EPS = 1e-6
ROPE_THETA = 500000.0
OFF = dict(diff_q=0, diff_k=512, diff_v=1024, fox_q=1536, fox_k=2048, fox_v=2560, fox_f=3072,
           mla_cq=3076, mla_ckv=3588, mla_kr=3844, sb_q=3908, sb_k=4420, sb_v=4932, gates=5444)
NEG = -30000.0
TWO_PI = 6.283185307179586

C_IDENT, C_ONES, C_TRIS, C_TRIL, C_RD, C_RM = 0, 128, 256, 384, 512, 640
C_MADD_CH, C_MADD_CA, C_MMUL = 768, 768 + 2048, 768 + 4096
C_INVF_D, C_INVF_M, C_ECAP = 768 + 6144, 768 + 6145, 768 + 6146
NCONST = 768 + 6146 + 32


class Sched:
    NDMA = 16

    def __init__(self, nc, es):
        self.nc = nc
        self.es = es
        self.eng = {'pe': nc.tensor, 'dve': nc.vector, 'act': nc.scalar, 'pool': nc.gpsimd, 'sp': nc.sync}
        self.sems = {}
        self.cnt = {}
        for e in self.eng:
            self.sems[e] = es.enter_context(nc.semaphore('s_' + e))
            self.cnt[e] = 0
        self.dma_rr = {}
        for q in ('sp', 'act', 'pool'):
            self.dma_rr[q] = 0
            for i in range(self.NDMA):
                k = ('d', q, i)
                self.sems[k] = es.enter_context(nc.semaphore('d_%s_%d' % (q, i)))
                self.cnt[k] = 0
        self.seen = {e: {} for e in self.eng}
        self.res = {}
        self.nins = 0

    def _need(self, reads, writes):
        need = {}
        for r in reads:
            st = self.res.get(r)
            if st and st[0] is not None:
                k, v = st[0]
                if need.get(k, 0) < v:
                    need[k] = v
        for w in writes:
            st = self.res.get(w)
            if st:
                if st[0] is not None:
                    k, v = st[0]
                    if need.get(k, 0) < v:
                        need[k] = v
                for k, v in st[1].items():
                    if need.get(k, 0) < v:
                        need[k] = v
        return need

    def _commit(self, ev, reads, writes):
        k, v = ev
        for r in reads:
            st = self.res.get(r)
            if st is None:
                st = self.res[r] = [None, {}]
            if st[1].get(k, 0) < v:
                st[1][k] = v
        for w in writes:
            self.res[w] = [ev, {}]

    def _waits(self, e, need):
        eng = self.eng[e]
        seen = self.seen[e]
        for k, v in need.items():
            if e == 'pe' and k == 'pe':
                continue
            if seen.get(k, 0) < v:
                eng.wait_ge(self.sems[k], v)
                seen[k] = v

    def op(self, e, fn, reads=(), writes=()):
        self._waits(e, self._need(reads, writes))
        ins = fn(self.eng[e])
        self.cnt[e] += 1
        ins.then_inc(self.sems[e], 1)
        self._commit((e, self.cnt[e]), reads, writes)
        self.nins += 1

    def dma(self, q, out, in_, reads=(), writes=(), fn=None, **kw):
        if q == 'sp' and fn is None and str(out.space) == 'DRAM' and str(in_.space) != 'DRAM':
            q = 'act'
        i = self.dma_rr[q]
        self.dma_rr[q] = (i + 1) % self.NDMA
        k = ('d', q, i)
        need = self._need(reads, writes)
        if self.cnt[k]:
            need[k] = max(need.get(k, 0), self.cnt[k])
        self._waits(q, need)
        if fn is not None:
            ins = fn(self.eng[q])
        else:
            ins = self.eng[q].dma_start(out=out, in_=in_, **kw)
        self.cnt[k] += 16
        ins.then_inc(self.sems[k], 16)
        self._commit((k, self.cnt[k]), reads, writes)
        self.nins += 1

    def finish(self, keys):
        self._waits('sp', self._need(keys, ()))

    def barrier(self):
        need = {k: v for k, v in self.cnt.items() if v > 0}
        for e in self.eng:
            eng = self.eng[e]
            seen = self.seen[e]
            for k, v in need.items():
                if seen.get(k, 0) < v:
                    eng.wait_ge(self.sems[k], v)
                    seen[k] = v


class Ring:
    def __init__(self, tiles):
        self.tiles = tiles
        self.i = 0

    def get(self):
        t = self.tiles[self.i]
        self.i = (self.i + 1) % len(self.tiles)
        return t


class T:
    def __init__(self, ap, key):
        self.t = ap
        self.k = key


class Prog:
    def __init__(self, S_len, nlayers=DEPTH, dbg=False, phases=None):
        self.SL = S_len
        self.TG = min(2048, S_len)
        self.NTG = S_len // self.TG
        self.NCH = S_len // 128
        self.NQT = S_len // 512
        avg = 2 * S_len // NEXP
        sd = math.sqrt(2 * S_len / 32.0)
        self.CAPB = int(math.ceil((avg + 8.0 * sd) / 128.0))
        self.CAP = self.CAPB * 128
        self.nlayers = nlayers
        self.WD = DEPTH if (nlayers == DEPTH) else nlayers
        self.NE_DECL = NEXP if (phases is None or 'exp' in phases) else 1
        self.dbg = dbg
        self.phases = phases
        self.nc = bass.Bass("TRN2", target_bir_lowering=False)
        self.scopes = []
        self.uid = 0
        self.cast_rr = 0

    def din(self, name, shape, dt=F32):
        return self.nc.dram_tensor(name, list(shape), dt, kind="ExternalInput").ap()

    def dscr(self, name, shape, dt):
        kind = "ExternalOutput" if (self.dbg and name in self.dbg) else "Internal"
        return self.nc.dram_tensor(name, list(shape), dt, kind=kind).ap()

    def sb(self, name, shape, dt):
        self.uid += 1
        t = self.cur.enter_context(self.nc.sbuf_tensor("%s_%d" % (name, self.uid), list(shape), dt))
        return T(t, name)

    def ps(self, name, shape, dt=F32):
        self.uid += 1
        t = self.cur.enter_context(self.nc.psum_tensor("%s_%d" % (name, self.uid), list(shape), dt))
        return T(t, name)

    def ring(self, name, n, shape, dt, psum=False):
        return Ring([(self.ps if psum else self.sb)("%s%d" % (name, i), shape, dt) for i in range(n)])

    def wload(self, src_ap, a, b, q='sp'):
        st = self.wst.get()
        wb = self.wbf.get()
        S = self.S
        stv = st.t[:, 0:a * b].rearrange("p (a b) -> p a b", a=a)
        step = max(1, 4 * 128 // 128) if b <= 512 else 1
        step = 4
        keys = []
        for j, a0 in enumerate(range(0, a, step)):
            a1 = min(a, a0 + step)
            S.dma(q, stv[:, a0:a1, :], src_ap[:, a0:a1, :], writes=[(st.k, j)])
            keys.append((st.k, j))
        self.cast(wb.t[:, 0:a * b], st.t[:, 0:a * b], keys, [wb.k])
        return wb

    def cast(self, out_ap, in_ap, reads, writes):
        i = self.cast_rr
        self.cast_rr = (i + 1) % 3
        if i != 2:
            self.S.op('act', lambda e: e.activation(out=out_ap, in_=in_ap, func=AF.Copy), reads=reads, writes=writes)
        else:
            self.S.op('dve', lambda e: e.tensor_copy(out=out_ap, in_=in_ap), reads=reads, writes=writes)

    def build(self):
        nc = self.nc
        SL, TG, NCH = self.SL, self.TG, self.NCH
        L = self.nlayers
        self.x_tm = self.din("x_tm", [SL, D])
        self.x_fm = self.din("x_fm", [D, SL])
        self.pT = self.din("pT", [DEPTH, 256, SL])
        self.pos = self.din("pos", [1, SL], I32)
        self.consts = self.din("consts", [128, NCONST])
        self.w_in = self.din("w_in", [DEPTH, D, IN_DIM])
        self.fox_f_bias = self.din("fox_f_bias", [DEPTH, 4, 1])
        self.diff_lambda = self.din("diff_lambda", [DEPTH, 1, 256])
        self.diff_subln = self.din("diff_subln", [DEPTH, 128, 1])
        self.mla_q_norm = self.din("mla_q_norm", [DEPTH, 128, 4])
        self.mla_kv_norm = self.din("mla_kv_norm", [DEPTH, 128, 2])
        self.mla_w_uq = self.din("mla_w_uq", [DEPTH, 512, 768])
        self.mla_w_ukv = self.din("mla_w_ukv", [DEPTH, 256, 1024])
        self.w_branch = self.din("w_branch", [DEPTH, 4, 512, D])
        self.w_o = self.din("w_o", [DEPTH, D, D])
        self.ln1_g = self.din("ln1_g", [DEPTH, 1, D])
        self.ln1_b = self.din("ln1_b", [DEPTH, 1, D])
        self.w_rg = self.din("w_router_group", [DEPTH, D, 4])
        self.b_rg = self.din("b_router_group", [DEPTH, 1, 4])
        self.w_re = self.din("w_router_expert", [DEPTH, D, 32])
        self.b_re = self.din("b_router_expert", [DEPTH, 1, 32])
        self.w_eg = self.din("w_expert_gate", [self.WD, self.NE_DECL, D, EH])
        self.w_eu = self.din("w_expert_up", [self.WD, self.NE_DECL, D, EH])
        self.w_ed = self.din("w_expert_down", [self.WD, self.NE_DECL, EH, D])
        self.w_pg = self.din("w_ple_gate", [DEPTH, D, D])
        self.w_pp = self.din("w_ple_proj", [DEPTH, 256, D])
        self.ln2_g = self.din("ln2_g", [DEPTH, 1, D])
        self.ln2_b = self.din("ln2_b", [DEPTH, 1, D])
        self.out = nc.dram_tensor("out", [SL, D], F32, kind="ExternalOutput").ap()
        self.xT = self.dscr("xT", [D, SL], BF16)
        self.xres = self.dscr("xres", [SL, D], F32)
        self.ropeD = self.dscr("ropeD", [2, 128, SL], BF16)
        self.ropeM = self.dscr("ropeM", [2, 64, SL], BF16)
        self.qT = self.dscr("qT", [16, 128, SL], BF16)
        self.kT = self.dscr("kT", [16, 128, SL], BF16)
        self.vv = self.dscr("vv", [16, SL, 128], BF16)
        self.qrT = self.dscr("qrT", [4, 64, SL], BF16)
        self.krT = self.dscr("krT", [64, SL], BF16)
        self.fT = self.dscr("fT", [4, SL], F32)
        self.cumT = self.dscr("cumT", [4, SL], F32)
        self.qaug = self.dscr("qaug", [4, 3, SL], BF16)
        self.gatesT = self.dscr("gatesT", [4 * D, SL], BF16)
        self.oT = self.dscr("oT", [16, 128, SL], BF16)
        self.mergedT = self.dscr("mergedT", [D, SL], BF16)
        self.hres = self.dscr("hres", [SL, D], F32)
        self.hT = self.dscr("hT", [D, SL], BF16)
        self.hrows = self.dscr("hrows", [NEXP * self.CAP, D], BF16)
        self.yrows = self.dscr("yrows", [NEXP * self.CAP, D], F32)

        with ExitStack() as es:
            self.S = Sched(nc, es)
            self.glob = es
            self.cur = es
            self.cb = self.sb("cb", [128, C_INVF_D], BF16)
            self.cf = self.sb("cf", [128, 34], F32)
            self.S.dma('sp', self.cf.t[:], self.consts[:, C_INVF_D:C_INVF_D + 34], writes=['cf'])
            with self.scope() as es0:
                for c0 in range(0, C_INVF_D, 2048):
                    c1 = min(C_INVF_D, c0 + 2048)
                    cft = self.sb("cft%d" % c0, [128, 2048], F32)
                    self.S.dma('sp', cft.t[:, 0:c1 - c0], self.consts[:, c0:c1], writes=[cft.k])
                    self.S.op('dve', lambda e: e.tensor_copy(out=self.cb.t[:, c0:c1], in_=cft.t[:, 0:c1 - c0]), reads=[cft.k], writes=['cb'])
            self.dest_all = self.sb("dest_all", [128, NCH, 2], I32)
            self.w_all = self.sb("w_all", [128, NCH, 2], F32)
            self.wst = self.ring("wst", 2, [128, 2048], F32)
            self.wbf = self.ring("wbf", 3, [128, 2048], BF16)
            self.epsln = self.sb("epsln", [128, 1], F32)
            self.S.op('dve', lambda e: e.memset(self.epsln.t[:], LN_EPS), writes=['epsln'])
            self.epsrms = self.sb("epsrms", [128, 1], F32)
            self.S.op('dve', lambda e: e.memset(self.epsrms.t[:], RMS_EPS), writes=['epsrms'])

            ph = self.phases
            if ph is None or 'pre' in ph:
                self.phase_pre()
            for l in range(L):
                if ph is None or 'proj' in ph:
                    self.phase_proj(l)
                if ph is None or 'fox' in ph:
                    self.phase_foxprep(l)
                if ph is None or 'attn' in ph:
                    self.phase_attn(l)
                if ph is None or 'merge' in ph:
                    self.phase_merge(l)
                if ph is None or 'wo' in ph:
                    self.phase_wo(l)
                if ph is None or 'exp' in ph:
                    self.phase_experts(l)
                if ph is None or 'ple' in ph:
                    self.phase_ple(l, last=(l == L - 1))
            keys = [k for k in self.S.res.keys() if isinstance(k, tuple) and k and k[0] == 'D']
            self.S.finish(keys)
        return nc

    def cbs(self, c0, n, p=128):
        return self.cb.t[0:p, c0:c0 + n]

    def scope(self):
        return _Scope(self)

    def phase_pre(self):
        S, SL = self.S, self.SL
        with self.scope() as es:
            CB = min(2048, SL)
            posi = self.sb("posi", [128, CB], I32)
            posf = self.sb("posf", [128, CB], F32)
            ang = self.sb("ang", [128, CB], F32)
            ki = self.sb("ki", [128, CB], I32)
            kf = self.sb("kf", [128, CB], F32)
            tb = self.ring("tb", 2, [128, CB], BF16)
            for cb0 in range(0, SL, CB):
                S.dma('sp', posi.t[:], self.pos[0:1, cb0:cb0 + CB].partition_broadcast(128), writes=['posi'])
                S.op('dve', lambda e: e.tensor_copy(out=posf.t[:], in_=posi.t[:]), reads=['posi'], writes=['posf'])
                for kind, (ccol, npart, dst) in enumerate([(0, 128, self.ropeD), (1, 64, self.ropeM)]):
                    for cs, shift in enumerate([math.pi / 2, 0.0]):
                        P = npart
                        S.op('dve', lambda e: e.tensor_scalar(out=ang.t[0:P], in0=posf.t[0:P], scalar1=self.cf.t[0:P, ccol:ccol + 1],
                                                              scalar2=shift, op0=ALU.mult, op1=ALU.add),
                             reads=['posf', 'cf'], writes=['ang'])
                        S.op('dve', lambda e: e.tensor_scalar(out=ki.t[0:P], in0=ang.t[0:P], scalar1=1.0 / TWO_PI, scalar2=None, op0=ALU.mult),
                             reads=['ang'], writes=['ki'])
                        S.op('dve', lambda e: e.tensor_copy(out=kf.t[0:P], in_=ki.t[0:P]), reads=['ki'], writes=['kf'])
                        S.op('dve', lambda e: e.scalar_tensor_tensor(out=ang.t[0:P], in0=kf.t[0:P], scalar=-TWO_PI, in1=ang.t[0:P],
                                                                     op0=ALU.mult, op1=ALU.add), reads=['kf', 'ang'], writes=['ang'])
                        S.op('dve', lambda e: e.tensor_scalar(out=ang.t[0:P], in0=ang.t[0:P], scalar1=-3.1415925, scalar2=3.1415925,
                                                              op0=ALU.max, op1=ALU.min), reads=['ang'], writes=['ang'])
                        o = tb.get()
                        S.op('act', lambda e: e.activation(out=o.t[0:P], in_=ang.t[0:P], func=AF.Sin), reads=['ang'], writes=[o.k])
                        S.dma('sp', dst[cs, :, cb0:cb0 + CB], o.t[0:P], reads=[o.k], writes=[('D', 'rope', kind, cs, cb0)])
            xs = self.ring("xs", 2, [128, CB], F32)
            xb = self.ring("xbp", 2, [128, CB], BF16)
            for k in range(16):
                for cb0 in range(0, SL, CB):
                    a = xs.get()
                    b = xb.get()
                    S.dma('sp', a.t[:], self.x_fm[k * 128:(k + 1) * 128, cb0:cb0 + CB], writes=[a.k])
                    S.op('pool', lambda e: e.tensor_copy(out=b.t[:], in_=a.t[:]), reads=[a.k], writes=[b.k])
                    S.dma('sp', self.xT[k * 128:(k + 1) * 128, cb0:cb0 + CB], b.t[:], reads=[b.k], writes=[('D', 'xT0', k, cb0 // self.TG)])

    def phase_proj(self, l):
        S, SL, TG = self.S, self.SL, self.TG
        NT = TG // 512
        w_in = self.w_in[l].rearrange("(k p) c -> p k c", p=128)
        for tg in range(self.NTG):
            t0 = tg * TG
            with self.scope() as es:
                xt = self.sb("xt", [128, 16, TG], BF16)
                for k in range(16):
                    S.dma('sp', xt.t[:, k, :], self.xT[k * 128:(k + 1) * 128, t0:t0 + TG], reads=([('D', 'xT0', k, tg)] if l == 0 else [('D', 'xT1', c_) for c_ in range(t0 // 128, (t0 + TG) // 128)]), writes=['xt'])
                rD = self.sb("rD", [128, 2, TG], BF16)
                rM = self.sb("rM", [64, 2, TG], BF16)
                for cs in range(2):
                    S.dma('sp', rD.t[:, cs, :], self.ropeD[cs, :, t0:t0 + TG], reads=[('D', 'rope', 0, cs, t0)], writes=['rD'])
                    S.dma('sp', rM.t[:, cs, :], self.ropeM[cs, :, t0:t0 + TG], reads=[('D', 'rope', 1, cs, t0)], writes=['rM'])
                pp = self.ring("pp", 4, [128, 512], F32, psum=True)
                pr = self.ring("prr", 2, [128, 512], F32, psum=True)
                ptr = self.ring("ptr", 2, [128, 1024], BF16, psum=True)
                ob = self.ring("ob", 3, [128, TG], BF16)
                vtb = self.ring("vtb", 2, [128, TG // 128, 128], BF16)
                tmp = self.ring("ptmp", 3, [128, 512], F32)
                cqg = self.sb("cqg", [128, 4, TG], BF16)
                sqq = self.sb("sqq", [128, 4, TG], BF16)
                rq = self.sb("rq", [128, TG], F32)
                gq = self.sb("gq", [128, 4], F32)
                gkv = self.sb("gkv", [128, 2], F32)
                fb = self.sb("fb", [4, 1], F32)
                S.dma('sp', gq.t[:], self.mla_q_norm[l], writes=['gq'])
                S.dma('sp', gkv.t[:], self.mla_kv_norm[l], writes=['gkv'])
                S.dma('sp', fb.t[:], self.fox_f_bias[l], writes=['fb'])
                eng_alt = [0]

                def evac_copy(dst_ap, src_ap, r, w, func=AF.Copy):
                    if func == AF.Copy and eng_alt[0] % 2 == 0:
                        S.op('dve', lambda e: e.tensor_copy(out=dst_ap, in_=src_ap), reads=r, writes=w)
                    else:
                        S.op('act', lambda e: e.activation(out=dst_ap, in_=src_ap, func=func), reads=r, writes=w)
                    eng_alt[0] += 1

                def project(wb, nk, M, rhs_t, rhs_key):
                    outs = []
                    for n in range(NT):
                        p = pp.get()
                        for k in range(nk):
                            S.op('pe', lambda e: e.matmul(p.t[0:M, :], lhsT=wb.t[:, k * M:(k + 1) * M],
                                                          rhs=rhs_t[:, k, n * 512:(n + 1) * 512], start=(k == 0), stop=(k == nk - 1)),
                                 reads=[wb.k, rhs_key], writes=[p.k])
                        outs.append(p)
                    return outs

                def rope_store(o, M, rtab, rmat_c, dst_ap, dkey):
                    o2 = ob.get()
                    for n in range(NT):
                        sl = slice(n * 512, (n + 1) * 512)
                        p = pr.get()
                        S.op('pe', lambda e: e.matmul(p.t[0:M, :], lhsT=self.cbs(rmat_c, M, M), rhs=o.t[0:M, sl], start=True, stop=True),
                             reads=[o.k, 'cb'], writes=[p.k])
                        t1 = tmp.get()
                        S.op('dve', lambda e: e.tensor_tensor(out=t1.t[0:M, :], in0=o.t[0:M, sl], in1=rtab.t[0:M, 0, sl], op=ALU.mult),
                             reads=[o.k, rtab.k], writes=[t1.k])
                        t2 = tmp.get()
                        S.op('dve', lambda e: e.tensor_tensor(out=t2.t[0:M, :], in0=p.t[0:M, :], in1=rtab.t[0:M, 1, sl], op=ALU.mult),
                             reads=[p.k, rtab.k], writes=[t2.k])
                        S.op('dve', lambda e: e.tensor_tensor(out=o2.t[0:M, sl], in0=t1.t[0:M, :], in1=t2.t[0:M, :], op=ALU.add),
                             reads=[t1.k, t2.k], writes=[o2.k])
                    S.dma('sp', dst_ap, o2.t[0:M, :], reads=[o2.k], writes=[dkey])

                def v_store(o, idx):
                    vt = vtb.get()
                    for c8 in range(0, TG // 128, 8):
                        p = ptr.get()
                        nn = min(8, TG // 128 - c8)
                        for j in range(nn):
                            c = c8 + j
                            S.op('pe', lambda e: e.transpose(p.t[:, j * 128:(j + 1) * 128], o.t[:, c * 128:(c + 1) * 128], self.cbs(C_IDENT, 128)),
                                 reads=[o.k, 'cb'], writes=[p.k])
                        evac_copy(vt.t[:, c8:c8 + nn, :], p.t[:, 0:nn * 128].rearrange("p (c d) -> p c d", c=nn), [p.k], [vt.k])
                    S.dma('sp', self.vv[idx, t0:t0 + TG, :].rearrange("(c p) d -> p c d", p=128), vt.t[:], reads=[vt.k],
                          writes=[('D', 'vv', idx, tg)])

                import os
                fams = os.environ.get("PROJ_FAMS")

                def do_tile(fam, c0, M, info, wb=None):
                    if fams and fam not in fams.split(','):
                        return
                    if wb is None:
                        wb = self.wload(w_in[:, :, c0:c0 + M], 16, M)
                    outs = project(wb, 16, M, xt.t, 'xt')
                    if fam in ('plain', 'gate', 'v', 'rope_d', 'kr'):
                        o = ob.get()
                        for n, p in enumerate(outs):
                            evac_copy(o.t[0:M, n * 512:(n + 1) * 512], p.t[0:M, :], [p.k], [o.k],
                                      func=(AF.Sigmoid if fam == 'gate' else AF.Copy))
                        if fam == 'plain':
                            dst = self.qT if info[0] == 'qT' else self.kT
                            S.dma('sp', dst[info[1], :, t0:t0 + TG], o.t[:], reads=[o.k], writes=[('D', info[0], info[1], tg)])
                        elif fam == 'gate':
                            S.dma('sp', self.gatesT[info * 128:(info + 1) * 128, t0:t0 + TG], o.t[:], reads=[o.k],
                                  writes=[('D', 'gatesT', info, tg)])
                        elif fam == 'v':
                            v_store(o, info)
                        elif fam == 'rope_d':
                            dst = self.qT if info[0] == 'qT' else self.kT
                            rope_store(o, 128, rD, C_RD, dst[info[1], :, t0:t0 + TG], ('D', info[0], info[1], tg))
                        elif fam == 'kr':
                            rope_store(o, 64, rM, C_RM, self.krT[:, t0:t0 + TG], ('D', 'krT', tg))
                    elif fam in ('cq', 'ckv'):
                        gg = gq if fam == 'cq' else gkv
                        for n, p in enumerate(outs):
                            sl = slice(n * 512, (n + 1) * 512)
                            tq = tmp.get()
                            S.op('act', lambda e: e.activation(out=tq.t[:], in_=p.t[:], func=AF.Copy), reads=[p.k], writes=[tq.k])
                            S.op('dve', lambda e: e.tensor_tensor(out=sqq.t[:, info, sl], in0=tq.t[:], in1=tq.t[:], op=ALU.mult), reads=[tq.k], writes=['sqq'])
                            S.op('dve', lambda e: e.tensor_scalar(out=cqg.t[:, info, sl], in0=tq.t[:], scalar1=gg.t[:, info:info + 1], scalar2=None,
                                                                  op0=ALU.mult), reads=[tq.k, gg.k], writes=['cqg'])
                    elif fam == 'f':
                        for n, p in enumerate(outs):
                            ft = tmp.get()
                            S.op('dve', lambda e: e.tensor_scalar(out=ft.t[0:4, :], in0=p.t[0:4, :], scalar1=fb.t[0:4, 0:1], scalar2=None, op0=ALU.add),
                                 reads=[p.k, 'fb'], writes=[ft.k])
                            S.dma('sp', self.fT[:, t0 + n * 512:t0 + (n + 1) * 512], ft.t[0:4, :], reads=[ft.k], writes=[('D', 'fT', tg, n)])

                def rms_scale(nk, dim):
                    for n in range(NT):
                        sl = slice(n * 512, (n + 1) * 512)
                        p = pp.get()
                        for k in range(nk):
                            S.op('pe', lambda e: e.matmul(p.t[:], lhsT=self.cbs(C_ONES, 128), rhs=sqq.t[:, k, sl], start=(k == 0), stop=(k == nk - 1)),
                                 reads=['sqq', 'cb'], writes=[p.k])
                        S.op('act', lambda e: e.activation(out=rq.t[:, sl], in_=p.t[:], func=AF.Sqrt, bias=self.epsrms.t[:, 0:1], scale=1.0 / dim),
                             reads=[p.k, 'epsrms'], writes=['rq'])
                        S.op('dve', lambda e: e.reciprocal(out=rq.t[:, sl], in_=rq.t[:, sl]), reads=['rq'], writes=['rq'])

                def scaled(outs, M):
                    o = ob.get()
                    for n, p in enumerate(outs):
                        sl = slice(n * 512, (n + 1) * 512)
                        S.op('dve', lambda e: e.tensor_tensor(out=o.t[0:M, sl], in0=p.t[0:M, :], in1=rq.t[0:M, sl], op=ALU.mult),
                             reads=[p.k, 'rq'], writes=[o.k])
                    return o
                wuq = self.mla_w_uq[l].rearrange("(k p) c -> p k c", p=128)
                wukv = self.mla_w_ukv[l].rearrange("(k p) c -> p k c", p=128)
                mla_on = os.environ.get("PROJ_MLA", "1") == "1"
                for h in range(4 if mla_on else 0):
                    do_tile('cq', OFF['mla_cq'] + h * 128, 128, h)
                if mla_on:
                    rms_scale(4, 512)
                for h in range(4 if mla_on else 0):
                    wb = self.wload(wuq[:, :, h * 192:h * 192 + 128], 4, 128)
                    o = scaled(project(wb, 4, 128, cqg.t, 'cqg'), 128)
                    S.dma('sp', self.qT[8 + h, :, t0:t0 + TG], o.t[:], reads=[o.k], writes=[('D', 'qT', 8 + h, tg)])
                    wb = self.wload(wuq[:, :, h * 192 + 128:h * 192 + 192], 4, 64)
                    o = scaled(project(wb, 4, 64, cqg.t, 'cqg'), 64)
                    rope_store(o, 64, rM, C_RM, self.qrT[h, :, t0:t0 + TG], ('D', 'qrT', h, tg))
                for j in range(2 if mla_on else 0):
                    do_tile('ckv', OFF['mla_ckv'] + j * 128, 128, j)
                if mla_on:
                    rms_scale(2, 256)
                for h in range(4 if mla_on else 0):
                    wb = self.wload(wukv[:, :, h * 256:h * 256 + 128], 2, 128)
                    o = scaled(project(wb, 2, 128, cqg.t, 'cqg'), 128)
                    S.dma('sp', self.kT[8 + h, :, t0:t0 + TG], o.t[:], reads=[o.k], writes=[('D', 'kT', 8 + h, tg)])
                    wb = self.wload(wukv[:, :, h * 256 + 128:h * 256 + 256], 2, 128)
                    o = scaled(project(wb, 2, 128, cqg.t, 'cqg'), 128)
                    v_store(o, 8 + h)
                do_tile('kr', OFF['mla_kr'], 64, 0)
                do_tile('f', OFF['fox_f'], 4, 0)
                lst = []
                for h in range(4):
                    lst.append(('rope_d', OFF['diff_q'] + h * 128, 128, ('qT', 0 + h)))
                    lst.append(('rope_d', OFF['diff_k'] + h * 128, 128, ('kT', 0 + h)))
                    lst.append(('v', OFF['diff_v'] + h * 128, 128, 0 + h))
                    lst.append(('plain', OFF['fox_q'] + h * 128, 128, ('qT', 4 + h)))
                    lst.append(('plain', OFF['fox_k'] + h * 128, 128, ('kT', 4 + h)))
                    lst.append(('v', OFF['fox_v'] + h * 128, 128, 4 + h))
                    lst.append(('plain', OFF['sb_q'] + h * 128, 128, ('qT', 12 + h)))
                    lst.append(('plain', OFF['sb_k'] + h * 128, 128, ('kT', 12 + h)))
                    lst.append(('v', OFF['sb_v'] + h * 128, 128, 12 + h))
                for g in range(64):
                    lst.append(('gate', OFF['gates'] + g * 128, 128, g))
                if fams:
                    for it in lst:
                        do_tile(*it)
                else:
                    nxt_wb = self.wload(w_in[:, :, lst[0][1]:lst[0][1] + 128], 16, 128)
                    for ii, it in enumerate(lst):
                        wb_c = nxt_wb
                        if ii + 1 < len(lst):
                            c0n = lst[ii + 1][1]
                            nxt_wb = self.wload(w_in[:, :, c0n:c0n + 128], 16, 128)
                        do_tile(*it, wb=wb_c)

    def phase_foxprep(self, l):
        S, SL = self.S, self.SL
        with self.scope() as es:
            z = self.sb("fz", [4, SL], F32)
            a = self.sb("fa", [4, SL], F32)
            b = self.sb("fb2", [4, SL], F32)
            hb = [self.sb("fh%d" % i, [4, SL], BF16) for i in range(3)]
            rk = [('D', 'fT', tg, n) for tg in range(self.NTG) for n in range(self.TG // 512)]
            S.dma('sp', z.t[:], self.fT[:, :], reads=rk, writes=['fz'])
            S.op('act', lambda e: e.activation(out=a.t[:], in_=z.t[:], func=AF.Exp, scale=-1.0), reads=['fz'], writes=['fa'])
            S.op('act', lambda e: e.activation(out=a.t[:], in_=a.t[:], func=AF.Ln, bias=1.0), reads=['fa'], writes=['fa'])
            S.op('dve', lambda e: e.tensor_scalar(out=a.t[:], in0=a.t[:], scalar1=-1.0, scalar2=None, op0=ALU.mult), reads=['fa'], writes=['fa'])
            S.op('dve', lambda e: e.memset(b.t[:], 1.0), writes=['fb2'])
            S.op('dve', lambda e: e.tensor_tensor_scan(out=z.t[:], data0=b.t[:], data1=a.t[:], initial=0.0, op0=ALU.mult, op1=ALU.add),
                 reads=['fa', 'fb2'], writes=['fz'])
            S.dma('sp', self.cumT[:, :], z.t[:], reads=['fz'], writes=[('D', 'cumT')])
            S.op('dve', lambda e: e.tensor_scalar(out=a.t[:], in0=z.t[:], scalar1=math.sqrt(128.0), scalar2=None, op0=ALU.mult), reads=['fz'], writes=['fa'])
            for i in range(3):
                S.op('dve', lambda e: e.tensor_copy(out=hb[i].t[:], in_=a.t[:]), reads=['fa'], writes=[hb[i].k])
                if i < 2:
                    S.op('dve', lambda e: e.tensor_tensor(out=a.t[:], in0=a.t[:], in1=hb[i].t[:], op=ALU.subtract), reads=['fa', hb[i].k], writes=['fa'])
                for h in range(4):
                    S.dma('sp', self.qaug[h, i:i + 1, :], hb[i].t[h:h + 1, :], reads=[hb[i].k], writes=[('D', 'qaug', h, i)])

    def phase_attn(self, l):
        S, SL, NCH, NQT = self.S, self.SL, self.NCH, self.NQT
        NTG = self.NTG
        with self.scope() as es:
            KT = self.ring("KT", 2, [128, SL], BF16)
            QT = self.ring("QT", 2, [128, SL], BF16)
            VV = self.ring("VV", 2, [128, NCH, 128], BF16)
            OT = self.ring("OT", 2, [128, SL], BF16)
            KR = self.sb("KR", [64, SL], BF16)
            QR = self.ring("QR", 2, [64, SL], BF16)
            QA = self.ring("QA", 2, [3, SL], BF16)
            NCUM = self.ring("NCUM", 2, [128, NCH], F32)
            sps = self.ring("sps", 4, [128, 512], F32, psum=True)
            ops_ = self.ring("ops", 2, [128, 512], F32, psum=True)
            dps = self.ring("dps", 2, [128, 512], F32, psum=True)
            lps = sps
            PT = self.ring("PT", 6, [128, 512], BF16)
            rec = self.ring("rec", 2, [128, 512], F32)
            o0 = self.ring("o0", 2, [128, 512], F32)
            o1 = self.ring("o1", 2, [128, 512], F32)
            f32t = self.ring("f32t", 4, [128, 512], F32)
            sqb = self.ring("sqb", 2, [128, 512], BF16)
            lp = self.sb("lp", [128, 256], F32)
            lpp = self.sb("lpp", [128, 128], F32)
            lam2 = self.sb("lam2", [128, 2], F32)
            neglam = self.sb("neglam", [128, 1], F32)
            gsub = self.sb("gsub", [128, 1], F32)
            lam_init = 0.8 - 0.6 * math.exp(-0.3 * l)
            S.dma('sp', lp.t[:], self.diff_lambda[l, 0:1, :].partition_broadcast(128), writes=['lp'])
            S.dma('sp', gsub.t[:], self.diff_subln[l], writes=['gsub'])
            S.op('dve', lambda e: e.tensor_scalar(out=gsub.t[:], in0=gsub.t[:], scalar1=(1.0 - lam_init), scalar2=None, op0=ALU.mult), reads=['gsub'], writes=['gsub'])
            lp4 = lp.t[:].rearrange("p (a d) -> p a d", a=4)
            lpp2 = lpp.t[:].rearrange("p (a d) -> p a d", a=2)
            S.op('dve', lambda e: e.tensor_tensor(out=lpp2[:, 0, :], in0=lp4[:, 0, :], in1=lp4[:, 1, :], op=ALU.mult), reads=['lp'], writes=['lpp'])
            S.op('dve', lambda e: e.tensor_tensor(out=lpp2[:, 1, :], in0=lp4[:, 2, :], in1=lp4[:, 3, :], op=ALU.mult), reads=['lp'], writes=['lpp'])
            S.op('dve', lambda e: e.reduce_sum(out=lam2.t[:], in_=lpp2, axis=mybir.AxisListType.X), reads=['lpp'], writes=['lam2'])
            S.op('act', lambda e: e.activation(out=lam2.t[:], in_=lam2.t[:], func=AF.Exp), reads=['lam2'], writes=['lam2'])
            S.op('dve', lambda e: e.tensor_tensor(out=neglam.t[:], in0=lam2.t[:, 1:2], in1=lam2.t[:, 0:1], op=ALU.subtract), reads=['lam2'], writes=['neglam'])
            S.op('dve', lambda e: e.tensor_scalar(out=neglam.t[:], in0=neglam.t[:], scalar1=-lam_init, scalar2=None, op0=ALU.add), reads=['neglam'], writes=['neglam'])

            def dkeys(name, idx):
                return [('D', name, idx, tg) for tg in range(NTG)]

            def load_head(idx, need_v=True):
                kt = KT.get()
                qt = QT.get()
                S.dma('sp', kt.t[:], self.kT[idx], reads=dkeys('kT', idx), writes=[kt.k])
                S.dma('sp', qt.t[:], self.qT[idx], reads=dkeys('qT', idx), writes=[qt.k])
                vt = VV.get()
                vsrc = self.vv[idx].rearrange("(c p) d -> p c d", p=128)
                for c0 in range(0, NCH, 8):
                    S.dma('sp', vt.t[:, c0:min(NCH, c0 + 8), :], vsrc[:, c0:min(NCH, c0 + 8), :], reads=dkeys('vv', idx), writes=[(vt.k, c0 // 8)])
                return kt, qt, vt

            def drive(streams):
                live = list(streams)
                while live:
                    nxt = []
                    for g_ in live:
                        try:
                            next(g_)
                            nxt.append(g_)
                        except StopIteration:
                            pass
                    live = nxt

            def softmax_tile(i, parts, scale, madd_c, vt, O, Dn, bias_t=None, aug=None):
                last = 4 * i + 3

                def emit_qk(g):
                    sp_ = sps.get()
                    j0 = g - 4 * i
                    nmm = len(parts) + (1 if aug is not None else 0) + (1 if j0 >= 0 else 0)
                    c = 0
                    for (kt, qt, p0, p1) in parts:
                        S.op('pe', lambda e: e.matmul(sp_.t[:], lhsT=kt.t[p0:p1, g * 128:(g + 1) * 128], rhs=qt.t[p0:p1, i * 512:(i + 1) * 512],
                                                      start=(c == 0), stop=(c == nmm - 1)), reads=[kt.k, qt.k], writes=[sp_.k])
                        c += 1
                    if aug is not None:
                        S.op('pe', lambda e: e.matmul(sp_.t[:], lhsT=self.cb.t[0:3, C_ONES:C_ONES + 128], rhs=aug.t[0:3, i * 512:(i + 1) * 512],
                                                      start=False, stop=(c == nmm - 1)), reads=[aug.k, 'cb'], writes=[sp_.k])
                        c += 1
                    if j0 >= 0:
                        S.op('pe', lambda e: e.matmul(sp_.t[:], lhsT=self.cbs(C_IDENT, 128), rhs=self.cbs(madd_c + j0 * 512, 512),
                                                      start=False, stop=True), reads=['cb'], writes=[sp_.k])
                    return sp_
                nxt = emit_qk(0)
                for g in range(last + 1):
                    sp_ = nxt
                    if g < last:
                        nxt = emit_qk(g + 1)
                    pt = PT.get()
                    if bias_t is not None:
                        S.op('act', lambda e: e.activation(out=pt.t[:], in_=sp_.t[:], func=AF.Exp, scale=scale, bias=bias_t.t[:, g:g + 1]),
                             reads=[sp_.k, bias_t.k], writes=[pt.k])
                    else:
                        S.op('act', lambda e: e.activation(out=pt.t[:], in_=sp_.t[:], func=AF.Exp, scale=scale), reads=[sp_.k], writes=[pt.k])
                    S.op('pe', lambda e: e.matmul(O.t[:], lhsT=vt.t[:, g, :], rhs=pt.t[:], start=(g == 0), stop=(g == last)),
                         reads=[(vt.k, g // 8), pt.k], writes=[O.k])
                    S.op('pe', lambda e: e.matmul(Dn.t[:], lhsT=self.cbs(C_ONES, 128), rhs=pt.t[:], start=(g == 0), stop=(g == last)),
                         reads=['cb', pt.k], writes=[Dn.k])
                    yield

            def normalize(O, Dn, dst_ap, wkey):
                r = rec.get()
                S.op('dve', lambda e: e.reciprocal(out=r.t[:], in_=Dn.t[:]), reads=[Dn.k], writes=[r.k])
                S.op('dve', lambda e: e.tensor_tensor(out=dst_ap, in0=O.t[:], in1=r.t[:], op=ALU.mult), reads=[O.k, r.k], writes=[wkey])

            for h in range(4):
                idx = h
                kt, qt, vt = load_head(idx)
                ot = OT.get()
                for i in range(NQT):
                    accs = [(ops_.get(), dps.get()) for c in range(2)]
                    drive([softmax_tile(i, [(kt, qt, c * 64, (c + 1) * 64)], 64 ** -0.5, C_MADD_CH, vt, accs[c][0], accs[c][1]) for c in range(2)])
                    oc = []
                    for c in range(2):
                        dst = (o0 if c == 0 else o1).get()
                        normalize(accs[c][0], accs[c][1], dst.t[:], dst.k)
                        oc.append(dst)
                    d = f32t.get()
                    S.op('dve', lambda e: e.scalar_tensor_tensor(out=d.t[:], in0=oc[1].t[:], scalar=neglam.t[:, 0:1], in1=oc[0].t[:],
                                                                 op0=ALU.mult, op1=ALU.add), reads=[oc[0].k, oc[1].k, 'neglam'], writes=[d.k])
                    sq = sqb.get()
                    S.op('pool', lambda e: e.tensor_tensor(out=sq.t[:], in0=d.t[:], in1=d.t[:], op=ALU.mult), reads=[d.k], writes=[sq.k])
                    nps = lps.get()
                    S.op('pe', lambda e: e.matmul(nps.t[:], lhsT=self.cbs(C_ONES, 128), rhs=sq.t[:], start=True, stop=True), reads=[sq.k, 'cb'], writes=[nps.k])
                    rt = f32t.get()
                    S.op('act', lambda e: e.activation(out=rt.t[:], in_=nps.t[:], func=AF.Sqrt, bias=self.epsrms.t[:, 0:1], scale=1.0 / 128),
                         reads=[nps.k, 'epsrms'], writes=[rt.k])
                    S.op('dve', lambda e: e.reciprocal(out=rt.t[:], in_=rt.t[:]), reads=[rt.k], writes=[rt.k])
                    S.op('dve', lambda e: e.scalar_tensor_tensor(out=ot.t[:, i * 512:(i + 1) * 512], in0=d.t[:], scalar=gsub.t[:, 0:1], in1=rt.t[:],
                                                                 op0=ALU.mult, op1=ALU.mult), reads=[d.k, rt.k, 'gsub'], writes=[ot.k])
                S.dma('sp', self.oT[idx], ot.t[:], reads=[ot.k], writes=[('D', 'oT', idx)])

            def head_stream(idx, parts_fn, scale, madd_c, bias_t=None, aug=None):
                kt, qt, vt = load_head(idx)
                parts = parts_fn(kt, qt)
                ot = OT.get()
                for i in range(NQT):
                    O = ops_.get()
                    Dn = dps.get()
                    for _ in softmax_tile(i, parts, scale, madd_c, vt, O, Dn, bias_t=bias_t, aug=aug):
                        yield
                    normalize(O, Dn, ot.t[:, i * 512:(i + 1) * 512], ot.k)
                S.dma('sp', self.oT[idx], ot.t[:], reads=[ot.k], writes=[('D', 'oT', idx)])

            def fox_stream(h):
                qa = QA.get()
                S.dma('sp', qa.t[:], self.qaug[h], reads=[('D', 'qaug', h, i) for i in range(3)], writes=[qa.k])
                ncum = NCUM.get()
                with self.nc.allow_non_contiguous_dma(reason="tiny strided column load"):
                    csrc = self.cumT[h].rearrange("(c p) -> p c", p=128)
                    for c0 in range(0, NCH, 8):
                        S.dma('sp', ncum.t[:, c0:min(NCH, c0 + 8)], csrc[:, c0:min(NCH, c0 + 8)], reads=[('D', 'cumT')], writes=[(ncum.k, 'ld', c0 // 8), ncum.k])
                S.op('dve', lambda e: e.tensor_scalar(out=ncum.t[:], in0=ncum.t[:], scalar1=-1.0, scalar2=None, op0=ALU.mult),
                     reads=[(ncum.k, 'ld', j) for j in range((NCH + 7) // 8)], writes=[ncum.k])
                return head_stream(4 + h, lambda kt, qt: [(kt, qt, 0, 128)], 128 ** -0.5, C_MADD_CA, bias_t=ncum, aug=qa)
            for h0 in (0, 2):
                drive([fox_stream(h0), fox_stream(h0 + 1)])

            S.dma('sp', KR.t[:], self.krT[:, :], reads=[('D', 'krT', tg) for tg in range(NTG)], writes=['KR'])

            def mla_stream(h):
                qr = QR.get()
                S.dma('sp', qr.t[:], self.qrT[h], reads=dkeys('qrT', h), writes=[qr.k])
                return head_stream(8 + h, lambda kt, qt: [(kt, qt, 0, 128), (KR, qr, 0, 64)], 192 ** -0.5, C_MADD_CH)
            for h0 in (0, 2):
                drive([mla_stream(h0), mla_stream(h0 + 1)])

        with self.scope() as es:
            KT = self.ring("KT", 2, [128, SL], BF16)
            QT = self.ring("QT", 2, [128, SL], BF16)
            VV = self.ring("VV", 2, [128, NCH, 128], BF16)
            OT = self.ring("OT", 2, [128, SL], BF16)
            sps = self.ring("sps", 4, [128, 512], F32, psum=True)
            ops_ = self.ring("ops", 2, [128, 512], F32, psum=True)
            lps = self.ring("lps", 2, [128, 512], F32, psum=True)
            PT = self.ring("PT", 8, [128, 512], BF16)
            f32t = self.ring("f32t", 10, [128, 512], F32)
            sp32 = self.ring("sp32", 6, [128, 512], F32)
            spb = self.ring("spb", 6, [128, 512], BF16)
            sc = 128 ** -0.5

            def sb_stream(h, cast_eng):
                idx = 12 + h
                kt = KT.get()
                qt = QT.get()
                S.dma('sp', kt.t[:], self.kT[idx], reads=dkeys('kT', idx), writes=[kt.k])
                S.dma('sp', qt.t[:], self.qT[idx], reads=dkeys('qT', idx), writes=[qt.k])
                vt = VV.get()
                vsrc = self.vv[idx].rearrange("(c p) d -> p c d", p=128)
                for c0 in range(0, NCH, 8):
                    S.dma('sp', vt.t[:, c0:min(NCH, c0 + 8), :], vsrc[:, c0:min(NCH, c0 + 8), :], reads=dkeys('vv', idx), writes=[(vt.k, c0 // 8)])
                ot = OT.get()
                spsum = self.sb("spsum%d" % h, [128, 512], F32)
                spsumb = self.ring("spsumb%d" % h, 3, [128, 512], BF16)
                for i in range(NQT):
                    O = ops_.get()
                    last = 4 * i + 3
                    order = list(range(last, -1, -1))
                    n = len(order)
                    st1 = {}
                    st2 = {}
                    ssb_prev = [None]

                    def stage1(g):
                        j0 = g - 4 * i
                        z = sps.get()
                        S.op('pe', lambda e: e.matmul(z.t[:], lhsT=kt.t[:, g * 128:(g + 1) * 128], rhs=qt.t[:, i * 512:(i + 1) * 512], start=True, stop=True),
                             reads=[kt.k, qt.k], writes=[z.k])
                        ee = f32t.get()
                        S.op('act', lambda e: e.activation(out=ee.t[:], in_=z.t[:], func=AF.Exp, scale=sc), reads=[z.k], writes=[ee.k])
                        s32 = sp32.get()
                        S.op('act', lambda e: e.activation(out=s32.t[:], in_=ee.t[:], func=AF.Ln, bias=1.0), reads=[ee.k], writes=[s32.k])
                        sb_ = spb.get()
                        if j0 >= 0:
                            S.op('pool', lambda e: e.tensor_tensor(out=sb_.t[:], in0=s32.t[:], in1=self.cbs(C_MMUL + j0 * 512, 512), op=ALU.mult),
                                 reads=[s32.k, 'cb'], writes=[sb_.k])
                        else:
                            S.op('dve', lambda e: e.tensor_copy(out=sb_.t[:], in_=s32.t[:]), reads=[s32.k], writes=[sb_.k])
                        st1[g] = (z, s32, sb_)

                    def stage2(g):
                        j0 = g - 4 * i
                        z, s32, sb_ = st1.pop(g)
                        first = (g == last)
                        Lp = lps.get()
                        S.op('pe', lambda e: e.matmul(Lp.t[:], lhsT=self.cbs(C_TRIS, 128), rhs=sb_.t[:], start=True, stop=first), reads=[sb_.k, 'cb'], writes=[Lp.k])
                        if not first:
                            ssb = ssb_prev[0]
                            S.op('pe', lambda e: e.matmul(Lp.t[:], lhsT=self.cbs(C_ONES, 128), rhs=ssb.t[:], start=False, stop=True), reads=[ssb.k, 'cb'], writes=[Lp.k])
                        t1 = f32t.get()
                        S.op('dve', lambda e: e.scalar_tensor_tensor(out=t1.t[:], in0=z.t[:], scalar=sc, in1=s32.t[:], op0=ALU.mult, op1=ALU.subtract),
                             reads=[z.k, s32.k], writes=[t1.k])
                        S.op('dve', lambda e: e.tensor_tensor(out=t1.t[:], in0=t1.t[:], in1=Lp.t[:], op=ALU.subtract), reads=[t1.k, Lp.k], writes=[t1.k])
                        wt = PT.get()
                        S.op('act', lambda e: e.activation(out=wt.t[:], in_=t1.t[:], func=AF.Exp), reads=[t1.k], writes=[wt.k])
                        if j0 >= 0:
                            S.op('pool', lambda e: e.tensor_tensor(out=wt.t[:], in0=wt.t[:], in1=self.cbs(C_MMUL + j0 * 512, 512), op=ALU.mult),
                                 reads=[wt.k, 'cb'], writes=[wt.k])
                        if g > 0:
                            if first:
                                S.op('pool', lambda e: e.tensor_copy(out=spsum.t[:], in_=sb_.t[:]), reads=[sb_.k], writes=[spsum.k])
                            else:
                                S.op('dve', lambda e: e.tensor_tensor(out=spsum.t[:], in0=spsum.t[:], in1=sb_.t[:], op=ALU.add), reads=[spsum.k, sb_.k], writes=[spsum.k])
                            ssb = spsumb.get()
                            S.op('act', lambda e: e.activation(out=ssb.t[:], in_=spsum.t[:], func=AF.Copy), reads=[spsum.k], writes=[ssb.k])
                            ssb_prev[0] = ssb
                        st2[g] = wt

                    def stage3(g):
                        wt = st2.pop(g)
                        S.op('pe', lambda e: e.matmul(O.t[:], lhsT=vt.t[:, g, :], rhs=wt.t[:], start=(g == last), stop=(g == 0)), reads=[(vt.k, g // 8), wt.k], writes=[O.k])
                    for step in range(n + 2):
                        if step < n:
                            stage1(order[step])
                        if 0 <= step - 1 < n:
                            stage2(order[step - 1])
                        if 0 <= step - 2 < n:
                            stage3(order[step - 2])
                        yield
                    S.op('act', lambda e: e.activation(out=ot.t[:, i * 512:(i + 1) * 512], in_=O.t[:], func=AF.Copy), reads=[O.k], writes=[ot.k])
                S.dma('sp', self.oT[idx], ot.t[:], reads=[ot.k], writes=[('D', 'oT', idx)])
            for h0 in (0, 2):
                drive([sb_stream(h0, 'dve'), sb_stream(h0 + 1, 'dve')])

    def phase_merge(self, l):
        S, SL, TG = self.S, self.SL, self.TG
        NT = TG // 512
        for tg in range(self.NTG):
            t0 = tg * TG
            with self.scope() as es:
                ot = self.sb("mot", [128, 16, TG], BF16)
                for i in range(16):
                    S.dma('sp', ot.t[:, i, :], self.oT[i, :, t0:t0 + TG], reads=[('D', 'oT', i)], writes=['mot'])
                pp = self.ring("mpp", 4, [128, 512], F32, psum=True)
                gt = self.ring("mgt", 3, [128, TG], BF16)
                acc = self.sb("macc", [128, TG], F32)
                mo = self.ring("mmo", 2, [128, TG], BF16)
                tmp = self.ring("mtmp", 2, [128, 512], F32)
                def fetch(c, n_):
                    wb_ = self.wload(self.w_branch[l, n_].rearrange("(k p) c -> p k c", p=128)[:, :, c * 128:(c + 1) * 128], 4, 128)
                    g_ = gt.get()
                    grow = n_ * 16 + c
                    S.dma('sp', g_.t[:], self.gatesT[grow * 128:(grow + 1) * 128, t0:t0 + TG], reads=[('D', 'gatesT', grow, tg)], writes=[g_.k])
                    return wb_, g_
                nxt_f = fetch(0, 0)
                for c in range(16):
                    for n_ in range(4):
                        wb, g = nxt_f
                        if not (c == 15 and n_ == 3):
                            nxt_f = fetch(c + (n_ + 1) // 4, (n_ + 1) % 4)
                        wv = wb.t[:, 0:512].rearrange("p (k m) -> p k m", k=4)
                        for n in range(NT):
                            sl = slice(n * 512, (n + 1) * 512)
                            p = pp.get()
                            for k in range(4):
                                S.op('pe', lambda e: e.matmul(p.t[:], lhsT=wv[:, k, :], rhs=ot.t[:, n_ * 4 + k, sl], start=(k == 0), stop=(k == 3)),
                                     reads=[wb.k, 'mot'], writes=[p.k])
                            if n_ == 0:
                                S.op('dve', lambda e: e.tensor_tensor(out=acc.t[:, sl], in0=p.t[:], in1=g.t[:, sl], op=ALU.mult), reads=[p.k, g.k], writes=['macc'])
                            else:
                                t = tmp.get()
                                S.op('dve', lambda e: e.tensor_tensor(out=t.t[:], in0=p.t[:], in1=g.t[:, sl], op=ALU.mult), reads=[p.k, g.k], writes=[t.k])
                                S.op('pool', lambda e: e.tensor_tensor(out=acc.t[:, sl], in0=acc.t[:, sl], in1=t.t[:], op=ALU.add), reads=['macc', t.k], writes=['macc'])
                    m = mo.get()
                    S.op('act', lambda e: e.activation(out=m.t[:], in_=acc.t[:], func=AF.Copy), reads=['macc'], writes=[m.k])
                    S.dma('sp', self.mergedT[c * 128:(c + 1) * 128, t0:t0 + TG], m.t[:], reads=[m.k], writes=[('D', 'mergedT', c, tg)])

    def load_resident(self, dst, w_ap, nk):
        S = self.S
        wv = w_ap.rearrange("(k p) c -> p k c", p=128)
        for k in range(nk):
            st = self.wst.get()
            S.dma('sp', st.t[:], wv[:, k, :], writes=[st.k])
            self.cast(dst.t[:, k, :], st.t[:], [st.k], [dst.k])

    def layer_norm(self, v, stt, mvt, g_t, b_t, out):
        S = self.S
        for j in range(4):
            S.op('dve', lambda e: e.bn_stats(out=stt.t[:, j * 6:(j + 1) * 6], in_=v.t[:, j * 512:(j + 1) * 512]), reads=[v.k], writes=[stt.k])
        S.op('dve', lambda e: e.bn_aggr(out=mvt.t[:, 0:2], in_=stt.t[:]), reads=[stt.k], writes=[mvt.k])
        S.op('act', lambda e: e.activation(out=mvt.t[:, 2:3], in_=mvt.t[:, 1:2], func=AF.Sqrt, bias=self.epsln.t[:, 0:1], scale=1.0),
             reads=[mvt.k, 'epsln'], writes=[mvt.k])
        S.op('dve', lambda e: e.reciprocal(out=mvt.t[:, 3:4], in_=mvt.t[:, 2:3]), reads=[mvt.k], writes=[mvt.k])
        S.op('dve', lambda e: e.tensor_scalar(out=out.t[:], in0=v.t[:], scalar1=mvt.t[:, 0:1], scalar2=mvt.t[:, 3:4], op0=ALU.subtract, op1=ALU.mult),
             reads=[v.k, mvt.k], writes=[out.k])
        S.op('pool', lambda e: e.tensor_tensor(out=out.t[:], in0=out.t[:], in1=g_t.t[:], op=ALU.mult), reads=[out.k, g_t.k], writes=[out.k])
        S.op('dve', lambda e: e.tensor_tensor(out=out.t[:], in0=out.t[:], in1=b_t.t[:], op=ALU.add), reads=[out.k, b_t.k], writes=[out.k])

    def transpose_store(self, hb, ptr, dstT_tile, evac_eng='act'):
        S = self.S
        for k8 in range(0, 16, 8):
            p = ptr.get()
            for j in range(8):
                k = k8 + j
                S.op('pe', lambda e: e.transpose(p.t[:, j * 128:(j + 1) * 128], hb.t[:, k * 128:(k + 1) * 128], self.cbs(C_IDENT, 128)),
                     reads=[hb.k, 'cb'], writes=[p.k])
            S.op('act', lambda e: e.activation(out=dstT_tile.t[:, k8:k8 + 8, :], in_=p.t[:].rearrange("p (c d) -> p c d", c=8), func=AF.Copy),
                 reads=[p.k], writes=[dstT_tile.k])

    def phase_wo(self, l):
        S, SL, NCH = self.S, self.SL, self.NCH
        CAP = self.CAP
        with self.scope() as es:
            wo = self.sb("wo", [128, 16, D], BF16)
            self.load_resident(wo, self.w_o[l], 16)
            wr = self.sb("wr", [128, 16, 36], BF16)
            wrs = self.sb("wrs", [128, 16, 36], F32)
            S.dma('sp', wrs.t[:, :, 0:4], self.w_rg[l].rearrange("(k p) c -> p k c", p=128), writes=['wrs'])
            S.dma('sp', wrs.t[:, :, 4:36], self.w_re[l].rearrange("(k p) c -> p k c", p=128), writes=['wrs'])
            S.op('dve', lambda e: e.tensor_copy(out=wr.t[:], in_=wrs.t[:]), reads=['wrs'], writes=['wr'])
            brt = self.sb("brt", [128, 36], F32)
            S.dma('sp', brt.t[:, 0:4], self.b_rg[l, 0:1, :].partition_broadcast(128), writes=['brt'])
            S.dma('sp', brt.t[:, 4:36], self.b_re[l, 0:1, :].partition_broadcast(128), writes=['brt'])
            g_t = self.sb("l1g", [128, D], F32)
            b_t = self.sb("l1b", [128, D], F32)
            S.dma('sp', g_t.t[:], self.ln1_g[l, 0:1, :].partition_broadcast(128), writes=['l1g'])
            S.dma('sp', b_t.t[:], self.ln1_b[l, 0:1, :].partition_broadcast(128), writes=['l1b'])
            base = self.sb("rbase", [128, 32], F32)
            S.op('dve', lambda e: e.memset(base.t[:], 0.0), writes=['rbase'])
            mT = self.ring("wmT", 2, [128, 16, 128], BF16)
            xr = self.ring("wxr", 2, [128, D], F32)
            ht = self.ring("wh", 2, [128, D], F32)
            hb = self.ring("whb", 2, [128, D], BF16)
            hTt = self.ring("whT", 2, [128, 16, 128], BF16)
            stt = self.ring("wstt", 2, [128, 24], F32)
            mvt = self.ring("wmv", 2, [128, 4], F32)
            pp = self.ring("wpp", 4, [128, 512], F32, psum=True)
            ptr = self.ring("wptr", 2, [128, 1024], BF16, psum=True)
            prt = self.ring("wprt", 2, [128, 64], F32, psum=True)
            sm = self.ring("wsm", 2, [128, 256], F32)
            mb = self.ring("wmb", 2, [128, 32], BF16)
            x_src = self.x_tm if l == 0 else self.xres
            mres = [('D', 'mergedT', c, tg) for c in range(16) for tg in range(self.NTG)]
            def stageA(c):
                r0 = c * 128
                m = mT.get()
                S.dma('sp', m.t[:], self.mergedT[:, r0:r0 + 128].rearrange("(k p) t -> p k t", p=128), reads=mres, writes=[m.k])
                x = xr.get()
                S.dma('sp', x.t[:], x_src[r0:r0 + 128, :], reads=([('D', 'xres', c)] if l > 0 else []), writes=[x.k])
                v = x
                for ct in range(4):
                    p = pp.get()
                    for k in range(16):
                        S.op('pe', lambda e: e.matmul(p.t[:], lhsT=m.t[:, k, :], rhs=wo.t[:, k, ct * 512:(ct + 1) * 512], start=(k == 0), stop=(k == 15)),
                             reads=[m.k, 'wo'], writes=[p.k])
                    S.op('dve', lambda e: e.scalar_tensor_tensor(out=v.t[:, ct * 512:(ct + 1) * 512], in0=x.t[:, ct * 512:(ct + 1) * 512], scalar=ALPHA,
                                                                 in1=p.t[:], op0=ALU.mult, op1=ALU.add), reads=[x.k, p.k], writes=[v.k])
                h = ht.get()
                self.layer_norm(v, stt.get(), mvt.get(), g_t, b_t, h)
                hbb = hb.get()
                S.op('act', lambda e: e.activation(out=hbb.t[:], in_=h.t[:], func=AF.Copy), reads=[h.k], writes=[hbb.k])
                return h, hbb

            def stageB(c, h, hbb):
                r0 = c * 128
                S.dma('sp', self.hres[r0:r0 + 128, :], h.t[:], reads=[h.k], writes=[('D', 'hres', c)])
                hT = hTt.get()
                self.transpose_store(hbb, ptr, hT)
                S.dma('sp', self.hT[:, r0:r0 + 128].rearrange("(k p) t -> p k t", p=128), hT.t[:], reads=[hT.k], writes=[('D', 'hT', c)])
                pr = prt.get()
                for k in range(16):
                    S.op('pe', lambda e: e.matmul(pr.t[:, 0:36], lhsT=hT.t[:, k, :], rhs=wr.t[:, k, :], start=(k == 0), stop=(k == 15)),
                         reads=[hT.k, 'wr'], writes=[pr.k])
                s = sm.get()
                sk = s.k
                A = s.t

                def dv(fn, extra_r=(), w=None):
                    S.op('dve', fn, reads=[sk] + list(extra_r), writes=[w or sk])
                dv(lambda e: e.tensor_tensor(out=A[:, 0:36], in0=pr.t[:, 0:36], in1=brt.t[:], op=ALU.add), extra_r=[pr.k, 'brt'])
                dv(lambda e: e.reduce_max(out=A[:, 36:37], in_=A[:, 0:4], axis=mybir.AxisListType.X))
                dv(lambda e: e.tensor_scalar(out=A[:, 224:228], in0=A[:, 0:4], scalar1=A[:, 36:37], scalar2=None, op0=ALU.subtract))
                S.op('act', lambda e: e.activation(out=A[:, 224:228], in_=A[:, 224:228], func=AF.Exp), reads=[sk], writes=[sk])
                dv(lambda e: e.reduce_sum(out=A[:, 37:38], in_=A[:, 224:228], axis=mybir.AxisListType.X))
                dv(lambda e: e.reciprocal(out=A[:, 38:39], in_=A[:, 37:38]))
                dv(lambda e: e.tensor_scalar(out=A[:, 40:44], in0=A[:, 0:4], scalar1=A[:, 36:37], scalar2=None, op0=ALU.is_equal))
                dv(lambda e: e.tensor_scalar(out=A[:, 44:52], in0=A[:, 4:12], scalar1=A[:, 40:41], scalar2=None, op0=ALU.mult))
                for j in range(1, 4):
                    dv(lambda e: e.scalar_tensor_tensor(out=A[:, 44:52], in0=A[:, 4 + 8 * j:12 + 8 * j], scalar=A[:, 40 + j:41 + j], in1=A[:, 44:52],
                                                        op0=ALU.mult, op1=ALU.add))
                dv(lambda e: e.reduce_max(out=A[:, 52:53], in_=A[:, 44:52], axis=mybir.AxisListType.X))
                dv(lambda e: e.tensor_scalar(out=A[:, 56:64], in0=A[:, 44:52], scalar1=A[:, 52:53], scalar2=None, op0=ALU.is_equal))
                dv(lambda e: e.scalar_tensor_tensor(out=A[:, 72:80], in0=A[:, 56:64], scalar=-1e30, in1=A[:, 44:52], op0=ALU.mult, op1=ALU.add))
                dv(lambda e: e.reduce_max(out=A[:, 53:54], in_=A[:, 72:80], axis=mybir.AxisListType.X))
                dv(lambda e: e.tensor_scalar(out=A[:, 64:72], in0=A[:, 72:80], scalar1=A[:, 53:54], scalar2=None, op0=ALU.is_equal))
                dv(lambda e: e.tensor_tensor(out=A[:, 80:81], in0=A[:, 53:54], in1=A[:, 52:53], op=ALU.subtract))
                S.op('act', lambda e: e.activation(out=A[:, 80:81], in_=A[:, 80:81], func=AF.Exp), reads=[sk], writes=[sk])
                dv(lambda e: e.tensor_scalar(out=A[:, 81:82], in0=A[:, 80:81], scalar1=1.0, scalar2=None, op0=ALU.add))
                dv(lambda e: e.reciprocal(out=A[:, 81:82], in_=A[:, 81:82]))
                dv(lambda e: e.tensor_tensor(out=A[:, 81:82], in0=A[:, 81:82], in1=A[:, 38:39], op=ALU.mult))
                dv(lambda e: e.tensor_tensor(out=A[:, 82:83], in0=A[:, 38:39], in1=A[:, 81:82], op=ALU.subtract))
                S.op('dve', lambda e: e.tensor_copy(out=self.w_all.t[:, c, :], in_=A[:, 81:83]), reads=[sk], writes=['w_all'])
                for j in range(4):
                    dv(lambda e: e.tensor_scalar(out=A[:, 96 + 8 * j:104 + 8 * j], in0=A[:, 56:64], scalar1=A[:, 40 + j:41 + j], scalar2=None, op0=ALU.mult))
                    dv(lambda e: e.tensor_scalar(out=A[:, 128 + 8 * j:136 + 8 * j], in0=A[:, 64:72], scalar1=A[:, 40 + j:41 + j], scalar2=None, op0=ALU.mult))
                dv(lambda e: e.tensor_tensor(out=A[:, 160:192], in0=A[:, 96:128], in1=A[:, 128:160], op=ALU.add))
                mbb = mb.get()
                S.op('dve', lambda e: e.tensor_copy(out=mbb.t[:], in_=A[:, 160:192]), reads=[sk], writes=[mbb.k])
                pc = prt.get()
                S.op('pe', lambda e: e.matmul(pc.t[:, 0:32], lhsT=self.cbs(C_TRIL, 128), rhs=mbb.t[:], start=True, stop=True), reads=[mbb.k, 'cb'], writes=[pc.k])
                S.op('pe', lambda e: e.matmul(pc.t[:, 32:64], lhsT=self.cbs(C_ONES, 128), rhs=mbb.t[:], start=True, stop=True), reads=[mbb.k, 'cb'], writes=[pc.k])
                dv(lambda e: e.tensor_tensor(out=A[:, 192:224], in0=pc.t[:, 0:32], in1=base.t[:], op=ALU.add), extra_r=[pc.k, 'rbase'])
                dv(lambda e: e.tensor_tensor(out=A[:, 192:224], in0=A[:, 192:224], in1=self.cf.t[:, 2:34], op=ALU.add), extra_r=['cf'])
                S.op('dve', lambda e: e.tensor_tensor(out=base.t[:], in0=base.t[:], in1=pc.t[:, 32:64], op=ALU.add), reads=['rbase', pc.k, sk], writes=['rbase'])
                dv(lambda e: e.tensor_tensor(out=A[:, 96:128], in0=A[:, 96:128], in1=A[:, 192:224], op=ALU.mult))
                dv(lambda e: e.tensor_tensor(out=A[:, 128:160], in0=A[:, 128:160], in1=A[:, 192:224], op=ALU.mult))
                dv(lambda e: e.reduce_sum(out=A[:, 228:229], in_=A[:, 96:128], axis=mybir.AxisListType.X))
                dv(lambda e: e.reduce_sum(out=A[:, 229:230], in_=A[:, 128:160], axis=mybir.AxisListType.X))
                S.op('dve', lambda e: e.tensor_copy(out=self.dest_all.t[:, c, :], in_=A[:, 228:230]), reads=[sk], writes=['dest_all'])
                for kk in range(2):
                    S.dma('pool', None, None, reads=[hbb.k, 'dest_all'], writes=[('D', 'hrows', c, kk)],
                          fn=lambda e: e.indirect_dma_start(out=self.hrows[:, :], out_offset=bass.IndirectOffsetOnAxis(ap=self.dest_all.t[:, c, kk:kk + 1], axis=0),
                                                            in_=hbb.t[:], in_offset=None))

            cur = stageA(0)
            for c in range(NCH):
                nxt = stageA(c + 1) if c + 1 < NCH else None
                stageB(c, *cur)
                cur = nxt

    def phase_experts(self, l):
        S, NCH, CAP, CAPB = self.S, self.NCH, self.CAP, self.CAPB
        with self.scope() as es:
            rows = self.ring("erow", 2, [128, D], BF16)
            rT = self.ring("erT", 2, [128, 16, CAP], BF16)
            hidT = self.ring("ehid", 2, [128, 4, CAP], BF16)
            sil = self.ring("esil", 2, [128, CAP], F32)
            ysb = self.ring("eysb", CAPB, [128, D], F32)
            ptr = self.ring("eptr", 2, [128, 1024], BF16, psum=True)
            pg = self.ring("epg", 2, [128, 512], F32, psum=True)
            pu = self.ring("epu", 2, [128, 512], F32, psum=True)
            py = self.ring("epy", 2, [128, 512], F32, psum=True)
            hr = [('D', 'hrows', c, kk) for c in range(NCH) for kk in range(2)]
            EST = self.ring("est", 4, [128, 2048], F32)
            WG = self.ring("ewg", 2, [128, 16, EH], BF16)
            WU = self.ring("ewu", 2, [128, 16, EH], BF16)
            def load_gu(ex_):
                weg_ = self.w_eg[l, ex_].rearrange("(k p) c -> p k c", p=128)
                weu_ = self.w_eu[l, ex_].rearrange("(k p) c -> p k c", p=128)
                wg_ = WG.get()
                wu_ = WU.get()
                for (src, dstb) in ((weg_, wg_), (weu_, wu_)):
                    for kq in range(4):
                        st = EST.get()
                        sk4 = [(st.k, jj) for jj in range(4)]
                        S.dma('sp', st.t[:].rearrange("p (a b) -> p a b", a=4), src[:, kq * 4:(kq + 1) * 4, :], writes=sk4)
                        self.cast(dstb.t[:, kq * 4:(kq + 1) * 4, :], st.t[:].rearrange("p (a b) -> p a b", a=4), sk4, [(dstb.k, kq)])
                        yield (wg_, wu_)
            nxt_w = None
            for ex in range(NEXP):
                rt = rT.get()
                for b in range(CAPB):
                    r = rows.get()
                    S.dma('act', r.t[:], self.hrows[ex * CAP + b * 128:ex * CAP + (b + 1) * 128, :], reads=hr, writes=[r.k])
                    for k8 in range(0, 16, 8):
                        p = ptr.get()
                        for j in range(8):
                            k = k8 + j
                            S.op('pe', lambda e: e.transpose(p.t[:, j * 128:(j + 1) * 128], r.t[:, k * 128:(k + 1) * 128], self.cbs(C_IDENT, 128)),
                                 reads=[r.k, 'cb'], writes=[p.k])
                        S.op('act' if k8 == 0 else 'dve',
                             (lambda e: e.activation(out=rt.t[:, k8:k8 + 8, b * 128:(b + 1) * 128], in_=p.t[:].rearrange("p (c d) -> p c d", c=8), func=AF.Copy)) if k8 == 0 else
                             (lambda e: e.tensor_copy(out=rt.t[:, k8:k8 + 8, b * 128:(b + 1) * 128], in_=p.t[:].rearrange("p (c d) -> p c d", c=8))),
                             reads=[p.k], writes=[rt.k])
                hd = hidT.get()
                weg = self.w_eg[l, ex].rearrange("(k p) c -> p k c", p=128)
                weu = self.w_eu[l, ex].rearrange("(k p) c -> p k c", p=128)
                if ex == 0:
                    for nxt_w in load_gu(0):
                        pass
                wg, wu = nxt_w
                gen = load_gu(ex + 1) if ex + 1 < NEXP else iter(())
                for j in range(4):
                    for _ in range(2):
                        nxt_w = next(gen, nxt_w)
                    g_ = pg.get()
                    u_ = pu.get()
                    for k in range(16):
                        S.op('pe', lambda e: e.matmul(g_.t[:, 0:CAP], lhsT=wg.t[:, k, j * 128:(j + 1) * 128], rhs=rt.t[:, k, :], start=(k == 0), stop=(k == 15)),
                             reads=[(wg.k, k // 4), rt.k], writes=[g_.k])
                    for k in range(16):
                        S.op('pe', lambda e: e.matmul(u_.t[:, 0:CAP], lhsT=wu.t[:, k, j * 128:(j + 1) * 128], rhs=rt.t[:, k, :], start=(k == 0), stop=(k == 15)),
                             reads=[(wu.k, k // 4), rt.k], writes=[u_.k])
                    s_ = sil.get()
                    S.op('act', lambda e: e.activation(out=s_.t[:], in_=g_.t[:, 0:CAP], func=AF.Silu), reads=[g_.k], writes=[s_.k])
                    S.op('dve', lambda e: e.tensor_tensor(out=hd.t[:, j, :], in0=s_.t[:], in1=u_.t[:, 0:CAP], op=ALU.mult), reads=[s_.k, u_.k], writes=[hd.k])
                wed = self.w_ed[l, ex].rearrange("(k p) c -> p k c", p=128)
                ys = [ysb.get() for _ in range(CAPB)]
                wd_n = self.wload(wed[:, :, 0:512], 4, 512)
                for ct in range(4):
                    wd = wd_n
                    if ct < 3:
                        wd_n = self.wload(wed[:, :, (ct + 1) * 512:(ct + 2) * 512], 4, 512)
                    wdv = wd.t[:].rearrange("p (k m) -> p k m", k=4)
                    for b in range(CAPB):
                        y_ = py.get()
                        for j in range(4):
                            S.op('pe', lambda e: e.matmul(y_.t[:], lhsT=hd.t[:, j, b * 128:(b + 1) * 128], rhs=wdv[:, j, :], start=(j == 0), stop=(j == 3)),
                                 reads=[hd.k, wd.k], writes=[y_.k])
                        if (ct + b) % 2 == 0:
                            S.op('act', lambda e: e.activation(out=ys[b].t[:, ct * 512:(ct + 1) * 512], in_=y_.t[:], func=AF.Copy), reads=[y_.k], writes=[ys[b].k])
                        else:
                            S.op('dve', lambda e: e.tensor_copy(out=ys[b].t[:, ct * 512:(ct + 1) * 512], in_=y_.t[:]), reads=[y_.k], writes=[ys[b].k])
                for b in range(CAPB):
                    S.dma('sp', self.yrows[ex * CAP + b * 128:ex * CAP + (b + 1) * 128, :], ys[b].t[:], reads=[ys[b].k], writes=[('D', 'yrows', ex, b)])

    def phase_ple(self, l, last):
        S, SL, NCH, CAPB = self.S, self.SL, self.NCH, self.CAPB
        with self.scope() as es:
            wpg = self.sb("wpg", [128, 16, D], BF16)
            self.load_resident(wpg, self.w_pg[l], 16)
            wpp = self.sb("wpp", [128, 2, D], BF16)
            self.load_resident(wpp, self.w_pp[l], 2)
            g_t = self.sb("l2g", [128, D], F32)
            b_t = self.sb("l2b", [128, D], F32)
            S.dma('sp', g_t.t[:], self.ln2_g[l, 0:1, :].partition_broadcast(128), writes=['l2g'])
            S.dma('sp', b_t.t[:], self.ln2_b[l, 0:1, :].partition_broadcast(128), writes=['l2b'])
            hTt = self.ring("phT", 2, [128, 16, 128], BF16)
            pTs = self.ring("ppTs", 2, [128, 2, 128], F32)
            pTb = self.ring("ppTb", 2, [128, 2, 128], BF16)
            hr = self.ring("phr", 2, [128, D], F32)
            y1 = self.ring("py1", 1, [128, D], F32)
            y2 = self.ring("py2", 1, [128, D], F32)
            sg = self.ring("psg", 1, [128, D], F32)
            xb = self.ring("pxb", 1, [128, D], BF16)
            xTt = self.ring("pxT", 2, [128, 16, 128], BF16)
            stt = self.ring("pstt", 2, [128, 24], F32)
            mvt = self.ring("pmv", 2, [128, 4], F32)
            ppg = self.ring("ppg", 3, [128, 512], F32, psum=True)
            ppp = self.ring("ppp", 3, [128, 512], F32, psum=True)
            ptr = self.ring("pptr", 2, [128, 1024], BF16, psum=True)
            yr = [('D', 'yrows', ex, b) for ex in range(NEXP) for b in range(CAPB)]
            for c in range(NCH):
                r0 = c * 128
                hT = hTt.get()
                S.dma('sp', hT.t[:], self.hT[:, r0:r0 + 128].rearrange("(k p) t -> p k t", p=128), reads=[('D', 'hT', c)], writes=[hT.k])
                ps_ = pTs.get()
                S.dma('sp', ps_.t[:], self.pT[l, :, r0:r0 + 128].rearrange("(k p) t -> p k t", p=128), writes=[ps_.k])
                pb = pTb.get()
                S.op('pool', lambda e: e.tensor_copy(out=pb.t[:], in_=ps_.t[:]), reads=[ps_.k], writes=[pb.k])
                h = hr.get()
                S.dma('sp', h.t[:], self.hres[r0:r0 + 128, :], reads=[('D', 'hres', c)], writes=[h.k])
                ya = y1.get()
                yb = y2.get()
                for kk, yy in ((0, ya), (1, yb)):
                    S.dma('pool', None, None, reads=yr + ['dest_all'], writes=[yy.k],
                          fn=lambda e: e.indirect_dma_start(out=yy.t[:], out_offset=None, in_=self.yrows[:, :],
                                                            in_offset=bass.IndirectOffsetOnAxis(ap=self.dest_all.t[:, c, kk:kk + 1], axis=0)))
                s_ = sg.get()
                v = s_
                for ct in range(4):
                    sl = slice(ct * 512, (ct + 1) * 512)
                    pg_ = ppg.get()
                    for k in range(16):
                        S.op('pe', lambda e: e.matmul(pg_.t[:], lhsT=hT.t[:, k, :], rhs=wpg.t[:, k, sl], start=(k == 0), stop=(k == 15)), reads=[hT.k, 'wpg'], writes=[pg_.k])
                    pp_ = ppp.get()
                    for k in range(2):
                        S.op('pe', lambda e: e.matmul(pp_.t[:], lhsT=pb.t[:, k, :], rhs=wpp.t[:, k, sl], start=(k == 0), stop=(k == 1)), reads=[pb.k, 'wpp'], writes=[pp_.k])
                    S.op('act', lambda e: e.activation(out=s_.t[:, sl], in_=pg_.t[:], func=AF.Sigmoid), reads=[pg_.k], writes=[s_.k])
                    S.op('dve', lambda e: e.tensor_tensor(out=s_.t[:, sl], in0=s_.t[:, sl], in1=pp_.t[:], op=ALU.mult), reads=[s_.k, pp_.k], writes=[s_.k])
                S.op('dve', lambda e: e.scalar_tensor_tensor(out=v.t[:], in0=h.t[:], scalar=ALPHA, in1=s_.t[:], op0=ALU.mult, op1=ALU.add), reads=[h.k, s_.k], writes=[v.k])
                S.op('dve', lambda e: e.scalar_tensor_tensor(out=v.t[:], in0=ya.t[:], scalar=self.w_all.t[:, c, 0:1], in1=v.t[:], op0=ALU.mult, op1=ALU.add),
                     reads=[ya.k, 'w_all', v.k], writes=[v.k])
                S.op('dve', lambda e: e.scalar_tensor_tensor(out=v.t[:], in0=yb.t[:], scalar=self.w_all.t[:, c, 1:2], in1=v.t[:], op0=ALU.mult, op1=ALU.add),
                     reads=[yb.k, 'w_all', v.k], writes=[v.k])
                x = h
                self.layer_norm(v, stt.get(), mvt.get(), g_t, b_t, x)
                if last:
                    S.dma('sp', self.out[r0:r0 + 128, :], x.t[:], reads=[x.k], writes=[('D', 'out', c)])
                else:
                    S.dma('sp', self.xres[r0:r0 + 128, :], x.t[:], reads=[x.k], writes=[('D', 'xres', c)])
                    xbb = xb.get()
                    S.op('act', lambda e: e.activation(out=xbb.t[:], in_=x.t[:], func=AF.Copy), reads=[x.k], writes=[xbb.k])
                    xT = xTt.get()
                    self.transpose_store(xbb, ptr, xT)
                    S.dma('sp', self.xT[:, r0:r0 + 128].rearrange("(k p) t -> p k t", p=128), xT.t[:], reads=[xT.k], writes=[('D', 'xT1', c)])


class _Scope:
    def __init__(self, prog):
        self.p = prog
        self.es = ExitStack()

    def __enter__(self):
        self.prev = self.p.cur
        self.p.cur = self.es
        self.es.__enter__()
        return self.es

    def __exit__(self, *a):
        self.p.S.barrier()
        r = self.es.__exit__(*a)
        self.p.cur = self.prev
        return r


class nc_allow:
    def __init__(self, nc):
        self.cm = nc.allow_non_contiguous_dma(reason="tiny strided column load")

    def __enter__(self):
        return self.cm.__enter__()

    def __exit__(self, *a):
        return self.cm.__exit__(*a)


def make_consts(CAP):
    c = np.zeros((128, NCONST), np.float32)
    i = np.arange(128)
    c[:, C_IDENT:C_IDENT + 128] = np.eye(128)
    c[:, C_ONES:C_ONES + 128] = 1.0
    c[:, C_TRIS:C_TRIS + 128] = (i[:, None] > i[None, :])
    c[:, C_TRIL:C_TRIL + 128] = (i[:, None] < i[None, :])
    Rd = np.zeros((128, 128), np.float32)
    for base in (0, 64):
        for j in range(8):
            Rd[base + j, base + j + 8] = -1.0
            Rd[base + j + 8, base + j] = 1.0
    c[:, C_RD:C_RD + 128] = Rd.T
    Rm = np.zeros((128, 128), np.float32)
    for j in range(32):
        Rm[j, j + 32] = -1.0
        Rm[j + 32, j] = 1.0
    c[:, C_RM:C_RM + 128] = Rm.T
    kk = i[:, None]
    qq = i[None, :]
    inblk = {
        'ch': (kk // 64 <= qq // 64),
        'ca': (kk <= qq),
        'st': (kk < qq),
    }
    for j0 in range(4):
        for b in range(4):
            for name, col, kind in (('ch', C_MADD_CH, 'add'), ('ca', C_MADD_CA, 'add'), ('st', C_MMUL, 'mul')):
                if b < j0:
                    valid = np.zeros((128, 128), bool)
                elif b == j0:
                    valid = inblk[name]
                else:
                    valid = np.ones((128, 128), bool)
                blk = np.where(valid, 0.0, NEG) if kind == 'add' else valid.astype(np.float32)
                c[:, col + j0 * 512 + b * 128:col + j0 * 512 + (b + 1) * 128] = blk
    invf_d = np.zeros(128, np.float32)
    fd = (np.float32(ROPE_THETA) ** (-np.arange(8, dtype=np.float32) / np.float32(8))).astype(np.float32)
    for base in (0, 64):
        invf_d[base:base + 8] = fd
        invf_d[base + 8:base + 16] = fd
    c[:, C_INVF_D] = invf_d
    fm = (np.float32(ROPE_THETA) ** (-np.arange(32, dtype=np.float32) / np.float32(32))).astype(np.float32)
    invf_m = np.zeros(128, np.float32)
    invf_m[0:32] = fm
    invf_m[32:64] = fm
    c[:, C_INVF_M] = invf_m
    c[:, C_ECAP:C_ECAP + 32] = (np.arange(32) * CAP)[None, :]
    return c


_CACHE = {}


def get_prog(S_len, **kw):
    key = (S_len, tuple(sorted((k, str(v)) for k, v in kw.items())))
    if key not in _CACHE:
        p = Prog(S_len, **kw)
        p.build()
        _CACHE[key] = p
    return _CACHE[key]


def make_in_map(p, b, inputs):
    f = lambda a: np.ascontiguousarray(np.asarray(a, dtype=np.float32))
    x = np.asarray(inputs['x'])[b]
    m = {
        'x_tm': f(x), 'x_fm': f(x.T),
        'pT': f(np.transpose(np.asarray(inputs['p'])[:, b], (0, 2, 1))),
        'pos': np.ascontiguousarray(np.asarray(inputs['positions'])[b:b + 1].astype(np.int32)),
        'consts': make_consts(p.CAP),
        'w_in': f(inputs['w_in']),
        'fox_f_bias': f(inputs['fox_f_bias']).reshape(DEPTH, 4, 1),
        'diff_lambda': f(inputs['diff_lambda']).reshape(DEPTH, 1, 256),
        'diff_subln': f(inputs['diff_subln']).reshape(DEPTH, 128, 1),
        'mla_q_norm': f(f(inputs['mla_q_norm']).reshape(DEPTH, 4, 128).transpose(0, 2, 1)),
        'mla_kv_norm': f(f(inputs['mla_kv_norm']).reshape(DEPTH, 2, 128).transpose(0, 2, 1)),
        'mla_w_uq': f(inputs['mla_w_uq']), 'mla_w_ukv': f(inputs['mla_w_ukv']),
        'w_branch': f(inputs['w_branch']), 'w_o': f(inputs['w_o']),
        'ln1_g': f(inputs['ln1_g']).reshape(DEPTH, 1, D), 'ln1_b': f(inputs['ln1_b']).reshape(DEPTH, 1, D),
        'w_router_group': f(inputs['w_router_group']), 'b_router_group': f(inputs['b_router_group']).reshape(DEPTH, 1, 4),
        'w_router_expert': f(inputs['w_router_expert']), 'b_router_expert': f(inputs['b_router_expert']).reshape(DEPTH, 1, 32),
        'w_expert_gate': f(inputs['w_expert_gate']), 'w_expert_up': f(inputs['w_expert_up']),
        'w_expert_down': f(inputs['w_expert_down']),
        'w_ple_gate': f(inputs['w_ple_gate']), 'w_ple_proj': f(inputs['w_ple_proj']),
        'ln2_g': f(inputs['ln2_g']).reshape(DEPTH, 1, D), 'ln2_b': f(inputs['ln2_b']).reshape(DEPTH, 1, D),
    }
    return m


def kernel(**inputs):
    x = np.asarray(inputs['x'])
    B, SL, _ = x.shape
    p = get_prog(SL)
    in_maps = [make_in_map(p, b, inputs) for b in range(B)]
    res = run_bass_kernel_spmd(p.nc, in_maps, core_ids=list(range(B)))
    out = np.stack([np.asarray(r['out']) for r in res.results], axis=0)
    return out.astype(np.float32)
```

```python
import math
import numpy as np
from contextlib import ExitStack
import concourse.bass as bass
import concourse.mybir as mybir
from concourse.bass_utils import run_bass_kernel_spmd

F32 = mybir.dt.float32
BF16 = mybir.dt.bfloat16
I32 = mybir.dt.int32
AF = mybir.ActivationFunctionType
ALU = mybir.AluOpType

D = 2048
DEPTH = 2
NEXP = 32
EH = 512
IN_DIM = 13636
ALPHA = 4.0 ** 0.25
LN_EPS = 1e-5
RMS_EPS = 1e-6
ROPE_THETA = 500000.0
OFF = dict(diff_q=0, diff_k=512, diff_v=1024, fox_q=1536, fox_k=2048, fox_v=2560, fox_f=3072,
           mla_cq=3076, mla_ckv=3588, mla_kr=3844, sb_q=3908, sb_k=4420, sb_v=4932, gates=5444)
NEG = -30000.0
TWO_PI = 6.283185307179586

C_IDENT, C_ONES, C_TRIS, C_TRIL, C_RD, C_RM = 0, 128, 256, 384, 512, 640
C_MADD_CH, C_MADD_CA, C_MMUL = 768, 768 + 2048, 768 + 4096
C_INVF_D, C_INVF_M, C_ECAP = 768 + 6144, 768 + 6145, 768 + 6146
NCONST = 768 + 6146 + 32


class Sched:
    NDMA = 16

    def __init__(self, nc, es):
        self.nc = nc
        self.es = es
        self.eng = {'pe': nc.tensor, 'dve': nc.vector, 'act': nc.scalar, 'pool': nc.gpsimd, 'sp': nc.sync}
        self.sems = {}
        self.cnt = {}
        for e in self.eng:
            self.sems[e] = es.enter_context(nc.semaphore('s_' + e))
            self.cnt[e] = 0
        self.dma_rr = {}
        for q in ('sp', 'act', 'pool'):
            self.dma_rr[q] = 0
            for i in range(self.NDMA):
                k = ('d', q, i)
                self.sems[k] = es.enter_context(nc.semaphore('d_%s_%d' % (q, i)))
                self.cnt[k] = 0
        self.seen = {e: {} for e in self.eng}
        self.res = {}
        self.nins = 0

    def _need(self, reads, writes):
        need = {}
        for r in reads:
            st = self.res.get(r)
            if st and st[0] is not None:
                k, v = st[0]
                if need.get(k, 0) < v:
                    need[k] = v
        for w in writes:
            st = self.res.get(w)
            if st:
                if st[0] is not None:
                    k, v = st[0]
                    if need.get(k, 0) < v:
                        need[k] = v
                for k, v in st[1].items():
                    if need.get(k, 0) < v:
                        need[k] = v
        return need

    def _commit(self, ev, reads, writes):
        k, v = ev
        for r in reads:
            st = self.res.get(r)
            if st is None:
                st = self.res[r] = [None, {}]
            if st[1].get(k, 0) < v:
                st[1][k] = v
        for w in writes:
            self.res[w] = [ev, {}]

    def _waits(self, e, need):
        eng = self.eng[e]
        seen = self.seen[e]
        for k, v in need.items():
            if e == 'pe' and k == 'pe':
                continue
            if seen.get(k, 0) < v:
                eng.wait_ge(self.sems[k], v)
                seen[k] = v

    def op(self, e, fn, reads=(), writes=()):
        self._waits(e, self._need(reads, writes))
        ins = fn(self.eng[e])
        self.cnt[e] += 1
        ins.then_inc(self.sems[e], 1)
        self._commit((e, self.cnt[e]), reads, writes)
        self.nins += 1

    def dma(self, q, out, in_, reads=(), writes=(), fn=None, **kw):
        if q == 'sp' and fn is None and str(out.space) == 'DRAM' and str(in_.space) != 'DRAM':
            q = 'act'
        i = self.dma_rr[q]
        self.dma_rr[q] = (i + 1) % self.NDMA
        k = ('d', q, i)
        need = self._need(reads, writes)
        if self.cnt[k]:
            need[k] = max(need.get(k, 0), self.cnt[k])
        self._waits(q, need)
        if fn is not None:
            ins = fn(self.eng[q])
        else:
            ins = self.eng[q].dma_start(out=out, in_=in_, **kw)
        self.cnt[k] += 16
        ins.then_inc(self.sems[k], 16)
        self._commit((k, self.cnt[k]), reads, writes)
        self.nins += 1

    def finish(self, keys):
        self._waits('sp', self._need(keys, ()))

    def barrier(self):
        need = {k: v for k, v in self.cnt.items() if v > 0}
        for e in self.eng:
            eng = self.eng[e]
            seen = self.seen[e]
            for k, v in need.items():
                if seen.get(k, 0) < v:
                    eng.wait_ge(self.sems[k], v)
                    seen[k] = v


class Ring:
    def __init__(self, tiles):
        self.tiles = tiles
        self.i = 0

    def get(self):
        t = self.tiles[self.i]
        self.i = (self.i + 1) % len(self.tiles)
        return t


class T:
    def __init__(self, ap, key):
        self.t = ap
        self.k = key


class Prog:
    def __init__(self, S_len, nlayers=DEPTH, dbg=False, phases=None):
        self.SL = S_len
        self.TG = min(2048, S_len)
        self.NTG = S_len // self.TG
        self.NCH = S_len // 128
        self.NQT = S_len // 512
        avg = 2 * S_len // NEXP
        sd = math.sqrt(2 * S_len / 32.0)
        self.CAPB = int(math.ceil((avg + 8.0 * sd) / 128.0))
        self.CAP = self.CAPB * 128
        self.nlayers = nlayers
        self.WD = DEPTH if (nlayers == DEPTH) else nlayers
        self.NE_DECL = NEXP if (phases is None or 'exp' in phases) else 1
        self.dbg = dbg
        self.phases = phases
        self.nc = bass.Bass("TRN2", target_bir_lowering=False)
        self.scopes = []
        self.uid = 0
        self.cast_rr = 0

    def din(self, name, shape, dt=F32):
        return self.nc.dram_tensor(name, list(shape), dt, kind="ExternalInput").ap()

    def dscr(self, name, shape, dt):
        kind = "ExternalOutput" if (self.dbg and name in self.dbg) else "Internal"
        return self.nc.dram_tensor(name, list(shape), dt, kind=kind).ap()

    def sb(self, name, shape, dt):
        self.uid += 1
        t = self.cur.enter_context(self.nc.sbuf_tensor("%s_%d" % (name, self.uid), list(shape), dt))
        return T(t, name)

    def ps(self, name, shape, dt=F32):
        self.uid += 1
        t = self.cur.enter_context(self.nc.psum_tensor("%s_%d" % (name, self.uid), list(shape), dt))
        return T(t, name)

    def ring(self, name, n, shape, dt, psum=False):
        return Ring([(self.ps if psum else self.sb)("%s%d" % (name, i), shape, dt) for i in range(n)])

    def wload(self, src_ap, a, b, q='sp'):
        st = self.wst.get()
        wb = self.wbf.get()
        S = self.S
        stv = st.t[:, 0:a * b].rearrange("p (a b) -> p a b", a=a)
        step = max(1, 4 * 128 // 128) if b <= 512 else 1
        step = 4
        keys = []
        for j, a0 in enumerate(range(0, a, step)):
            a1 = min(a, a0 + step)
            S.dma(q, stv[:, a0:a1, :], src_ap[:, a0:a1, :], writes=[(st.k, j)])
            keys.append((st.k, j))
        self.cast(wb.t[:, 0:a * b], st.t[:, 0:a * b], keys, [wb.k])
        return wb

    def cast(self, out_ap, in_ap, reads, writes):
        i = self.cast_rr
        self.cast_rr = (i + 1) % 3
        if i != 2:
            self.S.op('act', lambda e: e.activation(out=out_ap, in_=in_ap, func=AF.Copy), reads=reads, writes=writes)
        else:
            self.S.op('dve', lambda e: e.tensor_copy(out=out_ap, in_=in_ap), reads=reads, writes=writes)

    def build(self):
        nc = self.nc
        SL, TG, NCH = self.SL, self.TG, self.NCH
        L = self.nlayers
        self.x_tm = self.din("x_tm", [SL, D])
        self.x_fm = self.din("x_fm", [D, SL])
        self.pT = self.din("pT", [DEPTH, 256, SL])
        self.pos = self.din("pos", [1, SL], I32)
        self.consts = self.din("consts", [128, NCONST])
        self.w_in = self.din("w_in", [DEPTH, D, IN_DIM])
        self.fox_f_bias = self.din("fox_f_bias", [DEPTH, 4, 1])
        self.diff_lambda = self.din("diff_lambda", [DEPTH, 1, 256])
        self.diff_subln = self.din("diff_subln", [DEPTH, 128, 1])
        self.mla_q_norm = self.din("mla_q_norm", [DEPTH, 128, 4])
        self.mla_kv_norm = self.din("mla_kv_norm", [DEPTH, 128, 2])
        self.mla_w_uq = self.din("mla_w_uq", [DEPTH, 512, 768])
        self.mla_w_ukv = self.din("mla_w_ukv", [DEPTH, 256, 1024])
        self.w_branch = self.din("w_branch", [DEPTH, 4, 512, D])
        self.w_o = self.din("w_o", [DEPTH, D, D])
        self.ln1_g = self.din("ln1_g", [DEPTH, 1, D])
        self.ln1_b = self.din("ln1_b", [DEPTH, 1, D])
        self.w_rg = self.din("w_router_group", [DEPTH, D, 4])
        self.b_rg = self.din("b_router_group", [DEPTH, 1, 4])
        self.w_re = self.din("w_router_expert", [DEPTH, D, 32])
        self.b_re = self.din("b_router_expert", [DEPTH, 1, 32])
        self.w_eg = self.din("w_expert_gate", [self.WD, self.NE_DECL, D, EH])
        self.w_eu = self.din("w_expert_up", [self.WD, self.NE_DECL, D, EH])
        self.w_ed = self.din("w_expert_down", [self.WD, self.NE_DECL, EH, D])
        self.w_pg = self.din("w_ple_gate", [DEPTH, D, D])
        self.w_pp = self.din("w_ple_proj", [DEPTH, 256, D])
        self.ln2_g = self.din("ln2_g", [DEPTH, 1, D])
        self.ln2_b = self.din("ln2_b", [DEPTH, 1, D])
        self.out = nc.dram_tensor("out", [SL, D], F32, kind="ExternalOutput").ap()
        self.xT = self.dscr("xT", [D, SL], BF16)
        self.xres = self.dscr("xres", [SL, D], F32)
        self.ropeD = self.dscr("ropeD", [2, 128, SL], BF16)
        self.ropeM = self.dscr("ropeM", [2, 64, SL], BF16)
        self.qT = self.dscr("qT", [16, 128, SL], BF16)
        self.kT = self.dscr("kT", [16, 128, SL], BF16)
        self.vv = self.dscr("vv", [16, SL, 128], BF16)
        self.qrT = self.dscr("qrT", [4, 64, SL], BF16)
        self.krT = self.dscr("krT", [64, SL], BF16)
        self.fT = self.dscr("fT", [4, SL], F32)
        self.cumT = self.dscr("cumT", [4, SL], F32)
        self.qaug = self.dscr("qaug", [4, 3, SL], BF16)
        self.gatesT = self.dscr("gatesT", [4 * D, SL], BF16)
        self.oT = self.dscr("oT", [16, 128, SL], BF16)
        self.mergedT = self.dscr("mergedT", [D, SL], BF16)
        self.hres = self.dscr("hres", [SL, D], F32)
        self.hT = self.dscr("hT", [D, SL], BF16)
        self.hrows = self.dscr("hrows", [NEXP * self.CAP, D], BF16)
        self.yrows = self.dscr("yrows", [NEXP * self.CAP, D], F32)

        with ExitStack() as es:
            self.S = Sched(nc, es)
            self.glob = es
            self.cur = es
            self.cb = self.sb("cb", [128, C_INVF_D], BF16)
            self.cf = self.sb("cf", [128, 34], F32)
            self.S.dma('sp', self.cf.t[:], self.consts[:, C_INVF_D:C_INVF_D + 34], writes=['cf'])
            with self.scope() as es0:
                for c0 in range(0, C_INVF_D, 2048):
                    c1 = min(C_INVF_D, c0 + 2048)
                    cft = self.sb("cft%d" % c0, [128, 2048], F32)
                    self.S.dma('sp', cft.t[:, 0:c1 - c0], self.consts[:, c0:c1], writes=[cft.k])
                    self.S.op('dve', lambda e: e.tensor_copy(out=self.cb.t[:, c0:c1], in_=cft.t[:, 0:c1 - c0]), reads=[cft.k], writes=['cb'])
            self.dest_all = self.sb("dest_all", [128, NCH, 2], I32)
            self.w_all = self.sb("w_all", [128, NCH, 2], F32)
            self.wst = self.ring("wst", 2, [128, 2048], F32)
            self.wbf = self.ring("wbf", 3, [128, 2048], BF16)
            self.epsln = self.sb("epsln", [128, 1], F32)
            self.S.op('dve', lambda e: e.memset(self.epsln.t[:], LN_EPS), writes=['epsln'])
            self.epsrms = self.sb("epsrms", [128, 1], F32)
            self.S.op('dve', lambda e: e.memset(self.epsrms.t[:], RMS_EPS), writes=['epsrms'])

            ph = self.phases
            if ph is None or 'pre' in ph:
                self.phase_pre()
            for l in range(L):
                if ph is None or 'proj' in ph:
                    self.phase_proj(l)
                if ph is None or 'fox' in ph:
                    self.phase_foxprep(l)
                if ph is None or 'attn' in ph:
                    self.phase_attn(l)
                if ph is None or 'merge' in ph:
                    self.phase_merge(l)
                if ph is None or 'wo' in ph:
                    self.phase_wo(l)
                if ph is None or 'exp' in ph:
                    self.phase_experts(l)
                if ph is None or 'ple' in ph:
                    self.phase_ple(l, last=(l == L - 1))
            keys = [k for k in self.S.res.keys() if isinstance(k, tuple) and k and k[0] == 'D']
            self.S.finish(keys)
        return nc

    def cbs(self, c0, n, p=128):
        return self.cb.t[0:p, c0:c0 + n]

    def scope(self):
        return _Scope(self)

    def phase_pre(self):
        S, SL = self.S, self.SL
        with self.scope() as es:
            CB = min(2048, SL)
            posi = self.sb("posi", [128, CB], I32)
            posf = self.sb("posf", [128, CB], F32)
            ang = self.sb("ang", [128, CB], F32)
            ki = self.sb("ki", [128, CB], I32)
            kf = self.sb("kf", [128, CB], F32)
            tb = self.ring("tb", 2, [128, CB], BF16)
            for cb0 in range(0, SL, CB):
                S.dma('sp', posi.t[:], self.pos[0:1, cb0:cb0 + CB].partition_broadcast(128), writes=['posi'])
                S.op('dve', lambda e: e.tensor_copy(out=posf.t[:], in_=posi.t[:]), reads=['posi'], writes=['posf'])
                for kind, (ccol, npart, dst) in enumerate([(0, 128, self.ropeD), (1, 64, self.ropeM)]):
                    for cs, shift in enumerate([math.pi / 2, 0.0]):
                        P = npart
                        S.op('dve', lambda e: e.tensor_scalar(out=ang.t[0:P], in0=posf.t[0:P], scalar1=self.cf.t[0:P, ccol:ccol + 1],
                                                              scalar2=shift, op0=ALU.mult, op1=ALU.add),
                             reads=['posf', 'cf'], writes=['ang'])
                        S.op('dve', lambda e: e.tensor_scalar(out=ki.t[0:P], in0=ang.t[0:P], scalar1=1.0 / TWO_PI, scalar2=None, op0=ALU.mult),
                             reads=['ang'], writes=['ki'])
                        S.op('dve', lambda e: e.tensor_copy(out=kf.t[0:P], in_=ki.t[0:P]), reads=['ki'], writes=['kf'])
                        S.op('dve', lambda e: e.scalar_tensor_tensor(out=ang.t[0:P], in0=kf.t[0:P], scalar=-TWO_PI, in1=ang.t[0:P],
                                                                     op0=ALU.mult, op1=ALU.add), reads=['kf', 'ang'], writes=['ang'])
                        S.op('dve', lambda e: e.tensor_scalar(out=ang.t[0:P], in0=ang.t[0:P], scalar1=-3.1415925, scalar2=3.1415925,
                                                              op0=ALU.max, op1=ALU.min), reads=['ang'], writes=['ang'])
                        o = tb.get()
                        S.op('act', lambda e: e.activation(out=o.t[0:P], in_=ang.t[0:P], func=AF.Sin), reads=['ang'], writes=[o.k])
                        S.dma('sp', dst[cs, :, cb0:cb0 + CB], o.t[0:P], reads=[o.k], writes=[('D', 'rope', kind, cs, cb0)])
            xs = self.ring("xs", 2, [128, CB], F32)
            xb = self.ring("xbp", 2, [128, CB], BF16)
            for k in range(16):
                for cb0 in range(0, SL, CB):
                    a = xs.get()
                    b = xb.get()
                    S.dma('sp', a.t[:], self.x_fm[k * 128:(k + 1) * 128, cb0:cb0 + CB], writes=[a.k])
                    S.op('pool', lambda e: e.tensor_copy(out=b.t[:], in_=a.t[:]), reads=[a.k], writes=[b.k])
                    S.dma('sp', self.xT[k * 128:(k + 1) * 128, cb0:cb0 + CB], b.t[:], reads=[b.k], writes=[('D', 'xT0', k, cb0 // self.TG)])

    def phase_proj(self, l):
        S, SL, TG = self.S, self.SL, self.TG
        NT = TG // 512
        w_in = self.w_in[l].rearrange("(k p) c -> p k c", p=128)
        for tg in range(self.NTG):
            t0 = tg * TG
            with self.scope() as es:
                xt = self.sb("xt", [128, 16, TG], BF16)
                for k in range(16):
                    S.dma('sp', xt.t[:, k, :], self.xT[k * 128:(k + 1) * 128, t0:t0 + TG], reads=([('D', 'xT0', k, tg)] if l == 0 else [('D', 'xT1', c_) for c_ in range(t0 // 128, (t0 + TG) // 128)]), writes=['xt'])
                rD = self.sb("rD", [128, 2, TG], BF16)
                rM = self.sb("rM", [64, 2, TG], BF16)
                for cs in range(2):
                    S.dma('sp', rD.t[:, cs, :], self.ropeD[cs, :, t0:t0 + TG], reads=[('D', 'rope', 0, cs, t0)], writes=['rD'])
                    S.dma('sp', rM.t[:, cs, :], self.ropeM[cs, :, t0:t0 + TG], reads=[('D', 'rope', 1, cs, t0)], writes=['rM'])
                pp = self.ring("pp", 4, [128, 512], F32, psum=True)
                pr = self.ring("prr", 2, [128, 512], F32, psum=True)
                ptr = self.ring("ptr", 2, [128, 1024], BF16, psum=True)
                ob = self.ring("ob", 3, [128, TG], BF16)
                vtb = self.ring("vtb", 2, [128, TG // 128, 128], BF16)
                tmp = self.ring("ptmp", 3, [128, 512], F32)
                cqg = self.sb("cqg", [128, 4, TG], BF16)
                sqq = self.sb("sqq", [128, 4, TG], BF16)
                rq = self.sb("rq", [128, TG], F32)
                gq = self.sb("gq", [128, 4], F32)
                gkv = self.sb("gkv", [128, 2], F32)
                fb = self.sb("fb", [4, 1], F32)
                S.dma('sp', gq.t[:], self.mla_q_norm[l], writes=['gq'])
                S.dma('sp', gkv.t[:], self.mla_kv_norm[l], writes=['gkv'])
                S.dma('sp', fb.t[:], self.fox_f_bias[l], writes=['fb'])
                eng_alt = [0]

                def evac_copy(dst_ap, src_ap, r, w, func=AF.Copy):
                    if func == AF.Copy and eng_alt[0] % 2 == 0:
                        S.op('dve', lambda e: e.tensor_copy(out=dst_ap, in_=src_ap), reads=r, writes=w)
                    else:
                        S.op('act', lambda e: e.activation(out=dst_ap, in_=src_ap, func=func), reads=r, writes=w)
                    eng_alt[0] += 1

                def project(wb, nk, M, rhs_t, rhs_key):
                    outs = []
                    for n in range(NT):
                        p = pp.get()
                        for k in range(nk):
                            S.op('pe', lambda e: e.matmul(p.t[0:M, :], lhsT=wb.t[:, k * M:(k + 1) * M],
                                                          rhs=rhs_t[:, k, n * 512:(n + 1) * 512], start=(k == 0), stop=(k == nk - 1)),
                                 reads=[wb.k, rhs_key], writes=[p.k])
                        outs.append(p)
                    return outs

                def rope_store(o, M, rtab, rmat_c, dst_ap, dkey):
                    o2 = ob.get()
                    for n in range(NT):
                        sl = slice(n * 512, (n + 1) * 512)
                        p = pr.get()
                        S.op('pe', lambda e: e.matmul(p.t[0:M, :], lhsT=self.cbs(rmat_c, M, M), rhs=o.t[0:M, sl], start=True, stop=True),
                             reads=[o.k, 'cb'], writes=[p.k])
                        t1 = tmp.get()
                        S.op('dve', lambda e: e.tensor_tensor(out=t1.t[0:M, :], in0=o.t[0:M, sl], in1=rtab.t[0:M, 0, sl], op=ALU.mult),
                             reads=[o.k, rtab.k], writes=[t1.k])
                        t2 = tmp.get()
                        S.op('dve', lambda e: e.tensor_tensor(out=t2.t[0:M, :], in0=p.t[0:M, :], in1=rtab.t[0:M, 1, sl], op=ALU.mult),
                             reads=[p.k, rtab.k], writes=[t2.k])
                        S.op('dve', lambda e: e.tensor_tensor(out=o2.t[0:M, sl], in0=t1.t[0:M, :], in1=t2.t[0:M, :], op=ALU.add),
                             reads=[t1.k, t2.k], writes=[o2.k])
                    S.dma('sp', dst_ap, o2.t[0:M, :], reads=[o2.k], writes=[dkey])

                def v_store(o, idx):
                    vt = vtb.get()
                    for c8 in range(0, TG // 128, 8):
                        p = ptr.get()
                        nn = min(8, TG // 128 - c8)
                        for j in range(nn):
                            c = c8 + j
                            S.op('pe', lambda e: e.transpose(p.t[:, j * 128:(j + 1) * 128], o.t[:, c * 128:(c + 1) * 128], self.cbs(C_IDENT, 128)),
                                 reads=[o.k, 'cb'], writes=[p.k])
                        evac_copy(vt.t[:, c8:c8 + nn, :], p.t[:, 0:nn * 128].rearrange("p (c d) -> p c d", c=nn), [p.k], [vt.k])
                    S.dma('sp', self.vv[idx, t0:t0 + TG, :].rearrange("(c p) d -> p c d", p=128), vt.t[:], reads=[vt.k],
                          writes=[('D', 'vv', idx, tg)])

                import os
                fams = os.environ.get("PROJ_FAMS")

                def do_tile(fam, c0, M, info, wb=None):
                    if fams and fam not in fams.split(','):
                        return
                    if wb is None:
                        wb = self.wload(w_in[:, :, c0:c0 + M], 16, M)
                    outs = project(wb, 16, M, xt.t, 'xt')
                    if fam in ('plain', 'gate', 'v', 'rope_d', 'kr'):
                        o = ob.get()
                        for n, p in enumerate(outs):
                            evac_copy(o.t[0:M, n * 512:(n + 1) * 512], p.t[0:M, :], [p.k], [o.k],
                                      func=(AF.Sigmoid if fam == 'gate' else AF.Copy))
                        if fam == 'plain':
                            dst = self.qT if info[0] == 'qT' else self.kT
                            S.dma('sp', dst[info[1], :, t0:t0 + TG], o.t[:], reads=[o.k], writes=[('D', info[0], info[1], tg)])
                        elif fam == 'gate':
                            S.dma('sp', self.gatesT[info * 128:(info + 1) * 128, t0:t0 + TG], o.t[:], reads=[o.k],
                                  writes=[('D', 'gatesT', info, tg)])
                        elif fam == 'v':
                            v_store(o, info)
                        elif fam == 'rope_d':
                            dst = self.qT if info[0] == 'qT' else self.kT
                            rope_store(o, 128, rD, C_RD, dst[info[1], :, t0:t0 + TG], ('D', info[0], info[1], tg))
                        elif fam == 'kr':
                            rope_store(o, 64, rM, C_RM, self.krT[:, t0:t0 + TG], ('D', 'krT', tg))
                    elif fam in ('cq', 'ckv'):
                        gg = gq if fam == 'cq' else gkv
                        for n, p in enumerate(outs):
                            sl = slice(n * 512, (n + 1) * 512)
                            tq = tmp.get()
                            S.op('act', lambda e: e.activation(out=tq.t[:], in_=p.t[:], func=AF.Copy), reads=[p.k], writes=[tq.k])
                            S.op('dve', lambda e: e.tensor_tensor(out=sqq.t[:, info, sl], in0=tq.t[:], in1=tq.t[:], op=ALU.mult), reads=[tq.k], writes=['sqq'])
                            S.op('dve', lambda e: e.tensor_scalar(out=cqg.t[:, info, sl], in0=tq.t[:], scalar1=gg.t[:, info:info + 1], scalar2=None,
                                                                  op0=ALU.mult), reads=[tq.k, gg.k], writes=['cqg'])
                    elif fam == 'f':
                        for n, p in enumerate(outs):
                            ft = tmp.get()
                            S.op('dve', lambda e: e.tensor_scalar(out=ft.t[0:4, :], in0=p.t[0:4, :], scalar1=fb.t[0:4, 0:1], scalar2=None, op0=ALU.add),
                                 reads=[p.k, 'fb'], writes=[ft.k])
                            S.dma('sp', self.fT[:, t0 + n * 512:t0 + (n + 1) * 512], ft.t[0:4, :], reads=[ft.k], writes=[('D', 'fT', tg, n)])

                def rms_scale(nk, dim):
                    for n in range(NT):
                        sl = slice(n * 512, (n + 1) * 512)
                        p = pp.get()
                        for k in range(nk):
                            S.op('pe', lambda e: e.matmul(p.t[:], lhsT=self.cbs(C_ONES, 128), rhs=sqq.t[:, k, sl], start=(k == 0), stop=(k == nk - 1)),
                                 reads=['sqq', 'cb'], writes=[p.k])
                        S.op('act', lambda e: e.activation(out=rq.t[:, sl], in_=p.t[:], func=AF.Sqrt, bias=self.epsrms.t[:, 0:1], scale=1.0 / dim),
                             reads=[p.k, 'epsrms'], writes=['rq'])
                        S.op('dve', lambda e: e.reciprocal(out=rq.t[:, sl], in_=rq.t[:, sl]), reads=['rq'], writes=['rq'])

                def scaled(outs, M):
                    o = ob.get()
                    for n, p in enumerate(outs):
                        sl = slice(n * 512, (n + 1) * 512)
                        S.op('dve', lambda e: e.tensor_tensor(out=o.t[0:M, sl], in0=p.t[0:M, :], in1=rq.t[0:M, sl], op=ALU.mult),
                             reads=[p.k, 'rq'], writes=[o.k])
                    return o
                wuq = self.mla_w_uq[l].rearrange("(k p) c -> p k c", p=128)
                wukv = self.mla_w_ukv[l].rearrange("(k p) c -> p k c", p=128)
                mla_on = os.environ.get("PROJ_MLA", "1") == "1"
                for h in range(4 if mla_on else 0):
                    do_tile('cq', OFF['mla_cq'] + h * 128, 128, h)
                if mla_on:
                    rms_scale(4, 512)
                for h in range(4 if mla_on else 0):
                    wb = self.wload(wuq[:, :, h * 192:h * 192 + 128], 4, 128)
                    o = scaled(project(wb, 4, 128, cqg.t, 'cqg'), 128)
                    S.dma('sp', self.qT[8 + h, :, t0:t0 + TG], o.t[:], reads=[o.k], writes=[('D', 'qT', 8 + h, tg)])
                    wb = self.wload(wuq[:, :, h * 192 + 128:h * 192 + 192], 4, 64)
                    o = scaled(project(wb, 4, 64, cqg.t, 'cqg'), 64)
                    rope_store(o, 64, rM, C_RM, self.qrT[h, :, t0:t0 + TG], ('D', 'qrT', h, tg))
                for j in range(2 if mla_on else 0):
                    do_tile('ckv', OFF['mla_ckv'] + j * 128, 128, j)
                if mla_on:
                    rms_scale(2, 256)
                for h in range(4 if mla_on else 0):
                    wb = self.wload(wukv[:, :, h * 256:h * 256 + 128], 2, 128)
                    o = scaled(project(wb, 2, 128, cqg.t, 'cqg'), 128)
                    S.dma('sp', self.kT[8 + h, :, t0:t0 + TG], o.t[:], reads=[o.k], writes=[('D', 'kT', 8 + h, tg)])
                    wb = self.wload(wukv[:, :, h * 256 + 128:h * 256 + 256], 2, 128)
                    o = scaled(project(wb, 2, 128, cqg.t, 'cqg'), 128)
                    v_store(o, 8 + h)
                do_tile('kr', OFF['mla_kr'], 64, 0)
                do_tile('f', OFF['fox_f'], 4, 0)
                lst = []
                for h in range(4):
                    lst.append(('rope_d', OFF['diff_q'] + h * 128, 128, ('qT', 0 + h)))
                    lst.append(('rope_d', OFF['diff_k'] + h * 128, 128, ('kT', 0 + h)))
                    lst.append(('v', OFF['diff_v'] + h * 128, 128, 0 + h))
                    lst.append(('plain', OFF['fox_q'] + h * 128, 128, ('qT', 4 + h)))
                    lst.append(('plain', OFF['fox_k'] + h * 128, 128, ('kT', 4 + h)))
                    lst.append(('v', OFF['fox_v'] + h * 128, 128, 4 + h))
                    lst.append(('plain', OFF['sb_q'] + h * 128, 128, ('qT', 12 + h)))
                    lst.append(('plain', OFF['sb_k'] + h * 128, 128, ('kT', 12 + h)))
                    lst.append(('v', OFF['sb_v'] + h * 128, 128, 12 + h))
                for g in range(64):
                    lst.append(('gate', OFF['gates'] + g * 128, 128, g))
                if fams:
                    for it in lst:
                        do_tile(*it)
                else:
                    nxt_wb = self.wload(w_in[:, :, lst[0][1]:lst[0][1] + 128], 16, 128)
                    for ii, it in enumerate(lst):
                        wb_c = nxt_wb
                        if ii + 1 < len(lst):
                            c0n = lst[ii + 1][1]
                            nxt_wb = self.wload(w_in[:, :, c0n:c0n + 128], 16, 128)
                        do_tile(*it, wb=wb_c)

    def phase_foxprep(self, l):
        S, SL = self.S, self.SL
        with self.scope() as es:
            z = self.sb("fz", [4, SL], F32)
            a = self.sb("fa", [4, SL], F32)
            b = self.sb("fb2", [4, SL], F32)
            hb = [self.sb("fh%d" % i, [4, SL], BF16) for i in range(3)]
            rk = [('D', 'fT', tg, n) for tg in range(self.NTG) for n in range(self.TG // 512)]
            S.dma('sp', z.t[:], self.fT[:, :], reads=rk, writes=['fz'])
            S.op('act', lambda e: e.activation(out=a.t[:], in_=z.t[:], func=AF.Exp, scale=-1.0), reads=['fz'], writes=['fa'])
            S.op('act', lambda e: e.activation(out=a.t[:], in_=a.t[:], func=AF.Ln, bias=1.0), reads=['fa'], writes=['fa'])
            S.op('dve', lambda e: e.tensor_scalar(out=a.t[:], in0=a.t[:], scalar1=-1.0, scalar2=None, op0=ALU.mult), reads=['fa'], writes=['fa'])
            S.op('dve', lambda e: e.memset(b.t[:], 1.0), writes=['fb2'])
            S.op('dve', lambda e: e.tensor_tensor_scan(out=z.t[:], data0=b.t[:], data1=a.t[:], initial=0.0, op0=ALU.mult, op1=ALU.add),
                 reads=['fa', 'fb2'], writes=['fz'])
            S.dma('sp', self.cumT[:, :], z.t[:], reads=['fz'], writes=[('D', 'cumT')])
            S.op('dve', lambda e: e.tensor_scalar(out=a.t[:], in0=z.t[:], scalar1=math.sqrt(128.0), scalar2=None, op0=ALU.mult), reads=['fz'], writes=['fa'])
            for i in range(3):
                S.op('dve', lambda e: e.tensor_copy(out=hb[i].t[:], in_=a.t[:]), reads=['fa'], writes=[hb[i].k])
                if i < 2:
                    S.op('dve', lambda e: e.tensor_tensor(out=a.t[:], in0=a.t[:], in1=hb[i].t[:], op=ALU.subtract), reads=['fa', hb[i].k], writes=['fa'])
                for h in range(4):
                    S.dma('sp', self.qaug[h, i:i + 1, :], hb[i].t[h:h + 1, :], reads=[hb[i].k], writes=[('D', 'qaug', h, i)])

    def phase_attn(self, l):
        S, SL, NCH, NQT = self.S, self.SL, self.NCH, self.NQT
        NTG = self.NTG
        with self.scope() as es:
            KT = self.ring("KT", 2, [128, SL], BF16)
            QT = self.ring("QT", 2, [128, SL], BF16)
            VV = self.ring("VV", 2, [128, NCH, 128], BF16)
            OT = self.ring("OT", 2, [128, SL], BF16)
            KR = self.sb("KR", [64, SL], BF16)
            QR = self.ring("QR", 2, [64, SL], BF16)
            QA = self.ring("QA", 2, [3, SL], BF16)
            NCUM = self.ring("NCUM", 2, [128, NCH], F32)
            sps = self.ring("sps", 4, [128, 512], F32, psum=True)
            ops_ = self.ring("ops", 2, [128, 512], F32, psum=True)
            dps = self.ring("dps", 2, [128, 512], F32, psum=True)
            lps = sps
            PT = self.ring("PT", 6, [128, 512], BF16)
            rec = self.ring("rec", 2, [128, 512], F32)
            o0 = self.ring("o0", 2, [128, 512], F32)
            o1 = self.ring("o1", 2, [128, 512], F32)
            f32t = self.ring("f32t", 4, [128, 512], F32)
            sqb = self.ring("sqb", 2, [128, 512], BF16)
            lp = self.sb("lp", [128, 256], F32)
            lpp = self.sb("lpp", [128, 128], F32)
            lam2 = self.sb("lam2", [128, 2], F32)
            neglam = self.sb("neglam", [128, 1], F32)
            gsub = self.sb("gsub", [128, 1], F32)
            lam_init = 0.8 - 0.6 * math.exp(-0.3 * l)
            S.dma('sp', lp.t[:], self.diff_lambda[l, 0:1, :].partition_broadcast(128), writes=['lp'])
            S.dma('sp', gsub.t[:], self.diff_subln[l], writes=['gsub'])
            S.op('dve', lambda e: e.tensor_scalar(out=gsub.t[:], in0=gsub.t[:], scalar1=(1.0 - lam_init), scalar2=None, op0=ALU.mult), reads=['gsub'], writes=['gsub'])
            lp4 = lp.t[:].rearrange("p (a d) -> p a d", a=4)
            lpp2 = lpp.t[:].rearrange("p (a d) -> p a d", a=2)
            S.op('dve', lambda e: e.tensor_tensor(out=lpp2[:, 0, :], in0=lp4[:, 0, :], in1=lp4[:, 1, :], op=ALU.mult), reads=['lp'], writes=['lpp'])
            S.op('dve', lambda e: e.tensor_tensor(out=lpp2[:, 1, :], in0=lp4[:, 2, :], in1=lp4[:, 3, :], op=ALU.mult), reads=['lp'], writes=['lpp'])
            S.op('dve', lambda e: e.reduce_sum(out=lam2.t[:], in_=lpp2, axis=mybir.AxisListType.X), reads=['lpp'], writes=['lam2'])
            S.op('act', lambda e: e.activation(out=lam2.t[:], in_=lam2.t[:], func=AF.Exp), reads=['lam2'], writes=['lam2'])
            S.op('dve', lambda e: e.tensor_tensor(out=neglam.t[:], in0=lam2.t[:, 1:2], in1=lam2.t[:, 0:1], op=ALU.subtract), reads=['lam2'], writes=['neglam'])
            S.op('dve', lambda e: e.tensor_scalar(out=neglam.t[:], in0=neglam.t[:], scalar1=-lam_init, scalar2=None, op0=ALU.add), reads=['neglam'], writes=['neglam'])

            def dkeys(name, idx):
                return [('D', name, idx, tg) for tg in range(NTG)]

            def load_head(idx, need_v=True):
                kt = KT.get()
                qt = QT.get()
                S.dma('sp', kt.t[:], self.kT[idx], reads=dkeys('kT', idx), writes=[kt.k])
                S.dma('sp', qt.t[:], self.qT[idx], reads=dkeys('qT', idx), writes=[qt.k])
                vt = VV.get()
                vsrc = self.vv[idx].rearrange("(c p) d -> p c d", p=128)
                for c0 in range(0, NCH, 8):
                    S.dma('sp', vt.t[:, c0:min(NCH, c0 + 8), :], vsrc[:, c0:min(NCH, c0 + 8), :], reads=dkeys('vv', idx), writes=[(vt.k, c0 // 8)])
                return kt, qt, vt

            def drive(streams):
                live = list(streams)
                while live:
                    nxt = []
                    for g_ in live:
                        try:
                            next(g_)
                            nxt.append(g_)
                        except StopIteration:
                            pass
                    live = nxt

            def softmax_tile(i, parts, scale, madd_c, vt, O, Dn, bias_t=None, aug=None):
                last = 4 * i + 3

                def emit_qk(g):
                    sp_ = sps.get()
                    j0 = g - 4 * i
                    nmm = len(parts) + (1 if aug is not None else 0) + (1 if j0 >= 0 else 0)
                    c = 0
                    for (kt, qt, p0, p1) in parts:
                        S.op('pe', lambda e: e.matmul(sp_.t[:], lhsT=kt.t[p0:p1, g * 128:(g + 1) * 128], rhs=qt.t[p0:p1, i * 512:(i + 1) * 512],
                                                      start=(c == 0), stop=(c == nmm - 1)), reads=[kt.k, qt.k], writes=[sp_.k])
                        c += 1
                    if aug is not None:
                        S.op('pe', lambda e: e.matmul(sp_.t[:], lhsT=self.cb.t[0:3, C_ONES:C_ONES + 128], rhs=aug.t[0:3, i * 512:(i + 1) * 512],
                                                      start=False, stop=(c == nmm - 1)), reads=[aug.k, 'cb'], writes=[sp_.k])
                        c += 1
                    if j0 >= 0:
                        S.op('pe', lambda e: e.matmul(sp_.t[:], lhsT=self.cbs(C_IDENT, 128), rhs=self.cbs(madd_c + j0 * 512, 512),
                                                      start=False, stop=True), reads=['cb'], writes=[sp_.k])
                    return sp_
                nxt = emit_qk(0)
                for g in range(last + 1):
                    sp_ = nxt
                    if g < last:
                        nxt = emit_qk(g + 1)
                    pt = PT.get()
                    if bias_t is not None:
                        S.op('act', lambda e: e.activation(out=pt.t[:], in_=sp_.t[:], func=AF.Exp, scale=scale, bias=bias_t.t[:, g:g + 1]),
                             reads=[sp_.k, bias_t.k], writes=[pt.k])
                    else:
                        S.op('act', lambda e: e.activation(out=pt.t[:], in_=sp_.t[:], func=AF.Exp, scale=scale), reads=[sp_.k], writes=[pt.k])
                    S.op('pe', lambda e: e.matmul(O.t[:], lhsT=vt.t[:, g, :], rhs=pt.t[:], start=(g == 0), stop=(g == last)),
                         reads=[(vt.k, g // 8), pt.k], writes=[O.k])
                    S.op('pe', lambda e: e.matmul(Dn.t[:], lhsT=self.cbs(C_ONES, 128), rhs=pt.t[:], start=(g == 0), stop=(g == last)),
                         reads=['cb', pt.k], writes=[Dn.k])
                    yield

            def normalize(O, Dn, dst_ap, wkey):
                r = rec.get()
                S.op('dve', lambda e: e.reciprocal(out=r.t[:], in_=Dn.t[:]), reads=[Dn.k], writes=[r.k])
                S.op('dve', lambda e: e.tensor_tensor(out=dst_ap, in0=O.t[:], in1=r.t[:], op=ALU.mult), reads=[O.k, r.k], writes=[wkey])

            for h in range(4):
                idx = h
                kt, qt, vt = load_head(idx)
                ot = OT.get()
                for i in range(NQT):
                    accs = [(ops_.get(), dps.get()) for c in range(2)]
                    drive([softmax_tile(i, [(kt, qt, c * 64, (c + 1) * 64)], 64 ** -0.5, C_MADD_CH, vt, accs[c][0], accs[c][1]) for c in range(2)])
                    oc = []
                    for c in range(2):
                        dst = (o0 if c == 0 else o1).get()
                        normalize(accs[c][0], accs[c][1], dst.t[:], dst.k)
                        oc.append(dst)
                    d = f32t.get()
                    S.op('dve', lambda e: e.scalar_tensor_tensor(out=d.t[:], in0=oc[1].t[:], scalar=neglam.t[:, 0:1], in1=oc[0].t[:],
                                                                 op0=ALU.mult, op1=ALU.add), reads=[oc[0].k, oc[1].k, 'neglam'], writes=[d.k])
                    sq = sqb.get()
                    S.op('pool', lambda e: e.tensor_tensor(out=sq.t[:], in0=d.t[:], in1=d.t[:], op=ALU.mult), reads=[d.k], writes=[sq.k])
                    nps = lps.get()
                    S.op('pe', lambda e: e.matmul(nps.t[:], lhsT=self.cbs(C_ONES, 128), rhs=sq.t[:], start=True, stop=True), reads=[sq.k, 'cb'], writes=[nps.k])
                    rt = f32t.get()
                    S.op('act', lambda e: e.activation(out=rt.t[:], in_=nps.t[:], func=AF.Sqrt, bias=self.epsrms.t[:, 0:1], scale=1.0 / 128),
                         reads=[nps.k, 'epsrms'], writes=[rt.k])
                    S.op('dve', lambda e: e.reciprocal(out=rt.t[:], in_=rt.t[:]), reads=[rt.k], writes=[rt.k])
                    S.op('dve', lambda e: e.scalar_tensor_tensor(out=ot.t[:, i * 512:(i + 1) * 512], in0=d.t[:], scalar=gsub.t[:, 0:1], in1=rt.t[:],
                                                                 op0=ALU.mult, op1=ALU.mult), reads=[d.k, rt.k, 'gsub'], writes=[ot.k])
                S.dma('sp', self.oT[idx], ot.t[:], reads=[ot.k], writes=[('D', 'oT', idx)])

            def head_stream(idx, parts_fn, scale, madd_c, bias_t=None, aug=None):
                kt, qt, vt = load_head(idx)
                parts = parts_fn(kt, qt)
                ot = OT.get()
                for i in range(NQT):
                    O = ops_.get()
                    Dn = dps.get()
                    for _ in softmax_tile(i, parts, scale, madd_c, vt, O, Dn, bias_t=bias_t, aug=aug):
                        yield
                    normalize(O, Dn, ot.t[:, i * 512:(i + 1) * 512], ot.k)
                S.dma('sp', self.oT[idx], ot.t[:], reads=[ot.k], writes=[('D', 'oT', idx)])

            def fox_stream(h):
                qa = QA.get()
                S.dma('sp', qa.t[:], self.qaug[h], reads=[('D', 'qaug', h, i) for i in range(3)], writes=[qa.k])
                ncum = NCUM.get()
                with self.nc.allow_non_contiguous_dma(reason="tiny strided column load"):
                    csrc = self.cumT[h].rearrange("(c p) -> p c", p=128)
                    for c0 in range(0, NCH, 8):
                        S.dma('sp', ncum.t[:, c0:min(NCH, c0 + 8)], csrc[:, c0:min(NCH, c0 + 8)], reads=[('D', 'cumT')], writes=[(ncum.k, 'ld', c0 // 8), ncum.k])
                S.op('dve', lambda e: e.tensor_scalar(out=ncum.t[:], in0=ncum.t[:], scalar1=-1.0, scalar2=None, op0=ALU.mult),
                     reads=[(ncum.k, 'ld', j) for j in range((NCH + 7) // 8)], writes=[ncum.k])
                return head_stream(4 + h, lambda kt, qt: [(kt, qt, 0, 128)], 128 ** -0.5, C_MADD_CA, bias_t=ncum, aug=qa)
            for h0 in (0, 2):
                drive([fox_stream(h0), fox_stream(h0 + 1)])

            S.dma('sp', KR.t[:], self.krT[:, :], reads=[('D', 'krT', tg) for tg in range(NTG)], writes=['KR'])

            def mla_stream(h):
                qr = QR.get()
                S.dma('sp', qr.t[:], self.qrT[h], reads=dkeys('qrT', h), writes=[qr.k])
                return head_stream(8 + h, lambda kt, qt: [(kt, qt, 0, 128), (KR, qr, 0, 64)], 192 ** -0.5, C_MADD_CH)
            for h0 in (0, 2):
                drive([mla_stream(h0), mla_stream(h0 + 1)])

        with self.scope() as es:
            KT = self.ring("KT", 2, [128, SL], BF16)
            QT = self.ring("QT", 2, [128, SL], BF16)
            VV = self.ring("VV", 2, [128, NCH, 128], BF16)
            OT = self.ring("OT", 2, [128, SL], BF16)
            sps = self.ring("sps", 4, [128, 512], F32, psum=True)
            ops_ = self.ring("ops", 2, [128, 512], F32, psum=True)
            lps = self.ring("lps", 2, [128, 512], F32, psum=True)
            PT = self.ring("PT", 8, [128, 512], BF16)
            f32t = self.ring("f32t", 10, [128, 512], F32)
            sp32 = self.ring("sp32", 6, [128, 512], F32)
            spb = self.ring("spb", 6, [128, 512], BF16)
            sc = 128 ** -0.5

            def sb_stream(h, cast_eng):
                idx = 12 + h
                kt = KT.get()
                qt = QT.get()
                S.dma('sp', kt.t[:], self.kT[idx], reads=dkeys('kT', idx), writes=[kt.k])
                S.dma('sp', qt.t[:], self.qT[idx], reads=dkeys('qT', idx), writes=[qt.k])
                vt = VV.get()
                vsrc = self.vv[idx].rearrange("(c p) d -> p c d", p=128)
                for c0 in range(0, NCH, 8):
                    S.dma('sp', vt.t[:, c0:min(NCH, c0 + 8), :], vsrc[:, c0:min(NCH, c0 + 8), :], reads=dkeys('vv', idx), writes=[(vt.k, c0 // 8)])
                ot = OT.get()
                spsum = self.sb("spsum%d" % h, [128, 512], F32)
                spsumb = self.ring("spsumb%d" % h, 3, [128, 512], BF16)
                for i in range(NQT):
                    O = ops_.get()
                    last = 4 * i + 3
                    order = list(range(last, -1, -1))
                    n = len(order)
                    st1 = {}
                    st2 = {}
                    ssb_prev = [None]

                    def stage1(g):
                        j0 = g - 4 * i
                        z = sps.get()
                        S.op('pe', lambda e: e.matmul(z.t[:], lhsT=kt.t[:, g * 128:(g + 1) * 128], rhs=qt.t[:, i * 512:(i + 1) * 512], start=True, stop=True),
                             reads=[kt.k, qt.k], writes=[z.k])
                        ee = f32t.get()
                        S.op('act', lambda e: e.activation(out=ee.t[:], in_=z.t[:], func=AF.Exp, scale=sc), reads=[z.k], writes=[ee.k])
                        s32 = sp32.get()
                        S.op('act', lambda e: e.activation(out=s32.t[:], in_=ee.t[:], func=AF.Ln, bias=1.0), reads=[ee.k], writes=[s32.k])
                        sb_ = spb.get()
                        if j0 >= 0:
                            S.op('pool', lambda e: e.tensor_tensor(out=sb_.t[:], in0=s32.t[:], in1=self.cbs(C_MMUL + j0 * 512, 512), op=ALU.mult),
                                 reads=[s32.k, 'cb'], writes=[sb_.k])
                        else:
                            S.op('dve', lambda e: e.tensor_copy(out=sb_.t[:], in_=s32.t[:]), reads=[s32.k], writes=[sb_.k])
                        st1[g] = (z, s32, sb_)

                    def stage2(g):
                        j0 = g - 4 * i
                        z, s32, sb_ = st1.pop(g)
                        first = (g == last)
                        Lp = lps.get()
                        S.op('pe', lambda e: e.matmul(Lp.t[:], lhsT=self.cbs(C_TRIS, 128), rhs=sb_.t[:], start=True, stop=first), reads=[sb_.k, 'cb'], writes=[Lp.k])
                        if not first:
                            ssb = ssb_prev[0]
                            S.op('pe', lambda e: e.matmul(Lp.t[:], lhsT=self.cbs(C_ONES, 128), rhs=ssb.t[:], start=False, stop=True), reads=[ssb.k, 'cb'], writes=[Lp.k])
                        t1 = f32t.get()
                        S.op('dve', lambda e: e.scalar_tensor_tensor(out=t1.t[:], in0=z.t[:], scalar=sc, in1=s32.t[:], op0=ALU.mult, op1=ALU.subtract),
                             reads=[z.k, s32.k], writes=[t1.k])
                        S.op('dve', lambda e: e.tensor_tensor(out=t1.t[:], in0=t1.t[:], in1=Lp.t[:], op=ALU.subtract), reads=[t1.k, Lp.k], writes=[t1.k])
                        wt = PT.get()
                        S.op('act', lambda e: e.activation(out=wt.t[:], in_=t1.t[:], func=AF.Exp), reads=[t1.k], writes=[wt.k])
                        if j0 >= 0:
                            S.op('pool', lambda e: e.tensor_tensor(out=wt.t[:], in0=wt.t[:], in1=self.cbs(C_MMUL + j0 * 512, 512), op=ALU.mult),
                                 reads=[wt.k, 'cb'], writes=[wt.k])
                        if g > 0:
                            if first:
                                S.op('pool', lambda e: e.tensor_copy(out=spsum.t[:], in_=sb_.t[:]), reads=[sb_.k], writes=[spsum.k])
                            else:
                                S.op('dve', lambda e: e.tensor_tensor(out=spsum.t[:], in0=spsum.t[:], in1=sb_.t[:], op=ALU.add), reads=[spsum.k, sb_.k], writes=[spsum.k])
                            ssb = spsumb.get()
                            S.op('act', lambda e: e.activation(out=ssb.t[:], in_=spsum.t[:], func=AF.Copy), reads=[spsum.k], writes=[ssb.k])
                            ssb_prev[0] = ssb
                        st2[g] = wt

                    def stage3(g):
                        wt = st2.pop(g)
                        S.op('pe', lambda e: e.matmul(O.t[:], lhsT=vt.t[:, g, :], rhs=wt.t[:], start=(g == last), stop=(g == 0)), reads=[(vt.k, g // 8), wt.k], writes=[O.k])
                    for step in range(n + 2):
                        if step < n:
                            stage1(order[step])
                        if 0 <= step - 1 < n:
                            stage2(order[step - 1])
                        if 0 <= step - 2 < n:
                            stage3(order[step - 2])
                        yield
                    S.op('act', lambda e: e.activation(out=ot.t[:, i * 512:(i + 1) * 512], in_=O.t[:], func=AF.Copy), reads=[O.k], writes=[ot.k])
                S.dma('sp', self.oT[idx], ot.t[:], reads=[ot.k], writes=[('D', 'oT', idx)])
            for h0 in (0, 2):
                drive([sb_stream(h0, 'dve'), sb_stream(h0 + 1, 'dve')])

    def phase_merge(self, l):
        S, SL, TG = self.S, self.SL, self.TG
        NT = TG // 512
        for tg in range(self.NTG):
            t0 = tg * TG
            with self.scope() as es:
                ot = self.sb("mot", [128, 16, TG], BF16)
                for i in range(16):
                    S.dma('sp', ot.t[:, i, :], self.oT[i, :, t0:t0 + TG], reads=[('D', 'oT', i)], writes=['mot'])
                pp = self.ring("mpp", 4, [128, 512], F32, psum=True)
                gt = self.ring("mgt", 3, [128, TG], BF16)
                acc = self.sb("macc", [128, TG], F32)
                mo = self.ring("mmo", 2, [128, TG], BF16)
                tmp = self.ring("mtmp", 2, [128, 512], F32)
                def fetch(c, n_):
                    wb_ = self.wload(self.w_branch[l, n_].rearrange("(k p) c -> p k c", p=128)[:, :, c * 128:(c + 1) * 128], 4, 128)
                    g_ = gt.get()
                    grow = n_ * 16 + c
                    S.dma('sp', g_.t[:], self.gatesT[grow * 128:(grow + 1) * 128, t0:t0 + TG], reads=[('D', 'gatesT', grow, tg)], writes=[g_.k])
                    return wb_, g_
                nxt_f = fetch(0, 0)
                for c in range(16):
                    for n_ in range(4):
                        wb, g = nxt_f
                        if not (c == 15 and n_ == 3):
                            nxt_f = fetch(c + (n_ + 1) // 4, (n_ + 1) % 4)
                        wv = wb.t[:, 0:512].rearrange("p (k m) -> p k m", k=4)
                        for n in range(NT):
                            sl = slice(n * 512, (n + 1) * 512)
                            p = pp.get()
                            for k in range(4):
                                S.op('pe', lambda e: e.matmul(p.t[:], lhsT=wv[:, k, :], rhs=ot.t[:, n_ * 4 + k, sl], start=(k == 0), stop=(k == 3)),
                                     reads=[wb.k, 'mot'], writes=[p.k])
                            if n_ == 0:
                                S.op('dve', lambda e: e.tensor_tensor(out=acc.t[:, sl], in0=p.t[:], in1=g.t[:, sl], op=ALU.mult), reads=[p.k, g.k], writes=['macc'])
                            else:
                                t = tmp.get()
                                S.op('dve', lambda e: e.tensor_tensor(out=t.t[:], in0=p.t[:], in1=g.t[:, sl], op=ALU.mult), reads=[p.k, g.k], writes=[t.k])
                                S.op('pool', lambda e: e.tensor_tensor(out=acc.t[:, sl], in0=acc.t[:, sl], in1=t.t[:], op=ALU.add), reads=['macc', t.k], writes=['macc'])
                    m = mo.get()
                    S.op('act', lambda e: e.activation(out=m.t[:], in_=acc.t[:], func=AF.Copy), reads=['macc'], writes=[m.k])
                    S.dma('sp', self.mergedT[c * 128:(c + 1) * 128, t0:t0 + TG], m.t[:], reads=[m.k], writes=[('D', 'mergedT', c, tg)])

    def load_resident(self, dst, w_ap, nk):
        S = self.S
        wv = w_ap.rearrange("(k p) c -> p k c", p=128)
        for k in range(nk):
            st = self.wst.get()
            S.dma('sp', st.t[:], wv[:, k, :], writes=[st.k])
            self.cast(dst.t[:, k, :], st.t[:], [st.k], [dst.k])

    def layer_norm(self, v, stt, mvt, g_t, b_t, out):
        S = self.S
        for j in range(4):
            S.op('dve', lambda e: e.bn_stats(out=stt.t[:, j * 6:(j + 1) * 6], in_=v.t[:, j * 512:(j + 1) * 512]), reads=[v.k], writes=[stt.k])
        S.op('dve', lambda e: e.bn_aggr(out=mvt.t[:, 0:2], in_=stt.t[:]), reads=[stt.k], writes=[mvt.k])
        S.op('act', lambda e: e.activation(out=mvt.t[:, 2:3], in_=mvt.t[:, 1:2], func=AF.Sqrt, bias=self.epsln.t[:, 0:1], scale=1.0),
             reads=[mvt.k, 'epsln'], writes=[mvt.k])
        S.op('dve', lambda e: e.reciprocal(out=mvt.t[:, 3:4], in_=mvt.t[:, 2:3]), reads=[mvt.k], writes=[mvt.k])
        S.op('dve', lambda e: e.tensor_scalar(out=out.t[:], in0=v.t[:], scalar1=mvt.t[:, 0:1], scalar2=mvt.t[:, 3:4], op0=ALU.subtract, op1=ALU.mult),
             reads=[v.k, mvt.k], writes=[out.k])
        S.op('pool', lambda e: e.tensor_tensor(out=out.t[:], in0=out.t[:], in1=g_t.t[:], op=ALU.mult), reads=[out.k, g_t.k], writes=[out.k])
        S.op('dve', lambda e: e.tensor_tensor(out=out.t[:], in0=out.t[:], in1=b_t.t[:], op=ALU.add), reads=[out.k, b_t.k], writes=[out.k])

    def transpose_store(self, hb, ptr, dstT_tile, evac_eng='act'):
        S = self.S
        for k8 in range(0, 16, 8):
            p = ptr.get()
            for j in range(8):
                k = k8 + j
                S.op('pe', lambda e: e.transpose(p.t[:, j * 128:(j + 1) * 128], hb.t[:, k * 128:(k + 1) * 128], self.cbs(C_IDENT, 128)),
                     reads=[hb.k, 'cb'], writes=[p.k])
            S.op('act', lambda e: e.activation(out=dstT_tile.t[:, k8:k8 + 8, :], in_=p.t[:].rearrange("p (c d) -> p c d", c=8), func=AF.Copy),
                 reads=[p.k], writes=[dstT_tile.k])

    def phase_wo(self, l):
        S, SL, NCH = self.S, self.SL, self.NCH
        CAP = self.CAP
        with self.scope() as es:
            wo = self.sb("wo", [128, 16, D], BF16)
            self.load_resident(wo, self.w_o[l], 16)
            wr = self.sb("wr", [128, 16, 36], BF16)
            wrs = self.sb("wrs", [128, 16, 36], F32)
            S.dma('sp', wrs.t[:, :, 0:4], self.w_rg[l].rearrange("(k p) c -> p k c", p=128), writes=['wrs'])
            S.dma('sp', wrs.t[:, :, 4:36], self.w_re[l].rearrange("(k p) c -> p k c", p=128), writes=['wrs'])
            S.op('dve', lambda e: e.tensor_copy(out=wr.t[:], in_=wrs.t[:]), reads=['wrs'], writes=['wr'])
            brt = self.sb("brt", [128, 36], F32)
            S.dma('sp', brt.t[:, 0:4], self.b_rg[l, 0:1, :].partition_broadcast(128), writes=['brt'])
            S.dma('sp', brt.t[:, 4:36], self.b_re[l, 0:1, :].partition_broadcast(128), writes=['brt'])
            g_t = self.sb("l1g", [128, D], F32)
            b_t = self.sb("l1b", [128, D], F32)
            S.dma('sp', g_t.t[:], self.ln1_g[l, 0:1, :].partition_broadcast(128), writes=['l1g'])
            S.dma('sp', b_t.t[:], self.ln1_b[l, 0:1, :].partition_broadcast(128), writes=['l1b'])
            base = self.sb("rbase", [128, 32], F32)
            S.op('dve', lambda e: e.memset(base.t[:], 0.0), writes=['rbase'])
            mT = self.ring("wmT", 2, [128, 16, 128], BF16)
            xr = self.ring("wxr", 2, [128, D], F32)
            ht = self.ring("wh", 2, [128, D], F32)
            hb = self.ring("whb", 2, [128, D], BF16)
            hTt = self.ring("whT", 2, [128, 16, 128], BF16)
            stt = self.ring("wstt", 2, [128, 24], F32)
            mvt = self.ring("wmv", 2, [128, 4], F32)
            pp = self.ring("wpp", 4, [128, 512], F32, psum=True)
            ptr = self.ring("wptr", 2, [128, 1024], BF16, psum=True)
            prt = self.ring("wprt", 2, [128, 64], F32, psum=True)
            sm = self.ring("wsm", 2, [128, 256], F32)
            mb = self.ring("wmb", 2, [128, 32], BF16)
            x_src = self.x_tm if l == 0 else self.xres
            mres = [('D', 'mergedT', c, tg) for c in range(16) for tg in range(self.NTG)]
            def stageA(c):
                r0 = c * 128
                m = mT.get()
                S.dma('sp', m.t[:], self.mergedT[:, r0:r0 + 128].rearrange("(k p) t -> p k t", p=128), reads=mres, writes=[m.k])
                x = xr.get()
                S.dma('sp', x.t[:], x_src[r0:r0 + 128, :], reads=([('D', 'xres', c)] if l > 0 else []), writes=[x.k])
                v = x
                for ct in range(4):
                    p = pp.get()
                    for k in range(16):
                        S.op('pe', lambda e: e.matmul(p.t[:], lhsT=m.t[:, k, :], rhs=wo.t[:, k, ct * 512:(ct + 1) * 512], start=(k == 0), stop=(k == 15)),
                             reads=[m.k, 'wo'], writes=[p.k])
                    S.op('dve', lambda e: e.scalar_tensor_tensor(out=v.t[:, ct * 512:(ct + 1) * 512], in0=x.t[:, ct * 512:(ct + 1) * 512], scalar=ALPHA,
                                                                 in1=p.t[:], op0=ALU.mult, op1=ALU.add), reads=[x.k, p.k], writes=[v.k])
                h = ht.get()
                self.layer_norm(v, stt.get(), mvt.get(), g_t, b_t, h)
                hbb = hb.get()
                S.op('act', lambda e: e.activation(out=hbb.t[:], in_=h.t[:], func=AF.Copy), reads=[h.k], writes=[hbb.k])
                return h, hbb

            def stageB(c, h, hbb):
                r0 = c * 128
                S.dma('sp', self.hres[r0:r0 + 128, :], h.t[:], reads=[h.k], writes=[('D', 'hres', c)])
                hT = hTt.get()
                self.transpose_store(hbb, ptr, hT)
                S.dma('sp', self.hT[:, r0:r0 + 128].rearrange("(k p) t -> p k t", p=128), hT.t[:], reads=[hT.k], writes=[('D', 'hT', c)])
                pr = prt.get()
                for k in range(16):
                    S.op('pe', lambda e: e.matmul(pr.t[:, 0:36], lhsT=hT.t[:, k, :], rhs=wr.t[:, k, :], start=(k == 0), stop=(k == 15)),
                         reads=[hT.k, 'wr'], writes=[pr.k])
                s = sm.get()
                sk = s.k
                A = s.t

                def dv(fn, extra_r=(), w=None):
                    S.op('dve', fn, reads=[sk] + list(extra_r), writes=[w or sk])
                dv(lambda e: e.tensor_tensor(out=A[:, 0:36], in0=pr.t[:, 0:36], in1=brt.t[:], op=ALU.add), extra_r=[pr.k, 'brt'])
                dv(lambda e: e.reduce_max(out=A[:, 36:37], in_=A[:, 0:4], axis=mybir.AxisListType.X))
                dv(lambda e: e.tensor_scalar(out=A[:, 224:228], in0=A[:, 0:4], scalar1=A[:, 36:37], scalar2=None, op0=ALU.subtract))
                S.op('act', lambda e: e.activation(out=A[:, 224:228], in_=A[:, 224:228], func=AF.Exp), reads=[sk], writes=[sk])
                dv(lambda e: e.reduce_sum(out=A[:, 37:38], in_=A[:, 224:228], axis=mybir.AxisListType.X))
                dv(lambda e: e.reciprocal(out=A[:, 38:39], in_=A[:, 37:38]))
                dv(lambda e: e.tensor_scalar(out=A[:, 40:44], in0=A[:, 0:4], scalar1=A[:, 36:37], scalar2=None, op0=ALU.is_equal))
                dv(lambda e: e.tensor_scalar(out=A[:, 44:52], in0=A[:, 4:12], scalar1=A[:, 40:41], scalar2=None, op0=ALU.mult))
                for j in range(1, 4):
                    dv(lambda e: e.scalar_tensor_tensor(out=A[:, 44:52], in0=A[:, 4 + 8 * j:12 + 8 * j], scalar=A[:, 40 + j:41 + j], in1=A[:, 44:52],
                                                        op0=ALU.mult, op1=ALU.add))
                dv(lambda e: e.reduce_max(out=A[:, 52:53], in_=A[:, 44:52], axis=mybir.AxisListType.X))
                dv(lambda e: e.tensor_scalar(out=A[:, 56:64], in0=A[:, 44:52], scalar1=A[:, 52:53], scalar2=None, op0=ALU.is_equal))
                dv(lambda e: e.scalar_tensor_tensor(out=A[:, 72:80], in0=A[:, 56:64], scalar=-1e30, in1=A[:, 44:52], op0=ALU.mult, op1=ALU.add))
                dv(lambda e: e.reduce_max(out=A[:, 53:54], in_=A[:, 72:80], axis=mybir.AxisListType.X))
                dv(lambda e: e.tensor_scalar(out=A[:, 64:72], in0=A[:, 72:80], scalar1=A[:, 53:54], scalar2=None, op0=ALU.is_equal))
                dv(lambda e: e.tensor_tensor(out=A[:, 80:81], in0=A[:, 53:54], in1=A[:, 52:53], op=ALU.subtract))
                S.op('act', lambda e: e.activation(out=A[:, 80:81], in_=A[:, 80:81], func=AF.Exp), reads=[sk], writes=[sk])
                dv(lambda e: e.tensor_scalar(out=A[:, 81:82], in0=A[:, 80:81], scalar1=1.0, scalar2=None, op0=ALU.add))
                dv(lambda e: e.reciprocal(out=A[:, 81:82], in_=A[:, 81:82]))
                dv(lambda e: e.tensor_tensor(out=A[:, 81:82], in0=A[:, 81:82], in1=A[:, 38:39], op=ALU.mult))
                dv(lambda e: e.tensor_tensor(out=A[:, 82:83], in0=A[:, 38:39], in1=A[:, 81:82], op=ALU.subtract))
                S.op('dve', lambda e: e.tensor_copy(out=self.w_all.t[:, c, :], in_=A[:, 81:83]), reads=[sk], writes=['w_all'])
                for j in range(4):
                    dv(lambda e: e.tensor_scalar(out=A[:, 96 + 8 * j:104 + 8 * j], in0=A[:, 56:64], scalar1=A[:, 40 + j:41 + j], scalar2=None, op0=ALU.mult))
                    dv(lambda e: e.tensor_scalar(out=A[:, 128 + 8 * j:136 + 8 * j], in0=A[:, 64:72], scalar1=A[:, 40 + j:41 + j], scalar2=None, op0=ALU.mult))
                dv(lambda e: e.tensor_tensor(out=A[:, 160:192], in0=A[:, 96:128], in1=A[:, 128:160], op=ALU.add))
                mbb = mb.get()
                S.op('dve', lambda e: e.tensor_copy(out=mbb.t[:], in_=A[:, 160:192]), reads=[sk], writes=[mbb.k])
                pc = prt.get()
                S.op('pe', lambda e: e.matmul(pc.t[:, 0:32], lhsT=self.cbs(C_TRIL, 128), rhs=mbb.t[:], start=True, stop=True), reads=[mbb.k, 'cb'], writes=[pc.k])
                S.op('pe', lambda e: e.matmul(pc.t[:, 32:64], lhsT=self.cbs(C_ONES, 128), rhs=mbb.t[:], start=True, stop=True), reads=[mbb.k, 'cb'], writes=[pc.k])
                dv(lambda e: e.tensor_tensor(out=A[:, 192:224], in0=pc.t[:, 0:32], in1=base.t[:], op=ALU.add), extra_r=[pc.k, 'rbase'])
                dv(lambda e: e.tensor_tensor(out=A[:, 192:224], in0=A[:, 192:224], in1=self.cf.t[:, 2:34], op=ALU.add), extra_r=['cf'])
                S.op('dve', lambda e: e.tensor_tensor(out=base.t[:], in0=base.t[:], in1=pc.t[:, 32:64], op=ALU.add), reads=['rbase', pc.k, sk], writes=['rbase'])
                dv(lambda e: e.tensor_tensor(out=A[:, 96:128], in0=A[:, 96:128], in1=A[:, 192:224], op=ALU.mult))
                dv(lambda e: e.tensor_tensor(out=A[:, 128:160], in0=A[:, 128:160], in1=A[:, 192:224], op=ALU.mult))
                dv(lambda e: e.reduce_sum(out=A[:, 228:229], in_=A[:, 96:128], axis=mybir.AxisListType.X))
                dv(lambda e: e.reduce_sum(out=A[:, 229:230], in_=A[:, 128:160], axis=mybir.AxisListType.X))
                S.op('dve', lambda e: e.tensor_copy(out=self.dest_all.t[:, c, :], in_=A[:, 228:230]), reads=[sk], writes=['dest_all'])
                for kk in range(2):
                    S.dma('pool', None, None, reads=[hbb.k, 'dest_all'], writes=[('D', 'hrows', c, kk)],
                          fn=lambda e: e.indirect_dma_start(out=self.hrows[:, :], out_offset=bass.IndirectOffsetOnAxis(ap=self.dest_all.t[:, c, kk:kk + 1], axis=0),
                                                            in_=hbb.t[:], in_offset=None))

            cur = stageA(0)
            for c in range(NCH):
                nxt = stageA(c + 1) if c + 1 < NCH else None
                stageB(c, *cur)
                cur = nxt

    def phase_experts(self, l):
        S, NCH, CAP, CAPB = self.S, self.NCH, self.CAP, self.CAPB
        with self.scope() as es:
            rows = self.ring("erow", 2, [128, D], BF16)
            rT = self.ring("erT", 2, [128, 16, CAP], BF16)
            hidT = self.ring("ehid", 2, [128, 4, CAP], BF16)
            sil = self.ring("esil", 2, [128, CAP], F32)
            ysb = self.ring("eysb", CAPB, [128, D], F32)
            ptr = self.ring("eptr", 2, [128, 1024], BF16, psum=True)
            pg = self.ring("epg", 2, [128, 512], F32, psum=True)
            pu = self.ring("epu", 2, [128, 512], F32, psum=True)
            py = self.ring("epy", 2, [128, 512], F32, psum=True)
            hr = [('D', 'hrows', c, kk) for c in range(NCH) for kk in range(2)]
            EST = self.ring("est", 4, [128, 2048], F32)
            WG = self.ring("ewg", 2, [128, 16, EH], BF16)
            WU = self.ring("ewu", 2, [128, 16, EH], BF16)
            def load_gu(ex_):
                weg_ = self.w_eg[l, ex_].rearrange("(k p) c -> p k c", p=128)
                weu_ = self.w_eu[l, ex_].rearrange("(k p) c -> p k c", p=128)
                wg_ = WG.get()
                wu_ = WU.get()
                for (src, dstb) in ((weg_, wg_), (weu_, wu_)):
                    for kq in range(4):
                        st = EST.get()
                        sk4 = [(st.k, jj) for jj in range(4)]
                        S.dma('sp', st.t[:].rearrange("p (a b) -> p a b", a=4), src[:, kq * 4:(kq + 1) * 4, :], writes=sk4)
                        self.cast(dstb.t[:, kq * 4:(kq + 1) * 4, :], st.t[:].rearrange("p (a b) -> p a b", a=4), sk4, [(dstb.k, kq)])
                        yield (wg_, wu_)
            nxt_w = None
            for ex in range(NEXP):
                rt = rT.get()
                for b in range(CAPB):
                    r = rows.get()
                    S.dma('act', r.t[:], self.hrows[ex * CAP + b * 128:ex * CAP + (b + 1) * 128, :], reads=hr, writes=[r.k])
                    for k8 in range(0, 16, 8):
                        p = ptr.get()
                        for j in range(8):
                            k = k8 + j
                            S.op('pe', lambda e: e.transpose(p.t[:, j * 128:(j + 1) * 128], r.t[:, k * 128:(k + 1) * 128], self.cbs(C_IDENT, 128)),
                                 reads=[r.k, 'cb'], writes=[p.k])
                        S.op('act' if k8 == 0 else 'dve',
                             (lambda e: e.activation(out=rt.t[:, k8:k8 + 8, b * 128:(b + 1) * 128], in_=p.t[:].rearrange("p (c d) -> p c d", c=8), func=AF.Copy)) if k8 == 0 else
                             (lambda e: e.tensor_copy(out=rt.t[:, k8:k8 + 8, b * 128:(b + 1) * 128], in_=p.t[:].rearrange("p (c d) -> p c d", c=8))),
                             reads=[p.k], writes=[rt.k])
                hd = hidT.get()
                weg = self.w_eg[l, ex].rearrange("(k p) c -> p k c", p=128)
                weu = self.w_eu[l, ex].rearrange("(k p) c -> p k c", p=128)
                if ex == 0:
                    for nxt_w in load_gu(0):
                        pass
                wg, wu = nxt_w
                gen = load_gu(ex + 1) if ex + 1 < NEXP else iter(())
                for j in range(4):
                    for _ in range(2):
                        nxt_w = next(gen, nxt_w)
                    g_ = pg.get()
                    u_ = pu.get()
                    for k in range(16):
                        S.op('pe', lambda e: e.matmul(g_.t[:, 0:CAP], lhsT=wg.t[:, k, j * 128:(j + 1) * 128], rhs=rt.t[:, k, :], start=(k == 0), stop=(k == 15)),
                             reads=[(wg.k, k // 4), rt.k], writes=[g_.k])
                    for k in range(16):
                        S.op('pe', lambda e: e.matmul(u_.t[:, 0:CAP], lhsT=wu.t[:, k, j * 128:(j + 1) * 128], rhs=rt.t[:, k, :], start=(k == 0), stop=(k == 15)),
                             reads=[(wu.k, k // 4), rt.k], writes=[u_.k])
                    s_ = sil.get()
                    S.op('act', lambda e: e.activation(out=s_.t[:], in_=g_.t[:, 0:CAP], func=AF.Silu), reads=[g_.k], writes=[s_.k])
                    S.op('dve', lambda e: e.tensor_tensor(out=hd.t[:, j, :], in0=s_.t[:], in1=u_.t[:, 0:CAP], op=ALU.mult), reads=[s_.k, u_.k], writes=[hd.k])
                wed = self.w_ed[l, ex].rearrange("(k p) c -> p k c", p=128)
                ys = [ysb.get() for _ in range(CAPB)]
                wd_n = self.wload(wed[:, :, 0:512], 4, 512)
                for ct in range(4):
                    wd = wd_n
                    if ct < 3:
                        wd_n = self.wload(wed[:, :, (ct + 1) * 512:(ct + 2) * 512], 4, 512)
                    wdv = wd.t[:].rearrange("p (k m) -> p k m", k=4)
                    for b in range(CAPB):
                        y_ = py.get()
                        for j in range(4):
                            S.op('pe', lambda e: e.matmul(y_.t[:], lhsT=hd.t[:, j, b * 128:(b + 1) * 128], rhs=wdv[:, j, :], start=(j == 0), stop=(j == 3)),
                                 reads=[hd.k, wd.k], writes=[y_.k])
                        if (ct + b) % 2 == 0:
                            S.op('act', lambda e: e.activation(out=ys[b].t[:, ct * 512:(ct + 1) * 512], in_=y_.t[:], func=AF.Copy), reads=[y_.k], writes=[ys[b].k])
                        else:
                            S.op('dve', lambda e: e.tensor_copy(out=ys[b].t[:, ct * 512:(ct + 1) * 512], in_=y_.t[:]), reads=[y_.k], writes=[ys[b].k])
                for b in range(CAPB):
                    S.dma('sp', self.yrows[ex * CAP + b * 128:ex * CAP + (b + 1) * 128, :], ys[b].t[:], reads=[ys[b].k], writes=[('D', 'yrows', ex, b)])

    def phase_ple(self, l, last):
        S, SL, NCH, CAPB = self.S, self.SL, self.NCH, self.CAPB
        with self.scope() as es:
            wpg = self.sb("wpg", [128, 16, D], BF16)
            self.load_resident(wpg, self.w_pg[l], 16)
            wpp = self.sb("wpp", [128, 2, D], BF16)
            self.load_resident(wpp, self.w_pp[l], 2)
            g_t = self.sb("l2g", [128, D], F32)
            b_t = self.sb("l2b", [128, D], F32)
            S.dma('sp', g_t.t[:], self.ln2_g[l, 0:1, :].partition_broadcast(128), writes=['l2g'])
            S.dma('sp', b_t.t[:], self.ln2_b[l, 0:1, :].partition_broadcast(128), writes=['l2b'])
            hTt = self.ring("phT", 2, [128, 16, 128], BF16)
            pTs = self.ring("ppTs", 2, [128, 2, 128], F32)
            pTb = self.ring("ppTb", 2, [128, 2, 128], BF16)
            hr = self.ring("phr", 2, [128, D], F32)
            y1 = self.ring("py1", 2, [128, D], F32)
            y2 = self.ring("py2", 2, [128, D], F32)
            sg = self.ring("psg", 1, [128, D], F32)
            xb = self.ring("pxb", 1, [128, D], BF16)
            xTt = self.ring("pxT", 1, [128, 16, 128], BF16)
            stt = self.ring("pstt", 2, [128, 24], F32)
            mvt = self.ring("pmv", 2, [128, 4], F32)
            ppg = self.ring("ppg", 3, [128, 512], F32, psum=True)
            ppp = self.ring("ppp", 3, [128, 512], F32, psum=True)
            ptr = self.ring("pptr", 2, [128, 1024], BF16, psum=True)
            yr = [('D', 'yrows', ex, b) for ex in range(NEXP) for b in range(CAPB)]
            for c in range(NCH):
                r0 = c * 128
                hT = hTt.get()
                S.dma('sp', hT.t[:], self.hT[:, r0:r0 + 128].rearrange("(k p) t -> p k t", p=128), reads=[('D', 'hT', c)], writes=[hT.k])
                ps_ = pTs.get()
                S.dma('sp', ps_.t[:], self.pT[l, :, r0:r0 + 128].rearrange("(k p) t -> p k t", p=128), writes=[ps_.k])
                pb = pTb.get()
                S.op('pool', lambda e: e.tensor_copy(out=pb.t[:], in_=ps_.t[:]), reads=[ps_.k], writes=[pb.k])
                h = hr.get()
                S.dma('sp', h.t[:], self.hres[r0:r0 + 128, :], reads=[('D', 'hres', c)], writes=[h.k])
                ya = y1.get()
                yb = y2.get()
                for kk, yy in ((0, ya), (1, yb)):
                    S.dma('pool', None, None, reads=yr + ['dest_all'], writes=[yy.k],
                          fn=lambda e: e.indirect_dma_start(out=yy.t[:], out_offset=None, in_=self.yrows[:, :],
                                                            in_offset=bass.IndirectOffsetOnAxis(ap=self.dest_all.t[:, c, kk:kk + 1], axis=0)))
                s_ = sg.get()
                v = s_
                for ct in range(4):
                    sl = slice(ct * 512, (ct + 1) * 512)
                    pg_ = ppg.get()
                    for k in range(16):
                        S.op('pe', lambda e: e.matmul(pg_.t[:], lhsT=hT.t[:, k, :], rhs=wpg.t[:, k, sl], start=(k == 0), stop=(k == 15)), reads=[hT.k, 'wpg'], writes=[pg_.k])
                    pp_ = ppp.get()
                    for k in range(2):
                        S.op('pe', lambda e: e.matmul(pp_.t[:], lhsT=pb.t[:, k, :], rhs=wpp.t[:, k, sl], start=(k == 0), stop=(k == 1)), reads=[pb.k, 'wpp'], writes=[pp_.k])
                    S.op('act', lambda e: e.activation(out=s_.t[:, sl], in_=pg_.t[:], func=AF.Sigmoid), reads=[pg_.k], writes=[s_.k])
                    S.op('dve', lambda e: e.tensor_tensor(out=s_.t[:, sl], in0=s_.t[:, sl], in1=pp_.t[:], op=ALU.mult), reads=[s_.k, pp_.k], writes=[s_.k])
                S.op('dve', lambda e: e.scalar_tensor_tensor(out=v.t[:], in0=h.t[:], scalar=ALPHA, in1=s_.t[:], op0=ALU.mult, op1=ALU.add), reads=[h.k, s_.k], writes=[v.k])
                S.op('dve', lambda e: e.scalar_tensor_tensor(out=v.t[:], in0=ya.t[:], scalar=self.w_all.t[:, c, 0:1], in1=v.t[:], op0=ALU.mult, op1=ALU.add),
                     reads=[ya.k, 'w_all', v.k], writes=[v.k])
                S.op('dve', lambda e: e.scalar_tensor_tensor(out=v.t[:], in0=yb.t[:], scalar=self.w_all.t[:, c, 1:2], in1=v.t[:], op0=ALU.mult, op1=ALU.add),
                     reads=[yb.k, 'w_all', v.k], writes=[v.k])
                x = h
                self.layer_norm(v, stt.get(), mvt.get(), g_t, b_t, x)
                if last:
                    S.dma('sp', self.out[r0:r0 + 128, :], x.t[:], reads=[x.k], writes=[('D', 'out', c)])
                else:
                    S.dma('sp', self.xres[r0:r0 + 128, :], x.t[:], reads=[x.k], writes=[('D', 'xres', c)])
                    xbb = xb.get()
                    S.op('act', lambda e: e.activation(out=xbb.t[:], in_=x.t[:], func=AF.Copy), reads=[x.k], writes=[xbb.k])
                    xT = xTt.get()
                    self.transpose_store(xbb, ptr, xT)
                    S.dma('sp', self.xT[:, r0:r0 + 128].rearrange("(k p) t -> p k t", p=128), xT.t[:], reads=[xT.k], writes=[('D', 'xT1', c)])


class _Scope:
    def __init__(self, prog):
        self.p = prog
        self.es = ExitStack()

    def __enter__(self):
        self.prev = self.p.cur
        self.p.cur = self.es
        self.es.__enter__()
        return self.es

    def __exit__(self, *a):
        self.p.S.barrier()
        r = self.es.__exit__(*a)
        self.p.cur = self.prev
        return r


class nc_allow:
    def __init__(self, nc):
        self.cm = nc.allow_non_contiguous_dma(reason="tiny strided column load")

    def __enter__(self):
        return self.cm.__enter__()

    def __exit__(self, *a):
        return self.cm.__exit__(*a)


def make_consts(CAP):
    c = np.zeros((128, NCONST), np.float32)
    i = np.arange(128)
    c[:, C_IDENT:C_IDENT + 128] = np.eye(128)
    c[:, C_ONES:C_ONES + 128] = 1.0
    c[:, C_TRIS:C_TRIS + 128] = (i[:, None] > i[None, :])
    c[:, C_TRIL:C_TRIL + 128] = (i[:, None] < i[None, :])
    Rd = np.zeros((128, 128), np.float32)
    for base in (0, 64):
        for j in range(8):
            Rd[base + j, base + j + 8] = -1.0
            Rd[base + j + 8, base + j] = 1.0
    c[:, C_RD:C_RD + 128] = Rd.T
    Rm = np.zeros((128, 128), np.float32)
    for j in range(32):
        Rm[j, j + 32] = -1.0
        Rm[j + 32, j] = 1.0
    c[:, C_RM:C_RM + 128] = Rm.T
    kk = i[:, None]
    qq = i[None, :]
    inblk = {
        'ch': (kk // 64 <= qq // 64),
        'ca': (kk <= qq),
        'st': (kk < qq),
    }
    for j0 in range(4):
        for b in range(4):
            for name, col, kind in (('ch', C_MADD_CH, 'add'), ('ca', C_MADD_CA, 'add'), ('st', C_MMUL, 'mul')):
                if b < j0:
                    valid = np.zeros((128, 128), bool)
                elif b == j0:
                    valid = inblk[name]
                else:
                    valid = np.ones((128, 128), bool)
                blk = np.where(valid, 0.0, NEG) if kind == 'add' else valid.astype(np.float32)
                c[:, col + j0 * 512 + b * 128:col + j0 * 512 + (b + 1) * 128] = blk
    invf_d = np.zeros(128, np.float32)
    fd = (np.float32(ROPE_THETA) ** (-np.arange(8, dtype=np.float32) / np.float32(8))).astype(np.float32)
    for base in (0, 64):
        invf_d[base:base + 8] = fd
        invf_d[base + 8:base + 16] = fd
    c[:, C_INVF_D] = invf_d
    fm = (np.float32(ROPE_THETA) ** (-np.arange(32, dtype=np.float32) / np.float32(32))).astype(np.float32)
    invf_m = np.zeros(128, np.float32)
    invf_m[0:32] = fm
    invf_m[32:64] = fm
    c[:, C_INVF_M] = invf_m
    c[:, C_ECAP:C_ECAP + 32] = (np.arange(32) * CAP)[None, :]
    return c


_CACHE = {}


def get_prog(S_len, **kw):
    key = (S_len, tuple(sorted((k, str(v)) for k, v in kw.items())))
    if key not in _CACHE:
        p = Prog(S_len, **kw)
        p.build()
        _CACHE[key] = p
    return _CACHE[key]


def make_in_map(p, b, inputs):
    f = lambda a: np.ascontiguousarray(np.asarray(a, dtype=np.float32))
    x = np.asarray(inputs['x'])[b]
    m = {
        'x_tm': f(x), 'x_fm': f(x.T),
        'pT': f(np.transpose(np.asarray(inputs['p'])[:, b], (0, 2, 1))),
        'pos': np.ascontiguousarray(np.asarray(inputs['positions'])[b:b + 1].astype(np.int32)),
        'consts': make_consts(p.CAP),
        'w_in': f(inputs['w_in']),
        'fox_f_bias': f(inputs['fox_f_bias']).reshape(DEPTH, 4, 1),
        'diff_lambda': f(inputs['diff_lambda']).reshape(DEPTH, 1, 256),
        'diff_subln': f(inputs['diff_subln']).reshape(DEPTH, 128, 1),
        'mla_q_norm': f(f(inputs['mla_q_norm']).reshape(DEPTH, 4, 128).transpose(0, 2, 1)),
        'mla_kv_norm': f(f(inputs['mla_kv_norm']).reshape(DEPTH, 2, 128).transpose(0, 2, 1)),
        'mla_w_uq': f(inputs['mla_w_uq']), 'mla_w_ukv': f(inputs['mla_w_ukv']),
        'w_branch': f(inputs['w_branch']), 'w_o': f(inputs['w_o']),
        'ln1_g': f(inputs['ln1_g']).reshape(DEPTH, 1, D), 'ln1_b': f(inputs['ln1_b']).reshape(DEPTH, 1, D),
        'w_router_group': f(inputs['w_router_group']), 'b_router_group': f(inputs['b_router_group']).reshape(DEPTH, 1, 4),
        'w_router_expert': f(inputs['w_router_expert']), 'b_router_expert': f(inputs['b_router_expert']).reshape(DEPTH, 1, 32),
        'w_expert_gate': f(inputs['w_expert_gate']), 'w_expert_up': f(inputs['w_expert_up']),
        'w_expert_down': f(inputs['w_expert_down']),
        'w_ple_gate': f(inputs['w_ple_gate']), 'w_ple_proj': f(inputs['w_ple_proj']),
        'ln2_g': f(inputs['ln2_g']).reshape(DEPTH, 1, D), 'ln2_b': f(inputs['ln2_b']).reshape(DEPTH, 1, D),
    }
    return m


def kernel(**inputs):
    x = np.asarray(inputs['x'])
    B, SL, _ = x.shape
    p = get_prog(SL)
    in_maps = [make_in_map(p, b, inputs) for b in range(B)]
    res = run_bass_kernel_spmd(p.nc, in_maps, core_ids=list(range(B)))
    out = np.stack([np.asarray(r['out']) for r in res.results], axis=0)
    return out.astype(np.float32)
```
